# Optimizing a Trainium2 kernel written in Bass

```python
import math
import jax, jax.numpy as jnp
from jax import lax
import numpy as np

D_MODEL = 1024
BATCH = 4
SEQ = 4096
DEPTH = 1

HEAD_DIM = 64
ROT_DIM = HEAD_DIM // 4
ROPE_THETA = 500000.0
QBLK = 128
DA_HEADS = 4
DA_V_DIM = 2 * HEAD_DIM
DA_QK_W = DA_HEADS * 2 * HEAD_DIM
DA_V_W = DA_HEADS * DA_V_DIM
DIL_PAIRS = ((128, 1), (512, 4), (2048, 16))
DIL_GROUPS = len(DIL_PAIRS)
DIL_HEADS = 8
DIL_W = DIL_HEADS * HEAD_DIM
IN_COLS = 2 * DA_QK_W + DA_V_W + 3 * DIL_GROUPS * DIL_W + 2 * D_MODEL
N_EXPERTS = 32
TOP_K = 4
D_FF = D_MODEL
SWIGLU_ALPHA = 1.702
SWIGLU_LIMIT = 7.0
MOE_BLK = 128
DN_ALPHA = (2 * DEPTH) ** 0.25
DN_BETA = (8 * DEPTH) ** -0.25
EPS = 1e-5

kernel_name = "hybrid_diffattn_dilated_moe_deepnorm"


def rope_tables(seq):
    inv_freq = ROPE_THETA ** (-jnp.arange(0, ROT_DIM, 2, dtype=jnp.float32) / ROT_DIM)
    ang = jnp.arange(seq, dtype=jnp.float32)[:, None] * inv_freq[None, :]
    return jnp.cos(ang), jnp.sin(ang)


def apply_rope(t, cos, sin):
    half = ROT_DIM // 2
    shp = (t.shape[1],) + (1,) * (t.ndim - 3) + (half,)
    c = cos.reshape(shp).astype(t.dtype)
    s = sin.reshape(shp).astype(t.dtype)
    t1 = t[..., :half]
    t2 = t[..., half:ROT_DIM]
    return jnp.concatenate([t1 * c - t2 * s, t2 * c + t1 * s, t[..., ROT_DIM:]], axis=-1)


def layer_norm(x, g, b):
    xf = x.astype(jnp.float32)
    mu = jnp.mean(xf, axis=-1, keepdims=True)
    var = jnp.mean(jnp.square(xf - mu), axis=-1, keepdims=True)
    y = (xf - mu) * lax.rsqrt(var + EPS) * g.astype(jnp.float32) + b.astype(jnp.float32)
    return y.astype(x.dtype)


def diff_attention(q, k, v, lam_q1, lam_k1, lam_q2, lam_k2, subln_g, lambda_init):
    B, S = q.shape[0], q.shape[1]
    qh = q.transpose(0, 2, 3, 1, 4) * (HEAD_DIM ** -0.5)
    kh = k.transpose(0, 2, 3, 1, 4)
    vh = v.transpose(0, 2, 1, 3)
    lam = (jnp.exp(jnp.sum(lam_q1.astype(jnp.float32) * lam_k1.astype(jnp.float32)))
           - jnp.exp(jnp.sum(lam_q2.astype(jnp.float32) * lam_k2.astype(jnp.float32)))
           + lambda_init)
    outs = []
    for i in range(S // QBLK):
        q0 = i * QBLK
        kend = q0 + QBLK
        s = jnp.einsum('bhcqd,bhckd->bhcqk', qh[:, :, :, q0:kend], kh[:, :, :, :kend]).astype(jnp.float32)
        mask = (q0 + jnp.arange(QBLK))[:, None] >= jnp.arange(kend)[None, :]
        p = jax.nn.softmax(jnp.where(mask, s, -jnp.inf), axis=-1)
        a = p[:, :, 0] - lam * p[:, :, 1]
        outs.append(jnp.einsum('bhqk,bhkv->bhqv', a.astype(v.dtype), vh[:, :, :kend]))
    o = jnp.concatenate(outs, axis=2).astype(jnp.float32)
    o = o * lax.rsqrt(jnp.mean(jnp.square(o), axis=-1, keepdims=True) + EPS) * subln_g.astype(jnp.float32)
    o = o * (1.0 - lambda_init)
    return o.astype(v.dtype).transpose(0, 2, 1, 3).reshape(B, S, DA_HEADS * DA_V_DIM)


def dilated_group(q, k, v, window, dilation):
    B, S, H, dh = q.shape
    rel = window // dilation
    L = S // dilation
    nb = -(-L // QBLK)
    Lp = nb * QBLK

    def strided(t):
        t = t.reshape(B, L, dilation, H, dh).transpose(0, 2, 3, 1, 4)
        return jnp.pad(t, ((0, 0), (0, 0), (0, 0), (0, Lp - L), (0, 0)))

    def banded(t):
        tb = t.reshape(B, dilation, H, nb, QBLK, dh)
        prev = jnp.pad(tb, ((0, 0), (0, 0), (0, 0), (1, 0), (0, 0), (0, 0)))[:, :, :, :nb]
        return jnp.concatenate([prev, tb], axis=4)

    qb = (strided(q) * (dh ** -0.5)).reshape(B, dilation, H, nb, QBLK, dh)
    kk = banded(strided(k))
    vv = banded(strided(v))
    s = jnp.einsum('bdhnqe,bdhnke->bdhnqk', qb, kk).astype(jnp.float32)
    qi = jnp.arange(QBLK)[:, None]
    kj = jnp.arange(2 * QBLK)[None, :]
    dist = qi + QBLK - kj
    blk = jnp.arange(nb)[:, None, None]
    mask = (dist >= 0) & (dist <= rel) & (blk * QBLK + kj - QBLK >= 0)
    s = jnp.where(mask, s, -jnp.inf)
    m = jnp.max(s, axis=-1, keepdims=True)
    p = jnp.exp(s - m)
    den = jnp.sum(p, axis=-1, keepdims=True)
    o = jnp.einsum('bdhnqk,bdhnke->bdhnqe', (p / den).astype(v.dtype), vv)
    lse = (m + jnp.log(den))[..., 0]
    o = o.reshape(B, dilation, H, Lp, dh)[:, :, :, :L].transpose(0, 3, 1, 2, 4).reshape(B, S, H, dh)
    lse = lse.reshape(B, dilation, H, Lp)[..., :L].transpose(0, 3, 1, 2).reshape(B, S, H)
    return o, lse


def token_mixer(h, w_in, b_gate, lam_q1, lam_k1, lam_q2, lam_k2, subln_g, w_oa, w_ob, w_o,
                cos, sin, lambda_init):
    B, S, D = h.shape
    proj = h @ w_in
    off = 0
    qa = proj[..., off:off + DA_QK_W].reshape(B, S, DA_HEADS, 2, HEAD_DIM); off += DA_QK_W
    ka = proj[..., off:off + DA_QK_W].reshape(B, S, DA_HEADS, 2, HEAD_DIM); off += DA_QK_W
    va = proj[..., off:off + DA_V_W].reshape(B, S, DA_HEADS, DA_V_DIM); off += DA_V_W
    y_a = diff_attention(apply_rope(qa, cos, sin), apply_rope(ka, cos, sin), va,
                         lam_q1, lam_k1, lam_q2, lam_k2, subln_g, lambda_init)
    outs, lses = [], []
    for window, dilation in DIL_PAIRS:
        qg = proj[..., off:off + DIL_W].reshape(B, S, DIL_HEADS, HEAD_DIM); off += DIL_W
        kg = proj[..., off:off + DIL_W].reshape(B, S, DIL_HEADS, HEAD_DIM); off += DIL_W
        vg = proj[..., off:off + DIL_W].reshape(B, S, DIL_HEADS, HEAD_DIM); off += DIL_W
        o, lse = dilated_group(apply_rope(qg, cos, sin), apply_rope(kg, cos, sin), vg, window, dilation)
        outs.append(o)
        lses.append(lse)
    wts = jax.nn.softmax(jnp.stack(lses, axis=0), axis=0)
    y_b = jnp.sum(wts[..., None].astype(h.dtype) * jnp.stack(outs, axis=0), axis=0).reshape(B, S, DIL_W)
    gates = jax.nn.sigmoid(proj[..., off:off + 2 * D] + b_gate)
    merged = gates[..., :D] * (y_a @ w_oa) + gates[..., D:] * (y_b @ w_ob)
    return merged @ w_o


def moe_ffn(h, w_router, b_router, w_gu, b_gu, w_down, b_down):
    B, S, D = h.shape
    T = B * S
    xt = h.reshape(T, D)
    logits = (xt @ w_router).astype(jnp.float32) + b_router.astype(jnp.float32)
    top_val, top_idx = lax.top_k(logits, TOP_K)
    gate_w = jax.nn.softmax(top_val, axis=-1)
    n_assign = T * TOP_K
    flat_e = top_idx.reshape(-1).astype(jnp.int32)
    flat_tok = jnp.arange(n_assign, dtype=jnp.int32) // TOP_K
    flat_w = gate_w.reshape(-1)
    order = jnp.argsort(flat_e)
    sorted_e = flat_e[order]
    counts = jnp.bincount(flat_e, length=N_EXPERTS).astype(jnp.int32)
    padded = (counts + MOE_BLK - 1) // MOE_BLK * MOE_BLK
    ustart = jnp.cumsum(counts) - counts
    pend = jnp.cumsum(padded)
    pstart = pend - padded
    dest = pstart[sorted_e] + (jnp.arange(n_assign, dtype=jnp.int32) - ustart[sorted_e])
    n_rows = n_assign + N_EXPERTS * MOE_BLK
    n_blocks = n_rows // MOE_BLK
    row_tok = jnp.zeros((n_rows,), jnp.int32).at[dest].set(flat_tok[order])
    row_w = jnp.zeros((n_rows,), jnp.float32).at[dest].set(flat_w[order])
    block_e = jnp.minimum(jnp.searchsorted(pend, jnp.arange(n_blocks, dtype=jnp.int32) * MOE_BLK, side='right'),
                          N_EXPERTS - 1)
    xrows = xt[row_tok].reshape(n_blocks, MOE_BLK, D)

    def expert_block(args):
        xb, e = args
        gu = xb @ w_gu[e] + b_gu[e]
        gate = jnp.minimum(gu[:, :D_FF], SWIGLU_LIMIT)
        up = jnp.clip(gu[:, D_FF:], -SWIGLU_LIMIT, SWIGLU_LIMIT)
        act = (up + 1.0) * gate * jax.nn.sigmoid(SWIGLU_ALPHA * gate)
        return act @ w_down[e] + b_down[e]

    yrows = lax.map(expert_block, (xrows, block_e)).reshape(n_rows, D)
    y = jnp.zeros((T, D), jnp.float32).at[row_tok].add(yrows.astype(jnp.float32) * row_w[:, None])
    return y.astype(h.dtype).reshape(B, S, D)


def setup_inputs(seed: int = 0) -> dict:
    key = jax.random.key(seed)
    ks = jax.random.split(key, 24)
    f32 = jnp.float32
    L = DEPTH

    def nrm(k, shape, scale):
        return jax.random.normal(k, shape, f32) * scale

    return {
        "x": nrm(ks[0], (BATCH, SEQ, D_MODEL), 1.0),
        "w_in": nrm(ks[1], (L, D_MODEL, IN_COLS), D_MODEL ** -0.5),
        "b_gate": nrm(ks[2], (L, 2 * D_MODEL), 0.02),
        "lam_q1": nrm(ks[3], (L, HEAD_DIM), 0.1),
        "lam_k1": nrm(ks[4], (L, HEAD_DIM), 0.1),
        "lam_q2": nrm(ks[5], (L, HEAD_DIM), 0.1),
        "lam_k2": nrm(ks[6], (L, HEAD_DIM), 0.1),
        "subln_g": 1.0 + nrm(ks[7], (L, DA_V_DIM), 0.02),
        "w_oa": nrm(ks[8], (L, DA_V_W, D_MODEL), DA_V_W ** -0.5),
        "w_ob": nrm(ks[9], (L, DIL_W, D_MODEL), DIL_W ** -0.5),
        "w_o": nrm(ks[10], (L, D_MODEL, D_MODEL), DN_BETA * D_MODEL ** -0.5),
        "ln1_g": 1.0 + nrm(ks[11], (L, D_MODEL), 0.02),
        "ln1_b": nrm(ks[12], (L, D_MODEL), 0.02),
        "w_router": nrm(ks[13], (L, D_MODEL, N_EXPERTS), D_MODEL ** -0.5),
        "b_router": nrm(ks[14], (L, N_EXPERTS), 0.01),
        "w_gu": nrm(ks[15], (L, N_EXPERTS, D_MODEL, 2 * D_FF), D_MODEL ** -0.5),
        "b_gu": nrm(ks[16], (L, N_EXPERTS, 2 * D_FF), 0.02),
        "w_down": nrm(ks[17], (L, N_EXPERTS, D_FF, D_MODEL), DN_BETA * D_FF ** -0.5),
        "b_down": nrm(ks[18], (L, N_EXPERTS, D_MODEL), 0.02),
        "ln2_g": 1.0 + nrm(ks[19], (L, D_MODEL), 0.02),
        "ln2_b": nrm(ks[20], (L, D_MODEL), 0.02),
    }


def reference(x, w_in, b_gate, lam_q1, lam_k1, lam_q2, lam_k2, subln_g, w_oa, w_ob, w_o,
              ln1_g, ln1_b, w_router, b_router, w_gu, b_gu, w_down, b_down, ln2_g, ln2_b):
    cos, sin = rope_tables(x.shape[1])
    h = x
    for l in range(DEPTH):
        lambda_init = 0.8 - 0.6 * math.exp(-0.3 * l)
        m = token_mixer(h, w_in[l], b_gate[l], lam_q1[l], lam_k1[l], lam_q2[l], lam_k2[l], subln_g[l],
                        w_oa[l], w_ob[l], w_o[l], cos, sin, lambda_init)
        h = layer_norm(DN_ALPHA * h + m, ln1_g[l], ln1_b[l])
        f = moe_ffn(h, w_router[l], b_router[l], w_gu[l], b_gu[l], w_down[l], b_down[l])
        h = layer_norm(DN_ALPHA * h + f, ln2_g[l], ln2_b[l])
    return h
```

```python
import contextlib
import numpy as np
import ml_dtypes
import concourse.bass as bass
import concourse.mybir as mybir
from concourse.bass_utils import run_bass_kernel_spmd

F32 = mybir.dt.float32
BF16 = mybir.dt.bfloat16
ALU = mybir.AluOpType
AF = mybir.ActivationFunctionType
AX = mybir.AxisListType

S_OWN = 2048
D = 1024
NE = 32
ALPHA = 2.0 ** 0.25
EPS = 1e-5
LAMBDA_INIT = 0.2
NEG = -30000.0
DILS = (1, 4, 16)
SAME_ENGINE_WAITS = True
CAP = 384
I32 = mybir.dt.int32


def tab_index():
    idx = {}
    n = 0
    for b in range(32):
        idx[("n", 0, b)] = n; n += 1
    for g, d in enumerate(DILS):
        for ob in range(16):
            idx[("o", g, ob)] = n; n += 1
        for pb in range(d):
            idx[("p", g, pb)] = n; n += 1
    return idx, n


class Eng:
    def __init__(self, name, sem):
        self.name, self.sem, self.n, self.ops, self.seen = name, sem, 0, [], {}

    def wait(self, *evs):
        for ev in evs:
            if ev is None:
                continue
            if isinstance(ev, list):
                self.wait(*ev)
                continue
            sem, val, key = ev
            if key == self.name and not SAME_ENGINE_WAITS:
                continue
            if self.seen.get(key, 0) >= val:
                continue
            self.seen[key] = val
            self.ops.append(lambda h, sem=sem, val=val: h.wait_ge(sem, val))

    def op(self, fn, sig=True):
        if sig:
            self.n += 1
            self.ops.append(lambda h, fn=fn, sem=self.sem: fn(h).then_inc(sem, 1))
            return (self.sem, self.n, self.name)
        self.ops.append(lambda h, fn=fn: fn(h))
        return None

    def dma(self, fn, ds):
        ds.n += 16
        self.ops.append(lambda h, fn=fn, sem=ds.sem: fn(h).then_inc(sem, 16))
        return (ds.sem, ds.n, ds.name)


class DSem:
    def __init__(self, name, sem):
        self.name, self.sem, self.n = name, sem, 0

    def ev(self):
        return (self.sem, self.n, self.name)


def build_nc(debug=False):
    nc = bass.Bass("TRN2", target_bir_lowering=False)
    TIDX, NTAB = tab_index()

    def din(name, shape, dt=F32):
        return nc.dram_tensor(name, list(shape), dt, kind="ExternalInput").ap()

    x_own = din("x_own", [S_OWN, D])
    x_prev = din("x_prev", [S_OWN, D])
    w_in = din("w_in", [D, 8192])
    w_oa = din("w_oa", [512, D])
    w_ob = din("w_ob", [512, D])
    w_o = din("w_o", [D, D])
    w_router = din("w_router", [D, NE])
    w_gu = din("w_gu", [NE, D, 2048])
    w_down = din("w_down", [NE, D, D])
    rope = din("rope", [128, NTAB * 16])
    prevbias_d = din("prevbias", [128, 1])
    cmask_d = din("cmask", [128, 4 * 512], BF16)
    maskA_d = din("maskA", [128, 128], BF16)
    negI_d = din("negI", [128, 128], BF16)
    identb_d = din("identb", [128, 128], BF16)
    identf_d = din("identf", [128, 128])
    lamv_d = din("lamv", [128, 4 * 64])
    subg_d = din("subg", [128, 1])
    bgt_d = din("bgt", [128, 16])
    bgu_d = din("bgu_t", [128, NE * 16])
    brt_d = din("brt", [128, NE])
    bdn_d = din("bdn", [NE, D])
    lnp_d = din("lnp", [128, 4 * D])
    ebase_d = din("ebase", [128, NE])
    Xg = nc.dram_tensor("Xg", [NE * CAP, D], BF16, kind="Internal").ap()
    Yg = nc.dram_tensor("Yg", [NE * CAP, D], F32, kind="Internal").ap()
    out_d = nc.dram_tensor("out", [S_OWN, D], F32, kind="ExternalOutput").ap()
    h1d = nc.dram_tensor("h1d", [S_OWN, D], F32, kind="ExternalOutput" if debug else "Internal").ap()
    if debug:
        dbg_ya = nc.dram_tensor("dbg_ya", [128, 4 * 2048], BF16, kind="ExternalOutput").ap()
    ybd = nc.dram_tensor("ybd", [64, 8 * 2048], BF16, kind="ExternalOutput" if debug else "Internal").ap()

    es = contextlib.ExitStack()
    with es:
        def sb(name, shape, dt):
            return es.enter_context(nc.sbuf_tensor("sb_" + name, list(shape), dt))

        def pst(name):
            return es.enter_context(nc.psum_tensor(name, [128, 512], F32))

        _semc = [0]

        def newsem(name):
            _semc[0] += 1
            return es.enter_context(nc.semaphore(name))

        PE = Eng("pe", newsem("s_pe"))
        ACT = Eng("act", newsem("s_act"))
        DVE = Eng("dve", newsem("s_dve"))
        POOL = Eng("pool", newsem("s_pool"))
        SP = Eng("sp", newsem("s_sp"))

        def dsem(name):
            return DSem(name, newsem(name))

        K = 1024
        ARENA = sb("arena", [128, 92 * K], BF16)

        def av(off, shape, dt, parts=128):
            esz = 4 if dt == F32 else 2
            n = 1
            for d_ in shape[1:]:
                n *= d_
            a = ARENA[0:parts, off // 2: off // 2 + n * esz // 2]
            if dt == F32:
                a = a.bitcast(F32)
            if len(shape) == 3:
                a = a.rearrange("p (a b) -> p a b", a=shape[1])
            elif len(shape) == 4:
                a = a.rearrange("p (a b c) -> p a b c", a=shape[1], b=shape[2])
            return a

        xTp = av(0, [128, 8, 2048], BF16)
        xTo = av(32 * K, [128, 8, 2048], BF16)
        ybH = av(64 * K, [64, 4, 2048], BF16, parts=64)
        acc = av(80 * K, [128, 4, 2048], F32)
        yaT = av(80 * K, [128, 4, 2048], BF16)
        ft = [av(96 * K + i * 2 * K, [128, 512], F32) for i in range(5)]
        xld = [av(100 * K + i * 2 * K, [128, 1024], BF16) for i in range(2)]
        QT = av(112 * K, [128, 2, 2048], BF16)
        KT = av(120 * K, [128, 2, 4096], BF16)
        VVf = av(136 * K, [128, 32 * 260], BF16)
        Wr = [av(153 * K + i * 4 * K, [128, 8, 256], BF16) for i in range(3)]
        Tt = [av(165 * K + i * 512, [128, 256], BF16) for i in range(2)]
        rtmp = [av(166 * K + i * 512, [128, 4, 32], F32) for i in range(2)]
        ropet = av(167 * K, [128, NTAB, 16], F32)
        cmask = av(167 * K + 6656, [128, 4, 512], BF16)
        PT = [av(167 * K + 6656 + 4 * K + i * K, [128, 512], BF16) for i in range(4)]
        lamv = av(167 * K + 6656 + 8 * K, [128, 4, 64], F32)
        maskA = av(167 * K + 6656 + 9 * K, [128, 128], BF16)
        negI = av(167 * K + 6656 + 9 * K + 256, [128, 128], BF16)
        ybw = av(64 * K, [64, 8, 512], BF16, parts=64)
        mT = av(72 * K, [128, 8, 512], BF16)
        h1Tf = av(106 * K, [128, 8, 128], F32)
        wrt = av(110 * K, [128, 8, NE], F32)
        woa = av(112 * K, [128, 4, D], BF16)
        wob = av(120 * K, [64, 8, D], BF16, parts=64)
        wo = av(136 * K, [128, 8, D], BF16)
        lnp1 = av(165 * K, [128, 2, D], F32)
        xf3 = av(173 * K, [128, D], F32)
        xf5 = [av(128 * K + i * 4 * K, [128, D], F32) for i in range(2)]
        lnp2 = av(136 * K, [128, 2, D], F32)

        identb = sb("identb", [128, 128], BF16)
        identf = sb("identf", [128, 128], F32)
        ones_bf = sb("ones_bf", [128, 128], BF16)
        onesF = sb("onesF", [128, 128], F32)
        prevbias = sb("prevbias", [128, 1], F32)
        lamt = sb("lamt", [128, 2, 64], F32)
        lams = sb("lams", [128, 4], F32)
        subg = sb("subg", [128, 1], F32)
        epsc = sb("epsc", [128, 1], F32)
        bgt = sb("bgt", [128, 16], F32)
        bgu = sb("bgu", [128, NE * 16], F32)
        brt = sb("brt", [128, NE], F32)
        bdn = sb("bdn", [NE, D], F32)
        gwd = sb("gwd", [128, 16, NE], F32)
        rsm = sb("rsm", [128, 16], F32)
        ftA = sb("ftA", [128, 512], F32)
        stt = sb("stt", [128, 2, 6], F32)
        mv = sb("mv", [128, 2], F32)
        sm = sb("sm", [128, 8], F32)
        lg = sb("lg", [128, NE], F32)
        mx8 = sb("mx8", [128, 8], F32)
        el = sb("el", [128, NE], F32)
        gT = sb("gT", [NE, 128], F32)
        ebase = sb("ebase", [128, NE], F32)
        m01 = sb("m01", [128, 16, NE], BF16)
        slotv = sb("slotv", [128, NE], F32)
        rtm = sb("rtm", [128, NE], F32)
        destf = sb("destf", [128, 4], F32)
        desti = sb("desti", [128, 16, 4], I32)
        gw4 = sb("gw4", [128, 16, 4], F32)

        ps = [pst(f"ps{i}") for i in range(8)]
        psS0, psS1, psO0, psO1, psD0, psD1, psP, psT = ps
        psTb = psT[:, :].bitcast(BF16)

        d_const = dsem("d_const")
        for (dst, src) in [
            (ropet[:, :, :], rope.rearrange("p (n c) -> p n c", c=16)),
            (prevbias[:, :], prevbias_d), (cmask[:, :, :], cmask_d.rearrange("p (v q) -> p v q", v=4)),
            (maskA[:, :], maskA_d), (negI[:, :], negI_d), (identb[:, :], identb_d), (identf[:, :], identf_d),
            (lamv[:, :, :], lamv_d.rearrange("p (a b) -> p a b", a=4)), (subg[:, :], subg_d),
            (bgt[:, :], bgt_d), (bgu[:, :], bgu_d), (brt[:, :], brt_d), (bdn[:, :], bdn_d), (ebase[:, :], ebase_d),
        ]:
            SP.dma(lambda h, dst=dst, src=src: h.dma_start(out=dst, in_=src), d_const)
        CONST = d_const.ev()
        e1 = POOL.op(lambda h: h.memset(ones_bf[:, :], 1.0))
        e2 = POOL.op(lambda h: h.memset(onesF[:, :], 1.0))
        e3 = POOL.op(lambda h: h.memset(epsc[:, :], EPS))
        e4 = POOL.op(lambda h: h.memset(VVf[:, :], 1.0))
        MEMS = [e1, e2, e3, e4]
        zt = av(112 * K, [128, 8192], BF16)
        ez = POOL.op(lambda h: h.memset(zt[:, :], 0.0))
        d_z = dsem("d_z")
        SP.wait(ez)
        xg_flat = Xg.rearrange("(p n) d -> p (n d)", p=128)
        for i_ in range(NE * CAP * D // 128 // 8192):
            SP.dma(lambda h, i_=i_: h.dma_start(out=xg_flat[:, i_ * 8192:(i_ + 1) * 8192], in_=zt[:, :]), d_z)
        ZFILL = d_z.ev()
        DVE.wait(CONST)
        ev = DVE.op(lambda h: h.tensor_tensor(out=lamt[:, :, :], in0=lamv[:, 0:4:2, :], in1=lamv[:, 1:4:2, :], op=ALU.mult))
        DVE.wait(ev)
        ev = DVE.op(lambda h: h.reduce_sum(out=lams[:, 0:2], in_=lamt[:, :, :], axis=AX.X))
        ACT.wait(ev)
        ev = ACT.op(lambda h: h.activation(out=lams[:, 2:4], in_=lams[:, 0:2], func=AF.Exp))
        DVE.wait(ev)
        ev = DVE.op(lambda h: h.tensor_tensor(out=lams[:, 0:1], in0=lams[:, 3:4], in1=lams[:, 2:3], op=ALU.subtract))
        DVE.wait(ev)
        ev = DVE.op(lambda h: h.tensor_scalar(out=lams[:, 0:1], in0=lams[:, 0:1], scalar1=-LAMBDA_INIT, scalar2=None, op0=ALU.add))
        DVE.wait(ev)
        ev = DVE.op(lambda h: h.tensor_scalar(out=lams[:, 1:2], in0=subg[:, :], scalar1=1.0 - LAMBDA_INIT, scalar2=None, op0=ALU.mult))
        LAMEV = ev
        neglam = lams[:, 0:1]
        gsc = lams[:, 1:2]

        d_x = [dsem("d_x0"), dsem("d_x1")]
        xfree = [None, None]
        psT_free = None
        PE.wait(CONST)
        for blk in range(32):
            s = blk % 2
            src = (x_prev if blk < 16 else x_own)[(blk % 16) * 128:(blk % 16 + 1) * 128, :]
            POOL.wait(xfree[s])
            ld = POOL.dma(lambda h, s=s, src=src: h.dma_start(out=xld[s][:, :], in_=src), d_x[s])
            PE.wait(ld, psT_free)
            for kc in range(8):
                ev = PE.op(lambda h, s=s, kc=kc: h.transpose(out=psTb[:, kc * 128:(kc + 1) * 128], in_=xld[s][:, kc * 128:(kc + 1) * 128], identity=identb[:, :]), sig=(kc == 7))
            xfree[s] = ev
            ACT.wait(ev)
            dst = (xTp if blk < 16 else xTo)[:, :, (blk % 16) * 128:(blk % 16 + 1) * 128]
            psT_free = ACT.op(lambda h, dst=dst: h.activation(out=dst, in_=psTb[:, :].rearrange("p (k t) -> p k t", k=8), func=AF.Copy))
        XT_DONE = psT_free

        d_w = [dsem(f"d_w{i}") for i in range(3)]
        wfree = [None, None, None]
        wcnt = [0]

        def load_w(col0):
            s = wcnt[0] % 3
            wcnt[0] += 1
            POOL.wait(wfree[s])
            ev = POOL.dma(lambda h, s=s, col0=col0: h.dma_start(out=Wr[s][:, :, :], in_=w_in[:, col0:col0 + 256].rearrange("(k p) c -> p k c", p=128)), d_w[s])
            return s, ev

        st = {"psP_free": None, "psT_free": XT_DONE, "tcnt": 0, "Tfree": [None, None], "rfree": [None, None], "pend": None,
              "pf": [None, None], "pcnt": 0}
        pbanks = [psP, psD1]

        def proj_mm(xap_fn, ws, wev):
            pb = st["pcnt"] % 2
            st["pcnt"] += 1
            Pb = pbanks[pb]
            PE.wait(wev, st["pf"][pb], st["psP_free"] if pb == 0 else None, XT_DONE)
            for kc in range(8):
                ev = PE.op(lambda h, kc=kc, Pb=Pb: h.matmul(Pb[:, 0:256], lhsT=xap_fn(kc), rhs=Wr[ws][:, kc, :], start=(kc == 0), stop=(kc == 7)), sig=(kc == 7))
            wfree[ws] = ev
            return ev, pb

        def flush_pend():
            if st["pend"] is None:
                return
            ti, tev, dst = st["pend"]
            st["pend"] = None
            PE.wait(tev, st["psT_free"])
            for hh_ in range(2):
                ev = PE.op(lambda h, hh_=hh_, ti=ti: h.transpose(out=psTb[:, hh_ * 128:(hh_ + 1) * 128], in_=Tt[ti][:, hh_ * 128:(hh_ + 1) * 128], identity=identb[:, :]), sig=(hh_ == 1))
            st["Tfree"][ti] = ev
            ACT.wait(ev)
            st["psT_free"] = ACT.op(lambda h, dst=dst: h.activation(out=dst, in_=psTb[:, 0:256].rearrange("p (a t) -> p a t", a=2), func=AF.Copy))
            st["last_qk"] = st["psT_free"]

        def qk_tile(xap_fn, ws, wev, tab, dst):
            mmev, pb = proj_mm(xap_fn, ws, wev)
            flush_pend()
            ti = st["tcnt"] % 2
            st["tcnt"] += 1
            p3 = pbanks[pb][:, 0:256].rearrange("p (a c) -> p a c", a=4)
            t3 = Tt[ti][:, :].rearrange("p (a c) -> p a c", a=4)
            cos = ropet[:, tab, 0:8].unsqueeze(1).broadcast_to([128, 4, 8])
            sin = ropet[:, tab, 8:16].unsqueeze(1).broadcast_to([128, 4, 8])
            rt = rtmp[ti]
            DVE.wait(mmev, st["Tfree"][ti], CONST)
            a = DVE.op(lambda h: h.tensor_tensor(out=rt[:, :, 0:8], in0=p3[:, :, 0:8], in1=cos, op=ALU.mult), sig=False)
            a = DVE.op(lambda h: h.tensor_tensor(out=rt[:, :, 8:16], in0=p3[:, :, 8:16], in1=cos, op=ALU.mult), sig=False)
            a = DVE.op(lambda h: h.tensor_tensor(out=rt[:, :, 16:24], in0=p3[:, :, 8:16], in1=sin, op=ALU.mult), sig=False)
            a = DVE.op(lambda h: h.tensor_tensor(out=rt[:, :, 24:32], in0=p3[:, :, 0:8], in1=sin, op=ALU.mult), sig=False)
            a = DVE.op(lambda h: h.tensor_copy(out=t3[:, :, 16:64], in_=p3[:, :, 16:64]))
            st["pf"][pb] = a
            if pb == 0:
                st["psP_free"] = a
            DVE.wait(a)
            a = DVE.op(lambda h: h.tensor_tensor(out=t3[:, :, 0:8], in0=rt[:, :, 0:8], in1=rt[:, :, 16:24], op=ALU.subtract), sig=False)
            a = DVE.op(lambda h: h.tensor_tensor(out=t3[:, :, 8:16], in0=rt[:, :, 8:16], in1=rt[:, :, 24:32], op=ALU.add))
            st["pend"] = (ti, a, dst)

        def v_tile(xap_fn, ws, wev, dst, srcf):
            mmev, pb = proj_mm(xap_fn, ws, wev)
            ACT.wait(mmev, MEMS)
            a = ACT.op(lambda h: h.activation(out=dst, in_=srcf(pbanks[pb]), func=AF.Copy))
            st["pf"][pb] = a
            if pb == 0:
                st["psP_free"] = a
            st["last_v"] = a

        def xnat(blk):
            t = xTp if blk < 16 else xTo
            b = blk % 16
            return lambda kc: t[:, kc, b * 128:(b + 1) * 128]

        Vaug = VVf[:, :].rearrange("p (b h c) -> p b h c", b=32, h=4)
        bufc = [0]
        d_yb = dsem("d_yb")
        sfree = [None, None]
        ptfree = [None] * 4
        ofree = [None, None]
        ptc = [0]
        psSb = [psS0, psS1]
        psOb = [psO0, psO1]
        acc_last = [None]

        for hh in range(2):
            for g, d in enumerate(DILS):
                nb = 16 // d
                base = 1536 + g * 1536 + hh * 256
                stage_guard = acc_last[0]
                ACT.wait(stage_guard, ZFILL)
                sq, evq = load_w(base)
                sk, evk = load_w(base + 512)
                sv, evv = load_w(base + 1024)

                def xown(ob, d=d, nb=nb):
                    r, n = ob // nb, ob % nb
                    return lambda kc: xTo[:, kc, :].rearrange("p (l d) -> p d l", d=d)[:, r, n * 128:(n + 1) * 128]

                def xprev(pb, d=d):
                    return lambda kc: xTp[:, kc, 2048 - 128 * d:2048].rearrange("p (l d) -> p d l", d=d)[:, pb, :]

                for ob in range(16):
                    qk_tile(xown(ob), sq, evq, TIDX[("o", g, ob)], QT[:, :, ob * 128:(ob + 1) * 128])
                for ob in range(16):
                    qk_tile(xown(ob), sk, evk, TIDX[("o", g, ob)], KT[:, :, 2048 + ob * 128:2048 + (ob + 1) * 128])
                for pb in range(d):
                    qk_tile(xprev(pb), sk, evk, TIDX[("p", g, pb)], KT[:, :, pb * 128:(pb + 1) * 128])
                flush_pend()
                for ob in range(16):
                    v_tile(xown(ob), sv, evv, Vaug[:, 16 + ob, :, 0:64], lambda P_: P_[:, 0:256].rearrange("p (a c) -> p a c", a=4))
                for pb in range(d):
                    v_tile(xprev(pb), sv, evv, Vaug[:, pb, :, 0:64], lambda P_: P_[:, 0:256].rearrange("p (a c) -> p a c", a=4))
                PROJ_DONE = [st["last_qk"], st["last_v"]]
                PE.wait(PROJ_DONE)
                work = [(r, hl, n) for r in range(d) for hl in range(4) for n in range(nb)]

                def dil_S(item, d=d, nb=nb):
                    r, hl, n = item
                    p, s_ = hl // 2, hl % 2
                    lo, hi = s_ * 64, s_ * 64 + 64
                    ob = r * nb + n
                    qpos = ob * 128
                    if n == 0:
                        kposA, blkA = r * 128, r
                    else:
                        kposA, blkA = 2048 + (ob - 1) * 128, 16 + ob - 1
                    kposB, blkB = 2048 + ob * 128, 16 + ob
                    bi = bufc[0] % 2
                    bufc[0] += 1
                    S = psSb[bi]
                    PE.wait(sfree[bi])
                    qap = QT[lo:hi, p, qpos:qpos + 128]
                    PE.op(lambda h: h.matmul(S[:, 0:128], lhsT=KT[lo:hi, p, kposA:kposA + 128], rhs=qap, start=True, stop=False), sig=False)
                    PE.op(lambda h: h.matmul(S[:, 0:128], lhsT=negI[:, :], rhs=maskA[:, :], start=False, stop=True), sig=False)
                    PE.op(lambda h: h.matmul(S[:, 128:256], lhsT=KT[lo:hi, p, kposB:kposB + 128], rhs=qap, start=True, stop=False), sig=False)
                    sev = PE.op(lambda h: h.matmul(S[:, 128:256], lhsT=negI[:, :], rhs=cmask[:, 0, 0:128], start=False, stop=True))
                    pi = ptc[0] % 4
                    ptc[0] += 1
                    ACT.wait(sev, ptfree[pi])
                    if n == 0:
                        ACT.op(lambda h: h.activation(out=PT[pi][:, 0:128], in_=S[:, 0:128], func=AF.Exp, bias=prevbias[:, 0:1], scale=0.125), sig=False)
                        aev = ACT.op(lambda h: h.activation(out=PT[pi][:, 128:256], in_=S[:, 128:256], func=AF.Exp, scale=0.125))
                    else:
                        aev = ACT.op(lambda h: h.activation(out=PT[pi][:, 0:256], in_=S[:, 0:256], func=AF.Exp, scale=0.125))
                    sfree[bi] = aev
                    return (aev, pi, bi, blkA, blkB)

                def dil_AV(item, pend_, g=g, d=d):
                    r, hl, n = item
                    aev, pi, bi, blkA, blkB = pend_
                    O = psOb[bi]
                    PE.wait(aev, ofree[bi])
                    PE.op(lambda h: h.matmul(O[0:65, 0:128], lhsT=Vaug[:, blkA, hl, :], rhs=PT[pi][:, 0:128], start=True, stop=False), sig=False)
                    oev = PE.op(lambda h: h.matmul(O[0:65, 0:128], lhsT=Vaug[:, blkB, hl, :], rhs=PT[pi][:, 128:256], start=False, stop=True))
                    ptfree[pi] = oev
                    dst = acc[0:65, hl, :].rearrange("p (l d) -> p d l", d=d)[:, r, n * 128:(n + 1) * 128]
                    DVE.wait(oev, acc_last[0] if g > 0 else None)
                    if g == 0:
                        dev = DVE.op(lambda h: h.tensor_copy(out=dst, in_=O[0:65, 0:128]))
                    else:
                        dev = DVE.op(lambda h: h.tensor_tensor(out=dst, in0=dst, in1=O[0:65, 0:128], op=ALU.add))
                    ofree[bi] = dev
                    acc_last[0] = dev

                pend_ = dil_S(work[0])
                for wi, item in enumerate(work):
                    nxt = dil_S(work[wi + 1]) if wi + 1 < len(work) else None
                    dil_AV(item, pend_)
                    pend_ = nxt
            DVE.wait(st.get("ybstore"))
            for hl in range(4):
                for w in range(4):
                    PE.wait(acc_last[0], MEMS, st.get("dfree"))
                    bev = PE.op(lambda h, hl=hl, w=w: h.matmul(psD0[0:64, :], lhsT=onesF[64:65, 0:64], rhs=acc[64:65, hl, w * 512:(w + 1) * 512], start=True, stop=True))
                    DVE.wait(bev)
                    rev = DVE.op(lambda h: h.reciprocal(out=ftA[0:64, :], in_=psD0[0:64, :]))
                    st["dfree"] = rev
                    DVE.wait(rev)
                    fev = DVE.op(lambda h, hl=hl, w=w: h.tensor_tensor(out=ybH[0:64, hl, w * 512:(w + 1) * 512], in0=acc[0:64, hl, w * 512:(w + 1) * 512], in1=ftA[0:64, :], op=ALU.mult))
                    acc_last[0] = fev
            SP.wait(acc_last[0])
            st["ybstore"] = SP.dma(lambda h, hh=hh: h.dma_start(out=ybd[:, hh * 8192:(hh + 1) * 8192], in_=ybH[:, :, :].rearrange("p h t -> p (h t)")), d_yb)
        YB_DONE = [acc_last[0], st["ybstore"]]

        Vd = VVf[:, 0:32 * 256].rearrange("p (b c) -> p b c", b=32)
        psOd = [psO0, psO1]
        psDd = [psD0, psD1]
        fin_free = YB_DONE
        st["sbf"] = [[sfree[0], sfree[1]], None]
        st["acc_prev"] = [None, None]
        att_last = YB_DONE
        ya_last = None
        for pp in range(2):
            ACT.wait(att_last)
            DVE.wait(att_last)
            sq, evq = load_w(pp * 256)
            sk, evk = load_w(512 + pp * 256)
            sv, evv = load_w(1024 + pp * 256)
            for ob in range(16):
                qk_tile(xnat(16 + ob), sq, evq, TIDX[("n", 0, 16 + ob)], QT[:, :, ob * 128:(ob + 1) * 128])
            for blk in range(32):
                qk_tile(xnat(blk), sk, evk, TIDX[("n", 0, blk)], KT[:, :, blk * 128:(blk + 1) * 128])
            flush_pend()
            for blk in range(32):
                v_tile(xnat(blk), sv, evv, Vd[:, blk, :], lambda P_: P_[:, 0:256])
            PE.wait(st["last_qk"], st["last_v"])
            for hl in range(2):
                for j in range(4):
                    nkb = 16 + 4 * (j + 1)
                    PE.wait(fin_free, st["pf"][1], st["pf"][0], st["psP_free"], st["psT_free"])
                    SBK = [(psS0, psS1), (psP, psT)]
                    accD = [av(106 * K, [128, 512], F32), av(108 * K, [128, 512], F32)]
                    accE = [DVE, POOL]

                    def dif_S(kb, hl=hl, j=j):
                        bp = bufc[0] % 2
                        bufc[0] += 1
                        diag = kb >= 16 + 4 * j
                        PE.wait(st["sbf"][bp])
                        for c in range(2):
                            lo, hi = c * 64, c * 64 + 64
                            S = SBK[bp][c]
                            sev = PE.op(lambda h, S=S, lo=lo, hi=hi: h.matmul(S[:, :], lhsT=KT[lo:hi, hl, kb * 128:(kb + 1) * 128], rhs=QT[lo:hi, hl, j * 512:(j + 1) * 512], start=True, stop=not diag), sig=(c == 1 and not diag))
                            if diag:
                                v = kb - 16 - 4 * j
                                sev = PE.op(lambda h, S=S, v=v: h.matmul(S[:, :], lhsT=negI[:, :], rhs=cmask[:, v, :], start=False, stop=True), sig=(c == 1))
                        pis = [(2 * bp) % 4, (2 * bp + 1) % 4]
                        ACT.wait(sev, ptfree[pis[0]], ptfree[pis[1]])
                        for c in range(2):
                            S = SBK[bp][c]
                            if kb < 16:
                                aev = ACT.op(lambda h, S=S, c=c: h.activation(out=PT[pis[c]][:, :], in_=S[:, :], func=AF.Exp, bias=prevbias[:, 0:1], scale=0.125), sig=(c == 1))
                            else:
                                aev = ACT.op(lambda h, S=S, c=c: h.activation(out=PT[pis[c]][:, :], in_=S[:, :], func=AF.Exp, scale=0.125), sig=(c == 1))
                        st["sbf"][bp] = aev
                        if bp == 1:
                            st["psP_free"] = aev
                            st["psT_free"] = aev
                            st["pf"][0] = aev
                        else:
                            sfree[0] = aev
                            sfree[1] = aev
                        return (aev, pis)

                    def dif_AV(kb, pend_, hl=hl, nkb=nkb):
                        aev, pis = pend_
                        even = (kb % 2 == 0)
                        PE.wait(aev)
                        PE.op(lambda h: h.matmul(psOd[0][:, :], lhsT=Vd[:, kb, hl * 128:(hl + 1) * 128], rhs=PT[pis[0]][:, :], start=(kb == 0), stop=(kb == nkb - 1)), sig=False)
                        oev_ = PE.op(lambda h: h.matmul(psOd[1][:, :], lhsT=Vd[:, kb, hl * 128:(hl + 1) * 128], rhs=PT[pis[1]][:, :], start=(kb == 0), stop=(kb == nkb - 1)), sig=not even)
                        if even:
                            PE.op(lambda h: h.matmul(psDd[0][:, :], lhsT=ones_bf[:, :], rhs=PT[pis[0]][:, :], start=(kb == 0), stop=False), sig=False)
                            oev_ = PE.op(lambda h: h.matmul(psDd[1][:, :], lhsT=ones_bf[:, :], rhs=PT[pis[1]][:, :], start=(kb == 0), stop=False))
                            ptfree[pis[0]] = oev_
                            ptfree[pis[1]] = oev_
                            return oev_
                        devs = []
                        for c in range(2):
                            E_ = accE[c]
                            E_.wait(aev, st["acc_prev"][c])
                            if kb == 1:
                                dv = E_.op(lambda h, c=c: h.tensor_copy(out=accD[c][:, :], in_=PT[pis[c]][:, :]))
                            else:
                                dv = E_.op(lambda h, c=c: h.tensor_tensor(out=accD[c][:, :], in0=accD[c][:, :], in1=PT[pis[c]][:, :], op=ALU.add))
                            st["acc_prev"][c] = dv
                            devs.append(dv)
                        ptfree[pis[0]] = [oev_, devs[0]]
                        ptfree[pis[1]] = [oev_, devs[1]]
                        return oev_

                    pend_ = dif_S(0)
                    for kb in range(nkb):
                        nxt = dif_S(kb + 1) if kb + 1 < nkb else None
                        oev = dif_AV(kb, pend_)
                        pend_ = nxt
                    PE.wait(st["acc_prev"][0], st["acc_prev"][1], MEMS)
                    PE.op(lambda h: h.matmul(psD0[:, :], lhsT=onesF[:, :], rhs=accD[0][:, :], start=False, stop=True), sig=False)
                    oev = PE.op(lambda h: h.matmul(psD1[:, :], lhsT=onesF[:, :], rhs=accD[1][:, :], start=False, stop=True))
                    st["acc_prev"] = [oev, oev]
                    DVE.wait(oev, LAMEV, ya_last)
                    a = DVE.op(lambda h: h.reciprocal(out=ft[0][:, :], in_=psD0[:, :]), sig=False)
                    a = DVE.op(lambda h: h.reciprocal(out=ft[1][:, :], in_=psD1[:, :]))
                    DVE.wait(a)
                    a = DVE.op(lambda h: h.tensor_tensor(out=ft[0][:, :], in0=psO0[:, :], in1=ft[0][:, :], op=ALU.mult), sig=False)
                    a = DVE.op(lambda h: h.tensor_tensor(out=ft[1][:, :], in0=psO1[:, :], in1=ft[1][:, :], op=ALU.mult))
                    fin_free = a
                    st["pf"][1] = a
                    DVE.wait(a)
                    a = DVE.op(lambda h: h.scalar_tensor_tensor(out=ft[2][:, :], in0=ft[1][:, :], scalar=neglam, in1=ft[0][:, :], op0=ALU.mult, op1=ALU.add))
                    DVE.wait(a)
                    a = DVE.op(lambda h: h.tensor_tensor(out=ft[3][:, :], in0=ft[2][:, :], in1=ft[2][:, :], op=ALU.mult))
                    PE.wait(a, st["sbf"][0])
                    m = PE.op(lambda h: h.matmul(psS0[:, :], lhsT=onesF[:, :], rhs=ft[3][:, :], start=True, stop=True))
                    ACT.wait(m)
                    a = ACT.op(lambda h: h.activation(out=ft[4][:, :], in_=psS0[:, :], func=AF.Sqrt, bias=epsc[:, 0:1], scale=1.0 / 128.0))
                    st["sbf"][0] = a
                    sfree[0] = a
                    DVE.wait(a)
                    a = DVE.op(lambda h: h.reciprocal(out=ft[4][:, :], in_=ft[4][:, :]))
                    DVE.wait(a)
                    a = DVE.op(lambda h, pp=pp, hl=hl, j=j: h.scalar_tensor_tensor(out=yaT[:, 2 * pp + hl, j * 512:(j + 1) * 512], in0=ft[2][:, :], scalar=gsc, in1=ft[4][:, :], op0=ALU.mult, op1=ALU.mult))
                    ya_last = a
                    att_last = a
        YA_DONE = ya_last

        d_dbg = dsem("d_dbg")
        if debug:
            SP.wait(YA_DONE, YB_DONE)
            SP.dma(lambda h: h.dma_start(out=dbg_ya, in_=yaT[:, :, :].rearrange("p h t -> p (h t)")), d_dbg)

        d_pw = dsem("d_pw")
        POOL.wait(YA_DONE, YB_DONE)
        SP.wait(YA_DONE, YB_DONE)
        if debug:
            POOL.wait(d_dbg.ev())
            SP.wait(d_dbg.ev())
        POOL.dma(lambda h: h.dma_start(out=woa[:, :, :], in_=w_oa.rearrange("(k p) c -> p k c", p=128)), d_pw)
        POOL.dma(lambda h: h.dma_start(out=wob[:, :, :], in_=w_ob.rearrange("(k p) c -> p k c", p=64)), d_pw)
        POOL.dma(lambda h: h.dma_start(out=wo[:, :, :], in_=w_o.rearrange("(k p) c -> p k c", p=128)), d_pw)
        SP.dma(lambda h: h.dma_start(out=lnp1[:, :, :], in_=lnp_d[:, 0:2 * D].rearrange("p (a c) -> p a c", a=2)), d_pw)
        SP.dma(lambda h: h.dma_start(out=wrt[:, :, :], in_=w_router.rearrange("(k p) c -> p k c", p=128)), d_pw)
        PW = d_pw.ev()

        h1T = xTp
        d_xf = dsem("d_xf")
        d_h1 = dsem("d_h1")
        d_ybw = dsem("d_ybw")
        d_sc = dsem("d_sc")
        h1b = av(177 * K, [128, D], BF16)
        xf_free = None
        ybw_free = None
        g_last = {"ga": None, "gb": None, "mT": None, "h1Tf": None, "misc": None}

        def layer_norm(buf, lnv, pre_wait):
            DVE.wait(pre_wait)
            DVE.op(lambda h: h.bn_stats(out=stt[:, 0, :], in_=buf[:, 0:512]), sig=False)
            a = DVE.op(lambda h: h.bn_stats(out=stt[:, 1, :], in_=buf[:, 512:1024]))
            DVE.wait(a)
            a = DVE.op(lambda h: h.bn_aggr(out=mv[:, :], in_=stt[:, :, :].rearrange("p a b -> p (a b)")))
            ACT.wait(a)
            a = ACT.op(lambda h: h.activation(out=sm[:, 0:1], in_=mv[:, 1:2], func=AF.Sqrt, bias=epsc[:, 0:1], scale=1.0))
            DVE.wait(a)
            a = DVE.op(lambda h: h.reciprocal(out=sm[:, 1:2], in_=sm[:, 0:1]))
            DVE.wait(a)
            a = DVE.op(lambda h: h.tensor_scalar(out=buf[:, :], in0=buf[:, :], scalar1=mv[:, 0:1], scalar2=sm[:, 1:2], op0=ALU.subtract, op1=ALU.mult))
            DVE.wait(a)
            a = DVE.op(lambda h: h.tensor_tensor(out=buf[:, :], in0=buf[:, :], in1=lnv[:, 0, :], op=ALU.mult))
            DVE.wait(a)
            a = DVE.op(lambda h: h.tensor_tensor(out=buf[:, :], in0=buf[:, :], in1=lnv[:, 1, :], op=ALU.add))
            return a

        xf3r = [xf3, av(0, [128, D], F32)]
        h1Tfr = [h1Tf, av(4 * K, [128, 8, 128], F32)]
        d_xfr = [dsem("d_xfr0"), dsem("d_xfr1")]
        d_h1r = [dsem("d_h1r0"), dsem("d_h1r1")]
        xf_fr = [None, None]
        h1Tf_fr = [None, None]
        tr_ev = {}
        stv_ev = {}
        pendB = [None]

        def stageA(tb, tbl, mt_ready):
            s_ = tb % 2
            xf = xf3r[s_]
            hT = h1Tfr[s_]
            SP.wait(xf_fr[s_])
            xev = SP.dma(lambda h: h.dma_start(out=xf[:, :], in_=x_own[tb * 128:(tb + 1) * 128, :]), d_xfr[s_])
            PE.wait(mt_ready, st.get("dfree2"))
            for dh in range(2):
                for fc in range(8):
                    e = PE.op(lambda h, dh=dh, fc=fc: h.matmul(psDd[dh][:, :], lhsT=mT[:, fc, tbl * 128:(tbl + 1) * 128], rhs=wo[:, fc, dh * 512:(dh + 1) * 512], start=(fc == 0), stop=(fc == 7)), sig=(fc == 7 and dh == 1))
            mm_o = e
            if tbl == 3:
                g_last["mT"] = mm_o
            DVE.wait(mm_o, xev)
            DVE.op(lambda h: h.scalar_tensor_tensor(out=xf[:, 0:512], in0=xf[:, 0:512], scalar=ALPHA, in1=psD0[:, :], op0=ALU.mult, op1=ALU.add), sig=False)
            a = DVE.op(lambda h: h.scalar_tensor_tensor(out=xf[:, 512:1024], in0=xf[:, 512:1024], scalar=ALPHA, in1=psD1[:, :], op0=ALU.mult, op1=ALU.add))
            st["dfree2"] = a
            h1ev = layer_norm(xf, lnp1, a)
            SP.wait(h1ev)
            stv_ev[tb] = SP.dma(lambda h: h.dma_start(out=h1d[tb * 128:(tb + 1) * 128, :], in_=xf[:, :]), d_h1r[s_])
            for half in range(2):
                PE.wait(h1ev, st["psP_free"])
                for q_ in range(4):
                    kc = half * 4 + q_
                    e = PE.op(lambda h, kc=kc, q_=q_: h.transpose(out=psP[:, q_ * 128:(q_ + 1) * 128], in_=xf[:, kc * 128:(kc + 1) * 128], identity=identf[:, :]), sig=(q_ == 3))
                ACT.wait(e, h1Tf_fr[s_] if half == 0 else None)
                a = ACT.op(lambda h, half=half: h.activation(out=hT[:, half * 4:(half + 1) * 4, :], in_=psP[:, :].rearrange("p (k t) -> p k t", k=4), func=AF.Copy))
                st["psP_free"] = a
            tr_ev[tb] = (a, e)

        def stageB(tb):
            s_ = tb % 2
            xf = xf3r[s_]
            hT = h1Tfr[s_]
            a, e_tr = tr_ev[tb]
            PE.wait(a, sfree[0])
            for kc in range(8):
                e = PE.op(lambda h, kc=kc: h.matmul(psS0[:, 0:NE], lhsT=hT[:, kc, :], rhs=wrt[:, kc, :], start=(kc == 0), stop=(kc == 7)), sig=(kc == 7))
            h1Tf_fr[s_] = e
            DVE.wait(e)
            a = DVE.op(lambda h: h.tensor_tensor(out=lg[:, :], in0=psS0[:, 0:NE], in1=brt[:, :], op=ALU.add))
            sfree[0] = a
            DVE.wait(a)
            a = DVE.op(lambda h: h.max(out=mx8[:, :], in_=lg[:, :]))
            DVE.wait(a)
            a = DVE.op(lambda h: h.tensor_scalar(out=sm[:, 2:3], in0=mx8[:, 0:1], scalar1=-1.0, scalar2=None, op0=ALU.mult))
            ACT.wait(a)
            ACT.op(lambda h: h.activation(out=sm[:, 4:8], in_=mx8[:, 0:4], func=AF.Exp, bias=sm[:, 2:3], scale=1.0), sig=False)
            a = ACT.op(lambda h: h.activation(out=el[:, :], in_=lg[:, :], func=AF.Exp, bias=sm[:, 2:3], scale=1.0))
            DVE.wait(a)
            a = DVE.op(lambda h: h.reduce_sum(out=sm[:, 3:4], in_=sm[:, 4:8], axis=AX.X))
            DVE.wait(a)
            a = DVE.op(lambda h: h.reciprocal(out=rsm[:, tb:tb + 1], in_=sm[:, 3:4]))
            DVE.wait(a)
            a = DVE.op(lambda h: h.tensor_scalar(out=el[:, :], in0=el[:, :], scalar1=rsm[:, tb:tb + 1], scalar2=None, op0=ALU.mult))
            DVE.wait(a)
            a = DVE.op(lambda h: h.scalar_tensor_tensor(out=gwd[:, tb, :], in0=lg[:, :], scalar=mx8[:, 3:4], in1=el[:, :], op0=ALU.is_ge, op1=ALU.mult))
            a = DVE.op(lambda h: h.tensor_scalar(out=m01[:, tb, :], in0=lg[:, :], scalar1=mx8[:, 3:4], scalar2=None, op0=ALU.is_ge))
            PE.wait(a, sfree[1])
            for tb2 in range(tb + 1):
                e = PE.op(lambda h, tb2=tb2: h.matmul(psS1[:, 0:NE], lhsT=(maskA[:, :] if tb2 == tb else ones_bf[:, :]), rhs=m01[:, tb2, :], start=(tb2 == 0), stop=(tb2 == tb)), sig=(tb2 == tb))
            DVE.wait(e)
            a = DVE.op(lambda h: h.tensor_tensor(out=slotv[:, :], in0=psS1[:, 0:NE], in1=ebase[:, :], op=ALU.add))
            sfree[1] = a
            DVE.wait(a)
            for k_ in range(4):
                DVE.op(lambda h, k_=k_: h.scalar_tensor_tensor(out=rtm[:, :], in0=lg[:, :], scalar=mx8[:, k_:k_ + 1], in1=slotv[:, :], op0=ALU.is_equal, op1=ALU.mult))
                DVE.wait((DVE.sem, DVE.n, DVE.name))
                DVE.op(lambda h, k_=k_: h.reduce_sum(out=destf[:, k_:k_ + 1], in_=rtm[:, :], axis=AX.X))
                DVE.wait((DVE.sem, DVE.n, DVE.name))
            a = DVE.op(lambda h: h.tensor_copy(out=desti[:, tb, :], in_=destf[:, :]))
            DVE.op(lambda h: h.tensor_scalar(out=gw4[:, tb, :], in0=sm[:, 4:8], scalar1=rsm[:, tb:tb + 1], scalar2=None, op0=ALU.mult))
            DVE.wait(st.get("h1b_free"))
            a = DVE.op(lambda h: h.tensor_copy(out=h1b[:, :], in_=xf[:, :]))
            POOL.wait(a, ZFILL)
            for k_ in range(4):
                sc = POOL.dma(lambda h, k_=k_: h.indirect_dma_start(out=Xg[:, :], out_offset=bass.IndirectOffsetOnAxis(ap=desti[:, tb, k_:k_ + 1], axis=0), in_=h1b[:, :], in_offset=None), d_sc)
            st["h1b_free"] = sc
            xf_fr[s_] = [stv_ev[tb], e_tr, a]
            g_last["misc"] = a
            st["route_done"] = a

        PE.wait(PW, YA_DONE, YB_DONE)
        DVE.wait(PW)
        for w in range(4):
            tok = slice(w * 512, (w + 1) * 512)
            SP.wait(ybw_free)
            ybw_ev = SP.dma(lambda h, tok=tok: h.dma_start(out=ybw[:, :, :], in_=ybd.rearrange("p (h t) -> p h t", h=8)[:, :, tok]), d_ybw)
            for cp in range(4):
                sA, evA = load_w(6144 + cp * 256)
                sB, evB = load_w(7168 + cp * 256)
                for fcl in range(2):
                    fc = 2 * cp + fcl
                    PE.wait(evA, sfree[0])
                    for kc in range(8):
                        e = PE.op(lambda h, kc=kc, sA=sA, fcl=fcl, tok=tok: h.matmul(psS0[:, :], lhsT=Wr[sA][:, kc, fcl * 128:(fcl + 1) * 128], rhs=xTo[:, kc, tok], start=(kc == 0), stop=(kc == 7)), sig=(kc == 7))
                    if fcl == 1:
                        wfree[sA] = e
                    ACT.wait(e, g_last["ga"], CONST)
                    ga = ACT.op(lambda h, fc=fc: h.activation(out=ft[0][:, :], in_=psS0[:, :], func=AF.Sigmoid, bias=bgt[:, fc:fc + 1], scale=1.0))
                    sfree[0] = ga
                    PE.wait(evB, sfree[1])
                    for kc in range(8):
                        e = PE.op(lambda h, kc=kc, sB=sB, fcl=fcl, tok=tok: h.matmul(psS1[:, :], lhsT=Wr[sB][:, kc, fcl * 128:(fcl + 1) * 128], rhs=xTo[:, kc, tok], start=(kc == 0), stop=(kc == 7)), sig=(kc == 7))
                    if fcl == 1:
                        wfree[sB] = e
                    ACT.wait(e, g_last["gb"])
                    gb = ACT.op(lambda h, fc=fc: h.activation(out=ft[1][:, :], in_=psS1[:, :], func=AF.Sigmoid, bias=bgt[:, 8 + fc:9 + fc], scale=1.0))
                    sfree[1] = gb
                    PE.wait(ofree[0])
                    for kc in range(4):
                        e = PE.op(lambda h, kc=kc, fc=fc, tok=tok: h.matmul(psO0[:, :], lhsT=woa[:, kc, fc * 128:(fc + 1) * 128], rhs=yaT[:, kc, tok], start=(kc == 0), stop=(kc == 3)), sig=(kc == 3))
                    oa = e
                    PE.wait(ofree[1], ybw_ev)
                    for hh_ in range(8):
                        e = PE.op(lambda h, hh_=hh_, fc=fc: h.matmul(psO1[:, :], lhsT=wob[0:64, hh_, fc * 128:(fc + 1) * 128], rhs=ybw[0:64, hh_, :], start=(hh_ == 0), stop=(hh_ == 7)), sig=(hh_ == 7))
                    obv = e
                    if fc == 7:
                        ybw_free = e
                    DVE.wait(ga, oa, g_last["misc"])
                    a = DVE.op(lambda h: h.tensor_tensor(out=ft[2][:, :], in0=psO0[:, :], in1=ft[0][:, :], op=ALU.mult))
                    ofree[0] = a
                    g_last["ga"] = a
                    DVE.wait(gb, obv)
                    b = DVE.op(lambda h: h.tensor_tensor(out=ft[3][:, :], in0=psO1[:, :], in1=ft[1][:, :], op=ALU.mult))
                    ofree[1] = b
                    g_last["gb"] = b
                    DVE.wait(a, b, g_last["mT"] if fc == 0 else None)
                    c_ = DVE.op(lambda h, fc=fc: h.tensor_tensor(out=mT[:, fc, :], in0=ft[2][:, :], in1=ft[3][:, :], op=ALU.add))
                    g_last["misc"] = c_
            MT_READY = c_
            for tbl in range(4):
                tb = w * 4 + tbl
                stageA(tb, tbl, MT_READY)
                if pendB[0] is not None:
                    stageB(pendB[0])
                pendB[0] = tb
        stageB(pendB[0])
        ROUTE_DONE = st["route_done"]
        xf_free = [xf_fr[0], xf_fr[1]]
        P3_DONE = [ROUTE_DONE, st["psP_free"], xf_free]
        H1D_DONE = [d_h1r[0].ev(), d_h1r[1].ev()]

        Wd = [av(i * 16 * K, [128, 8, D], BF16) for i in range(2)]
        Xe = [av(32 * K + i * 6 * K, [128, 3, D], BF16) for i in range(2)]
        XT = [av(44 * K + i * 6 * K, [128, 8, CAP], BF16) for i in range(2)]
        actT = [av(56 * K + i * 6 * K, [128, 8, CAP], BF16) for i in range(2)]
        Yt = [av(68 * K + i * 4 * K, [128, D], F32) for i in range(2)]
        Wgu = [av(112 * K + i * 32 * K, [128, 8, 2048], BF16) for i in range(2)]
        tg = [av(100 * K + i * 6 * K, [128, CAP], F32) for i in range(2)]
        tsg = [av(102 * K + i * 6 * K, [128, CAP], F32) for i in range(2)]
        tu = [av(104 * K + i * 6 * K, [128, CAP], F32) for i in range(2)]
        d_gu = [dsem(f"d_gu{i}") for i in range(2)]
        d_dn = [dsem(f"d_dn{i}") for i in range(2)]
        d_xe = [dsem("d_xe0"), dsem("d_xe1")]
        d_yg = [dsem("d_yg0"), dsem("d_yg1")]
        gu_free = [None] * 2
        dn_free = [None] * 2
        xe_free = [None, None]
        yt_free = [None, None]
        items = [(e, q) for e in range(NE) for q in range(4)]
        gu_ev = {}
        dn_ev = {}
        xe_ev = {}
        xt_ready = {}

        def issue_gu(e):
            s = e % 2
            POOL.wait(gu_free[s])
            gu_ev[e] = POOL.dma(lambda h, s=s, e=e: h.dma_start(out=Wgu[s][:, :, :], in_=w_gu[e].rearrange("(k p) c -> p k c", p=128)), d_gu[s])

        def issue_dn(e):
            s = e % 2
            POOL.wait(dn_free[s])
            dn_ev[e] = POOL.dma(lambda h, s=s, e=e: h.dma_start(out=Wd[s][:, :, :], in_=w_down[e].rearrange("(k p) c -> p k c", p=128)), d_dn[s])

        def issue_xe(e):
            s = e % 2
            SP.wait(xe_free[s])
            xe_ev[e] = SP.dma(lambda h, s=s, e=e: h.dma_start(out=Xe[s][:, :, :], in_=Xg[e * CAP:(e + 1) * CAP, :].rearrange("(n p) d -> p n d", p=128)), d_xe[s])

        def do_transposes(e):
            s = e % 2
            for n in range(3):
                PE.wait(xe_ev[e], st["psT_free"])
                for kc in range(8):
                    ev_ = PE.op(lambda h, kc=kc, n=n, s=s: h.transpose(out=psTb[:, kc * 128:(kc + 1) * 128], in_=Xe[s][:, n, kc * 128:(kc + 1) * 128], identity=identb[:, :]), sig=(kc == 7))
                ACT.wait(ev_)
                st["psT_free"] = ACT.op(lambda h, n=n, s=s: h.activation(out=XT[s][:, :, n * 128:(n + 1) * 128], in_=psTb[:, :].rearrange("p (k t) -> p k t", k=8), func=AF.Copy))
            xe_free[s] = ev_
            xt_ready[e] = st["psT_free"]

        SCAT_DONE = d_sc.ev()
        POOL.wait(P3_DONE, H1D_DONE)
        PE.wait(P3_DONE)
        DVE.wait(P3_DONE, H1D_DONE)
        ACT.wait(P3_DONE, H1D_DONE)
        SP.wait(P3_DONE, SCAT_DONE)
        issue_gu(0)
        issue_dn(0)
        issue_xe(0)
        issue_xe(1)
        do_transposes(0)
        banks = [(psS0, psS1), (psD0, psD1)]
        bfree = [[sfree[0], sfree[1]], [st.get("dfree2"), st.get("dfree2")]]
        obk = [psO0, psO1, psP]
        obkfree = [ofree[0], ofree[1], st["psP_free"]]
        tfree = [None, None]
        stepc = 0
        ocnt = 0
        act_last = None
        for e in range(NE):
            xs = e % 2
            if e + 1 < NE:
                issue_gu(e + 1)
                issue_dn(e + 1)
            s = e % 2
            for q in range(4):
                i = e
                for jj in range(2):
                    j = 2 * q + jj
                    b = stepc % 2
                    stepc += 1
                    Pg, Pu = banks[b]
                    PE.wait(gu_ev[i], xt_ready[e], bfree[b][0], bfree[b][1])
                    for kc in range(8):
                        eg = PE.op(lambda h, kc=kc, Pg=Pg, s=s, j=j, xs=xs: h.matmul(Pg[:, 0:CAP], lhsT=Wgu[s][:, kc, j * 128:(j + 1) * 128], rhs=XT[xs][:, kc, :], start=(kc == 0), stop=(kc == 7)), sig=(kc == 7))
                    for kc in range(8):
                        eu = PE.op(lambda h, kc=kc, Pu=Pu, s=s, j=j, xs=xs: h.matmul(Pu[:, 0:CAP], lhsT=Wgu[s][:, kc, 1024 + j * 128:1024 + (j + 1) * 128], rhs=XT[xs][:, kc, :], start=(kc == 0), stop=(kc == 7)), sig=(kc == 7))
                    if j == 7:
                        gu_free[s] = eu
                    DVE.wait(eg, tfree[b])
                    a1 = DVE.op(lambda h, Pg=Pg, e=e, j=j, b=b: h.tensor_scalar(out=tg[b][:, :], in0=Pg[:, 0:CAP], scalar1=bgu[:, e * 16 + j:e * 16 + j + 1], scalar2=7.0, op0=ALU.add, op1=ALU.min))
                    bfree[b][0] = a1
                    ACT.wait(a1, eu)
                    ACT.op(lambda h, b=b: h.activation(out=tsg[b][:, :], in_=tg[b][:, :], func=AF.Sigmoid, scale=1.702), sig=False)
                    a2 = ACT.op(lambda h, Pu=Pu, e=e, j=j, b=b: h.activation(out=tu[b][:, :], in_=Pu[:, 0:CAP], func=AF.Identity, bias=bgu[:, e * 16 + 8 + j:e * 16 + 9 + j], scale=1.0))
                    bfree[b][1] = a2
                    DVE.wait(a2)
                    a3 = DVE.op(lambda h, b=b: h.tensor_scalar(out=tu[b][:, :], in0=tu[b][:, :], scalar1=7.0, scalar2=-7.0, op0=ALU.min, op1=ALU.max))
                    DVE.wait(a3)
                    a4 = DVE.op(lambda h, b=b: h.scalar_tensor_tensor(out=tu[b][:, :], in0=tu[b][:, :], scalar=1.0, in1=tg[b][:, :], op0=ALU.add, op1=ALU.mult))
                    DVE.wait(a4)
                    a5 = DVE.op(lambda h, j=j, b=b, xs=xs: h.tensor_tensor(out=actT[xs][:, j, :], in0=tu[b][:, :], in1=tsg[b][:, :], op=ALU.mult))
                    tfree[b] = a5
                    act_last = a5
            if e + 1 < NE:
                do_transposes(e + 1)
            if e + 2 < NE:
                issue_xe(e + 2)
            ds_ = e % 2
            PE.wait(act_last, dn_ev[e])
            for nb_ in range(3):
                ys = (e * 3 + nb_) % 2
                for dh in range(2):
                    oi = ocnt % 3
                    ocnt += 1
                    O = obk[oi]
                    PE.wait(obkfree[oi])
                    for fc in range(8):
                        em = PE.op(lambda h, O=O, fc=fc, nb_=nb_, dh=dh, ds_=ds_, xs=xs: h.matmul(O[:, :], lhsT=actT[xs][:, fc, nb_ * 128:(nb_ + 1) * 128], rhs=Wd[ds_][:, fc, dh * 512:(dh + 1) * 512], start=(fc == 0), stop=(fc == 7)), sig=(fc == 7))
                    ACT.wait(em, yt_free[ys] if dh == 0 else None)
                    ac = ACT.op(lambda h, O=O, ys=ys, dh=dh: h.activation(out=Yt[ys][:, dh * 512:(dh + 1) * 512], in_=O[:, :], func=AF.Copy))
                    obkfree[oi] = ac
                SP.wait(ac)
                yt_free[ys] = SP.dma(lambda h, ys=ys, e=e, nb_=nb_: h.dma_start(out=Yg[e * CAP + nb_ * 128:e * CAP + (nb_ + 1) * 128, :], in_=Yt[ys][:, :]), d_yg[ys])
            dn_free[ds_] = em
        MOE_DONE = [ac, em, d_yg[0].ev(), d_yg[1].ev()]

        G = [[av((s_ * 4 + k_) * 4 * K, [128, D], F32) for k_ in range(4)] for s_ in range(2)]
        d_hl = [dsem("d_hl0"), dsem("d_hl1")]
        d_out = [dsem("d_out0"), dsem("d_out1")]
        d_g = [dsem("d_g0"), dsem("d_g1")]
        d_l2 = dsem("d_l2")
        buf_free = [None, None]
        g_free = [None, None]
        gT_free = None
        SP.wait(H1D_DONE, MOE_DONE)
        POOL.wait(MOE_DONE)
        PE.wait(MOE_DONE)
        DVE.wait(MOE_DONE)
        l2ev = SP.dma(lambda h: h.dma_start(out=lnp2[:, :, :], in_=lnp_d[:, 2 * D:4 * D].rearrange("p (a c) -> p a c", a=2)), d_l2)
        o5free = [obkfree[0], obkfree[1]]

        def issue_gather(tb):
            s = tb % 2
            POOL.wait(g_free[s])
            for k_ in range(4):
                gev_ = POOL.dma(lambda h, s=s, tb=tb, k_=k_: h.indirect_dma_start(out=G[s][k_][:, :], out_offset=None, in_=Yg[:, :], in_offset=bass.IndirectOffsetOnAxis(ap=desti[:, tb, k_:k_ + 1], axis=0)), d_g[s])
            return gev_

        gq = {0: issue_gather(0)}
        for tb in range(16):
            s = tb % 2
            if tb + 1 < 16:
                gq[tb + 1] = issue_gather(tb + 1)
            SP.wait(buf_free[s])
            hev = SP.dma(lambda h, s=s, tb=tb: h.dma_start(out=xf5[s][:, :], in_=h1d[tb * 128:(tb + 1) * 128, :]), d_hl[s])
            PE.wait(bfree[0][0], bfree[0][1], gT_free)
            e = PE.op(lambda h, tb=tb: h.transpose(out=psS0[0:NE, 0:128], in_=gwd[:, tb, :], identity=identf[:, :]))
            ACT.wait(e, gT_free)
            a = ACT.op(lambda h: h.activation(out=gT[:, :], in_=psS0[0:NE, 0:128], func=AF.Copy))
            bfree[0][0] = a
            PE.wait(a, o5free[0], o5free[1])
            for dh in range(2):
                e = PE.op(lambda h, dh=dh: h.matmul([psO0, psO1][dh][:, :], lhsT=gT[:, :], rhs=bdn[:, dh * 512:(dh + 1) * 512], start=True, stop=True))
            gT_free = e
            G0 = G[s][0]
            POOL.wait(gq[tb])
            a = POOL.op(lambda h, G0=G0, tb=tb: h.tensor_scalar(out=G0[:, :], in0=G0[:, :], scalar1=gw4[:, tb, 0:1], scalar2=None, op0=ALU.mult))
            for k_ in range(1, 4):
                b_ = POOL.op(lambda h, s=s, k_=k_, tb=tb: h.tensor_scalar(out=G[s][k_][:, :], in0=G[s][k_][:, :], scalar1=gw4[:, tb, k_:k_ + 1], scalar2=None, op0=ALU.mult))
                POOL.wait(a, b_)
                a = POOL.op(lambda h, G0=G0, s=s, k_=k_: h.tensor_tensor(out=G0[:, :], in0=G0[:, :], in1=G[s][k_][:, :], op=ALU.add))
            DVE.wait(a, e)
            DVE.op(lambda h, G0=G0: h.tensor_tensor(out=G0[:, 0:512], in0=G0[:, 0:512], in1=psO0[:, :], op=ALU.add), sig=False)
            a = DVE.op(lambda h, G0=G0: h.tensor_tensor(out=G0[:, 512:1024], in0=G0[:, 512:1024], in1=psO1[:, :], op=ALU.add))
            o5free = [a, a]
            DVE.wait(a, hev)
            a = DVE.op(lambda h, s=s, G0=G0: h.scalar_tensor_tensor(out=xf5[s][:, :], in0=xf5[s][:, :], scalar=ALPHA, in1=G0[:, :], op0=ALU.mult, op1=ALU.add))
            g_free[s] = a
            DVE.wait(l2ev)
            oev = layer_norm(xf5[s], lnp2, a)
            SP.wait(oev)
            buf_free[s] = SP.dma(lambda h, s=s, tb=tb: h.dma_start(out=out_d[tb * 128:(tb + 1) * 128, :], in_=xf5[s][:, :]), d_out[s])
        SP.wait(d_out[0].ev(), d_out[1].ev())
        if debug:
            SP.wait(d_dbg.ev())

        with nc.Block() as block:
            @block.tensor
            def _(h):
                for f in PE.ops:
                    f(h)

            @block.scalar
            def _(h):
                for f in ACT.ops:
                    f(h)

            @block.vector
            def _(h):
                for f in DVE.ops:
                    f(h)

            @block.gpsimd
            def _(h):
                for f in POOL.ops:
                    f(h)

            @block.sync
            def _(h):
                for f in SP.ops:
                    f(h)
    return nc


def rope_tables_np(positions):
    inv = 500000.0 ** (-np.arange(0, 16, 2, dtype=np.float32) / 16.0)
    ang = positions.astype(np.float32)[:, None] * inv[None, :]
    return np.cos(ang).astype(np.float32), np.sin(ang).astype(np.float32)


def make_consts(hf):
    TIDX, NTAB = tab_index()
    tab = np.zeros((128, NTAB, 16), np.float32)
    p = np.arange(128)
    pos0 = hf * 2048

    def put(slot, positions):
        c, s = rope_tables_np(positions)
        tab[:, slot, 0:8] = c
        tab[:, slot, 8:16] = s

    for b in range(32):
        if b < 16:
            put(TIDX[("n", 0, b)], b * 128 + p)
        else:
            put(TIDX[("n", 0, b)], pos0 + (b - 16) * 128 + p)
    for g, d in enumerate(DILS):
        nb = 16 // d
        for ob in range(16):
            r, n = ob // nb, ob % nb
            put(TIDX[("o", g, ob)], pos0 + (n * 128 + p) * d + r)
        for pb in range(d):
            put(TIDX[("p", g, pb)], 2048 - 128 * d + p * d + pb)
    k = np.arange(128)[:, None]
    q = np.arange(512)[None, :]
    cm = np.stack([(128 * v + k > q) for v in range(4)], axis=1).astype(np.float32)
    mA = (np.arange(128)[:, None] < np.arange(128)[None, :]).astype(np.float32)
    bf = ml_dtypes.bfloat16
    return {
        "rope": tab.reshape(128, NTAB * 16),
        "prevbias": np.full((128, 1), 0.0 if hf == 1 else NEG, np.float32),
        "cmask": cm.reshape(128, 4 * 512).astype(bf),
        "maskA": mA.astype(bf),
        "negI": (NEG * np.eye(128, dtype=np.float32)).astype(bf),
        "identb": np.eye(128, dtype=np.float32).astype(bf),
        "identf": np.eye(128, dtype=np.float32),
    }


_NC_CACHE = {}


def kernel(x, w_in, b_gate, lam_q1, lam_k1, lam_q2, lam_k2, subln_g, w_oa, w_ob, w_o,
           ln1_g, ln1_b, w_router, b_router, w_gu, b_gu, w_down, b_down, ln2_g, ln2_b, _debug=False):
    f32 = np.float32
    x = np.asarray(x, f32)
    key = bool(_debug)
    if key not in _NC_CACHE:
        _NC_CACHE[key] = build_nc(debug=_debug)
    nc = _NC_CACHE[key]
    c = lambda a: np.ascontiguousarray(np.asarray(a, f32))
    shared = {
        "w_in": c(w_in[0]), "w_oa": c(w_oa[0]), "w_ob": c(w_ob[0]), "w_o": c(w_o[0]),
        "w_router": c(w_router[0]), "w_gu": c(w_gu[0]), "w_down": c(w_down[0]),
        "lamv": c(np.broadcast_to(np.stack([lam_q1[0], lam_k1[0], lam_q2[0], lam_k2[0]])[None], (128, 4, 64)).reshape(128, 256)),
        "subg": c(np.asarray(subln_g[0]).reshape(128, 1)),
        "bgt": c(np.asarray(b_gate[0]).reshape(16, 128).T),
        "bgu_t": c(np.asarray(b_gu[0]).reshape(NE, 16, 128).transpose(2, 0, 1).reshape(128, NE * 16)),
        "brt": c(np.broadcast_to(np.asarray(b_router[0])[None], (128, NE))),
        "bdn": c(b_down[0]),
        "ebase": c(np.broadcast_to((np.arange(NE, dtype=np.float32) * CAP)[None], (128, NE))),
        "lnp": c(np.broadcast_to(np.stack([ln1_g[0], ln1_b[0], ln2_g[0], ln2_b[0]])[None], (128, 4, D)).reshape(128, 4 * D)),
    }
    consts = [make_consts(0), make_consts(1)]
    in_maps = []
    for core in range(8):
        b, hf = core // 2, core % 2
        m = dict(shared)
        m.update(consts[hf])
        m["x_own"] = c(x[b, hf * 2048:(hf + 1) * 2048])
        m["x_prev"] = c(x[b, 0:2048])
        in_maps.append(m)
    res = run_bass_kernel_spmd(nc, in_maps, core_ids=list(range(8)))
    out = np.empty((4, 4096, D), f32)
    for core in range(8):
        b, hf = core // 2, core % 2
        out[b, hf * 2048:(hf + 1) * 2048] = res.results[core]["out"]
    if _debug:
        return out, res.results
    return out
```

```python
import contextlib
import numpy as np
import ml_dtypes
import concourse.bass as bass
import concourse.mybir as mybir
from concourse.bass_utils import run_bass_kernel_spmd

F32 = mybir.dt.float32
BF16 = mybir.dt.bfloat16
ALU = mybir.AluOpType
AF = mybir.ActivationFunctionType
AX = mybir.AxisListType

S_OWN = 2048
D = 1024
NE = 32
ALPHA = 2.0 ** 0.25
EPS = 1e-5
LAMBDA_INIT = 0.2
NEG = -30000.0
DILS = (1, 4, 16)
SAME_ENGINE_WAITS = True
CAP = 384
I32 = mybir.dt.int32


def tab_index():
    idx = {}
    n = 0
    for b in range(32):
        idx[("n", 0, b)] = n; n += 1
    for g, d in enumerate(DILS):
        for ob in range(16):
            idx[("o", g, ob)] = n; n += 1
        for pb in range(d):
            idx[("p", g, pb)] = n; n += 1
    return idx, n


class Eng:
    def __init__(self, name, sem):
        self.name, self.sem, self.n, self.ops, self.seen = name, sem, 0, [], {}

    def wait(self, *evs):
        for ev in evs:
            if ev is None:
                continue
            if isinstance(ev, list):
                self.wait(*ev)
                continue
            sem, val, key = ev
            if key == self.name and not SAME_ENGINE_WAITS:
                continue
            if self.seen.get(key, 0) >= val:
                continue
            self.seen[key] = val
            self.ops.append(lambda h, sem=sem, val=val: h.wait_ge(sem, val))

    def op(self, fn, sig=True):
        if sig:
            self.n += 1
            self.ops.append(lambda h, fn=fn, sem=self.sem: fn(h).then_inc(sem, 1))
            return (self.sem, self.n, self.name)
        self.ops.append(lambda h, fn=fn: fn(h))
        return None

    def dma(self, fn, ds):
        ds.n += 16
        self.ops.append(lambda h, fn=fn, sem=ds.sem: fn(h).then_inc(sem, 16))
        return (ds.sem, ds.n, ds.name)


class DSem:
    def __init__(self, name, sem):
        self.name, self.sem, self.n = name, sem, 0

    def ev(self):
        return (self.sem, self.n, self.name)


def build_nc(debug=False):
    nc = bass.Bass("TRN2", target_bir_lowering=False)
    TIDX, NTAB = tab_index()

    def din(name, shape, dt=F32):
        return nc.dram_tensor(name, list(shape), dt, kind="ExternalInput").ap()

    x_own = din("x_own", [S_OWN, D])
    x_prev = din("x_prev", [S_OWN, D])
    w_in = din("w_in", [D, 8192])
    w_oa = din("w_oa", [512, D])
    w_ob = din("w_ob", [512, D])
    w_o = din("w_o", [D, D])
    w_router = din("w_router", [D, NE])
    w_gu = din("w_gu", [NE, D, 2048])
    w_down = din("w_down", [NE, D, D])
    rope = din("rope", [128, NTAB * 16])
    prevbias_d = din("prevbias", [128, 1])
    cmask_d = din("cmask", [128, 4 * 512], BF16)
    maskA_d = din("maskA", [128, 128], BF16)
    negI_d = din("negI", [128, 128], BF16)
    identb_d = din("identb", [128, 128], BF16)
    identf_d = din("identf", [128, 128])
    lamv_d = din("lamv", [128, 4 * 64])
    subg_d = din("subg", [128, 1])
    bgt_d = din("bgt", [128, 16])
    bgu_d = din("bgu_t", [128, NE * 16])
    brt_d = din("brt", [128, NE])
    bdn_d = din("bdn", [NE, D])
    lnp_d = din("lnp", [128, 4 * D])
    ebase_d = din("ebase", [128, NE])
    Xg = nc.dram_tensor("Xg", [NE * CAP, D], BF16, kind="Internal").ap()
    Yg = nc.dram_tensor("Yg", [NE * CAP, D], F32, kind="Internal").ap()
    out_d = nc.dram_tensor("out", [S_OWN, D], F32, kind="ExternalOutput").ap()
    h1d = nc.dram_tensor("h1d", [S_OWN, D], F32, kind="ExternalOutput" if debug else "Internal").ap()
    if debug:
        dbg_ya = nc.dram_tensor("dbg_ya", [128, 4 * 2048], BF16, kind="ExternalOutput").ap()
    ybd = nc.dram_tensor("ybd", [64, 8 * 2048], BF16, kind="ExternalOutput" if debug else "Internal").ap()

    es = contextlib.ExitStack()
    with es:
        def sb(name, shape, dt):
            return es.enter_context(nc.sbuf_tensor("sb_" + name, list(shape), dt))

        def pst(name):
            return es.enter_context(nc.psum_tensor(name, [128, 512], F32))

        _semc = [0]

        def newsem(name):
            _semc[0] += 1
            return es.enter_context(nc.semaphore(name))

        PE = Eng("pe", newsem("s_pe"))
        ACT = Eng("act", newsem("s_act"))
        DVE = Eng("dve", newsem("s_dve"))
        POOL = Eng("pool", newsem("s_pool"))
        SP = Eng("sp", newsem("s_sp"))

        def dsem(name):
            return DSem(name, newsem(name))

        K = 1024
        ARENA = sb("arena", [128, 92 * K], BF16)

        def av(off, shape, dt, parts=128):
            esz = 4 if dt == F32 else 2
            n = 1
            for d_ in shape[1:]:
                n *= d_
            a = ARENA[0:parts, off // 2: off // 2 + n * esz // 2]
            if dt == F32:
                a = a.bitcast(F32)
            if len(shape) == 3:
                a = a.rearrange("p (a b) -> p a b", a=shape[1])
            elif len(shape) == 4:
                a = a.rearrange("p (a b c) -> p a b c", a=shape[1], b=shape[2])
            return a

        xTp = av(0, [128, 8, 2048], BF16)
        xTo = av(32 * K, [128, 8, 2048], BF16)
        ybH = av(64 * K, [64, 4, 2048], BF16, parts=64)
        acc = av(80 * K, [128, 4, 2048], F32)
        yaT = av(80 * K, [128, 4, 2048], BF16)
        ft = [av(96 * K + i * 2 * K, [128, 512], F32) for i in range(5)]
        xld = [av(100 * K + i * 2 * K, [128, 1024], BF16) for i in range(2)]
        QT = av(112 * K, [128, 2, 2048], BF16)
        KT = av(120 * K, [128, 2, 4096], BF16)
        VVf = av(136 * K, [128, 32 * 260], BF16)
        Wr = [av(153 * K + i * 4 * K, [128, 8, 256], BF16) for i in range(3)]
        Tt = [av(165 * K + i * 512, [128, 256], BF16) for i in range(2)]
        rtmp = [av(166 * K + i * 512, [128, 4, 32], F32) for i in range(2)]
        ropet = av(167 * K, [128, NTAB, 16], F32)
        cmask = av(167 * K + 6656, [128, 4, 512], BF16)
        PT = [av(167 * K + 6656 + 4 * K + i * K, [128, 512], BF16) for i in range(4)]
        PT2 = [av(167 * K + 6656 + 4 * K + i * 2 * K, [128, 1024], BF16) for i in range(2)]
        lamv = av(167 * K + 6656 + 8 * K, [128, 4, 64], F32)
        maskA = av(167 * K + 6656 + 9 * K, [128, 128], BF16)
        negI = av(167 * K + 6656 + 9 * K + 256, [128, 128], BF16)
        ybw = av(64 * K, [64, 8, 512], BF16, parts=64)
        mT = av(72 * K, [128, 8, 512], BF16)
        h1Tf = av(106 * K, [128, 8, 128], F32)
        wrt = av(110 * K, [128, 8, NE], F32)
        woa = av(112 * K, [128, 4, D], BF16)
        wob = av(120 * K, [64, 8, D], BF16, parts=64)
        wo = av(136 * K, [128, 8, D], BF16)
        lnp1 = av(165 * K, [128, 2, D], F32)
        xf3 = av(173 * K, [128, D], F32)
        xf5 = [av(128 * K + i * 4 * K, [128, D], F32) for i in range(2)]
        lnp2 = av(136 * K, [128, 2, D], F32)

        identb = sb("identb", [128, 128], BF16)
        identf = sb("identf", [128, 128], F32)
        ones_bf = sb("ones_bf", [128, 128], BF16)
        onesF = sb("onesF", [128, 128], F32)
        prevbias = sb("prevbias", [128, 1], F32)
        lamt = sb("lamt", [128, 2, 64], F32)
        lams = sb("lams", [128, 4], F32)
        subg = sb("subg", [128, 1], F32)
        epsc = sb("epsc", [128, 1], F32)
        bgt = sb("bgt", [128, 16], F32)
        bgu = sb("bgu", [128, NE * 16], F32)
        brt = sb("brt", [128, NE], F32)
        bdn = sb("bdn", [NE, D], F32)
        gwd = sb("gwd", [128, 16, NE], F32)
        rsm = sb("rsm", [128, 16], F32)
        ftA = sb("ftA", [128, 512], F32)
        stt = sb("stt", [128, 2, 6], F32)
        mv = sb("mv", [128, 2], F32)
        sm = sb("sm", [128, 8], F32)
        lg = sb("lg", [128, NE], F32)
        mx8 = sb("mx8", [128, 8], F32)
        el = sb("el", [128, NE], F32)
        gT = sb("gT", [NE, 128], F32)
        ebase = sb("ebase", [128, NE], F32)
        m01 = sb("m01", [128, 16, NE], BF16)
        slotv = sb("slotv", [128, NE], F32)
        rtm = sb("rtm", [128, NE], F32)
        destf = sb("destf", [128, 4], F32)
        desti = sb("desti", [128, 16, 4], I32)
        gw4 = sb("gw4", [128, 16, 4], F32)

        pairA = es.enter_context(nc.psum_tensor("psA", [128, 1024], F32))
        pairB = es.enter_context(nc.psum_tensor("psB", [128, 1024], F32))
        psS0, psS1 = pairA[:, 0:512], pairA[:, 512:1024]
        psP, psT = pairB[:, 0:512], pairB[:, 512:1024]
        psO0, psO1, psD0, psD1 = [pst(f"ps{i}") for i in range(4)]
        psTb = psT[:, :].bitcast(BF16)

        d_const = dsem("d_const")
        for (dst, src) in [
            (ropet[:, :, :], rope.rearrange("p (n c) -> p n c", c=16)),
            (prevbias[:, :], prevbias_d), (cmask[:, :, :], cmask_d.rearrange("p (v q) -> p v q", v=4)),
            (maskA[:, :], maskA_d), (negI[:, :], negI_d), (identb[:, :], identb_d), (identf[:, :], identf_d),
            (lamv[:, :, :], lamv_d.rearrange("p (a b) -> p a b", a=4)), (subg[:, :], subg_d),
            (bgt[:, :], bgt_d), (bgu[:, :], bgu_d), (brt[:, :], brt_d), (bdn[:, :], bdn_d), (ebase[:, :], ebase_d),
        ]:
            SP.dma(lambda h, dst=dst, src=src: h.dma_start(out=dst, in_=src), d_const)
        CONST = d_const.ev()
        e1 = POOL.op(lambda h: h.memset(ones_bf[:, :], 1.0))
        e2 = POOL.op(lambda h: h.memset(onesF[:, :], 1.0))
        e3 = POOL.op(lambda h: h.memset(epsc[:, :], EPS))
        e4 = POOL.op(lambda h: h.memset(VVf[:, :], 1.0))
        MEMS = [e1, e2, e3, e4]
        zt = av(112 * K, [128, 8192], BF16)
        ez = POOL.op(lambda h: h.memset(zt[:, :], 0.0))
        d_z = dsem("d_z")
        SP.wait(ez)
        xg_flat = Xg.rearrange("(p n) d -> p (n d)", p=128)
        for i_ in range(NE * CAP * D // 128 // 8192):
            SP.dma(lambda h, i_=i_: h.dma_start(out=xg_flat[:, i_ * 8192:(i_ + 1) * 8192], in_=zt[:, :]), d_z)
        ZFILL = d_z.ev()
        DVE.wait(CONST)
        ev = DVE.op(lambda h: h.tensor_tensor(out=lamt[:, :, :], in0=lamv[:, 0:4:2, :], in1=lamv[:, 1:4:2, :], op=ALU.mult))
        DVE.wait(ev)
        ev = DVE.op(lambda h: h.reduce_sum(out=lams[:, 0:2], in_=lamt[:, :, :], axis=AX.X))
        ACT.wait(ev)
        ev = ACT.op(lambda h: h.activation(out=lams[:, 2:4], in_=lams[:, 0:2], func=AF.Exp))
        DVE.wait(ev)
        ev = DVE.op(lambda h: h.tensor_tensor(out=lams[:, 0:1], in0=lams[:, 3:4], in1=lams[:, 2:3], op=ALU.subtract))
        DVE.wait(ev)
        ev = DVE.op(lambda h: h.tensor_scalar(out=lams[:, 0:1], in0=lams[:, 0:1], scalar1=-LAMBDA_INIT, scalar2=None, op0=ALU.add))
        DVE.wait(ev)
        ev = DVE.op(lambda h: h.tensor_scalar(out=lams[:, 1:2], in0=subg[:, :], scalar1=1.0 - LAMBDA_INIT, scalar2=None, op0=ALU.mult))
        LAMEV = ev
        neglam = lams[:, 0:1]
        gsc = lams[:, 1:2]

        d_x = [dsem("d_x0"), dsem("d_x1")]
        xfree = [None, None]
        psT_free = None
        PE.wait(CONST)
        for blk in range(32):
            s = blk % 2
            src = (x_prev if blk < 16 else x_own)[(blk % 16) * 128:(blk % 16 + 1) * 128, :]
            POOL.wait(xfree[s])
            ld = POOL.dma(lambda h, s=s, src=src: h.dma_start(out=xld[s][:, :], in_=src), d_x[s])
            PE.wait(ld, psT_free)
            for kc in range(8):
                ev = PE.op(lambda h, s=s, kc=kc: h.transpose(out=psTb[:, kc * 128:(kc + 1) * 128], in_=xld[s][:, kc * 128:(kc + 1) * 128], identity=identb[:, :]), sig=(kc == 7))
            xfree[s] = ev
            ACT.wait(ev)
            dst = (xTp if blk < 16 else xTo)[:, :, (blk % 16) * 128:(blk % 16 + 1) * 128]
            psT_free = ACT.op(lambda h, dst=dst: h.activation(out=dst, in_=psTb[:, :].rearrange("p (k t) -> p k t", k=8), func=AF.Copy))
        XT_DONE = psT_free

        d_w = [dsem(f"d_w{i}") for i in range(3)]
        wfree = [None, None, None]
        wcnt = [0]

        def load_w(col0):
            s = wcnt[0] % 3
            wcnt[0] += 1
            POOL.wait(wfree[s])
            ev = POOL.dma(lambda h, s=s, col0=col0: h.dma_start(out=Wr[s][:, :, :], in_=w_in[:, col0:col0 + 256].rearrange("(k p) c -> p k c", p=128)), d_w[s])
            return s, ev

        st = {"psP_free": None, "psT_free": XT_DONE, "tcnt": 0, "Tfree": [None, None], "rfree": [None, None], "pend": None,
              "pf": [None, None], "pcnt": 0}
        pbanks = [psP, psD1]

        def proj_mm(xap_fn, ws, wev):
            pb = st["pcnt"] % 2
            st["pcnt"] += 1
            Pb = pbanks[pb]
            PE.wait(wev, st["pf"][pb], st["psP_free"] if pb == 0 else None, XT_DONE)
            for kc in range(8):
                ev = PE.op(lambda h, kc=kc, Pb=Pb: h.matmul(Pb[:, 0:256], lhsT=xap_fn(kc), rhs=Wr[ws][:, kc, :], start=(kc == 0), stop=(kc == 7)), sig=(kc == 7))
            wfree[ws] = ev
            return ev, pb

        def flush_pend():
            if st["pend"] is None:
                return
            ti, tev, dst = st["pend"]
            st["pend"] = None
            PE.wait(tev, st["psT_free"])
            for hh_ in range(2):
                ev = PE.op(lambda h, hh_=hh_, ti=ti: h.transpose(out=psTb[:, hh_ * 128:(hh_ + 1) * 128], in_=Tt[ti][:, hh_ * 128:(hh_ + 1) * 128], identity=identb[:, :]), sig=(hh_ == 1))
            st["Tfree"][ti] = ev
            ACT.wait(ev)
            st["psT_free"] = ACT.op(lambda h, dst=dst: h.activation(out=dst, in_=psTb[:, 0:256].rearrange("p (a t) -> p a t", a=2), func=AF.Copy))
            st["last_qk"] = st["psT_free"]

        def qk_tile(xap_fn, ws, wev, tab, dst):
            mmev, pb = proj_mm(xap_fn, ws, wev)
            flush_pend()
            ti = st["tcnt"] % 2
            st["tcnt"] += 1
            p3 = pbanks[pb][:, 0:256].rearrange("p (a c) -> p a c", a=4)
            t3 = Tt[ti][:, :].rearrange("p (a c) -> p a c", a=4)
            cos = ropet[:, tab, 0:8].unsqueeze(1).broadcast_to([128, 4, 8])
            sin = ropet[:, tab, 8:16].unsqueeze(1).broadcast_to([128, 4, 8])
            rt = rtmp[ti]
            DVE.wait(mmev, st["Tfree"][ti], CONST)
            a = DVE.op(lambda h: h.tensor_tensor(out=rt[:, :, 0:8], in0=p3[:, :, 0:8], in1=cos, op=ALU.mult), sig=False)
            a = DVE.op(lambda h: h.tensor_tensor(out=rt[:, :, 8:16], in0=p3[:, :, 8:16], in1=cos, op=ALU.mult), sig=False)
            a = DVE.op(lambda h: h.tensor_tensor(out=rt[:, :, 16:24], in0=p3[:, :, 8:16], in1=sin, op=ALU.mult), sig=False)
            a = DVE.op(lambda h: h.tensor_tensor(out=rt[:, :, 24:32], in0=p3[:, :, 0:8], in1=sin, op=ALU.mult), sig=False)
            a = DVE.op(lambda h: h.tensor_copy(out=t3[:, :, 16:64], in_=p3[:, :, 16:64]))
            st["pf"][pb] = a
            if pb == 0:
                st["psP_free"] = a
            DVE.wait(a)
            a = DVE.op(lambda h: h.tensor_tensor(out=t3[:, :, 0:8], in0=rt[:, :, 0:8], in1=rt[:, :, 16:24], op=ALU.subtract), sig=False)
            a = DVE.op(lambda h: h.tensor_tensor(out=t3[:, :, 8:16], in0=rt[:, :, 8:16], in1=rt[:, :, 24:32], op=ALU.add))
            st["pend"] = (ti, a, dst)

        def v_tile(xap_fn, ws, wev, dst, srcf):
            mmev, pb = proj_mm(xap_fn, ws, wev)
            ACT.wait(mmev, MEMS)
            a = ACT.op(lambda h: h.activation(out=dst, in_=srcf(pbanks[pb]), func=AF.Copy))
            st["pf"][pb] = a
            if pb == 0:
                st["psP_free"] = a
            st["last_v"] = a

        def xnat(blk):
            t = xTp if blk < 16 else xTo
            b = blk % 16
            return lambda kc: t[:, kc, b * 128:(b + 1) * 128]

        Vaug = VVf[:, :].rearrange("p (b h c) -> p b h c", b=32, h=4)
        bufc = [0]
        d_yb = dsem("d_yb")
        sfree = [None, None]
        ptfree = [None] * 4
        ofree = [None, None]
        ptc = [0]
        psSb = [psS0, psS1]
        psOb = [psO0, psO1]
        acc_last = [None]

        for hh in range(2):
            for g, d in enumerate(DILS):
                nb = 16 // d
                base = 1536 + g * 1536 + hh * 256
                stage_guard = acc_last[0]
                ACT.wait(stage_guard, ZFILL)
                sq, evq = load_w(base)
                sk, evk = load_w(base + 512)
                sv, evv = load_w(base + 1024)

                def xown(ob, d=d, nb=nb):
                    r, n = ob // nb, ob % nb
                    return lambda kc: xTo[:, kc, :].rearrange("p (l d) -> p d l", d=d)[:, r, n * 128:(n + 1) * 128]

                def xprev(pb, d=d):
                    return lambda kc: xTp[:, kc, 2048 - 128 * d:2048].rearrange("p (l d) -> p d l", d=d)[:, pb, :]

                for ob in range(16):
                    qk_tile(xown(ob), sq, evq, TIDX[("o", g, ob)], QT[:, :, ob * 128:(ob + 1) * 128])
                for ob in range(16):
                    qk_tile(xown(ob), sk, evk, TIDX[("o", g, ob)], KT[:, :, 2048 + ob * 128:2048 + (ob + 1) * 128])
                for pb in range(d):
                    qk_tile(xprev(pb), sk, evk, TIDX[("p", g, pb)], KT[:, :, pb * 128:(pb + 1) * 128])
                flush_pend()
                for ob in range(16):
                    v_tile(xown(ob), sv, evv, Vaug[:, 16 + ob, :, 0:64], lambda P_: P_[:, 0:256].rearrange("p (a c) -> p a c", a=4))
                for pb in range(d):
                    v_tile(xprev(pb), sv, evv, Vaug[:, pb, :, 0:64], lambda P_: P_[:, 0:256].rearrange("p (a c) -> p a c", a=4))
                PROJ_DONE = [st["last_qk"], st["last_v"]]
                PE.wait(PROJ_DONE)
                work = [(r, hl, n) for r in range(d) for hl in range(4) for n in range(nb)]

                def dil_S(item, d=d, nb=nb):
                    r, hl, n = item
                    p, s_ = hl // 2, hl % 2
                    lo, hi = s_ * 64, s_ * 64 + 64
                    ob = r * nb + n
                    qpos = ob * 128
                    if n == 0:
                        kposA, blkA = r * 128, r
                    else:
                        kposA, blkA = 2048 + (ob - 1) * 128, 16 + ob - 1
                    kposB, blkB = 2048 + ob * 128, 16 + ob
                    bi = bufc[0] % 2
                    bufc[0] += 1
                    S = psSb[bi]
                    PE.wait(sfree[bi])
                    qap = QT[lo:hi, p, qpos:qpos + 128]
                    PE.op(lambda h: h.matmul(S[:, 0:128], lhsT=KT[lo:hi, p, kposA:kposA + 128], rhs=qap, start=True, stop=False), sig=False)
                    PE.op(lambda h: h.matmul(S[:, 0:128], lhsT=negI[:, :], rhs=maskA[:, :], start=False, stop=True), sig=False)
                    PE.op(lambda h: h.matmul(S[:, 128:256], lhsT=KT[lo:hi, p, kposB:kposB + 128], rhs=qap, start=True, stop=False), sig=False)
                    sev = PE.op(lambda h: h.matmul(S[:, 128:256], lhsT=negI[:, :], rhs=cmask[:, 0, 0:128], start=False, stop=True))
                    pi = ptc[0] % 4
                    ptc[0] += 1
                    ACT.wait(sev, ptfree[pi])
                    if n == 0:
                        ACT.op(lambda h: h.activation(out=PT[pi][:, 0:128], in_=S[:, 0:128], func=AF.Exp, bias=prevbias[:, 0:1], scale=0.125), sig=False)
                        aev = ACT.op(lambda h: h.activation(out=PT[pi][:, 128:256], in_=S[:, 128:256], func=AF.Exp, scale=0.125))
                    else:
                        aev = ACT.op(lambda h: h.activation(out=PT[pi][:, 0:256], in_=S[:, 0:256], func=AF.Exp, scale=0.125))
                    sfree[bi] = aev
                    return (aev, pi, bi, blkA, blkB)

                def dil_AV(item, pend_, g=g, d=d):
                    r, hl, n = item
                    aev, pi, bi, blkA, blkB = pend_
                    O = psOb[bi]
                    PE.wait(aev, ofree[bi])
                    PE.op(lambda h: h.matmul(O[0:65, 0:128], lhsT=Vaug[:, blkA, hl, :], rhs=PT[pi][:, 0:128], start=True, stop=False), sig=False)
                    oev = PE.op(lambda h: h.matmul(O[0:65, 0:128], lhsT=Vaug[:, blkB, hl, :], rhs=PT[pi][:, 128:256], start=False, stop=True))
                    ptfree[pi] = oev
                    dst = acc[0:65, hl, :].rearrange("p (l d) -> p d l", d=d)[:, r, n * 128:(n + 1) * 128]
                    DVE.wait(oev, acc_last[0] if g > 0 else None)
                    if g == 0:
                        dev = DVE.op(lambda h: h.tensor_copy(out=dst, in_=O[0:65, 0:128]))
                    else:
                        dev = DVE.op(lambda h: h.tensor_tensor(out=dst, in0=dst, in1=O[0:65, 0:128], op=ALU.add))
                    ofree[bi] = dev
                    acc_last[0] = dev

                pend_ = dil_S(work[0])
                for wi, item in enumerate(work):
                    nxt = dil_S(work[wi + 1]) if wi + 1 < len(work) else None
                    dil_AV(item, pend_)
                    pend_ = nxt
            DVE.wait(st.get("ybstore"))
            for hl in range(4):
                for w in range(4):
                    PE.wait(acc_last[0], MEMS, st.get("dfree"))
                    bev = PE.op(lambda h, hl=hl, w=w: h.matmul(psD0[0:64, :], lhsT=onesF[64:65, 0:64], rhs=acc[64:65, hl, w * 512:(w + 1) * 512], start=True, stop=True))
                    DVE.wait(bev)
                    rev = DVE.op(lambda h: h.reciprocal(out=ftA[0:64, :], in_=psD0[0:64, :]))
                    st["dfree"] = rev
                    DVE.wait(rev)
                    fev = DVE.op(lambda h, hl=hl, w=w: h.tensor_tensor(out=ybH[0:64, hl, w * 512:(w + 1) * 512], in0=acc[0:64, hl, w * 512:(w + 1) * 512], in1=ftA[0:64, :], op=ALU.mult))
                    acc_last[0] = fev
            SP.wait(acc_last[0])
            st["ybstore"] = SP.dma(lambda h, hh=hh: h.dma_start(out=ybd[:, hh * 8192:(hh + 1) * 8192], in_=ybH[:, :, :].rearrange("p h t -> p (h t)")), d_yb)
        YB_DONE = [acc_last[0], st["ybstore"]]

        Vd = VVf[:, 0:32 * 256].rearrange("p (b c) -> p b c", b=32)
        psOd = [psO0, psO1]
        psDd = [psD0, psD1]
        fin_free = YB_DONE
        st["sbf"] = [[sfree[0], sfree[1]], None]
        st["acc_prev"] = [None, None]
        att_last = YB_DONE
        ya_last = None
        for pp in range(2):
            ACT.wait(att_last)
            DVE.wait(att_last)
            sq, evq = load_w(pp * 256)
            sk, evk = load_w(512 + pp * 256)
            sv, evv = load_w(1024 + pp * 256)
            for ob in range(16):
                qk_tile(xnat(16 + ob), sq, evq, TIDX[("n", 0, 16 + ob)], QT[:, :, ob * 128:(ob + 1) * 128])
            for blk in range(32):
                qk_tile(xnat(blk), sk, evk, TIDX[("n", 0, blk)], KT[:, :, blk * 128:(blk + 1) * 128])
            flush_pend()
            for blk in range(32):
                v_tile(xnat(blk), sv, evv, Vd[:, blk, :], lambda P_: P_[:, 0:256])
            PE.wait(st["last_qk"], st["last_v"])
            for hl in range(2):
                for j in range(4):
                    nkb = 16 + 4 * (j + 1)
                    PE.wait(fin_free, st["pf"][1], st["pf"][0], st["psP_free"], st["psT_free"])
                    SBK = [(psS0, psS1), (psP, psT)]
                    accD = [av(106 * K, [128, 512], F32), av(108 * K, [128, 512], F32)]
                    accE = [DVE, POOL]

                    def dif_S(kb, hl=hl, j=j):
                        bp = bufc[0] % 2
                        bufc[0] += 1
                        diag = kb >= 16 + 4 * j
                        PE.wait(st["sbf"][bp])
                        for c in range(2):
                            lo, hi = c * 64, c * 64 + 64
                            S = SBK[bp][c]
                            sev = PE.op(lambda h, S=S, lo=lo, hi=hi: h.matmul(S[:, :], lhsT=KT[lo:hi, hl, kb * 128:(kb + 1) * 128], rhs=QT[lo:hi, hl, j * 512:(j + 1) * 512], start=True, stop=not diag), sig=(c == 1 and not diag))
                            if diag:
                                v = kb - 16 - 4 * j
                                sev = PE.op(lambda h, S=S, v=v: h.matmul(S[:, :], lhsT=negI[:, :], rhs=cmask[:, v, :], start=False, stop=True), sig=(c == 1))
                        pis = [(2 * bp) % 4, (2 * bp + 1) % 4]
                        ACT.wait(sev, ptfree[pis[0]], ptfree[pis[1]])
                        SS = [pairA, pairB][bp]
                        if kb < 16:
                            aev = ACT.op(lambda h: h.activation(out=PT2[bp][:, :], in_=SS[:, :], func=AF.Exp, bias=prevbias[:, 0:1], scale=0.125))
                        else:
                            aev = ACT.op(lambda h: h.activation(out=PT2[bp][:, :], in_=SS[:, :], func=AF.Exp, scale=0.125))
                        st["sbf"][bp] = aev
                        if bp == 1:
                            st["psP_free"] = aev
                            st["psT_free"] = aev
                            st["pf"][0] = aev
                        else:
                            sfree[0] = aev
                            sfree[1] = aev
                        return (aev, pis)

                    def dif_AV(kb, pend_, hl=hl, nkb=nkb):
                        aev, pis = pend_
                        even = (kb % 2 == 0)
                        PE.wait(aev)
                        PE.op(lambda h: h.matmul(psOd[0][:, :], lhsT=Vd[:, kb, hl * 128:(hl + 1) * 128], rhs=PT[pis[0]][:, :], start=(kb == 0), stop=(kb == nkb - 1)), sig=False)
                        oev_ = PE.op(lambda h: h.matmul(psOd[1][:, :], lhsT=Vd[:, kb, hl * 128:(hl + 1) * 128], rhs=PT[pis[1]][:, :], start=(kb == 0), stop=(kb == nkb - 1)), sig=not even)
                        if even:
                            PE.op(lambda h: h.matmul(psDd[0][:, :], lhsT=ones_bf[:, :], rhs=PT[pis[0]][:, :], start=(kb == 0), stop=False), sig=False)
                            oev_ = PE.op(lambda h: h.matmul(psDd[1][:, :], lhsT=ones_bf[:, :], rhs=PT[pis[1]][:, :], start=(kb == 0), stop=False))
                            ptfree[pis[0]] = oev_
                            ptfree[pis[1]] = oev_
                            return oev_
                        devs = []
                        for c in range(2):
                            E_ = accE[c]
                            E_.wait(aev, st["acc_prev"][c])
                            if kb == 1:
                                dv = E_.op(lambda h, c=c: h.tensor_copy(out=accD[c][:, :], in_=PT[pis[c]][:, :]))
                            else:
                                dv = E_.op(lambda h, c=c: h.tensor_tensor(out=accD[c][:, :], in0=accD[c][:, :], in1=PT[pis[c]][:, :], op=ALU.add))
                            st["acc_prev"][c] = dv
                            devs.append(dv)
                        ptfree[pis[0]] = [oev_, devs[0]]
                        ptfree[pis[1]] = [oev_, devs[1]]
                        return oev_

                    pend_ = dif_S(0)
                    for kb in range(nkb):
                        nxt = dif_S(kb + 1) if kb + 1 < nkb else None
                        oev = dif_AV(kb, pend_)
                        pend_ = nxt
                    PE.wait(st["acc_prev"][0], st["acc_prev"][1], MEMS)
                    PE.op(lambda h: h.matmul(psD0[:, :], lhsT=onesF[:, :], rhs=accD[0][:, :], start=False, stop=True), sig=False)
                    oev = PE.op(lambda h: h.matmul(psD1[:, :], lhsT=onesF[:, :], rhs=accD[1][:, :], start=False, stop=True))
                    st["acc_prev"] = [oev, oev]
                    DVE.wait(oev, LAMEV, ya_last)
                    a = DVE.op(lambda h: h.reciprocal(out=ft[0][:, :], in_=psD0[:, :]), sig=False)
                    a = DVE.op(lambda h: h.reciprocal(out=ft[1][:, :], in_=psD1[:, :]))
                    DVE.wait(a)
                    a = DVE.op(lambda h: h.tensor_tensor(out=ft[0][:, :], in0=psO0[:, :], in1=ft[0][:, :], op=ALU.mult), sig=False)
                    a = DVE.op(lambda h: h.tensor_tensor(out=ft[1][:, :], in0=psO1[:, :], in1=ft[1][:, :], op=ALU.mult))
                    fin_free = a
                    st["pf"][1] = a
                    DVE.wait(a)
                    a = DVE.op(lambda h: h.scalar_tensor_tensor(out=ft[2][:, :], in0=ft[1][:, :], scalar=neglam, in1=ft[0][:, :], op0=ALU.mult, op1=ALU.add))
                    DVE.wait(a)
                    a = DVE.op(lambda h: h.tensor_tensor(out=ft[3][:, :], in0=ft[2][:, :], in1=ft[2][:, :], op=ALU.mult))
                    PE.wait(a, st["sbf"][0])
                    m = PE.op(lambda h: h.matmul(psS0[:, :], lhsT=onesF[:, :], rhs=ft[3][:, :], start=True, stop=True))
                    ACT.wait(m)
                    a = ACT.op(lambda h: h.activation(out=ft[4][:, :], in_=psS0[:, :], func=AF.Sqrt, bias=epsc[:, 0:1], scale=1.0 / 128.0))
                    st["sbf"][0] = a
                    sfree[0] = a
                    DVE.wait(a)
                    a = DVE.op(lambda h: h.reciprocal(out=ft[4][:, :], in_=ft[4][:, :]))
                    DVE.wait(a)
                    a = DVE.op(lambda h, pp=pp, hl=hl, j=j: h.scalar_tensor_tensor(out=yaT[:, 2 * pp + hl, j * 512:(j + 1) * 512], in0=ft[2][:, :], scalar=gsc, in1=ft[4][:, :], op0=ALU.mult, op1=ALU.mult))
                    ya_last = a
                    att_last = a
        YA_DONE = ya_last

        d_dbg = dsem("d_dbg")
        if debug:
            SP.wait(YA_DONE, YB_DONE)
            SP.dma(lambda h: h.dma_start(out=dbg_ya, in_=yaT[:, :, :].rearrange("p h t -> p (h t)")), d_dbg)

        d_pw = dsem("d_pw")
        POOL.wait(YA_DONE, YB_DONE)
        SP.wait(YA_DONE, YB_DONE)
        if debug:
            POOL.wait(d_dbg.ev())
            SP.wait(d_dbg.ev())
        POOL.dma(lambda h: h.dma_start(out=woa[:, :, :], in_=w_oa.rearrange("(k p) c -> p k c", p=128)), d_pw)
        POOL.dma(lambda h: h.dma_start(out=wob[:, :, :], in_=w_ob.rearrange("(k p) c -> p k c", p=64)), d_pw)
        POOL.dma(lambda h: h.dma_start(out=wo[:, :, :], in_=w_o.rearrange("(k p) c -> p k c", p=128)), d_pw)
        SP.dma(lambda h: h.dma_start(out=lnp1[:, :, :], in_=lnp_d[:, 0:2 * D].rearrange("p (a c) -> p a c", a=2)), d_pw)
        SP.dma(lambda h: h.dma_start(out=wrt[:, :, :], in_=w_router.rearrange("(k p) c -> p k c", p=128)), d_pw)
        PW = d_pw.ev()

        h1T = xTp
        d_xf = dsem("d_xf")
        d_h1 = dsem("d_h1")
        d_ybw = dsem("d_ybw")
        d_sc = dsem("d_sc")
        h1b = av(177 * K, [128, D], BF16)
        xf_free = None
        ybw_free = None
        g_last = {"ga": None, "gb": None, "mT": None, "h1Tf": None, "misc": None}

        def layer_norm(buf, lnv, pre_wait):
            DVE.wait(pre_wait)
            DVE.op(lambda h: h.bn_stats(out=stt[:, 0, :], in_=buf[:, 0:512]), sig=False)
            a = DVE.op(lambda h: h.bn_stats(out=stt[:, 1, :], in_=buf[:, 512:1024]))
            DVE.wait(a)
            a = DVE.op(lambda h: h.bn_aggr(out=mv[:, :], in_=stt[:, :, :].rearrange("p a b -> p (a b)")))
            ACT.wait(a)
            a = ACT.op(lambda h: h.activation(out=sm[:, 0:1], in_=mv[:, 1:2], func=AF.Sqrt, bias=epsc[:, 0:1], scale=1.0))
            DVE.wait(a)
            a = DVE.op(lambda h: h.reciprocal(out=sm[:, 1:2], in_=sm[:, 0:1]))
            DVE.wait(a)
            a = DVE.op(lambda h: h.tensor_scalar(out=buf[:, :], in0=buf[:, :], scalar1=mv[:, 0:1], scalar2=sm[:, 1:2], op0=ALU.subtract, op1=ALU.mult))
            DVE.wait(a)
            a = DVE.op(lambda h: h.tensor_tensor(out=buf[:, :], in0=buf[:, :], in1=lnv[:, 0, :], op=ALU.mult))
            DVE.wait(a)
            a = DVE.op(lambda h: h.tensor_tensor(out=buf[:, :], in0=buf[:, :], in1=lnv[:, 1, :], op=ALU.add))
            return a

        xf3r = [xf3, av(0, [128, D], F32)]
        h1Tfr = [h1Tf, av(4 * K, [128, 8, 128], F32)]
        d_xfr = [dsem("d_xfr0"), dsem("d_xfr1")]
        d_h1r = [dsem("d_h1r0"), dsem("d_h1r1")]
        xf_fr = [None, None]
        h1Tf_fr = [None, None]
        tr_ev = {}
        stv_ev = {}
        pendB = [None]

        def stageA(tb, tbl, mt_ready):
            s_ = tb % 2
            xf = xf3r[s_]
            hT = h1Tfr[s_]
            SP.wait(xf_fr[s_])
            xev = SP.dma(lambda h: h.dma_start(out=xf[:, :], in_=x_own[tb * 128:(tb + 1) * 128, :]), d_xfr[s_])
            PE.wait(mt_ready, st.get("dfree2"))
            for dh in range(2):
                for fc in range(8):
                    e = PE.op(lambda h, dh=dh, fc=fc: h.matmul(psDd[dh][:, :], lhsT=mT[:, fc, tbl * 128:(tbl + 1) * 128], rhs=wo[:, fc, dh * 512:(dh + 1) * 512], start=(fc == 0), stop=(fc == 7)), sig=(fc == 7 and dh == 1))
            mm_o = e
            if tbl == 3:
                g_last["mT"] = mm_o
            DVE.wait(mm_o, xev)
            DVE.op(lambda h: h.scalar_tensor_tensor(out=xf[:, 0:512], in0=xf[:, 0:512], scalar=ALPHA, in1=psD0[:, :], op0=ALU.mult, op1=ALU.add), sig=False)
            a = DVE.op(lambda h: h.scalar_tensor_tensor(out=xf[:, 512:1024], in0=xf[:, 512:1024], scalar=ALPHA, in1=psD1[:, :], op0=ALU.mult, op1=ALU.add))
            st["dfree2"] = a
            h1ev = layer_norm(xf, lnp1, a)
            SP.wait(h1ev)
            stv_ev[tb] = SP.dma(lambda h: h.dma_start(out=h1d[tb * 128:(tb + 1) * 128, :], in_=xf[:, :]), d_h1r[s_])
            for half in range(2):
                PE.wait(h1ev, st["psP_free"])
                for q_ in range(4):
                    kc = half * 4 + q_
                    e = PE.op(lambda h, kc=kc, q_=q_: h.transpose(out=psP[:, q_ * 128:(q_ + 1) * 128], in_=xf[:, kc * 128:(kc + 1) * 128], identity=identf[:, :]), sig=(q_ == 3))
                ACT.wait(e, h1Tf_fr[s_] if half == 0 else None)
                a = ACT.op(lambda h, half=half: h.activation(out=hT[:, half * 4:(half + 1) * 4, :], in_=psP[:, :].rearrange("p (k t) -> p k t", k=4), func=AF.Copy))
                st["psP_free"] = a
            tr_ev[tb] = (a, e)

        def stageB(tb):
            s_ = tb % 2
            xf = xf3r[s_]
            hT = h1Tfr[s_]
            a, e_tr = tr_ev[tb]
            PE.wait(a, sfree[0])
            for kc in range(8):
                e = PE.op(lambda h, kc=kc: h.matmul(psS0[:, 0:NE], lhsT=hT[:, kc, :], rhs=wrt[:, kc, :], start=(kc == 0), stop=(kc == 7)), sig=(kc == 7))
            h1Tf_fr[s_] = e
            DVE.wait(e)
            a = DVE.op(lambda h: h.tensor_tensor(out=lg[:, :], in0=psS0[:, 0:NE], in1=brt[:, :], op=ALU.add))
            sfree[0] = a
            DVE.wait(a)
            a = DVE.op(lambda h: h.max(out=mx8[:, :], in_=lg[:, :]))
            DVE.wait(a)
            a = DVE.op(lambda h: h.tensor_scalar(out=sm[:, 2:3], in0=mx8[:, 0:1], scalar1=-1.0, scalar2=None, op0=ALU.mult))
            ACT.wait(a)
            ACT.op(lambda h: h.activation(out=sm[:, 4:8], in_=mx8[:, 0:4], func=AF.Exp, bias=sm[:, 2:3], scale=1.0), sig=False)
            a = ACT.op(lambda h: h.activation(out=el[:, :], in_=lg[:, :], func=AF.Exp, bias=sm[:, 2:3], scale=1.0))
            DVE.wait(a)
            a = DVE.op(lambda h: h.reduce_sum(out=sm[:, 3:4], in_=sm[:, 4:8], axis=AX.X))
            DVE.wait(a)
            a = DVE.op(lambda h: h.reciprocal(out=rsm[:, tb:tb + 1], in_=sm[:, 3:4]))
            DVE.wait(a)
            a = DVE.op(lambda h: h.tensor_scalar(out=el[:, :], in0=el[:, :], scalar1=rsm[:, tb:tb + 1], scalar2=None, op0=ALU.mult))
            DVE.wait(a)
            a = DVE.op(lambda h: h.scalar_tensor_tensor(out=gwd[:, tb, :], in0=lg[:, :], scalar=mx8[:, 3:4], in1=el[:, :], op0=ALU.is_ge, op1=ALU.mult))
            a = DVE.op(lambda h: h.tensor_scalar(out=m01[:, tb, :], in0=lg[:, :], scalar1=mx8[:, 3:4], scalar2=None, op0=ALU.is_ge))
            PE.wait(a, sfree[1])
            for tb2 in range(tb + 1):
                e = PE.op(lambda h, tb2=tb2: h.matmul(psS1[:, 0:NE], lhsT=(maskA[:, :] if tb2 == tb else ones_bf[:, :]), rhs=m01[:, tb2, :], start=(tb2 == 0), stop=(tb2 == tb)), sig=(tb2 == tb))
            DVE.wait(e)
            a = DVE.op(lambda h: h.tensor_tensor(out=slotv[:, :], in0=psS1[:, 0:NE], in1=ebase[:, :], op=ALU.add))
            sfree[1] = a
            DVE.wait(a)
            for k_ in range(4):
                DVE.op(lambda h, k_=k_: h.scalar_tensor_tensor(out=rtm[:, :], in0=lg[:, :], scalar=mx8[:, k_:k_ + 1], in1=slotv[:, :], op0=ALU.is_equal, op1=ALU.mult))
                DVE.wait((DVE.sem, DVE.n, DVE.name))
                DVE.op(lambda h, k_=k_: h.reduce_sum(out=destf[:, k_:k_ + 1], in_=rtm[:, :], axis=AX.X))
                DVE.wait((DVE.sem, DVE.n, DVE.name))
            a = DVE.op(lambda h: h.tensor_copy(out=desti[:, tb, :], in_=destf[:, :]))
            DVE.op(lambda h: h.tensor_scalar(out=gw4[:, tb, :], in0=sm[:, 4:8], scalar1=rsm[:, tb:tb + 1], scalar2=None, op0=ALU.mult))
            DVE.wait(st.get("h1b_free"))
            a = DVE.op(lambda h: h.tensor_copy(out=h1b[:, :], in_=xf[:, :]))
            POOL.wait(a, ZFILL)
            for k_ in range(4):
                sc = POOL.dma(lambda h, k_=k_: h.indirect_dma_start(out=Xg[:, :], out_offset=bass.IndirectOffsetOnAxis(ap=desti[:, tb, k_:k_ + 1], axis=0), in_=h1b[:, :], in_offset=None), d_sc)
            st["h1b_free"] = sc
            xf_fr[s_] = [stv_ev[tb], e_tr, a]
            g_last["misc"] = a
            st["route_done"] = a

        PE.wait(PW, YA_DONE, YB_DONE)
        DVE.wait(PW)
        for w in range(4):
            tok = slice(w * 512, (w + 1) * 512)
            SP.wait(ybw_free)
            ybw_ev = SP.dma(lambda h, tok=tok: h.dma_start(out=ybw[:, :, :], in_=ybd.rearrange("p (h t) -> p h t", h=8)[:, :, tok]), d_ybw)
            for cp in range(4):
                sA, evA = load_w(6144 + cp * 256)
                sB, evB = load_w(7168 + cp * 256)
                for fcl in range(2):
                    fc = 2 * cp + fcl
                    PE.wait(evA, sfree[0])
                    for kc in range(8):
                        e = PE.op(lambda h, kc=kc, sA=sA, fcl=fcl, tok=tok: h.matmul(psS0[:, :], lhsT=Wr[sA][:, kc, fcl * 128:(fcl + 1) * 128], rhs=xTo[:, kc, tok], start=(kc == 0), stop=(kc == 7)), sig=(kc == 7))
                    if fcl == 1:
                        wfree[sA] = e
                    ACT.wait(e, g_last["ga"], CONST)
                    ga = ACT.op(lambda h, fc=fc: h.activation(out=ft[0][:, :], in_=psS0[:, :], func=AF.Sigmoid, bias=bgt[:, fc:fc + 1], scale=1.0))
                    sfree[0] = ga
                    PE.wait(evB, sfree[1])
                    for kc in range(8):
                        e = PE.op(lambda h, kc=kc, sB=sB, fcl=fcl, tok=tok: h.matmul(psS1[:, :], lhsT=Wr[sB][:, kc, fcl * 128:(fcl + 1) * 128], rhs=xTo[:, kc, tok], start=(kc == 0), stop=(kc == 7)), sig=(kc == 7))
                    if fcl == 1:
                        wfree[sB] = e
                    ACT.wait(e, g_last["gb"])
                    gb = ACT.op(lambda h, fc=fc: h.activation(out=ft[1][:, :], in_=psS1[:, :], func=AF.Sigmoid, bias=bgt[:, 8 + fc:9 + fc], scale=1.0))
                    sfree[1] = gb
                    PE.wait(ofree[0])
                    for kc in range(4):
                        e = PE.op(lambda h, kc=kc, fc=fc, tok=tok: h.matmul(psO0[:, :], lhsT=woa[:, kc, fc * 128:(fc + 1) * 128], rhs=yaT[:, kc, tok], start=(kc == 0), stop=(kc == 3)), sig=(kc == 3))
                    oa = e
                    PE.wait(ofree[1], ybw_ev)
                    for hh_ in range(8):
                        e = PE.op(lambda h, hh_=hh_, fc=fc: h.matmul(psO1[:, :], lhsT=wob[0:64, hh_, fc * 128:(fc + 1) * 128], rhs=ybw[0:64, hh_, :], start=(hh_ == 0), stop=(hh_ == 7)), sig=(hh_ == 7))
                    obv = e
                    if fc == 7:
                        ybw_free = e
                    DVE.wait(ga, oa, g_last["misc"])
                    a = DVE.op(lambda h: h.tensor_tensor(out=ft[2][:, :], in0=psO0[:, :], in1=ft[0][:, :], op=ALU.mult))
                    ofree[0] = a
                    g_last["ga"] = a
                    DVE.wait(gb, obv)
                    b = DVE.op(lambda h: h.tensor_tensor(out=ft[3][:, :], in0=psO1[:, :], in1=ft[1][:, :], op=ALU.mult))
                    ofree[1] = b
                    g_last["gb"] = b
                    DVE.wait(a, b, g_last["mT"] if fc == 0 else None)
                    c_ = DVE.op(lambda h, fc=fc: h.tensor_tensor(out=mT[:, fc, :], in0=ft[2][:, :], in1=ft[3][:, :], op=ALU.add))
                    g_last["misc"] = c_
            MT_READY = c_
            for tbl in range(4):
                tb = w * 4 + tbl
                stageA(tb, tbl, MT_READY)
                if pendB[0] is not None:
                    stageB(pendB[0])
                pendB[0] = tb
        stageB(pendB[0])
        ROUTE_DONE = st["route_done"]
        xf_free = [xf_fr[0], xf_fr[1]]
        P3_DONE = [ROUTE_DONE, st["psP_free"], xf_free]
        H1D_DONE = [d_h1r[0].ev(), d_h1r[1].ev()]

        Wd = [av(i * 16 * K, [128, 8, D], BF16) for i in range(2)]
        Xe = [av(32 * K + i * 6 * K, [128, 3, D], BF16) for i in range(2)]
        XT = [av(44 * K + i * 6 * K, [128, 8, CAP], BF16) for i in range(2)]
        actT = [av(56 * K + i * 6 * K, [128, 8, CAP], BF16) for i in range(2)]
        Yt = [av(68 * K + i * 4 * K, [128, D], F32) for i in range(2)]
        Wgu = [av(112 * K + i * 32 * K, [128, 8, 2048], BF16) for i in range(2)]
        tg = [av(100 * K + i * 6 * K, [128, CAP], F32) for i in range(2)]
        tsg = [av(102 * K + i * 6 * K, [128, CAP], F32) for i in range(2)]
        tu = [av(104 * K + i * 6 * K, [128, CAP], F32) for i in range(2)]
        d_gu = [dsem(f"d_gu{i}") for i in range(2)]
        d_dn = [dsem(f"d_dn{i}") for i in range(2)]
        d_xe = [dsem("d_xe0"), dsem("d_xe1")]
        d_yg = [dsem("d_yg0"), dsem("d_yg1")]
        gu_free = [None] * 2
        dn_free = [None] * 2
        xe_free = [None, None]
        yt_free = [None, None]
        items = [(e, q) for e in range(NE) for q in range(4)]
        gu_ev = {}
        dn_ev = {}
        xe_ev = {}
        xt_ready = {}

        def issue_gu(e):
            s = e % 2
            POOL.wait(gu_free[s])
            gu_ev[e] = POOL.dma(lambda h, s=s, e=e: h.dma_start(out=Wgu[s][:, :, :], in_=w_gu[e].rearrange("(k p) c -> p k c", p=128)), d_gu[s])

        def issue_dn(e):
            s = e % 2
            POOL.wait(dn_free[s])
            dn_ev[e] = POOL.dma(lambda h, s=s, e=e: h.dma_start(out=Wd[s][:, :, :], in_=w_down[e].rearrange("(k p) c -> p k c", p=128)), d_dn[s])

        def issue_xe(e):
            s = e % 2
            SP.wait(xe_free[s])
            xe_ev[e] = SP.dma(lambda h, s=s, e=e: h.dma_start(out=Xe[s][:, :, :], in_=Xg[e * CAP:(e + 1) * CAP, :].rearrange("(n p) d -> p n d", p=128)), d_xe[s])

        def do_transposes(e):
            s = e % 2
            for n in range(3):
                PE.wait(xe_ev[e], st["psT_free"])
                for kc in range(8):
                    ev_ = PE.op(lambda h, kc=kc, n=n, s=s: h.transpose(out=psTb[:, kc * 128:(kc + 1) * 128], in_=Xe[s][:, n, kc * 128:(kc + 1) * 128], identity=identb[:, :]), sig=(kc == 7))
                ACT.wait(ev_)
                st["psT_free"] = ACT.op(lambda h, n=n, s=s: h.activation(out=XT[s][:, :, n * 128:(n + 1) * 128], in_=psTb[:, :].rearrange("p (k t) -> p k t", k=8), func=AF.Copy))
            xe_free[s] = ev_
            xt_ready[e] = st["psT_free"]

        SCAT_DONE = d_sc.ev()
        POOL.wait(P3_DONE, H1D_DONE)
        PE.wait(P3_DONE)
        DVE.wait(P3_DONE, H1D_DONE)
        ACT.wait(P3_DONE, H1D_DONE)
        SP.wait(P3_DONE, SCAT_DONE)
        issue_gu(0)
        issue_dn(0)
        issue_xe(0)
        issue_xe(1)
        do_transposes(0)
        banks = [(psS0, psS1), (psD0, psD1)]
        bfree = [[sfree[0], sfree[1]], [st.get("dfree2"), st.get("dfree2")]]
        obk = [psO0, psO1, psP]
        obkfree = [ofree[0], ofree[1], st["psP_free"]]
        tfree = [None, None]
        stepc = 0
        ocnt = 0
        act_last = None
        for e in range(NE):
            xs = e % 2
            if e + 1 < NE:
                issue_gu(e + 1)
                issue_dn(e + 1)
            s = e % 2
            for q in range(4):
                i = e
                for jj in range(2):
                    j = 2 * q + jj
                    b = stepc % 2
                    stepc += 1
                    Pg, Pu = banks[b]
                    PE.wait(gu_ev[i], xt_ready[e], bfree[b][0], bfree[b][1])
                    for kc in range(8):
                        eg = PE.op(lambda h, kc=kc, Pg=Pg, s=s, j=j, xs=xs: h.matmul(Pg[:, 0:CAP], lhsT=Wgu[s][:, kc, j * 128:(j + 1) * 128], rhs=XT[xs][:, kc, :], start=(kc == 0), stop=(kc == 7)), sig=(kc == 7))
                    for kc in range(8):
                        eu = PE.op(lambda h, kc=kc, Pu=Pu, s=s, j=j, xs=xs: h.matmul(Pu[:, 0:CAP], lhsT=Wgu[s][:, kc, 1024 + j * 128:1024 + (j + 1) * 128], rhs=XT[xs][:, kc, :], start=(kc == 0), stop=(kc == 7)), sig=(kc == 7))
                    if j == 7:
                        gu_free[s] = eu
                    DVE.wait(eg, tfree[b])
                    a1 = DVE.op(lambda h, Pg=Pg, e=e, j=j, b=b: h.tensor_scalar(out=tg[b][:, :], in0=Pg[:, 0:CAP], scalar1=bgu[:, e * 16 + j:e * 16 + j + 1], scalar2=7.0, op0=ALU.add, op1=ALU.min))
                    bfree[b][0] = a1
                    ACT.wait(a1, eu)
                    ACT.op(lambda h, b=b: h.activation(out=tsg[b][:, :], in_=tg[b][:, :], func=AF.Sigmoid, scale=1.702), sig=False)
                    a2 = ACT.op(lambda h, Pu=Pu, e=e, j=j, b=b: h.activation(out=tu[b][:, :], in_=Pu[:, 0:CAP], func=AF.Identity, bias=bgu[:, e * 16 + 8 + j:e * 16 + 9 + j], scale=1.0))
                    bfree[b][1] = a2
                    DVE.wait(a2)
                    a3 = DVE.op(lambda h, b=b: h.tensor_scalar(out=tu[b][:, :], in0=tu[b][:, :], scalar1=7.0, scalar2=-7.0, op0=ALU.min, op1=ALU.max))
                    DVE.wait(a3)
                    a4 = DVE.op(lambda h, b=b: h.scalar_tensor_tensor(out=tu[b][:, :], in0=tu[b][:, :], scalar=1.0, in1=tg[b][:, :], op0=ALU.add, op1=ALU.mult))
                    DVE.wait(a4)
                    a5 = DVE.op(lambda h, j=j, b=b, xs=xs: h.tensor_tensor(out=actT[xs][:, j, :], in0=tu[b][:, :], in1=tsg[b][:, :], op=ALU.mult))
                    tfree[b] = a5
                    act_last = a5
            if e + 1 < NE:
                do_transposes(e + 1)
            if e + 2 < NE:
                issue_xe(e + 2)
            ds_ = e % 2
            PE.wait(act_last, dn_ev[e])
            for nb_ in range(3):
                ys = (e * 3 + nb_) % 2
                for dh in range(2):
                    oi = ocnt % 3
                    ocnt += 1
                    O = obk[oi]
                    PE.wait(obkfree[oi])
                    for fc in range(8):
                        em = PE.op(lambda h, O=O, fc=fc, nb_=nb_, dh=dh, ds_=ds_, xs=xs: h.matmul(O[:, :], lhsT=actT[xs][:, fc, nb_ * 128:(nb_ + 1) * 128], rhs=Wd[ds_][:, fc, dh * 512:(dh + 1) * 512], start=(fc == 0), stop=(fc == 7)), sig=(fc == 7))
                    ACT.wait(em, yt_free[ys] if dh == 0 else None)
                    ac = ACT.op(lambda h, O=O, ys=ys, dh=dh: h.activation(out=Yt[ys][:, dh * 512:(dh + 1) * 512], in_=O[:, :], func=AF.Copy))
                    obkfree[oi] = ac
                SP.wait(ac)
                yt_free[ys] = SP.dma(lambda h, ys=ys, e=e, nb_=nb_: h.dma_start(out=Yg[e * CAP + nb_ * 128:e * CAP + (nb_ + 1) * 128, :], in_=Yt[ys][:, :]), d_yg[ys])
            dn_free[ds_] = em
        MOE_DONE = [ac, em, d_yg[0].ev(), d_yg[1].ev()]

        G = [[av((s_ * 4 + k_) * 4 * K, [128, D], F32) for k_ in range(4)] for s_ in range(2)]
        d_hl = [dsem("d_hl0"), dsem("d_hl1")]
        d_out = [dsem("d_out0"), dsem("d_out1")]
        d_g = [dsem("d_g0"), dsem("d_g1")]
        d_l2 = dsem("d_l2")
        buf_free = [None, None]
        g_free = [None, None]
        gT_free = None
        SP.wait(H1D_DONE, MOE_DONE)
        POOL.wait(MOE_DONE)
        PE.wait(MOE_DONE)
        DVE.wait(MOE_DONE)
        l2ev = SP.dma(lambda h: h.dma_start(out=lnp2[:, :, :], in_=lnp_d[:, 2 * D:4 * D].rearrange("p (a c) -> p a c", a=2)), d_l2)
        o5free = [obkfree[0], obkfree[1]]

        def issue_gather(tb):
            s = tb % 2
            POOL.wait(g_free[s])
            for k_ in range(4):
                gev_ = POOL.dma(lambda h, s=s, tb=tb, k_=k_: h.indirect_dma_start(out=G[s][k_][:, :], out_offset=None, in_=Yg[:, :], in_offset=bass.IndirectOffsetOnAxis(ap=desti[:, tb, k_:k_ + 1], axis=0)), d_g[s])
            return gev_

        gq = {0: issue_gather(0)}
        for tb in range(16):
            s = tb % 2
            if tb + 1 < 16:
                gq[tb + 1] = issue_gather(tb + 1)
            SP.wait(buf_free[s])
            hev = SP.dma(lambda h, s=s, tb=tb: h.dma_start(out=xf5[s][:, :], in_=h1d[tb * 128:(tb + 1) * 128, :]), d_hl[s])
            PE.wait(bfree[0][0], bfree[0][1], gT_free)
            e = PE.op(lambda h, tb=tb: h.transpose(out=psS0[0:NE, 0:128], in_=gwd[:, tb, :], identity=identf[:, :]))
            ACT.wait(e, gT_free)
            a = ACT.op(lambda h: h.activation(out=gT[:, :], in_=psS0[0:NE, 0:128], func=AF.Copy))
            bfree[0][0] = a
            PE.wait(a, o5free[0], o5free[1])
            for dh in range(2):
                e = PE.op(lambda h, dh=dh: h.matmul([psO0, psO1][dh][:, :], lhsT=gT[:, :], rhs=bdn[:, dh * 512:(dh + 1) * 512], start=True, stop=True))
            gT_free = e
            G0 = G[s][0]
            DVE.wait(gq[tb])
            a = DVE.op(lambda h, G0=G0, tb=tb: h.tensor_scalar(out=G0[:, :], in0=G0[:, :], scalar1=gw4[:, tb, 0:1], scalar2=None, op0=ALU.mult))
            for k_ in range(1, 4):
                DVE.wait(a)
                a = DVE.op(lambda h, G0=G0, s=s, k_=k_, tb=tb: h.scalar_tensor_tensor(out=G0[:, :], in0=G[s][k_][:, :], scalar=gw4[:, tb, k_:k_ + 1], in1=G0[:, :], op0=ALU.mult, op1=ALU.add))
            DVE.wait(a, e)
            DVE.op(lambda h, G0=G0: h.tensor_tensor(out=G0[:, 0:512], in0=G0[:, 0:512], in1=psO0[:, :], op=ALU.add), sig=False)
            a = DVE.op(lambda h, G0=G0: h.tensor_tensor(out=G0[:, 512:1024], in0=G0[:, 512:1024], in1=psO1[:, :], op=ALU.add))
            o5free = [a, a]
            DVE.wait(a, hev)
            a = DVE.op(lambda h, s=s, G0=G0: h.scalar_tensor_tensor(out=xf5[s][:, :], in0=xf5[s][:, :], scalar=ALPHA, in1=G0[:, :], op0=ALU.mult, op1=ALU.add))
            g_free[s] = a
            DVE.wait(l2ev)
            oev = layer_norm(xf5[s], lnp2, a)
            SP.wait(oev)
            buf_free[s] = SP.dma(lambda h, s=s, tb=tb: h.dma_start(out=out_d[tb * 128:(tb + 1) * 128, :], in_=xf5[s][:, :]), d_out[s])
        SP.wait(d_out[0].ev(), d_out[1].ev())
        if debug:
            SP.wait(d_dbg.ev())

        with nc.Block() as block:
            @block.tensor
            def _(h):
                for f in PE.ops:
                    f(h)

            @block.scalar
            def _(h):
                for f in ACT.ops:
                    f(h)

            @block.vector
            def _(h):
                for f in DVE.ops:
                    f(h)

            @block.gpsimd
            def _(h):
                for f in POOL.ops:
                    f(h)

            @block.sync
            def _(h):
                for f in SP.ops:
                    f(h)
    return nc


def rope_tables_np(positions):
    inv = 500000.0 ** (-np.arange(0, 16, 2, dtype=np.float32) / 16.0)
    ang = positions.astype(np.float32)[:, None] * inv[None, :]
    return np.cos(ang).astype(np.float32), np.sin(ang).astype(np.float32)


def make_consts(hf):
    TIDX, NTAB = tab_index()
    tab = np.zeros((128, NTAB, 16), np.float32)
    p = np.arange(128)
    pos0 = hf * 2048

    def put(slot, positions):
        c, s = rope_tables_np(positions)
        tab[:, slot, 0:8] = c
        tab[:, slot, 8:16] = s

    for b in range(32):
        if b < 16:
            put(TIDX[("n", 0, b)], b * 128 + p)
        else:
            put(TIDX[("n", 0, b)], pos0 + (b - 16) * 128 + p)
    for g, d in enumerate(DILS):
        nb = 16 // d
        for ob in range(16):
            r, n = ob // nb, ob % nb
            put(TIDX[("o", g, ob)], pos0 + (n * 128 + p) * d + r)
        for pb in range(d):
            put(TIDX[("p", g, pb)], 2048 - 128 * d + p * d + pb)
    k = np.arange(128)[:, None]
    q = np.arange(512)[None, :]
    cm = np.stack([(128 * v + k > q) for v in range(4)], axis=1).astype(np.float32)
    mA = (np.arange(128)[:, None] < np.arange(128)[None, :]).astype(np.float32)
    bf = ml_dtypes.bfloat16
    return {
        "rope": tab.reshape(128, NTAB * 16),
        "prevbias": np.full((128, 1), 0.0 if hf == 1 else NEG, np.float32),
        "cmask": cm.reshape(128, 4 * 512).astype(bf),
        "maskA": mA.astype(bf),
        "negI": (NEG * np.eye(128, dtype=np.float32)).astype(bf),
        "identb": np.eye(128, dtype=np.float32).astype(bf),
        "identf": np.eye(128, dtype=np.float32),
    }


_NC_CACHE = {}


def kernel(x, w_in, b_gate, lam_q1, lam_k1, lam_q2, lam_k2, subln_g, w_oa, w_ob, w_o,
           ln1_g, ln1_b, w_router, b_router, w_gu, b_gu, w_down, b_down, ln2_g, ln2_b, _debug=False):
    f32 = np.float32
    x = np.asarray(x, f32)
    key = bool(_debug)
    if key not in _NC_CACHE:
        _NC_CACHE[key] = build_nc(debug=_debug)
    nc = _NC_CACHE[key]
    c = lambda a: np.ascontiguousarray(np.asarray(a, f32))
    shared = {
        "w_in": c(w_in[0]), "w_oa": c(w_oa[0]), "w_ob": c(w_ob[0]), "w_o": c(w_o[0]),
        "w_router": c(w_router[0]), "w_gu": c(w_gu[0]), "w_down": c(w_down[0]),
        "lamv": c(np.broadcast_to(np.stack([lam_q1[0], lam_k1[0], lam_q2[0], lam_k2[0]])[None], (128, 4, 64)).reshape(128, 256)),
        "subg": c(np.asarray(subln_g[0]).reshape(128, 1)),
        "bgt": c(np.asarray(b_gate[0]).reshape(16, 128).T),
        "bgu_t": c(np.asarray(b_gu[0]).reshape(NE, 16, 128).transpose(2, 0, 1).reshape(128, NE * 16)),
        "brt": c(np.broadcast_to(np.asarray(b_router[0])[None], (128, NE))),
        "bdn": c(b_down[0]),
        "ebase": c(np.broadcast_to((np.arange(NE, dtype=np.float32) * CAP)[None], (128, NE))),
        "lnp": c(np.broadcast_to(np.stack([ln1_g[0], ln1_b[0], ln2_g[0], ln2_b[0]])[None], (128, 4, D)).reshape(128, 4 * D)),
    }
    consts = [make_consts(0), make_consts(1)]
    in_maps = []
    for core in range(8):
        b, hf = core // 2, core % 2
        m = dict(shared)
        m.update(consts[hf])
        m["x_own"] = c(x[b, hf * 2048:(hf + 1) * 2048])
        m["x_prev"] = c(x[b, 0:2048])
        in_maps.append(m)
    res = run_bass_kernel_spmd(nc, in_maps, core_ids=list(range(8)))
    out = np.empty((4, 4096, D), f32)
    for core in range(8):
        b, hf = core // 2, core % 2
        out[b, hf * 2048:(hf + 1) * 2048] = res.results[core]["out"]
    if _debug:
        return out, res.results
    return out
```

```python
import contextlib
import numpy as np
import ml_dtypes
import concourse.bass as bass
import concourse.mybir as mybir
from concourse.bass_utils import run_bass_kernel_spmd

F32 = mybir.dt.float32
BF16 = mybir.dt.bfloat16
ALU = mybir.AluOpType
AF = mybir.ActivationFunctionType
AX = mybir.AxisListType

S_OWN = 2048
D = 1024
NE = 32
ALPHA = 2.0 ** 0.25
EPS = 1e-5
LAMBDA_INIT = 0.2
NEG = -30000.0
DILS = (1, 4, 16)
SAME_ENGINE_WAITS = True
CAP = 384
I32 = mybir.dt.int32


def tab_index():
    idx = {}
    n = 0
    for b in range(32):
        idx[("n", 0, b)] = n; n += 1
    for g, d in enumerate(DILS):
        for ob in range(16):
            idx[("o", g, ob)] = n; n += 1
        for pb in range(d):
            idx[("p", g, pb)] = n; n += 1
    return idx, n


class Eng:
    def __init__(self, name, sem):
        self.name, self.sem, self.n, self.ops, self.seen = name, sem, 0, [], {}

    def wait(self, *evs):
        for ev in evs:
            if ev is None:
                continue
            if isinstance(ev, list):
                self.wait(*ev)
                continue
            sem, val, key = ev
            if key == self.name and not SAME_ENGINE_WAITS:
                continue
            if self.seen.get(key, 0) >= val:
                continue
            self.seen[key] = val
            self.ops.append(lambda h, sem=sem, val=val: h.wait_ge(sem, val))

    def op(self, fn, sig=True):
        if sig:
            self.n += 1
            self.ops.append(lambda h, fn=fn, sem=self.sem: fn(h).then_inc(sem, 1))
            return (self.sem, self.n, self.name)
        self.ops.append(lambda h, fn=fn: fn(h))
        return None

    def dma(self, fn, ds):
        ds.n += 16
        self.ops.append(lambda h, fn=fn, sem=ds.sem: fn(h).then_inc(sem, 16))
        return (ds.sem, ds.n, ds.name)


class DSem:
    def __init__(self, name, sem):
        self.name, self.sem, self.n = name, sem, 0

    def ev(self):
        return (self.sem, self.n, self.name)


def build_nc(debug=False):
    nc = bass.Bass("TRN2", target_bir_lowering=False)
    TIDX, NTAB = tab_index()

    def din(name, shape, dt=F32):
        return nc.dram_tensor(name, list(shape), dt, kind="ExternalInput").ap()

    x_own = din("x_own", [S_OWN, D])
    x_prev = din("x_prev", [S_OWN, D])
    w_in = din("w_in", [D, 8192])
    w_oa = din("w_oa", [512, D])
    w_ob = din("w_ob", [512, D])
    w_o = din("w_o", [D, D])
    w_router = din("w_router", [D, NE])
    w_gu = din("w_gu", [NE, D, 2048])
    w_down = din("w_down", [NE, D, D])
    rope = din("rope", [128, NTAB * 16])
    prevbias_d = din("prevbias", [128, 1])
    cmask_d = din("cmask", [128, 4 * 512], BF16)
    maskA_d = din("maskA", [128, 128], BF16)
    negI_d = din("negI", [128, 128], BF16)
    identb_d = din("identb", [128, 128], BF16)
    identf_d = din("identf", [128, 128])
    lamv_d = din("lamv", [128, 4 * 64])
    subg_d = din("subg", [128, 1])
    bgt_d = din("bgt", [128, 16])
    bgu_d = din("bgu_t", [128, NE * 16])
    brt_d = din("brt", [128, NE])
    bdn_d = din("bdn", [NE, D])
    lnp_d = din("lnp", [128, 4 * D])
    ebase_d = din("ebase", [128, NE])
    Xg = nc.dram_tensor("Xg", [NE * CAP, D], BF16, kind="Internal").ap()
    Yg = nc.dram_tensor("Yg", [NE * CAP, D], F32, kind="Internal").ap()
    out_d = nc.dram_tensor("out", [S_OWN, D], F32, kind="ExternalOutput").ap()
    h1d = nc.dram_tensor("h1d", [S_OWN, D], F32, kind="ExternalOutput" if debug else "Internal").ap()
    if debug:
        dbg_ya = nc.dram_tensor("dbg_ya", [128, 4 * 2048], BF16, kind="ExternalOutput").ap()
    ybd = nc.dram_tensor("ybd", [64, 8 * 2048], BF16, kind="ExternalOutput" if debug else "Internal").ap()

    es = contextlib.ExitStack()
    with es:
        def sb(name, shape, dt):
            return es.enter_context(nc.sbuf_tensor("sb_" + name, list(shape), dt))

        def pst(name):
            return es.enter_context(nc.psum_tensor(name, [128, 512], F32))

        _semc = [0]

        def newsem(name):
            _semc[0] += 1
            return es.enter_context(nc.semaphore(name))

        PE = Eng("pe", newsem("s_pe"))
        ACT = Eng("act", newsem("s_act"))
        DVE = Eng("dve", newsem("s_dve"))
        POOL = Eng("pool", newsem("s_pool"))
        SP = Eng("sp", newsem("s_sp"))

        def dsem(name):
            return DSem(name, newsem(name))

        K = 1024
        ARENA = sb("arena", [128, 92 * K], BF16)

        def av(off, shape, dt, parts=128):
            esz = 4 if dt == F32 else 2
            n = 1
            for d_ in shape[1:]:
                n *= d_
            a = ARENA[0:parts, off // 2: off // 2 + n * esz // 2]
            if dt == F32:
                a = a.bitcast(F32)
            if len(shape) == 3:
                a = a.rearrange("p (a b) -> p a b", a=shape[1])
            elif len(shape) == 4:
                a = a.rearrange("p (a b c) -> p a b c", a=shape[1], b=shape[2])
            return a

        xTp = av(0, [128, 8, 2048], BF16)
        xTo = av(32 * K, [128, 8, 2048], BF16)
        ybH = av(64 * K, [64, 4, 2048], BF16, parts=64)
        acc = av(80 * K, [128, 4, 2048], F32)
        yaT = av(80 * K, [128, 4, 2048], BF16)
        ft = [av(96 * K + i * 2 * K, [128, 512], F32) for i in range(5)]
        xld = [av(100 * K + i * 2 * K, [128, 1024], BF16) for i in range(2)]
        QT = av(112 * K, [128, 2, 2048], BF16)
        KT = av(120 * K, [128, 2, 4096], BF16)
        VVf = av(136 * K, [128, 32 * 260], BF16)
        Wr = [av(153 * K + i * 4 * K, [128, 8, 256], BF16) for i in range(3)]
        Tt = [av(165 * K + i * 512, [128, 256], BF16) for i in range(2)]
        rtmp = [av(166 * K + i * 512, [128, 4, 32], F32) for i in range(2)]
        ropet = av(167 * K, [128, NTAB, 16], F32)
        cmask = av(167 * K + 6656, [128, 4, 512], BF16)
        PT = [av(167 * K + 6656 + 4 * K + i * K, [128, 512], BF16) for i in range(4)]
        PT2 = [av(167 * K + 6656 + 4 * K + i * 2 * K, [128, 1024], BF16) for i in range(2)]
        lamv = av(167 * K + 6656 + 8 * K, [128, 4, 64], F32)
        maskA = av(167 * K + 6656 + 9 * K, [128, 128], BF16)
        negI = av(167 * K + 6656 + 9 * K + 256, [128, 128], BF16)
        ybw = av(64 * K, [64, 8, 512], BF16, parts=64)
        mT = av(72 * K, [128, 8, 512], BF16)
        h1Tf = av(106 * K, [128, 8, 128], F32)
        wrt = av(110 * K, [128, 8, NE], F32)
        woa = av(112 * K, [128, 4, D], BF16)
        wob = av(120 * K, [64, 8, D], BF16, parts=64)
        wo = av(136 * K, [128, 8, D], BF16)
        lnp1 = av(165 * K, [128, 2, D], F32)
        xf3 = av(173 * K, [128, D], F32)
        xf5 = [av(128 * K + i * 4 * K, [128, D], F32) for i in range(2)]
        lnp2 = av(136 * K, [128, 2, D], F32)

        identb = sb("identb", [128, 128], BF16)
        identf = sb("identf", [128, 128], F32)
        ones_bf = sb("ones_bf", [128, 128], BF16)
        onesF = sb("onesF", [128, 128], F32)
        prevbias = sb("prevbias", [128, 1], F32)
        lamt = sb("lamt", [128, 2, 64], F32)
        lams = sb("lams", [128, 4], F32)
        subg = sb("subg", [128, 1], F32)
        epsc = sb("epsc", [128, 1], F32)
        bgt = sb("bgt", [128, 16], F32)
        bgu = sb("bgu", [128, NE * 16], F32)
        brt = sb("brt", [128, NE], F32)
        bdn = sb("bdn", [NE, D], F32)
        gwd = sb("gwd", [128, 16, NE], F32)
        rsm = sb("rsm", [128, 16], F32)
        ftA = sb("ftA", [128, 512], F32)
        stt = sb("stt", [128, 2, 6], F32)
        mv = sb("mv", [128, 2], F32)
        sm = sb("sm", [128, 8], F32)
        lg = sb("lg", [128, NE], F32)
        mx8 = sb("mx8", [128, 8], F32)
        el = sb("el", [128, NE], F32)
        gT = sb("gT", [NE, 128], F32)
        ebase = sb("ebase", [128, NE], F32)
        m01 = sb("m01", [128, 16, NE], BF16)
        slotv = sb("slotv", [128, NE], F32)
        rtm = sb("rtm", [128, NE], F32)
        destf = sb("destf", [128, 4], F32)
        desti = sb("desti", [128, 16, 4], I32)
        gw4 = sb("gw4", [128, 16, 4], F32)

        pairA = es.enter_context(nc.psum_tensor("psA", [128, 1024], F32))
        pairB = es.enter_context(nc.psum_tensor("psB", [128, 1024], F32))
        psS0, psS1 = pairA[:, 0:512], pairA[:, 512:1024]
        psP, psT = pairB[:, 0:512], pairB[:, 512:1024]
        psO0, psO1, psD0, psD1 = [pst(f"ps{i}") for i in range(4)]
        psTb = psT[:, :].bitcast(BF16)

        d_const = dsem("d_const")
        for (dst, src) in [
            (ropet[:, :, :], rope.rearrange("p (n c) -> p n c", c=16)),
            (prevbias[:, :], prevbias_d), (cmask[:, :, :], cmask_d.rearrange("p (v q) -> p v q", v=4)),
            (maskA[:, :], maskA_d), (negI[:, :], negI_d), (identb[:, :], identb_d), (identf[:, :], identf_d),
            (lamv[:, :, :], lamv_d.rearrange("p (a b) -> p a b", a=4)), (subg[:, :], subg_d),
            (bgt[:, :], bgt_d), (bgu[:, :], bgu_d), (brt[:, :], brt_d), (bdn[:, :], bdn_d), (ebase[:, :], ebase_d),
        ]:
            SP.dma(lambda h, dst=dst, src=src: h.dma_start(out=dst, in_=src), d_const)
        CONST = d_const.ev()
        e1 = POOL.op(lambda h: h.memset(ones_bf[:, :], 1.0))
        e2 = POOL.op(lambda h: h.memset(onesF[:, :], 1.0))
        e3 = POOL.op(lambda h: h.memset(epsc[:, :], EPS))
        e4 = POOL.op(lambda h: h.memset(VVf[:, :], 1.0))
        MEMS = [e1, e2, e3, e4]
        zt = av(112 * K, [128, 8192], BF16)
        ez = POOL.op(lambda h: h.memset(zt[:, :], 0.0))
        d_z = dsem("d_z")
        SP.wait(ez)
        xg_flat = Xg.rearrange("(p n) d -> p (n d)", p=128)
        for i_ in range(NE * CAP * D // 128 // 8192):
            SP.dma(lambda h, i_=i_: h.dma_start(out=xg_flat[:, i_ * 8192:(i_ + 1) * 8192], in_=zt[:, :]), d_z)
        ZFILL = d_z.ev()
        DVE.wait(CONST)
        ev = DVE.op(lambda h: h.tensor_tensor(out=lamt[:, :, :], in0=lamv[:, 0:4:2, :], in1=lamv[:, 1:4:2, :], op=ALU.mult))
        DVE.wait(ev)
        ev = DVE.op(lambda h: h.reduce_sum(out=lams[:, 0:2], in_=lamt[:, :, :], axis=AX.X))
        ACT.wait(ev)
        ev = ACT.op(lambda h: h.activation(out=lams[:, 2:4], in_=lams[:, 0:2], func=AF.Exp))
        DVE.wait(ev)
        ev = DVE.op(lambda h: h.tensor_tensor(out=lams[:, 0:1], in0=lams[:, 3:4], in1=lams[:, 2:3], op=ALU.subtract))
        DVE.wait(ev)
        ev = DVE.op(lambda h: h.tensor_scalar(out=lams[:, 0:1], in0=lams[:, 0:1], scalar1=-LAMBDA_INIT, scalar2=None, op0=ALU.add))
        DVE.wait(ev)
        ev = DVE.op(lambda h: h.tensor_scalar(out=lams[:, 1:2], in0=subg[:, :], scalar1=1.0 - LAMBDA_INIT, scalar2=None, op0=ALU.mult))
        LAMEV = ev
        neglam = lams[:, 0:1]
        gsc = lams[:, 1:2]

        d_x = [dsem("d_x0"), dsem("d_x1")]
        xfree = [None, None]
        psT_free = None
        PE.wait(CONST)
        for blk in range(32):
            s = blk % 2
            src = (x_prev if blk < 16 else x_own)[(blk % 16) * 128:(blk % 16 + 1) * 128, :]
            POOL.wait(xfree[s])
            ld = POOL.dma(lambda h, s=s, src=src: h.dma_start(out=xld[s][:, :], in_=src), d_x[s])
            PE.wait(ld, psT_free)
            for kc in range(8):
                ev = PE.op(lambda h, s=s, kc=kc: h.transpose(out=psTb[:, kc * 128:(kc + 1) * 128], in_=xld[s][:, kc * 128:(kc + 1) * 128], identity=identb[:, :]), sig=(kc == 7))
            xfree[s] = ev
            ACT.wait(ev)
            dst = (xTp if blk < 16 else xTo)[:, :, (blk % 16) * 128:(blk % 16 + 1) * 128]
            psT_free = ACT.op(lambda h, dst=dst: h.activation(out=dst, in_=psTb[:, :].rearrange("p (k t) -> p k t", k=8), func=AF.Copy))
        XT_DONE = psT_free

        d_w = [dsem(f"d_w{i}") for i in range(3)]
        wfree = [None, None, None]
        wcnt = [0]

        def load_w(col0):
            s = wcnt[0] % 3
            wcnt[0] += 1
            POOL.wait(wfree[s])
            ev = POOL.dma(lambda h, s=s, col0=col0: h.dma_start(out=Wr[s][:, :, :], in_=w_in[:, col0:col0 + 256].rearrange("(k p) c -> p k c", p=128)), d_w[s])
            return s, ev

        st = {"psP_free": None, "psT_free": XT_DONE, "tcnt": 0, "Tfree": [None, None], "rfree": [None, None], "pend": None,
              "pf": [None, None], "pcnt": 0}
        pbanks = [psP, psD1]

        def proj_mm(xap_fn, ws, wev):
            pb = st["pcnt"] % 2
            st["pcnt"] += 1
            Pb = pbanks[pb]
            PE.wait(wev, st["pf"][pb], st["psP_free"] if pb == 0 else None, XT_DONE)
            for kc in range(8):
                ev = PE.op(lambda h, kc=kc, Pb=Pb: h.matmul(Pb[:, 0:256], lhsT=xap_fn(kc), rhs=Wr[ws][:, kc, :], start=(kc == 0), stop=(kc == 7)), sig=(kc == 7))
            wfree[ws] = ev
            return ev, pb

        def flush_pend():
            if st["pend"] is None:
                return
            ti, tev, dst = st["pend"]
            st["pend"] = None
            PE.wait(tev, st["psT_free"])
            for hh_ in range(2):
                ev = PE.op(lambda h, hh_=hh_, ti=ti: h.transpose(out=psTb[:, hh_ * 128:(hh_ + 1) * 128], in_=Tt[ti][:, hh_ * 128:(hh_ + 1) * 128], identity=identb[:, :]), sig=(hh_ == 1))
            st["Tfree"][ti] = ev
            ACT.wait(ev)
            st["psT_free"] = ACT.op(lambda h, dst=dst: h.activation(out=dst, in_=psTb[:, 0:256].rearrange("p (a t) -> p a t", a=2), func=AF.Copy))
            st["last_qk"] = st["psT_free"]

        def qk_tile(xap_fn, ws, wev, tab, dst):
            mmev, pb = proj_mm(xap_fn, ws, wev)
            flush_pend()
            ti = st["tcnt"] % 2
            st["tcnt"] += 1
            p3 = pbanks[pb][:, 0:256].rearrange("p (a c) -> p a c", a=4)
            t3 = Tt[ti][:, :].rearrange("p (a c) -> p a c", a=4)
            cos = ropet[:, tab, 0:8].unsqueeze(1).broadcast_to([128, 4, 8])
            sin = ropet[:, tab, 8:16].unsqueeze(1).broadcast_to([128, 4, 8])
            rt = rtmp[ti]
            DVE.wait(mmev, st["Tfree"][ti], CONST)
            a = DVE.op(lambda h: h.tensor_tensor(out=rt[:, :, 0:8], in0=p3[:, :, 0:8], in1=cos, op=ALU.mult), sig=False)
            a = DVE.op(lambda h: h.tensor_tensor(out=rt[:, :, 8:16], in0=p3[:, :, 8:16], in1=cos, op=ALU.mult), sig=False)
            a = DVE.op(lambda h: h.tensor_tensor(out=rt[:, :, 16:24], in0=p3[:, :, 8:16], in1=sin, op=ALU.mult), sig=False)
            a = DVE.op(lambda h: h.tensor_tensor(out=rt[:, :, 24:32], in0=p3[:, :, 0:8], in1=sin, op=ALU.mult), sig=False)
            a = DVE.op(lambda h: h.tensor_copy(out=t3[:, :, 16:64], in_=p3[:, :, 16:64]))
            st["pf"][pb] = a
            if pb == 0:
                st["psP_free"] = a
            DVE.wait(a)
            a = DVE.op(lambda h: h.tensor_tensor(out=t3[:, :, 0:8], in0=rt[:, :, 0:8], in1=rt[:, :, 16:24], op=ALU.subtract), sig=False)
            a = DVE.op(lambda h: h.tensor_tensor(out=t3[:, :, 8:16], in0=rt[:, :, 8:16], in1=rt[:, :, 24:32], op=ALU.add))
            st["pend"] = (ti, a, dst)

        def v_tile(xap_fn, ws, wev, dst, srcf):
            mmev, pb = proj_mm(xap_fn, ws, wev)
            ACT.wait(mmev, MEMS)
            a = ACT.op(lambda h: h.activation(out=dst, in_=srcf(pbanks[pb]), func=AF.Copy))
            st["pf"][pb] = a
            if pb == 0:
                st["psP_free"] = a
            st["last_v"] = a

        def xnat(blk):
            t = xTp if blk < 16 else xTo
            b = blk % 16
            return lambda kc: t[:, kc, b * 128:(b + 1) * 128]

        Vaug = VVf[:, :].rearrange("p (b h c) -> p b h c", b=32, h=4)
        bufc = [0]
        d_yb = dsem("d_yb")
        sfree = [None, None]
        ptfree = [None] * 4
        ofree = [None, None]
        ptc = [0]
        psSb = [psS0, psS1]
        psOb = [psO0, psO1]
        acc_last = [None]

        for hh in range(2):
            for g, d in enumerate(DILS):
                nb = 16 // d
                base = 1536 + g * 1536 + hh * 256
                stage_guard = acc_last[0]
                ACT.wait(stage_guard, ZFILL)
                sq, evq = load_w(base)
                sk, evk = load_w(base + 512)
                sv, evv = load_w(base + 1024)

                def xown(ob, d=d, nb=nb):
                    r, n = ob // nb, ob % nb
                    return lambda kc: xTo[:, kc, :].rearrange("p (l d) -> p d l", d=d)[:, r, n * 128:(n + 1) * 128]

                def xprev(pb, d=d):
                    return lambda kc: xTp[:, kc, 2048 - 128 * d:2048].rearrange("p (l d) -> p d l", d=d)[:, pb, :]

                for ob in range(16):
                    qk_tile(xown(ob), sq, evq, TIDX[("o", g, ob)], QT[:, :, ob * 128:(ob + 1) * 128])
                for ob in range(16):
                    qk_tile(xown(ob), sk, evk, TIDX[("o", g, ob)], KT[:, :, 2048 + ob * 128:2048 + (ob + 1) * 128])
                for pb in range(d):
                    qk_tile(xprev(pb), sk, evk, TIDX[("p", g, pb)], KT[:, :, pb * 128:(pb + 1) * 128])
                flush_pend()
                for ob in range(16):
                    v_tile(xown(ob), sv, evv, Vaug[:, 16 + ob, :, 0:64], lambda P_: P_[:, 0:256].rearrange("p (a c) -> p a c", a=4))
                for pb in range(d):
                    v_tile(xprev(pb), sv, evv, Vaug[:, pb, :, 0:64], lambda P_: P_[:, 0:256].rearrange("p (a c) -> p a c", a=4))
                PROJ_DONE = [st["last_qk"], st["last_v"]]
                PE.wait(PROJ_DONE)
                work = [(r, hl, n) for r in range(d) for hl in range(4) for n in range(nb)]

                def dil_S(item, d=d, nb=nb):
                    r, hl, n = item
                    p, s_ = hl // 2, hl % 2
                    lo, hi = s_ * 64, s_ * 64 + 64
                    ob = r * nb + n
                    qpos = ob * 128
                    if n == 0:
                        kposA, blkA = r * 128, r
                    else:
                        kposA, blkA = 2048 + (ob - 1) * 128, 16 + ob - 1
                    kposB, blkB = 2048 + ob * 128, 16 + ob
                    bi = bufc[0] % 2
                    bufc[0] += 1
                    S = psSb[bi]
                    PE.wait(sfree[bi])
                    qap = QT[lo:hi, p, qpos:qpos + 128]
                    PE.op(lambda h: h.matmul(S[:, 0:128], lhsT=KT[lo:hi, p, kposA:kposA + 128], rhs=qap, start=True, stop=False), sig=False)
                    PE.op(lambda h: h.matmul(S[:, 0:128], lhsT=negI[:, :], rhs=maskA[:, :], start=False, stop=True), sig=False)
                    PE.op(lambda h: h.matmul(S[:, 128:256], lhsT=KT[lo:hi, p, kposB:kposB + 128], rhs=qap, start=True, stop=False), sig=False)
                    sev = PE.op(lambda h: h.matmul(S[:, 128:256], lhsT=negI[:, :], rhs=cmask[:, 0, 0:128], start=False, stop=True))
                    pi = ptc[0] % 4
                    ptc[0] += 1
                    ACT.wait(sev, ptfree[pi])
                    if n == 0:
                        ACT.op(lambda h: h.activation(out=PT[pi][:, 0:128], in_=S[:, 0:128], func=AF.Exp, bias=prevbias[:, 0:1], scale=0.125), sig=False)
                        aev = ACT.op(lambda h: h.activation(out=PT[pi][:, 128:256], in_=S[:, 128:256], func=AF.Exp, scale=0.125))
                    else:
                        aev = ACT.op(lambda h: h.activation(out=PT[pi][:, 0:256], in_=S[:, 0:256], func=AF.Exp, scale=0.125))
                    sfree[bi] = aev
                    return (aev, pi, bi, blkA, blkB)

                def dil_AV(item, pend_, g=g, d=d):
                    r, hl, n = item
                    aev, pi, bi, blkA, blkB = pend_
                    O = psOb[bi]
                    PE.wait(aev, ofree[bi])
                    PE.op(lambda h: h.matmul(O[0:65, 0:128], lhsT=Vaug[:, blkA, hl, :], rhs=PT[pi][:, 0:128], start=True, stop=False), sig=False)
                    oev = PE.op(lambda h: h.matmul(O[0:65, 0:128], lhsT=Vaug[:, blkB, hl, :], rhs=PT[pi][:, 128:256], start=False, stop=True))
                    ptfree[pi] = oev
                    dst = acc[0:65, hl, :].rearrange("p (l d) -> p d l", d=d)[:, r, n * 128:(n + 1) * 128]
                    DVE.wait(oev, acc_last[0] if g > 0 else None)
                    if g == 0:
                        dev = DVE.op(lambda h: h.tensor_copy(out=dst, in_=O[0:65, 0:128]))
                    else:
                        dev = DVE.op(lambda h: h.tensor_tensor(out=dst, in0=dst, in1=O[0:65, 0:128], op=ALU.add))
                    ofree[bi] = dev
                    acc_last[0] = dev

                pend_ = dil_S(work[0])
                for wi, item in enumerate(work):
                    nxt = dil_S(work[wi + 1]) if wi + 1 < len(work) else None
                    dil_AV(item, pend_)
                    pend_ = nxt
            DVE.wait(st.get("ybstore"))
            for hl in range(4):
                for w in range(4):
                    PE.wait(acc_last[0], MEMS, st.get("dfree"))
                    bev = PE.op(lambda h, hl=hl, w=w: h.matmul(psD0[0:64, :], lhsT=onesF[64:65, 0:64], rhs=acc[64:65, hl, w * 512:(w + 1) * 512], start=True, stop=True))
                    ACT.wait(bev, acc_last[0])
                    rev = ACT.op(lambda h: h.activation(out=ftA[0:64, :], in_=psD0[0:64, :], func=AF.Ln))
                    st["dfree"] = rev
                    ACT.wait(rev)
                    rev = ACT.op(lambda h: h.activation(out=ftA[0:64, :], in_=ftA[0:64, :], func=AF.Exp, scale=-1.0))
                    DVE.wait(rev)
                    fev = DVE.op(lambda h, hl=hl, w=w: h.tensor_tensor(out=ybH[0:64, hl, w * 512:(w + 1) * 512], in0=acc[0:64, hl, w * 512:(w + 1) * 512], in1=ftA[0:64, :], op=ALU.mult))
                    acc_last[0] = fev
            SP.wait(acc_last[0])
            st["ybstore"] = SP.dma(lambda h, hh=hh: h.dma_start(out=ybd[:, hh * 8192:(hh + 1) * 8192], in_=ybH[:, :, :].rearrange("p h t -> p (h t)")), d_yb)
        YB_DONE = [acc_last[0], st["ybstore"]]

        Vd = VVf[:, 0:32 * 256].rearrange("p (b c) -> p b c", b=32)
        psOd = [psO0, psO1]
        psDd = [psD0, psD1]
        fin_free = YB_DONE
        st["sbf"] = [[sfree[0], sfree[1]], None]
        st["acc_prev"] = [None, None]
        att_last = YB_DONE
        ya_last = None
        for pp in range(2):
            ACT.wait(att_last)
            DVE.wait(att_last)
            sq, evq = load_w(pp * 256)
            sk, evk = load_w(512 + pp * 256)
            sv, evv = load_w(1024 + pp * 256)
            for ob in range(16):
                qk_tile(xnat(16 + ob), sq, evq, TIDX[("n", 0, 16 + ob)], QT[:, :, ob * 128:(ob + 1) * 128])
            for blk in range(32):
                qk_tile(xnat(blk), sk, evk, TIDX[("n", 0, blk)], KT[:, :, blk * 128:(blk + 1) * 128])
            flush_pend()
            for blk in range(32):
                v_tile(xnat(blk), sv, evv, Vd[:, blk, :], lambda P_: P_[:, 0:256])
            PE.wait(st["last_qk"], st["last_v"])
            for hl in range(2):
                for j in range(4):
                    nkb = 16 + 4 * (j + 1)
                    PE.wait(fin_free, st["pf"][1], st["pf"][0], st["psP_free"], st["psT_free"])
                    SBK = [(psS0, psS1), (psP, psT)]
                    accD = [av(106 * K, [128, 512], F32), av(108 * K, [128, 512], F32)]
                    accE = [DVE, POOL]

                    def dif_S(kb, hl=hl, j=j):
                        bp = bufc[0] % 2
                        bufc[0] += 1
                        diag = kb >= 16 + 4 * j
                        PE.wait(st["sbf"][bp])
                        for c in range(2):
                            lo, hi = c * 64, c * 64 + 64
                            S = SBK[bp][c]
                            sev = PE.op(lambda h, S=S, lo=lo, hi=hi: h.matmul(S[:, :], lhsT=KT[lo:hi, hl, kb * 128:(kb + 1) * 128], rhs=QT[lo:hi, hl, j * 512:(j + 1) * 512], start=True, stop=not diag), sig=(c == 1 and not diag))
                            if diag:
                                v = kb - 16 - 4 * j
                                sev = PE.op(lambda h, S=S, v=v: h.matmul(S[:, :], lhsT=negI[:, :], rhs=cmask[:, v, :], start=False, stop=True), sig=(c == 1))
                        pis = [(2 * bp) % 4, (2 * bp + 1) % 4]
                        ACT.wait(sev, ptfree[pis[0]], ptfree[pis[1]])
                        SS = [pairA, pairB][bp]
                        if kb < 16:
                            aev = ACT.op(lambda h: h.activation(out=PT2[bp][:, :], in_=SS[:, :], func=AF.Exp, bias=prevbias[:, 0:1], scale=0.125))
                        else:
                            aev = ACT.op(lambda h: h.activation(out=PT2[bp][:, :], in_=SS[:, :], func=AF.Exp, scale=0.125))
                        st["sbf"][bp] = aev
                        if bp == 1:
                            st["psP_free"] = aev
                            st["psT_free"] = aev
                            st["pf"][0] = aev
                        else:
                            sfree[0] = aev
                            sfree[1] = aev
                        return (aev, pis)

                    def dif_AV(kb, pend_, hl=hl, nkb=nkb):
                        aev, pis = pend_
                        even = (kb % 2 == 0)
                        PE.wait(aev)
                        PE.op(lambda h: h.matmul(psOd[0][:, :], lhsT=Vd[:, kb, hl * 128:(hl + 1) * 128], rhs=PT[pis[0]][:, :], start=(kb == 0), stop=(kb == nkb - 1)), sig=False)
                        oev_ = PE.op(lambda h: h.matmul(psOd[1][:, :], lhsT=Vd[:, kb, hl * 128:(hl + 1) * 128], rhs=PT[pis[1]][:, :], start=(kb == 0), stop=(kb == nkb - 1)), sig=not even)
                        if even:
                            PE.op(lambda h: h.matmul(psDd[0][:, :], lhsT=ones_bf[:, :], rhs=PT[pis[0]][:, :], start=(kb == 0), stop=False), sig=False)
                            oev_ = PE.op(lambda h: h.matmul(psDd[1][:, :], lhsT=ones_bf[:, :], rhs=PT[pis[1]][:, :], start=(kb == 0), stop=False))
                            ptfree[pis[0]] = oev_
                            ptfree[pis[1]] = oev_
                            return oev_
                        devs = []
                        for c in range(2):
                            E_ = accE[c]
                            E_.wait(aev, st["acc_prev"][c])
                            if kb == 1:
                                dv = E_.op(lambda h, c=c: h.tensor_copy(out=accD[c][:, :], in_=PT[pis[c]][:, :]))
                            else:
                                dv = E_.op(lambda h, c=c: h.tensor_tensor(out=accD[c][:, :], in0=accD[c][:, :], in1=PT[pis[c]][:, :], op=ALU.add))
                            st["acc_prev"][c] = dv
                            devs.append(dv)
                        ptfree[pis[0]] = [oev_, devs[0]]
                        ptfree[pis[1]] = [oev_, devs[1]]
                        return oev_

                    pend_ = dif_S(0)
                    for kb in range(nkb):
                        nxt = dif_S(kb + 1) if kb + 1 < nkb else None
                        oev = dif_AV(kb, pend_)
                        pend_ = nxt
                    PE.wait(st["acc_prev"][0], st["acc_prev"][1], MEMS)
                    PE.op(lambda h: h.matmul(psD0[:, :], lhsT=onesF[:, :], rhs=accD[0][:, :], start=False, stop=True), sig=False)
                    oev = PE.op(lambda h: h.matmul(psD1[:, :], lhsT=onesF[:, :], rhs=accD[1][:, :], start=False, stop=True))
                    st["acc_prev"] = [oev, oev]
                    DVE.wait(oev, LAMEV, ya_last)
                    ACT.wait(oev, ya_last)
                    ACT.op(lambda h: h.activation(out=ft[0][:, :], in_=psD0[:, :], func=AF.Ln), sig=False)
                    a = ACT.op(lambda h: h.activation(out=ft[1][:, :], in_=psD1[:, :], func=AF.Ln))
                    ACT.wait(a)
                    ACT.op(lambda h: h.activation(out=ft[0][:, :], in_=ft[0][:, :], func=AF.Exp, scale=-1.0), sig=False)
                    a = ACT.op(lambda h: h.activation(out=ft[1][:, :], in_=ft[1][:, :], func=AF.Exp, scale=-1.0))
                    DVE.wait(a)
                    a = DVE.op(lambda h: h.tensor_tensor(out=ft[0][:, :], in0=psO0[:, :], in1=ft[0][:, :], op=ALU.mult), sig=False)
                    a = DVE.op(lambda h: h.tensor_tensor(out=ft[1][:, :], in0=psO1[:, :], in1=ft[1][:, :], op=ALU.mult))
                    fin_free = a
                    st["pf"][1] = a
                    DVE.wait(a)
                    a = DVE.op(lambda h: h.scalar_tensor_tensor(out=ft[2][:, :], in0=ft[1][:, :], scalar=neglam, in1=ft[0][:, :], op0=ALU.mult, op1=ALU.add))
                    DVE.wait(a)
                    a = DVE.op(lambda h: h.tensor_tensor(out=ft[3][:, :], in0=ft[2][:, :], in1=ft[2][:, :], op=ALU.mult))
                    PE.wait(a, st["sbf"][0])
                    m = PE.op(lambda h: h.matmul(psS0[:, :], lhsT=onesF[:, :], rhs=ft[3][:, :], start=True, stop=True))
                    ACT.wait(m)
                    a = ACT.op(lambda h: h.activation(out=ft[4][:, :], in_=psS0[:, :], func=AF.Ln, bias=epsc[:, 0:1], scale=1.0 / 128.0))
                    st["sbf"][0] = a
                    sfree[0] = a
                    ACT.wait(a)
                    a = ACT.op(lambda h: h.activation(out=ft[4][:, :], in_=ft[4][:, :], func=AF.Exp, scale=-0.5))
                    DVE.wait(a)
                    a = DVE.op(lambda h, pp=pp, hl=hl, j=j: h.scalar_tensor_tensor(out=yaT[:, 2 * pp + hl, j * 512:(j + 1) * 512], in0=ft[2][:, :], scalar=gsc, in1=ft[4][:, :], op0=ALU.mult, op1=ALU.mult))
                    ya_last = a
                    att_last = a
        YA_DONE = ya_last

        d_dbg = dsem("d_dbg")
        if debug:
            SP.wait(YA_DONE, YB_DONE)
            SP.dma(lambda h: h.dma_start(out=dbg_ya, in_=yaT[:, :, :].rearrange("p h t -> p (h t)")), d_dbg)

        d_pw = dsem("d_pw")
        POOL.wait(YA_DONE, YB_DONE)
        SP.wait(YA_DONE, YB_DONE)
        if debug:
            POOL.wait(d_dbg.ev())
            SP.wait(d_dbg.ev())
        POOL.dma(lambda h: h.dma_start(out=woa[:, :, :], in_=w_oa.rearrange("(k p) c -> p k c", p=128)), d_pw)
        POOL.dma(lambda h: h.dma_start(out=wob[:, :, :], in_=w_ob.rearrange("(k p) c -> p k c", p=64)), d_pw)
        POOL.dma(lambda h: h.dma_start(out=wo[:, :, :], in_=w_o.rearrange("(k p) c -> p k c", p=128)), d_pw)
        SP.dma(lambda h: h.dma_start(out=lnp1[:, :, :], in_=lnp_d[:, 0:2 * D].rearrange("p (a c) -> p a c", a=2)), d_pw)
        SP.dma(lambda h: h.dma_start(out=wrt[:, :, :], in_=w_router.rearrange("(k p) c -> p k c", p=128)), d_pw)
        PW = d_pw.ev()

        h1T = xTp
        d_xf = dsem("d_xf")
        d_h1 = dsem("d_h1")
        d_ybw = dsem("d_ybw")
        d_sc = dsem("d_sc")
        h1b = av(177 * K, [128, D], BF16)
        xf_free = None
        ybw_free = None
        g_last = {"ga": None, "gb": None, "mT": None, "h1Tf": None, "misc": None}

        def layer_norm(buf, lnv, pre_wait):
            DVE.wait(pre_wait)
            DVE.op(lambda h: h.bn_stats(out=stt[:, 0, :], in_=buf[:, 0:512]), sig=False)
            a = DVE.op(lambda h: h.bn_stats(out=stt[:, 1, :], in_=buf[:, 512:1024]))
            DVE.wait(a)
            a = DVE.op(lambda h: h.bn_aggr(out=mv[:, :], in_=stt[:, :, :].rearrange("p a b -> p (a b)")))
            ACT.wait(a)
            a = ACT.op(lambda h: h.activation(out=sm[:, 0:1], in_=mv[:, 1:2], func=AF.Ln, bias=epsc[:, 0:1], scale=1.0))
            ACT.wait(a)
            a = ACT.op(lambda h: h.activation(out=sm[:, 1:2], in_=sm[:, 0:1], func=AF.Exp, scale=-0.5))
            DVE.wait(a)
            a = DVE.op(lambda h: h.tensor_scalar(out=buf[:, :], in0=buf[:, :], scalar1=mv[:, 0:1], scalar2=sm[:, 1:2], op0=ALU.subtract, op1=ALU.mult))
            DVE.wait(a)
            a = DVE.op(lambda h: h.tensor_tensor(out=buf[:, :], in0=buf[:, :], in1=lnv[:, 0, :], op=ALU.mult))
            DVE.wait(a)
            a = DVE.op(lambda h: h.tensor_tensor(out=buf[:, :], in0=buf[:, :], in1=lnv[:, 1, :], op=ALU.add))
            return a

        xf3r = [xf3, av(0, [128, D], F32)]
        h1Tfr = [h1Tf, av(4 * K, [128, 8, 128], F32)]
        d_xfr = [dsem("d_xfr0"), dsem("d_xfr1")]
        d_h1r = [dsem("d_h1r0"), dsem("d_h1r1")]
        xf_fr = [None, None]
        h1Tf_fr = [None, None]
        tr_ev = {}
        stv_ev = {}
        pendB = [None]

        def stageA(tb, tbl, mt_ready):
            s_ = tb % 2
            xf = xf3r[s_]
            hT = h1Tfr[s_]
            SP.wait(xf_fr[s_])
            xev = SP.dma(lambda h: h.dma_start(out=xf[:, :], in_=x_own[tb * 128:(tb + 1) * 128, :]), d_xfr[s_])
            PE.wait(mt_ready, st.get("dfree2"))
            for dh in range(2):
                for fc in range(8):
                    e = PE.op(lambda h, dh=dh, fc=fc: h.matmul(psDd[dh][:, :], lhsT=mT[:, fc, tbl * 128:(tbl + 1) * 128], rhs=wo[:, fc, dh * 512:(dh + 1) * 512], start=(fc == 0), stop=(fc == 7)), sig=(fc == 7 and dh == 1))
            mm_o = e
            if tbl == 3:
                g_last["mT"] = mm_o
            DVE.wait(mm_o, xev)
            DVE.op(lambda h: h.scalar_tensor_tensor(out=xf[:, 0:512], in0=xf[:, 0:512], scalar=ALPHA, in1=psD0[:, :], op0=ALU.mult, op1=ALU.add), sig=False)
            a = DVE.op(lambda h: h.scalar_tensor_tensor(out=xf[:, 512:1024], in0=xf[:, 512:1024], scalar=ALPHA, in1=psD1[:, :], op0=ALU.mult, op1=ALU.add))
            st["dfree2"] = a
            h1ev = layer_norm(xf, lnp1, a)
            SP.wait(h1ev)
            stv_ev[tb] = SP.dma(lambda h: h.dma_start(out=h1d[tb * 128:(tb + 1) * 128, :], in_=xf[:, :]), d_h1r[s_])
            for half in range(2):
                PE.wait(h1ev, st["psP_free"])
                for q_ in range(4):
                    kc = half * 4 + q_
                    e = PE.op(lambda h, kc=kc, q_=q_: h.transpose(out=psP[:, q_ * 128:(q_ + 1) * 128], in_=xf[:, kc * 128:(kc + 1) * 128], identity=identf[:, :]), sig=(q_ == 3))
                ACT.wait(e, h1Tf_fr[s_] if half == 0 else None)
                a = ACT.op(lambda h, half=half: h.activation(out=hT[:, half * 4:(half + 1) * 4, :], in_=psP[:, :].rearrange("p (k t) -> p k t", k=4), func=AF.Copy))
                st["psP_free"] = a
            tr_ev[tb] = (a, e)

        def stageB(tb):
            s_ = tb % 2
            xf = xf3r[s_]
            hT = h1Tfr[s_]
            a, e_tr = tr_ev[tb]
            PE.wait(a, sfree[0])
            for kc in range(8):
                e = PE.op(lambda h, kc=kc: h.matmul(psS0[:, 0:NE], lhsT=hT[:, kc, :], rhs=wrt[:, kc, :], start=(kc == 0), stop=(kc == 7)), sig=(kc == 7))
            h1Tf_fr[s_] = e
            DVE.wait(e)
            a = DVE.op(lambda h: h.tensor_tensor(out=lg[:, :], in0=psS0[:, 0:NE], in1=brt[:, :], op=ALU.add))
            sfree[0] = a
            DVE.wait(a)
            a = DVE.op(lambda h: h.max(out=mx8[:, :], in_=lg[:, :]))
            DVE.wait(a)
            a = DVE.op(lambda h: h.tensor_scalar(out=sm[:, 2:3], in0=mx8[:, 0:1], scalar1=-1.0, scalar2=None, op0=ALU.mult))
            ACT.wait(a)
            ACT.op(lambda h: h.activation(out=sm[:, 4:8], in_=mx8[:, 0:4], func=AF.Exp, bias=sm[:, 2:3], scale=1.0), sig=False)
            a = ACT.op(lambda h: h.activation(out=el[:, :], in_=lg[:, :], func=AF.Exp, bias=sm[:, 2:3], scale=1.0))
            DVE.wait(a)
            a = DVE.op(lambda h: h.reduce_sum(out=sm[:, 3:4], in_=sm[:, 4:8], axis=AX.X))
            DVE.wait(a)
            a = DVE.op(lambda h: h.reciprocal(out=rsm[:, tb:tb + 1], in_=sm[:, 3:4]))
            DVE.wait(a)
            a = DVE.op(lambda h: h.tensor_scalar(out=el[:, :], in0=el[:, :], scalar1=rsm[:, tb:tb + 1], scalar2=None, op0=ALU.mult))
            DVE.wait(a)
            a = DVE.op(lambda h: h.scalar_tensor_tensor(out=gwd[:, tb, :], in0=lg[:, :], scalar=mx8[:, 3:4], in1=el[:, :], op0=ALU.is_ge, op1=ALU.mult))
            a = DVE.op(lambda h: h.tensor_scalar(out=m01[:, tb, :], in0=lg[:, :], scalar1=mx8[:, 3:4], scalar2=None, op0=ALU.is_ge))
            PE.wait(a, sfree[1])
            for tb2 in range(tb + 1):
                e = PE.op(lambda h, tb2=tb2: h.matmul(psS1[:, 0:NE], lhsT=(maskA[:, :] if tb2 == tb else ones_bf[:, :]), rhs=m01[:, tb2, :], start=(tb2 == 0), stop=(tb2 == tb)), sig=(tb2 == tb))
            DVE.wait(e)
            a = DVE.op(lambda h: h.tensor_tensor(out=slotv[:, :], in0=psS1[:, 0:NE], in1=ebase[:, :], op=ALU.add))
            sfree[1] = a
            DVE.wait(a)
            for k_ in range(4):
                DVE.op(lambda h, k_=k_: h.scalar_tensor_tensor(out=rtm[:, :], in0=lg[:, :], scalar=mx8[:, k_:k_ + 1], in1=slotv[:, :], op0=ALU.is_equal, op1=ALU.mult))
                DVE.wait((DVE.sem, DVE.n, DVE.name))
                DVE.op(lambda h, k_=k_: h.reduce_sum(out=destf[:, k_:k_ + 1], in_=rtm[:, :], axis=AX.X))
                DVE.wait((DVE.sem, DVE.n, DVE.name))
            a = DVE.op(lambda h: h.tensor_copy(out=desti[:, tb, :], in_=destf[:, :]))
            DVE.op(lambda h: h.tensor_scalar(out=gw4[:, tb, :], in0=sm[:, 4:8], scalar1=rsm[:, tb:tb + 1], scalar2=None, op0=ALU.mult))
            DVE.wait(st.get("h1b_free"))
            a = DVE.op(lambda h: h.tensor_copy(out=h1b[:, :], in_=xf[:, :]))
            POOL.wait(a, ZFILL)
            for k_ in range(4):
                sc = POOL.dma(lambda h, k_=k_: h.indirect_dma_start(out=Xg[:, :], out_offset=bass.IndirectOffsetOnAxis(ap=desti[:, tb, k_:k_ + 1], axis=0), in_=h1b[:, :], in_offset=None), d_sc)
            st["h1b_free"] = sc
            xf_fr[s_] = [stv_ev[tb], e_tr, a]
            g_last["misc"] = a
            st["route_done"] = a

        PE.wait(PW, YA_DONE, YB_DONE)
        DVE.wait(PW)
        for w in range(4):
            tok = slice(w * 512, (w + 1) * 512)
            SP.wait(ybw_free)
            ybw_ev = SP.dma(lambda h, tok=tok: h.dma_start(out=ybw[:, :, :], in_=ybd.rearrange("p (h t) -> p h t", h=8)[:, :, tok]), d_ybw)
            for cp in range(4):
                sA, evA = load_w(6144 + cp * 256)
                sB, evB = load_w(7168 + cp * 256)
                for fcl in range(2):
                    fc = 2 * cp + fcl
                    PE.wait(evA, sfree[0])
                    for kc in range(8):
                        e = PE.op(lambda h, kc=kc, sA=sA, fcl=fcl, tok=tok: h.matmul(psS0[:, :], lhsT=Wr[sA][:, kc, fcl * 128:(fcl + 1) * 128], rhs=xTo[:, kc, tok], start=(kc == 0), stop=(kc == 7)), sig=(kc == 7))
                    if fcl == 1:
                        wfree[sA] = e
                    ACT.wait(e, g_last["ga"], CONST)
                    ga = ACT.op(lambda h, fc=fc: h.activation(out=ft[0][:, :], in_=psS0[:, :], func=AF.Sigmoid, bias=bgt[:, fc:fc + 1], scale=1.0))
                    sfree[0] = ga
                    PE.wait(evB, sfree[1])
                    for kc in range(8):
                        e = PE.op(lambda h, kc=kc, sB=sB, fcl=fcl, tok=tok: h.matmul(psS1[:, :], lhsT=Wr[sB][:, kc, fcl * 128:(fcl + 1) * 128], rhs=xTo[:, kc, tok], start=(kc == 0), stop=(kc == 7)), sig=(kc == 7))
                    if fcl == 1:
                        wfree[sB] = e
                    ACT.wait(e, g_last["gb"])
                    gb = ACT.op(lambda h, fc=fc: h.activation(out=ft[1][:, :], in_=psS1[:, :], func=AF.Sigmoid, bias=bgt[:, 8 + fc:9 + fc], scale=1.0))
                    sfree[1] = gb
                    PE.wait(ofree[0])
                    for kc in range(4):
                        e = PE.op(lambda h, kc=kc, fc=fc, tok=tok: h.matmul(psO0[:, :], lhsT=woa[:, kc, fc * 128:(fc + 1) * 128], rhs=yaT[:, kc, tok], start=(kc == 0), stop=(kc == 3)), sig=(kc == 3))
                    oa = e
                    PE.wait(ofree[1], ybw_ev)
                    for hh_ in range(8):
                        e = PE.op(lambda h, hh_=hh_, fc=fc: h.matmul(psO1[:, :], lhsT=wob[0:64, hh_, fc * 128:(fc + 1) * 128], rhs=ybw[0:64, hh_, :], start=(hh_ == 0), stop=(hh_ == 7)), sig=(hh_ == 7))
                    obv = e
                    if fc == 7:
                        ybw_free = e
                    DVE.wait(ga, oa, g_last["misc"])
                    a = DVE.op(lambda h: h.tensor_tensor(out=ft[2][:, :], in0=psO0[:, :], in1=ft[0][:, :], op=ALU.mult))
                    ofree[0] = a
                    g_last["ga"] = a
                    DVE.wait(gb, obv)
                    b = DVE.op(lambda h: h.tensor_tensor(out=ft[3][:, :], in0=psO1[:, :], in1=ft[1][:, :], op=ALU.mult))
                    ofree[1] = b
                    g_last["gb"] = b
                    DVE.wait(a, b, g_last["mT"] if fc == 0 else None)
                    c_ = DVE.op(lambda h, fc=fc: h.tensor_tensor(out=mT[:, fc, :], in0=ft[2][:, :], in1=ft[3][:, :], op=ALU.add))
                    g_last["misc"] = c_
            MT_READY = c_
            for tbl in range(4):
                tb = w * 4 + tbl
                stageA(tb, tbl, MT_READY)
                if pendB[0] is not None:
                    stageB(pendB[0])
                pendB[0] = tb
        stageB(pendB[0])
        ROUTE_DONE = st["route_done"]
        xf_free = [xf_fr[0], xf_fr[1]]
        P3_DONE = [ROUTE_DONE, st["psP_free"], xf_free]
        H1D_DONE = [d_h1r[0].ev(), d_h1r[1].ev()]

        Wd = [av(i * 16 * K, [128, 8, D], BF16) for i in range(2)]
        Xe = [av(32 * K + i * 6 * K, [128, 3, D], BF16) for i in range(2)]
        XT = [av(44 * K + i * 6 * K, [128, 8, CAP], BF16) for i in range(2)]
        actT = [av(56 * K + i * 6 * K, [128, 8, CAP], BF16) for i in range(2)]
        Yt = [av(68 * K + i * 4 * K, [128, D], F32) for i in range(2)]
        Wgu = [av(112 * K + i * 32 * K, [128, 8, 2048], BF16) for i in range(2)]
        tg = [av(100 * K + i * 6 * K, [128, CAP], F32) for i in range(2)]
        tsg = [av(102 * K + i * 6 * K, [128, CAP], F32) for i in range(2)]
        tu = [av(104 * K + i * 6 * K, [128, CAP], F32) for i in range(2)]
        d_gu = [dsem(f"d_gu{i}") for i in range(2)]
        d_dn = [dsem(f"d_dn{i}") for i in range(2)]
        d_xe = [dsem("d_xe0"), dsem("d_xe1")]
        d_yg = [dsem("d_yg0"), dsem("d_yg1")]
        gu_free = [None] * 2
        dn_free = [None] * 2
        xe_free = [None, None]
        yt_free = [None, None]
        items = [(e, q) for e in range(NE) for q in range(4)]
        gu_ev = {}
        dn_ev = {}
        xe_ev = {}
        xt_ready = {}

        def issue_gu(e):
            s = e % 2
            POOL.wait(gu_free[s])
            gu_ev[e] = POOL.dma(lambda h, s=s, e=e: h.dma_start(out=Wgu[s][:, :, :], in_=w_gu[e].rearrange("(k p) c -> p k c", p=128)), d_gu[s])

        def issue_dn(e):
            s = e % 2
            POOL.wait(dn_free[s])
            dn_ev[e] = POOL.dma(lambda h, s=s, e=e: h.dma_start(out=Wd[s][:, :, :], in_=w_down[e].rearrange("(k p) c -> p k c", p=128)), d_dn[s])

        def issue_xe(e):
            s = e % 2
            SP.wait(xe_free[s])
            xe_ev[e] = SP.dma(lambda h, s=s, e=e: h.dma_start(out=Xe[s][:, :, :], in_=Xg[e * CAP:(e + 1) * CAP, :].rearrange("(n p) d -> p n d", p=128)), d_xe[s])

        def do_transposes(e):
            s = e % 2
            for n in range(3):
                PE.wait(xe_ev[e], st["psT_free"])
                for kc in range(8):
                    ev_ = PE.op(lambda h, kc=kc, n=n, s=s: h.transpose(out=psTb[:, kc * 128:(kc + 1) * 128], in_=Xe[s][:, n, kc * 128:(kc + 1) * 128], identity=identb[:, :]), sig=(kc == 7))
                ACT.wait(ev_)
                st["psT_free"] = ACT.op(lambda h, n=n, s=s: h.activation(out=XT[s][:, :, n * 128:(n + 1) * 128], in_=psTb[:, :].rearrange("p (k t) -> p k t", k=8), func=AF.Copy))
            xe_free[s] = ev_
            xt_ready[e] = st["psT_free"]

        SCAT_DONE = d_sc.ev()
        POOL.wait(P3_DONE, H1D_DONE)
        PE.wait(P3_DONE)
        DVE.wait(P3_DONE, H1D_DONE)
        ACT.wait(P3_DONE, H1D_DONE)
        SP.wait(P3_DONE, SCAT_DONE)
        issue_gu(0)
        issue_dn(0)
        issue_xe(0)
        issue_xe(1)
        do_transposes(0)
        banks = [(psS0, psS1), (psD0, psD1)]
        bfree = [[sfree[0], sfree[1]], [st.get("dfree2"), st.get("dfree2")]]
        obk = [psO0, psO1, psP]
        obkfree = [ofree[0], ofree[1], st["psP_free"]]
        tfree = [None, None]
        stepc = 0
        ocnt = 0
        act_last = None
        for e in range(NE):
            xs = e % 2
            if e + 1 < NE:
                issue_gu(e + 1)
                issue_dn(e + 1)
            s = e % 2
            for q in range(4):
                i = e
                for jj in range(2):
                    j = 2 * q + jj
                    b = stepc % 2
                    stepc += 1
                    Pg, Pu = banks[b]
                    PE.wait(gu_ev[i], xt_ready[e], bfree[b][0], bfree[b][1])
                    for kc in range(8):
                        eg = PE.op(lambda h, kc=kc, Pg=Pg, s=s, j=j, xs=xs: h.matmul(Pg[:, 0:CAP], lhsT=Wgu[s][:, kc, j * 128:(j + 1) * 128], rhs=XT[xs][:, kc, :], start=(kc == 0), stop=(kc == 7)), sig=(kc == 7))
                    for kc in range(8):
                        eu = PE.op(lambda h, kc=kc, Pu=Pu, s=s, j=j, xs=xs: h.matmul(Pu[:, 0:CAP], lhsT=Wgu[s][:, kc, 1024 + j * 128:1024 + (j + 1) * 128], rhs=XT[xs][:, kc, :], start=(kc == 0), stop=(kc == 7)), sig=(kc == 7))
                    if j == 7:
                        gu_free[s] = eu
                    DVE.wait(eg, tfree[b])
                    a1 = DVE.op(lambda h, Pg=Pg, e=e, j=j, b=b: h.tensor_scalar(out=tg[b][:, :], in0=Pg[:, 0:CAP], scalar1=bgu[:, e * 16 + j:e * 16 + j + 1], scalar2=7.0, op0=ALU.add, op1=ALU.min))
                    bfree[b][0] = a1
                    ACT.wait(a1, eu)
                    ACT.op(lambda h, b=b: h.activation(out=tsg[b][:, :], in_=tg[b][:, :], func=AF.Sigmoid, scale=1.702), sig=False)
                    a2 = ACT.op(lambda h, Pu=Pu, e=e, j=j, b=b: h.activation(out=tu[b][:, :], in_=Pu[:, 0:CAP], func=AF.Identity, bias=bgu[:, e * 16 + 8 + j:e * 16 + 9 + j], scale=1.0))
                    bfree[b][1] = a2
                    DVE.wait(a2)
                    a3 = DVE.op(lambda h, b=b: h.tensor_scalar(out=tu[b][:, :], in0=tu[b][:, :], scalar1=7.0, scalar2=-7.0, op0=ALU.min, op1=ALU.max))
                    DVE.wait(a3)
                    a4 = DVE.op(lambda h, b=b: h.scalar_tensor_tensor(out=tu[b][:, :], in0=tu[b][:, :], scalar=1.0, in1=tg[b][:, :], op0=ALU.add, op1=ALU.mult))
                    DVE.wait(a4)
                    a5 = DVE.op(lambda h, j=j, b=b, xs=xs: h.tensor_tensor(out=actT[xs][:, j, :], in0=tu[b][:, :], in1=tsg[b][:, :], op=ALU.mult))
                    tfree[b] = a5
                    act_last = a5
            if e + 1 < NE:
                do_transposes(e + 1)
            if e + 2 < NE:
                issue_xe(e + 2)
            ds_ = e % 2
            PE.wait(act_last, dn_ev[e])
            for nb_ in range(3):
                ys = (e * 3 + nb_) % 2
                for dh in range(2):
                    oi = ocnt % 3
                    ocnt += 1
                    O = obk[oi]
                    PE.wait(obkfree[oi])
                    for fc in range(8):
                        em = PE.op(lambda h, O=O, fc=fc, nb_=nb_, dh=dh, ds_=ds_, xs=xs: h.matmul(O[:, :], lhsT=actT[xs][:, fc, nb_ * 128:(nb_ + 1) * 128], rhs=Wd[ds_][:, fc, dh * 512:(dh + 1) * 512], start=(fc == 0), stop=(fc == 7)), sig=(fc == 7))
                    ACT.wait(em, yt_free[ys] if dh == 0 else None)
                    ac = ACT.op(lambda h, O=O, ys=ys, dh=dh: h.activation(out=Yt[ys][:, dh * 512:(dh + 1) * 512], in_=O[:, :], func=AF.Copy))
                    obkfree[oi] = ac
                SP.wait(ac)
                yt_free[ys] = SP.dma(lambda h, ys=ys, e=e, nb_=nb_: h.dma_start(out=Yg[e * CAP + nb_ * 128:e * CAP + (nb_ + 1) * 128, :], in_=Yt[ys][:, :]), d_yg[ys])
            dn_free[ds_] = em
        MOE_DONE = [ac, em, d_yg[0].ev(), d_yg[1].ev()]

        G = [[av((s_ * 4 + k_) * 4 * K, [128, D], F32) for k_ in range(4)] for s_ in range(2)]
        d_hl = [dsem("d_hl0"), dsem("d_hl1")]
        d_out = [dsem("d_out0"), dsem("d_out1")]
        d_g = [dsem("d_g0"), dsem("d_g1")]
        d_l2 = dsem("d_l2")
        buf_free = [None, None]
        g_free = [None, None]
        gT_free = None
        SP.wait(H1D_DONE, MOE_DONE)
        POOL.wait(MOE_DONE)
        PE.wait(MOE_DONE)
        DVE.wait(MOE_DONE)
        l2ev = SP.dma(lambda h: h.dma_start(out=lnp2[:, :, :], in_=lnp_d[:, 2 * D:4 * D].rearrange("p (a c) -> p a c", a=2)), d_l2)
        o5free = [obkfree[0], obkfree[1]]

        def issue_gather(tb):
            s = tb % 2
            POOL.wait(g_free[s])
            for k_ in range(4):
                gev_ = POOL.dma(lambda h, s=s, tb=tb, k_=k_: h.indirect_dma_start(out=G[s][k_][:, :], out_offset=None, in_=Yg[:, :], in_offset=bass.IndirectOffsetOnAxis(ap=desti[:, tb, k_:k_ + 1], axis=0)), d_g[s])
            return gev_

        gq = {0: issue_gather(0)}
        for tb in range(16):
            s = tb % 2
            if tb + 1 < 16:
                gq[tb + 1] = issue_gather(tb + 1)
            SP.wait(buf_free[s])
            hev = SP.dma(lambda h, s=s, tb=tb: h.dma_start(out=xf5[s][:, :], in_=h1d[tb * 128:(tb + 1) * 128, :]), d_hl[s])
            PE.wait(bfree[0][0], bfree[0][1], gT_free)
            e = PE.op(lambda h, tb=tb: h.transpose(out=psS0[0:NE, 0:128], in_=gwd[:, tb, :], identity=identf[:, :]))
            ACT.wait(e, gT_free)
            a = ACT.op(lambda h: h.activation(out=gT[:, :], in_=psS0[0:NE, 0:128], func=AF.Copy))
            bfree[0][0] = a
            PE.wait(a, o5free[0], o5free[1])
            for dh in range(2):
                e = PE.op(lambda h, dh=dh: h.matmul([psO0, psO1][dh][:, :], lhsT=gT[:, :], rhs=bdn[:, dh * 512:(dh + 1) * 512], start=True, stop=True))
            gT_free = e
            G0 = G[s][0]
            DVE.wait(gq[tb])
            a = DVE.op(lambda h, G0=G0, tb=tb: h.tensor_scalar(out=G0[:, :], in0=G0[:, :], scalar1=gw4[:, tb, 0:1], scalar2=None, op0=ALU.mult))
            for k_ in range(1, 4):
                DVE.wait(a)
                a = DVE.op(lambda h, G0=G0, s=s, k_=k_, tb=tb: h.scalar_tensor_tensor(out=G0[:, :], in0=G[s][k_][:, :], scalar=gw4[:, tb, k_:k_ + 1], in1=G0[:, :], op0=ALU.mult, op1=ALU.add))
            DVE.wait(a, e)
            DVE.op(lambda h, G0=G0: h.tensor_tensor(out=G0[:, 0:512], in0=G0[:, 0:512], in1=psO0[:, :], op=ALU.add), sig=False)
            a = DVE.op(lambda h, G0=G0: h.tensor_tensor(out=G0[:, 512:1024], in0=G0[:, 512:1024], in1=psO1[:, :], op=ALU.add))
            o5free = [a, a]
            DVE.wait(a, hev)
            a = DVE.op(lambda h, s=s, G0=G0: h.scalar_tensor_tensor(out=xf5[s][:, :], in0=xf5[s][:, :], scalar=ALPHA, in1=G0[:, :], op0=ALU.mult, op1=ALU.add))
            g_free[s] = a
            DVE.wait(l2ev)
            oev = layer_norm(xf5[s], lnp2, a)
            SP.wait(oev)
            buf_free[s] = SP.dma(lambda h, s=s, tb=tb: h.dma_start(out=out_d[tb * 128:(tb + 1) * 128, :], in_=xf5[s][:, :]), d_out[s])
        SP.wait(d_out[0].ev(), d_out[1].ev())
        if debug:
            SP.wait(d_dbg.ev())

        with nc.Block() as block:
            @block.tensor
            def _(h):
                for f in PE.ops:
                    f(h)

            @block.scalar
            def _(h):
                for f in ACT.ops:
                    f(h)

            @block.vector
            def _(h):
                for f in DVE.ops:
                    f(h)

            @block.gpsimd
            def _(h):
                for f in POOL.ops:
                    f(h)

            @block.sync
            def _(h):
                for f in SP.ops:
                    f(h)
    return nc


def rope_tables_np(positions):
    inv = 500000.0 ** (-np.arange(0, 16, 2, dtype=np.float32) / 16.0)
    ang = positions.astype(np.float32)[:, None] * inv[None, :]
    return np.cos(ang).astype(np.float32), np.sin(ang).astype(np.float32)


def make_consts(hf):
    TIDX, NTAB = tab_index()
    tab = np.zeros((128, NTAB, 16), np.float32)
    p = np.arange(128)
    pos0 = hf * 2048

    def put(slot, positions):
        c, s = rope_tables_np(positions)
        tab[:, slot, 0:8] = c
        tab[:, slot, 8:16] = s

    for b in range(32):
        if b < 16:
            put(TIDX[("n", 0, b)], b * 128 + p)
        else:
            put(TIDX[("n", 0, b)], pos0 + (b - 16) * 128 + p)
    for g, d in enumerate(DILS):
        nb = 16 // d
        for ob in range(16):
            r, n = ob // nb, ob % nb
            put(TIDX[("o", g, ob)], pos0 + (n * 128 + p) * d + r)
        for pb in range(d):
            put(TIDX[("p", g, pb)], 2048 - 128 * d + p * d + pb)
    k = np.arange(128)[:, None]
    q = np.arange(512)[None, :]
    cm = np.stack([(128 * v + k > q) for v in range(4)], axis=1).astype(np.float32)
    mA = (np.arange(128)[:, None] < np.arange(128)[None, :]).astype(np.float32)
    bf = ml_dtypes.bfloat16
    return {
        "rope": tab.reshape(128, NTAB * 16),
        "prevbias": np.full((128, 1), 0.0 if hf == 1 else NEG, np.float32),
        "cmask": cm.reshape(128, 4 * 512).astype(bf),
        "maskA": mA.astype(bf),
        "negI": (NEG * np.eye(128, dtype=np.float32)).astype(bf),
        "identb": np.eye(128, dtype=np.float32).astype(bf),
        "identf": np.eye(128, dtype=np.float32),
    }


_NC_CACHE = {}


def kernel(x, w_in, b_gate, lam_q1, lam_k1, lam_q2, lam_k2, subln_g, w_oa, w_ob, w_o,
           ln1_g, ln1_b, w_router, b_router, w_gu, b_gu, w_down, b_down, ln2_g, ln2_b, _debug=False):
    f32 = np.float32
    x = np.asarray(x, f32)
    key = bool(_debug)
    if key not in _NC_CACHE:
        _NC_CACHE[key] = build_nc(debug=_debug)
    nc = _NC_CACHE[key]
    c = lambda a: np.ascontiguousarray(np.asarray(a, f32))
    shared = {
        "w_in": c(w_in[0]), "w_oa": c(w_oa[0]), "w_ob": c(w_ob[0]), "w_o": c(w_o[0]),
        "w_router": c(w_router[0]), "w_gu": c(w_gu[0]), "w_down": c(w_down[0]),
        "lamv": c(np.broadcast_to(np.stack([lam_q1[0], lam_k1[0], lam_q2[0], lam_k2[0]])[None], (128, 4, 64)).reshape(128, 256)),
        "subg": c(np.asarray(subln_g[0]).reshape(128, 1)),
        "bgt": c(np.asarray(b_gate[0]).reshape(16, 128).T),
        "bgu_t": c(np.asarray(b_gu[0]).reshape(NE, 16, 128).transpose(2, 0, 1).reshape(128, NE * 16)),
        "brt": c(np.broadcast_to(np.asarray(b_router[0])[None], (128, NE))),
        "bdn": c(b_down[0]),
        "ebase": c(np.broadcast_to((np.arange(NE, dtype=np.float32) * CAP)[None], (128, NE))),
        "lnp": c(np.broadcast_to(np.stack([ln1_g[0], ln1_b[0], ln2_g[0], ln2_b[0]])[None], (128, 4, D)).reshape(128, 4 * D)),
    }
    consts = [make_consts(0), make_consts(1)]
    in_maps = []
    for core in range(8):
        b, hf = core // 2, core % 2
        m = dict(shared)
        m.update(consts[hf])
        m["x_own"] = c(x[b, hf * 2048:(hf + 1) * 2048])
        m["x_prev"] = c(x[b, 0:2048])
        in_maps.append(m)
    res = run_bass_kernel_spmd(nc, in_maps, core_ids=list(range(8)))
    out = np.empty((4, 4096, D), f32)
    for core in range(8):
        b, hf = core // 2, core % 2
        out[b, hf * 2048:(hf + 1) * 2048] = res.results[core]["out"]
    if _debug:
        return out, res.results
    return out
```

```python
import contextlib
import numpy as np
import ml_dtypes
import concourse.bass as bass
import concourse.mybir as mybir
from concourse.bass_utils import run_bass_kernel_spmd

F32 = mybir.dt.float32
BF16 = mybir.dt.bfloat16
ALU = mybir.AluOpType
AF = mybir.ActivationFunctionType
AX = mybir.AxisListType

S_OWN = 2048
D = 1024
NE = 32
ALPHA = 2.0 ** 0.25
EPS = 1e-5
LAMBDA_INIT = 0.2
NEG = -30000.0
DILS = (1, 4, 16)
SAME_ENGINE_WAITS = True
CAP = 384
I32 = mybir.dt.int32


def tab_index():
    idx = {}
    n = 0
    for b in range(32):
        idx[("n", 0, b)] = n; n += 1
    for g, d in enumerate(DILS):
        for ob in range(16):
            idx[("o", g, ob)] = n; n += 1
        for pb in range(d):
            idx[("p", g, pb)] = n; n += 1
    return idx, n


class Eng:
    def __init__(self, name, sem):
        self.name, self.sem, self.n, self.ops, self.seen = name, sem, 0, [], {}

    def wait(self, *evs):
        for ev in evs:
            if ev is None:
                continue
            if isinstance(ev, list):
                self.wait(*ev)
                continue
            sem, val, key = ev
            if key == self.name and not SAME_ENGINE_WAITS:
                continue
            if self.seen.get(key, 0) >= val:
                continue
            self.seen[key] = val
            self.ops.append(lambda h, sem=sem, val=val: h.wait_ge(sem, val))

    def op(self, fn, sig=True):
        if sig:
            self.n += 1
            self.ops.append(lambda h, fn=fn, sem=self.sem: fn(h).then_inc(sem, 1))
            return (self.sem, self.n, self.name)
        self.ops.append(lambda h, fn=fn: fn(h))
        return None

    def dma(self, fn, ds):
        ds.n += 16
        self.ops.append(lambda h, fn=fn, sem=ds.sem: fn(h).then_inc(sem, 16))
        return (ds.sem, ds.n, ds.name)


class DSem:
    def __init__(self, name, sem):
        self.name, self.sem, self.n = name, sem, 0

    def ev(self):
        return (self.sem, self.n, self.name)


def build_nc(debug=False):
    nc = bass.Bass("TRN2", target_bir_lowering=False)
    TIDX, NTAB = tab_index()

    def din(name, shape, dt=F32):
        return nc.dram_tensor(name, list(shape), dt, kind="ExternalInput").ap()

    x_own = din("x_own", [S_OWN, D])
    x_prev = din("x_prev", [S_OWN, D])
    w_in = din("w_in", [D, 8192])
    w_oa = din("w_oa", [512, D])
    w_ob = din("w_ob", [512, D])
    w_o = din("w_o", [D, D])
    w_router = din("w_router", [D, NE])
    w_gu = din("w_gu", [NE, D, 2048])
    w_down = din("w_down", [NE, D, D])
    rope = din("rope", [128, NTAB * 16])
    prevbias_d = din("prevbias", [128, 1])
    cmask_d = din("cmask", [128, 4 * 512], BF16)
    maskA_d = din("maskA", [128, 128], BF16)
    negI_d = din("negI", [128, 128], BF16)
    identb_d = din("identb", [128, 128], BF16)
    identf_d = din("identf", [128, 128])
    lamv_d = din("lamv", [128, 4 * 64])
    subg_d = din("subg", [128, 1])
    bgt_d = din("bgt", [128, 16])
    bgu_d = din("bgu_t", [128, NE * 16])
    brt_d = din("brt", [128, NE])
    bdn_d = din("bdn", [NE, D])
    lnp_d = din("lnp", [128, 4 * D])
    ebase_d = din("ebase", [128, NE])
    Xg = nc.dram_tensor("Xg", [NE * CAP, D], BF16, kind="Internal").ap()
    Yg = nc.dram_tensor("Yg", [NE * CAP, D], F32, kind="Internal").ap()
    out_d = nc.dram_tensor("out", [S_OWN, D], F32, kind="ExternalOutput").ap()
    h1d = nc.dram_tensor("h1d", [S_OWN, D], F32, kind="ExternalOutput" if debug else "Internal").ap()
    if debug:
        dbg_ya = nc.dram_tensor("dbg_ya", [128, 4 * 2048], BF16, kind="ExternalOutput").ap()
    ybd = nc.dram_tensor("ybd", [64, 8 * 2048], BF16, kind="ExternalOutput" if debug else "Internal").ap()

    es = contextlib.ExitStack()
    with es:
        def sb(name, shape, dt):
            return es.enter_context(nc.sbuf_tensor("sb_" + name, list(shape), dt))

        def pst(name):
            return es.enter_context(nc.psum_tensor(name, [128, 512], F32))

        _semc = [0]

        def newsem(name):
            _semc[0] += 1
            return es.enter_context(nc.semaphore(name))

        PE = Eng("pe", newsem("s_pe"))
        ACT = Eng("act", newsem("s_act"))
        DVE = Eng("dve", newsem("s_dve"))
        POOL = Eng("pool", newsem("s_pool"))
        SP = Eng("sp", newsem("s_sp"))

        def dsem(name):
            return DSem(name, newsem(name))

        K = 1024
        ARENA = sb("arena", [128, 92 * K], BF16)

        def av(off, shape, dt, parts=128):
            esz = 4 if dt == F32 else 2
            n = 1
            for d_ in shape[1:]:
                n *= d_
            a = ARENA[0:parts, off // 2: off // 2 + n * esz // 2]
            if dt == F32:
                a = a.bitcast(F32)
            if len(shape) == 3:
                a = a.rearrange("p (a b) -> p a b", a=shape[1])
            elif len(shape) == 4:
                a = a.rearrange("p (a b c) -> p a b c", a=shape[1], b=shape[2])
            return a

        xTp = av(0, [128, 8, 2048], BF16)
        xTo = av(32 * K, [128, 8, 2048], BF16)
        ybH = av(64 * K, [64, 4, 2048], BF16, parts=64)
        acc = av(80 * K, [128, 4, 2048], F32)
        yaT = av(80 * K, [128, 4, 2048], BF16)
        ft = [av(96 * K + i * 2 * K, [128, 512], F32) for i in range(5)]
        xld = [av(100 * K + i * 2 * K, [128, 1024], BF16) for i in range(2)]
        QT = av(112 * K, [128, 2, 2048], BF16)
        KT = av(120 * K, [128, 2, 4096], BF16)
        VVf = av(136 * K, [128, 32 * 260], BF16)
        Wr = [av(153 * K + i * 4 * K, [128, 8, 256], BF16) for i in range(3)]
        Tt = [av(165 * K + i * 512, [128, 256], BF16) for i in range(2)]
        rtmp = [av(166 * K + i * 512, [128, 4, 32], F32) for i in range(2)]
        ropet = av(167 * K, [128, NTAB, 16], F32)
        cmask = av(167 * K + 6656, [128, 4, 512], BF16)
        PT = [av(167 * K + 6656 + 4 * K + i * K, [128, 512], BF16) for i in range(4)]
        PT2 = [av(167 * K + 6656 + 4 * K + i * 2 * K, [128, 1024], BF16) for i in range(2)]
        lamv = av(167 * K + 6656 + 8 * K, [128, 4, 64], F32)
        maskA = av(167 * K + 6656 + 9 * K, [128, 128], BF16)
        negI = av(167 * K + 6656 + 9 * K + 256, [128, 128], BF16)
        ybw = av(64 * K, [64, 8, 512], BF16, parts=64)
        mT = av(72 * K, [128, 8, 512], BF16)
        h1Tf = av(106 * K, [128, 8, 128], F32)
        wrt = av(110 * K, [128, 8, NE], F32)
        woa = av(112 * K, [128, 4, D], BF16)
        wob = av(120 * K, [64, 8, D], BF16, parts=64)
        wo = av(136 * K, [128, 8, D], BF16)
        lnp1 = av(165 * K, [128, 2, D], F32)
        xf3 = av(173 * K, [128, D], F32)
        xf5 = [av(128 * K + i * 4 * K, [128, D], F32) for i in range(2)]
        lnp2 = av(136 * K, [128, 2, D], F32)

        identb = sb("identb", [128, 128], BF16)
        identf = sb("identf", [128, 128], F32)
        ones_bf = sb("ones_bf", [128, 128], BF16)
        onesF = sb("onesF", [128, 128], F32)
        prevbias = sb("prevbias", [128, 1], F32)
        lamt = sb("lamt", [128, 2, 64], F32)
        lams = sb("lams", [128, 4], F32)
        subg = sb("subg", [128, 1], F32)
        epsc = sb("epsc", [128, 1], F32)
        bgt = sb("bgt", [128, 16], F32)
        bgu = sb("bgu", [128, NE * 16], F32)
        brt = sb("brt", [128, NE], F32)
        bdn = sb("bdn", [NE, D], F32)
        gwd = sb("gwd", [128, 16, NE], F32)
        rsm = sb("rsm", [128, 16], F32)
        ftA = sb("ftA", [128, 512], F32)
        stt = sb("stt", [128, 2, 6], F32)
        mv = sb("mv", [128, 2], F32)
        sm = sb("sm", [128, 8], F32)
        lg = sb("lg", [128, NE], F32)
        mx8 = sb("mx8", [128, 8], F32)
        el = sb("el", [128, NE], F32)
        gT = sb("gT", [NE, 128], F32)
        ebase = sb("ebase", [128, NE], F32)
        m01 = sb("m01", [128, 16, NE], BF16)
        slotv = sb("slotv", [128, NE], F32)
        rtm = sb("rtm", [128, NE], F32)
        destf = sb("destf", [128, 4], F32)
        desti = sb("desti", [128, 16, 4], I32)
        gw4 = sb("gw4", [128, 16, 4], F32)

        pairA = es.enter_context(nc.psum_tensor("psA", [128, 1024], F32))
        pairB = es.enter_context(nc.psum_tensor("psB", [128, 1024], F32))
        psS0, psS1 = pairA[:, 0:512], pairA[:, 512:1024]
        psP, psT = pairB[:, 0:512], pairB[:, 512:1024]
        psO0, psO1, psD0, psD1 = [pst(f"ps{i}") for i in range(4)]
        psTb = psT[:, :].bitcast(BF16)

        d_const = dsem("d_const")
        for (dst, src) in [
            (ropet[:, :, :], rope.rearrange("p (n c) -> p n c", c=16)),
            (prevbias[:, :], prevbias_d), (cmask[:, :, :], cmask_d.rearrange("p (v q) -> p v q", v=4)),
            (maskA[:, :], maskA_d), (negI[:, :], negI_d), (identb[:, :], identb_d), (identf[:, :], identf_d),
            (lamv[:, :, :], lamv_d.rearrange("p (a b) -> p a b", a=4)), (subg[:, :], subg_d),
            (bgt[:, :], bgt_d), (bgu[:, :], bgu_d), (brt[:, :], brt_d), (bdn[:, :], bdn_d), (ebase[:, :], ebase_d),
        ]:
            SP.dma(lambda h, dst=dst, src=src: h.dma_start(out=dst, in_=src), d_const)
        CONST = d_const.ev()
        e1 = POOL.op(lambda h: h.memset(ones_bf[:, :], 1.0))
        e2 = POOL.op(lambda h: h.memset(onesF[:, :], 1.0))
        e3 = POOL.op(lambda h: h.memset(epsc[:, :], EPS))
        e4 = POOL.op(lambda h: h.memset(VVf[:, :], 1.0))
        MEMS = [e1, e2, e3, e4]
        zt = av(112 * K, [128, 8192], BF16)
        ez = POOL.op(lambda h: h.memset(zt[:, :], 0.0))
        d_z = dsem("d_z")
        SP.wait(ez)
        xg_flat = Xg.rearrange("(p n) d -> p (n d)", p=128)
        for i_ in range(NE * CAP * D // 128 // 8192):
            SP.dma(lambda h, i_=i_: h.dma_start(out=xg_flat[:, i_ * 8192:(i_ + 1) * 8192], in_=zt[:, :]), d_z)
        ZFILL = d_z.ev()
        DVE.wait(CONST)
        ev = DVE.op(lambda h: h.tensor_tensor(out=lamt[:, :, :], in0=lamv[:, 0:4:2, :], in1=lamv[:, 1:4:2, :], op=ALU.mult))
        DVE.wait(ev)
        ev = DVE.op(lambda h: h.reduce_sum(out=lams[:, 0:2], in_=lamt[:, :, :], axis=AX.X))
        ACT.wait(ev)
        ev = ACT.op(lambda h: h.activation(out=lams[:, 2:4], in_=lams[:, 0:2], func=AF.Exp))
        DVE.wait(ev)
        ev = DVE.op(lambda h: h.tensor_tensor(out=lams[:, 0:1], in0=lams[:, 3:4], in1=lams[:, 2:3], op=ALU.subtract))
        DVE.wait(ev)
        ev = DVE.op(lambda h: h.tensor_scalar(out=lams[:, 0:1], in0=lams[:, 0:1], scalar1=-LAMBDA_INIT, scalar2=None, op0=ALU.add))
        DVE.wait(ev)
        ev = DVE.op(lambda h: h.tensor_scalar(out=lams[:, 1:2], in0=subg[:, :], scalar1=1.0 - LAMBDA_INIT, scalar2=None, op0=ALU.mult))
        LAMEV = ev
        neglam = lams[:, 0:1]
        gsc = lams[:, 1:2]

        d_x = [dsem("d_x0"), dsem("d_x1")]
        xfree = [None, None]
        psT_free = None
        PE.wait(CONST)
        for blk in range(32):
            s = blk % 2
            src = (x_prev if blk < 16 else x_own)[(blk % 16) * 128:(blk % 16 + 1) * 128, :]
            POOL.wait(xfree[s])
            ld = POOL.dma(lambda h, s=s, src=src: h.dma_start(out=xld[s][:, :], in_=src), d_x[s])
            PE.wait(ld, psT_free)
            for kc in range(8):
                ev = PE.op(lambda h, s=s, kc=kc: h.transpose(out=psTb[:, kc * 128:(kc + 1) * 128], in_=xld[s][:, kc * 128:(kc + 1) * 128], identity=identb[:, :]), sig=(kc == 7))
            xfree[s] = ev
            ACT.wait(ev)
            dst = (xTp if blk < 16 else xTo)[:, :, (blk % 16) * 128:(blk % 16 + 1) * 128]
            psT_free = ACT.op(lambda h, dst=dst: h.activation(out=dst, in_=psTb[:, :].rearrange("p (k t) -> p k t", k=8), func=AF.Copy))
        XT_DONE = psT_free

        d_w = [dsem(f"d_w{i}") for i in range(3)]
        wfree = [None, None, None]
        wcnt = [0]

        def load_w(col0):
            s = wcnt[0] % 3
            wcnt[0] += 1
            POOL.wait(wfree[s])
            ev = POOL.dma(lambda h, s=s, col0=col0: h.dma_start(out=Wr[s][:, :, :], in_=w_in[:, col0:col0 + 256].rearrange("(k p) c -> p k c", p=128)), d_w[s])
            return s, ev

        st = {"psP_free": None, "psT_free": XT_DONE, "tcnt": 0, "Tfree": [None, None], "rfree": [None, None], "pend": None,
              "pf": [None, None], "pcnt": 0}
        pbanks = [psP, psD1]

        def proj_mm(xap_fn, ws, wev):
            pb = st["pcnt"] % 2
            st["pcnt"] += 1
            Pb = pbanks[pb]
            PE.wait(wev, st["pf"][pb], st["psP_free"] if pb == 0 else None, XT_DONE)
            for kc in range(8):
                ev = PE.op(lambda h, kc=kc, Pb=Pb: h.matmul(Pb[:, 0:256], lhsT=xap_fn(kc), rhs=Wr[ws][:, kc, :], start=(kc == 0), stop=(kc == 7)), sig=(kc == 7))
            wfree[ws] = ev
            return ev, pb

        def flush_pend():
            if st["pend"] is None:
                return
            ti, tev, dst = st["pend"]
            st["pend"] = None
            PE.wait(tev, st["psT_free"])
            for hh_ in range(2):
                ev = PE.op(lambda h, hh_=hh_, ti=ti: h.transpose(out=psTb[:, hh_ * 128:(hh_ + 1) * 128], in_=Tt[ti][:, hh_ * 128:(hh_ + 1) * 128], identity=identb[:, :]), sig=(hh_ == 1))
            st["Tfree"][ti] = ev
            ACT.wait(ev)
            st["psT_free"] = ACT.op(lambda h, dst=dst: h.activation(out=dst, in_=psTb[:, 0:256].rearrange("p (a t) -> p a t", a=2), func=AF.Copy))
            st["last_qk"] = st["psT_free"]

        def qk_tile(xap_fn, ws, wev, tab, dst):
            mmev, pb = proj_mm(xap_fn, ws, wev)
            flush_pend()
            ti = st["tcnt"] % 2
            st["tcnt"] += 1
            p3 = pbanks[pb][:, 0:256].rearrange("p (a c) -> p a c", a=4)
            t3 = Tt[ti][:, :].rearrange("p (a c) -> p a c", a=4)
            cos = ropet[:, tab, 0:8].unsqueeze(1).broadcast_to([128, 4, 8])
            sin = ropet[:, tab, 8:16].unsqueeze(1).broadcast_to([128, 4, 8])
            rt = rtmp[ti]
            DVE.wait(mmev, st["Tfree"][ti], CONST)
            a = DVE.op(lambda h: h.tensor_tensor(out=rt[:, :, 0:8], in0=p3[:, :, 0:8], in1=cos, op=ALU.mult), sig=False)
            a = DVE.op(lambda h: h.tensor_tensor(out=rt[:, :, 8:16], in0=p3[:, :, 8:16], in1=cos, op=ALU.mult), sig=False)
            a = DVE.op(lambda h: h.tensor_tensor(out=rt[:, :, 16:24], in0=p3[:, :, 8:16], in1=sin, op=ALU.mult), sig=False)
            a = DVE.op(lambda h: h.tensor_tensor(out=rt[:, :, 24:32], in0=p3[:, :, 0:8], in1=sin, op=ALU.mult), sig=False)
            a = DVE.op(lambda h: h.tensor_copy(out=t3[:, :, 16:64], in_=p3[:, :, 16:64]))
            st["pf"][pb] = a
            if pb == 0:
                st["psP_free"] = a
            DVE.wait(a)
            a = DVE.op(lambda h: h.tensor_tensor(out=t3[:, :, 0:8], in0=rt[:, :, 0:8], in1=rt[:, :, 16:24], op=ALU.subtract), sig=False)
            a = DVE.op(lambda h: h.tensor_tensor(out=t3[:, :, 8:16], in0=rt[:, :, 8:16], in1=rt[:, :, 24:32], op=ALU.add))
            st["pend"] = (ti, a, dst)

        def v_tile(xap_fn, ws, wev, dst, srcf):
            mmev, pb = proj_mm(xap_fn, ws, wev)
            ACT.wait(mmev, MEMS)
            a = ACT.op(lambda h: h.activation(out=dst, in_=srcf(pbanks[pb]), func=AF.Copy))
            st["pf"][pb] = a
            if pb == 0:
                st["psP_free"] = a
            st["last_v"] = a

        def xnat(blk):
            t = xTp if blk < 16 else xTo
            b = blk % 16
            return lambda kc: t[:, kc, b * 128:(b + 1) * 128]

        Vaug = VVf[:, :].rearrange("p (b h c) -> p b h c", b=32, h=4)
        bufc = [0]
        d_yb = dsem("d_yb")
        sfree = [None, None]
        ptfree = [None] * 4
        ofree = [None, None]
        ptc = [0]
        psSb = [psS0, psS1]
        psOb = [psO0, psO1]
        acc_last = [None]

        for hh in range(2):
            for g, d in enumerate(DILS):
                nb = 16 // d
                base = 1536 + g * 1536 + hh * 256
                stage_guard = acc_last[0]
                ACT.wait(stage_guard, ZFILL)
                sq, evq = load_w(base)
                sk, evk = load_w(base + 512)
                sv, evv = load_w(base + 1024)

                def xown(ob, d=d, nb=nb):
                    r, n = ob // nb, ob % nb
                    return lambda kc: xTo[:, kc, :].rearrange("p (l d) -> p d l", d=d)[:, r, n * 128:(n + 1) * 128]

                def xprev(pb, d=d):
                    return lambda kc: xTp[:, kc, 2048 - 128 * d:2048].rearrange("p (l d) -> p d l", d=d)[:, pb, :]

                for ob in range(16):
                    qk_tile(xown(ob), sq, evq, TIDX[("o", g, ob)], QT[:, :, ob * 128:(ob + 1) * 128])
                for ob in range(16):
                    qk_tile(xown(ob), sk, evk, TIDX[("o", g, ob)], KT[:, :, 2048 + ob * 128:2048 + (ob + 1) * 128])
                for pb in range(d):
                    qk_tile(xprev(pb), sk, evk, TIDX[("p", g, pb)], KT[:, :, pb * 128:(pb + 1) * 128])
                flush_pend()
                for ob in range(16):
                    v_tile(xown(ob), sv, evv, Vaug[:, 16 + ob, :, 0:64], lambda P_: P_[:, 0:256].rearrange("p (a c) -> p a c", a=4))
                for pb in range(d):
                    v_tile(xprev(pb), sv, evv, Vaug[:, pb, :, 0:64], lambda P_: P_[:, 0:256].rearrange("p (a c) -> p a c", a=4))
                PROJ_DONE = [st["last_qk"], st["last_v"]]
                PE.wait(PROJ_DONE)
                work = [(r, hl, n) for r in range(d) for hl in range(4) for n in range(nb)]

                def dil_S(item, d=d, nb=nb):
                    r, hl, n = item
                    p, s_ = hl // 2, hl % 2
                    lo, hi = s_ * 64, s_ * 64 + 64
                    ob = r * nb + n
                    qpos = ob * 128
                    if n == 0:
                        kposA, blkA = r * 128, r
                    else:
                        kposA, blkA = 2048 + (ob - 1) * 128, 16 + ob - 1
                    kposB, blkB = 2048 + ob * 128, 16 + ob
                    bi = bufc[0] % 2
                    bufc[0] += 1
                    S = psSb[bi]
                    PE.wait(sfree[bi])
                    qap = QT[lo:hi, p, qpos:qpos + 128]
                    PE.op(lambda h: h.matmul(S[:, 0:128], lhsT=KT[lo:hi, p, kposA:kposA + 128], rhs=qap, start=True, stop=False), sig=False)
                    PE.op(lambda h: h.matmul(S[:, 0:128], lhsT=negI[:, :], rhs=maskA[:, :], start=False, stop=True), sig=False)
                    PE.op(lambda h: h.matmul(S[:, 128:256], lhsT=KT[lo:hi, p, kposB:kposB + 128], rhs=qap, start=True, stop=False), sig=False)
                    sev = PE.op(lambda h: h.matmul(S[:, 128:256], lhsT=negI[:, :], rhs=cmask[:, 0, 0:128], start=False, stop=True))
                    pi = ptc[0] % 4
                    ptc[0] += 1
                    ACT.wait(sev, ptfree[pi])
                    if n == 0:
                        ACT.op(lambda h: h.activation(out=PT[pi][:, 0:128], in_=S[:, 0:128], func=AF.Exp, bias=prevbias[:, 0:1], scale=0.125), sig=False)
                        aev = ACT.op(lambda h: h.activation(out=PT[pi][:, 128:256], in_=S[:, 128:256], func=AF.Exp, scale=0.125))
                    else:
                        aev = ACT.op(lambda h: h.activation(out=PT[pi][:, 0:256], in_=S[:, 0:256], func=AF.Exp, scale=0.125))
                    sfree[bi] = aev
                    return (aev, pi, bi, blkA, blkB)

                def dil_AV(item, pend_, g=g, d=d):
                    r, hl, n = item
                    aev, pi, bi, blkA, blkB = pend_
                    O = psOb[bi]
                    PE.wait(aev, ofree[bi])
                    PE.op(lambda h: h.matmul(O[0:65, 0:128], lhsT=Vaug[:, blkA, hl, :], rhs=PT[pi][:, 0:128], start=True, stop=False), sig=False)
                    oev = PE.op(lambda h: h.matmul(O[0:65, 0:128], lhsT=Vaug[:, blkB, hl, :], rhs=PT[pi][:, 128:256], start=False, stop=True))
                    ptfree[pi] = oev
                    dst = acc[0:65, hl, :].rearrange("p (l d) -> p d l", d=d)[:, r, n * 128:(n + 1) * 128]
                    DVE.wait(oev, acc_last[0] if g > 0 else None)
                    if g == 0:
                        dev = DVE.op(lambda h: h.tensor_copy(out=dst, in_=O[0:65, 0:128]))
                    else:
                        dev = DVE.op(lambda h: h.tensor_tensor(out=dst, in0=dst, in1=O[0:65, 0:128], op=ALU.add))
                    ofree[bi] = dev
                    acc_last[0] = dev

                pend_ = dil_S(work[0])
                for wi, item in enumerate(work):
                    nxt = dil_S(work[wi + 1]) if wi + 1 < len(work) else None
                    dil_AV(item, pend_)
                    pend_ = nxt
            DVE.wait(st.get("ybstore"))
            for hl in range(4):
                for w in range(4):
                    PE.wait(acc_last[0], MEMS, st.get("dfree"))
                    bev = PE.op(lambda h, hl=hl, w=w: h.matmul(psD0[0:64, :], lhsT=onesF[64:65, 0:64], rhs=acc[64:65, hl, w * 512:(w + 1) * 512], start=True, stop=True))
                    ACT.wait(bev, acc_last[0])
                    rev = ACT.op(lambda h: h.activation(out=ftA[0:64, :], in_=psD0[0:64, :], func=AF.Ln))
                    st["dfree"] = rev
                    ACT.wait(rev)
                    rev = ACT.op(lambda h: h.activation(out=ftA[0:64, :], in_=ftA[0:64, :], func=AF.Exp, scale=-1.0))
                    DVE.wait(rev)
                    fev = DVE.op(lambda h, hl=hl, w=w: h.tensor_tensor(out=ybH[0:64, hl, w * 512:(w + 1) * 512], in0=acc[0:64, hl, w * 512:(w + 1) * 512], in1=ftA[0:64, :], op=ALU.mult))
                    acc_last[0] = fev
            SP.wait(acc_last[0])
            st["ybstore"] = SP.dma(lambda h, hh=hh: h.dma_start(out=ybd[:, hh * 8192:(hh + 1) * 8192], in_=ybH[:, :, :].rearrange("p h t -> p (h t)")), d_yb)
        YB_DONE = [acc_last[0], st["ybstore"]]

        Vd = VVf[:, 0:32 * 256].rearrange("p (b c) -> p b c", b=32)
        psOd = [psO0, psO1]
        psDd = [psD0, psD1]
        fin_free = YB_DONE
        st["sbf"] = [[sfree[0], sfree[1]], None]
        st["acc_prev"] = [None, None]
        att_last = YB_DONE
        ya_last = None
        for pp in range(2):
            ACT.wait(att_last)
            DVE.wait(att_last)
            sq, evq = load_w(pp * 256)
            sk, evk = load_w(512 + pp * 256)
            sv, evv = load_w(1024 + pp * 256)
            for ob in range(16):
                qk_tile(xnat(16 + ob), sq, evq, TIDX[("n", 0, 16 + ob)], QT[:, :, ob * 128:(ob + 1) * 128])
            for blk in range(32):
                qk_tile(xnat(blk), sk, evk, TIDX[("n", 0, blk)], KT[:, :, blk * 128:(blk + 1) * 128])
            flush_pend()
            for blk in range(32):
                v_tile(xnat(blk), sv, evv, Vd[:, blk, :], lambda P_: P_[:, 0:256])
            PE.wait(st["last_qk"], st["last_v"])
            for hl in range(2):
                for j in range(4):
                    nkb = 16 + 4 * (j + 1)
                    PE.wait(fin_free, st["pf"][1], st["pf"][0], st["psP_free"], st["psT_free"])
                    SBK = [(psS0, psS1), (psP, psT)]
                    accD = [av(106 * K, [128, 512], F32), av(108 * K, [128, 512], F32)]
                    accE = [DVE, DVE]

                    def dif_S(kb, hl=hl, j=j):
                        bp = bufc[0] % 2
                        bufc[0] += 1
                        diag = kb >= 16 + 4 * j
                        PE.wait(st["sbf"][bp])
                        for c in range(2):
                            lo, hi = c * 64, c * 64 + 64
                            S = SBK[bp][c]
                            sev = PE.op(lambda h, S=S, lo=lo, hi=hi: h.matmul(S[:, :], lhsT=KT[lo:hi, hl, kb * 128:(kb + 1) * 128], rhs=QT[lo:hi, hl, j * 512:(j + 1) * 512], start=True, stop=not diag), sig=(c == 1 and not diag))
                            if diag:
                                v = kb - 16 - 4 * j
                                sev = PE.op(lambda h, S=S, v=v: h.matmul(S[:, :], lhsT=negI[:, :], rhs=cmask[:, v, :], start=False, stop=True), sig=(c == 1))
                        pis = [(2 * bp) % 4, (2 * bp + 1) % 4]
                        ACT.wait(sev, ptfree[pis[0]], ptfree[pis[1]])
                        SS = [pairA, pairB][bp]
                        if kb < 16:
                            aev = ACT.op(lambda h: h.activation(out=PT2[bp][:, :], in_=SS[:, :], func=AF.Exp, bias=prevbias[:, 0:1], scale=0.125))
                        else:
                            aev = ACT.op(lambda h: h.activation(out=PT2[bp][:, :], in_=SS[:, :], func=AF.Exp, scale=0.125))
                        st["sbf"][bp] = aev
                        if bp == 1:
                            st["psP_free"] = aev
                            st["psT_free"] = aev
                            st["pf"][0] = aev
                        else:
                            sfree[0] = aev
                            sfree[1] = aev
                        return (aev, pis)

                    def dif_AV(kb, pend_, hl=hl, nkb=nkb):
                        aev, pis = pend_
                        even = (kb % 2 == 0)
                        PE.wait(aev)
                        PE.op(lambda h: h.matmul(psOd[0][:, :], lhsT=Vd[:, kb, hl * 128:(hl + 1) * 128], rhs=PT[pis[0]][:, :], start=(kb == 0), stop=(kb == nkb - 1)), sig=False)
                        oev_ = PE.op(lambda h: h.matmul(psOd[1][:, :], lhsT=Vd[:, kb, hl * 128:(hl + 1) * 128], rhs=PT[pis[1]][:, :], start=(kb == 0), stop=(kb == nkb - 1)), sig=not even)
                        if even:
                            PE.op(lambda h: h.matmul(psDd[0][:, :], lhsT=ones_bf[:, :], rhs=PT[pis[0]][:, :], start=(kb == 0), stop=False), sig=False)
                            oev_ = PE.op(lambda h: h.matmul(psDd[1][:, :], lhsT=ones_bf[:, :], rhs=PT[pis[1]][:, :], start=(kb == 0), stop=False))
                            ptfree[pis[0]] = oev_
                            ptfree[pis[1]] = oev_
                            return oev_
                        devs = []
                        for c in range(2):
                            E_ = accE[c]
                            E_.wait(aev, st["acc_prev"][c])
                            if kb == 1:
                                dv = E_.op(lambda h, c=c: h.tensor_copy(out=accD[c][:, :], in_=PT[pis[c]][:, :]))
                            else:
                                dv = E_.op(lambda h, c=c: h.tensor_tensor(out=accD[c][:, :], in0=accD[c][:, :], in1=PT[pis[c]][:, :], op=ALU.add))
                            st["acc_prev"][c] = dv
                            devs.append(dv)
                        ptfree[pis[0]] = [oev_, devs[0]]
                        ptfree[pis[1]] = [oev_, devs[1]]
                        return oev_

                    pend_ = dif_S(0)
                    for kb in range(nkb):
                        nxt = dif_S(kb + 1) if kb + 1 < nkb else None
                        oev = dif_AV(kb, pend_)
                        pend_ = nxt
                    PE.wait(st["acc_prev"][0], st["acc_prev"][1], MEMS)
                    PE.op(lambda h: h.matmul(psD0[:, :], lhsT=onesF[:, :], rhs=accD[0][:, :], start=False, stop=True), sig=False)
                    oev = PE.op(lambda h: h.matmul(psD1[:, :], lhsT=onesF[:, :], rhs=accD[1][:, :], start=False, stop=True))
                    st["acc_prev"] = [oev, oev]
                    DVE.wait(oev, LAMEV, ya_last)
                    ACT.wait(oev, ya_last)
                    ACT.op(lambda h: h.activation(out=ft[0][:, :], in_=psD0[:, :], func=AF.Ln), sig=False)
                    a = ACT.op(lambda h: h.activation(out=ft[1][:, :], in_=psD1[:, :], func=AF.Ln))
                    ACT.wait(a)
                    ACT.op(lambda h: h.activation(out=ft[0][:, :], in_=ft[0][:, :], func=AF.Exp, scale=-1.0), sig=False)
                    a = ACT.op(lambda h: h.activation(out=ft[1][:, :], in_=ft[1][:, :], func=AF.Exp, scale=-1.0))
                    DVE.wait(a)
                    a = DVE.op(lambda h: h.tensor_tensor(out=ft[0][:, :], in0=psO0[:, :], in1=ft[0][:, :], op=ALU.mult), sig=False)
                    a = DVE.op(lambda h: h.tensor_tensor(out=ft[1][:, :], in0=psO1[:, :], in1=ft[1][:, :], op=ALU.mult))
                    fin_free = a
                    st["pf"][1] = a
                    DVE.wait(a)
                    a = DVE.op(lambda h: h.scalar_tensor_tensor(out=ft[2][:, :], in0=ft[1][:, :], scalar=neglam, in1=ft[0][:, :], op0=ALU.mult, op1=ALU.add))
                    DVE.wait(a)
                    a = DVE.op(lambda h: h.tensor_tensor(out=ft[3][:, :], in0=ft[2][:, :], in1=ft[2][:, :], op=ALU.mult))
                    PE.wait(a, st["sbf"][0])
                    m = PE.op(lambda h: h.matmul(psS0[:, :], lhsT=onesF[:, :], rhs=ft[3][:, :], start=True, stop=True))
                    ACT.wait(m)
                    a = ACT.op(lambda h: h.activation(out=ft[4][:, :], in_=psS0[:, :], func=AF.Ln, bias=epsc[:, 0:1], scale=1.0 / 128.0))
                    st["sbf"][0] = a
                    sfree[0] = a
                    ACT.wait(a)
                    a = ACT.op(lambda h: h.activation(out=ft[4][:, :], in_=ft[4][:, :], func=AF.Exp, scale=-0.5))
                    DVE.wait(a)
                    a = DVE.op(lambda h, pp=pp, hl=hl, j=j: h.scalar_tensor_tensor(out=yaT[:, 2 * pp + hl, j * 512:(j + 1) * 512], in0=ft[2][:, :], scalar=gsc, in1=ft[4][:, :], op0=ALU.mult, op1=ALU.mult))
                    ya_last = a
                    att_last = a
        YA_DONE = ya_last

        d_dbg = dsem("d_dbg")
        if debug:
            SP.wait(YA_DONE, YB_DONE)
            SP.dma(lambda h: h.dma_start(out=dbg_ya, in_=yaT[:, :, :].rearrange("p h t -> p (h t)")), d_dbg)

        d_pw = dsem("d_pw")
        POOL.wait(YA_DONE, YB_DONE)
        SP.wait(YA_DONE, YB_DONE)
        if debug:
            POOL.wait(d_dbg.ev())
            SP.wait(d_dbg.ev())
        POOL.dma(lambda h: h.dma_start(out=woa[:, :, :], in_=w_oa.rearrange("(k p) c -> p k c", p=128)), d_pw)
        POOL.dma(lambda h: h.dma_start(out=wob[:, :, :], in_=w_ob.rearrange("(k p) c -> p k c", p=64)), d_pw)
        POOL.dma(lambda h: h.dma_start(out=wo[:, :, :], in_=w_o.rearrange("(k p) c -> p k c", p=128)), d_pw)
        SP.dma(lambda h: h.dma_start(out=lnp1[:, :, :], in_=lnp_d[:, 0:2 * D].rearrange("p (a c) -> p a c", a=2)), d_pw)
        SP.dma(lambda h: h.dma_start(out=wrt[:, :, :], in_=w_router.rearrange("(k p) c -> p k c", p=128)), d_pw)
        PW = d_pw.ev()

        h1T = xTp
        d_xf = dsem("d_xf")
        d_h1 = dsem("d_h1")
        d_ybw = dsem("d_ybw")
        d_sc = dsem("d_sc")
        h1b = av(177 * K, [128, D], BF16)
        xf_free = None
        ybw_free = None
        g_last = {"ga": None, "gb": None, "mT": None, "h1Tf": None, "misc": None}

        def layer_norm(buf, lnv, pre_wait):
            DVE.wait(pre_wait)
            DVE.op(lambda h: h.bn_stats(out=stt[:, 0, :], in_=buf[:, 0:512]), sig=False)
            a = DVE.op(lambda h: h.bn_stats(out=stt[:, 1, :], in_=buf[:, 512:1024]))
            DVE.wait(a)
            a = DVE.op(lambda h: h.bn_aggr(out=mv[:, :], in_=stt[:, :, :].rearrange("p a b -> p (a b)")))
            ACT.wait(a)
            a = ACT.op(lambda h: h.activation(out=sm[:, 0:1], in_=mv[:, 1:2], func=AF.Ln, bias=epsc[:, 0:1], scale=1.0))
            ACT.wait(a)
            a = ACT.op(lambda h: h.activation(out=sm[:, 1:2], in_=sm[:, 0:1], func=AF.Exp, scale=-0.5))
            DVE.wait(a)
            a = DVE.op(lambda h: h.tensor_scalar(out=buf[:, :], in0=buf[:, :], scalar1=mv[:, 0:1], scalar2=sm[:, 1:2], op0=ALU.subtract, op1=ALU.mult))
            DVE.wait(a)
            a = DVE.op(lambda h: h.tensor_tensor(out=buf[:, :], in0=buf[:, :], in1=lnv[:, 0, :], op=ALU.mult))
            DVE.wait(a)
            a = DVE.op(lambda h: h.tensor_tensor(out=buf[:, :], in0=buf[:, :], in1=lnv[:, 1, :], op=ALU.add))
            return a

        xf3r = [xf3, av(0, [128, D], F32)]
        h1Tfr = [h1Tf, av(4 * K, [128, 8, 128], F32)]
        d_xfr = [dsem("d_xfr0"), dsem("d_xfr1")]
        d_h1r = [dsem("d_h1r0"), dsem("d_h1r1")]
        xf_fr = [None, None]
        h1Tf_fr = [None, None]
        tr_ev = {}
        stv_ev = {}
        pendB = [None]

        def stageA(tb, tbl, mt_ready):
            s_ = tb % 2
            xf = xf3r[s_]
            hT = h1Tfr[s_]
            SP.wait(xf_fr[s_])
            xev = SP.dma(lambda h: h.dma_start(out=xf[:, :], in_=x_own[tb * 128:(tb + 1) * 128, :]), d_xfr[s_])
            PE.wait(mt_ready, st.get("dfree2"))
            for dh in range(2):
                for fc in range(8):
                    e = PE.op(lambda h, dh=dh, fc=fc: h.matmul(psDd[dh][:, :], lhsT=mT[:, fc, tbl * 128:(tbl + 1) * 128], rhs=wo[:, fc, dh * 512:(dh + 1) * 512], start=(fc == 0), stop=(fc == 7)), sig=(fc == 7 and dh == 1))
            mm_o = e
            if tbl == 3:
                g_last["mT"] = mm_o
            DVE.wait(mm_o, xev)
            DVE.op(lambda h: h.scalar_tensor_tensor(out=xf[:, 0:512], in0=xf[:, 0:512], scalar=ALPHA, in1=psD0[:, :], op0=ALU.mult, op1=ALU.add), sig=False)
            a = DVE.op(lambda h: h.scalar_tensor_tensor(out=xf[:, 512:1024], in0=xf[:, 512:1024], scalar=ALPHA, in1=psD1[:, :], op0=ALU.mult, op1=ALU.add))
            st["dfree2"] = a
            h1ev = layer_norm(xf, lnp1, a)
            SP.wait(h1ev)
            stv_ev[tb] = SP.dma(lambda h: h.dma_start(out=h1d[tb * 128:(tb + 1) * 128, :], in_=xf[:, :]), d_h1r[s_])
            for half in range(2):
                PE.wait(h1ev, st["psP_free"])
                for q_ in range(4):
                    kc = half * 4 + q_
                    e = PE.op(lambda h, kc=kc, q_=q_: h.transpose(out=psP[:, q_ * 128:(q_ + 1) * 128], in_=xf[:, kc * 128:(kc + 1) * 128], identity=identf[:, :]), sig=(q_ == 3))
                ACT.wait(e, h1Tf_fr[s_] if half == 0 else None)
                a = ACT.op(lambda h, half=half: h.activation(out=hT[:, half * 4:(half + 1) * 4, :], in_=psP[:, :].rearrange("p (k t) -> p k t", k=4), func=AF.Copy))
                st["psP_free"] = a
            tr_ev[tb] = (a, e)

        def stageB(tb):
            s_ = tb % 2
            xf = xf3r[s_]
            hT = h1Tfr[s_]
            a, e_tr = tr_ev[tb]
            PE.wait(a, sfree[0])
            for kc in range(8):
                e = PE.op(lambda h, kc=kc: h.matmul(psS0[:, 0:NE], lhsT=hT[:, kc, :], rhs=wrt[:, kc, :], start=(kc == 0), stop=(kc == 7)), sig=(kc == 7))
            h1Tf_fr[s_] = e
            DVE.wait(e)
            a = DVE.op(lambda h: h.tensor_tensor(out=lg[:, :], in0=psS0[:, 0:NE], in1=brt[:, :], op=ALU.add))
            sfree[0] = a
            DVE.wait(a)
            a = DVE.op(lambda h: h.max(out=mx8[:, :], in_=lg[:, :]))
            DVE.wait(a)
            a = DVE.op(lambda h: h.tensor_scalar(out=sm[:, 2:3], in0=mx8[:, 0:1], scalar1=-1.0, scalar2=None, op0=ALU.mult))
            ACT.wait(a)
            ACT.op(lambda h: h.activation(out=sm[:, 4:8], in_=mx8[:, 0:4], func=AF.Exp, bias=sm[:, 2:3], scale=1.0), sig=False)
            a = ACT.op(lambda h: h.activation(out=el[:, :], in_=lg[:, :], func=AF.Exp, bias=sm[:, 2:3], scale=1.0))
            DVE.wait(a)
            a = DVE.op(lambda h: h.reduce_sum(out=sm[:, 3:4], in_=sm[:, 4:8], axis=AX.X))
            DVE.wait(a)
            a = DVE.op(lambda h: h.reciprocal(out=rsm[:, tb:tb + 1], in_=sm[:, 3:4]))
            DVE.wait(a)
            a = DVE.op(lambda h: h.tensor_scalar(out=el[:, :], in0=el[:, :], scalar1=rsm[:, tb:tb + 1], scalar2=None, op0=ALU.mult))
            DVE.wait(a)
            a = DVE.op(lambda h: h.scalar_tensor_tensor(out=gwd[:, tb, :], in0=lg[:, :], scalar=mx8[:, 3:4], in1=el[:, :], op0=ALU.is_ge, op1=ALU.mult))
            a = DVE.op(lambda h: h.tensor_scalar(out=m01[:, tb, :], in0=lg[:, :], scalar1=mx8[:, 3:4], scalar2=None, op0=ALU.is_ge))
            PE.wait(a, sfree[1])
            for tb2 in range(tb + 1):
                e = PE.op(lambda h, tb2=tb2: h.matmul(psS1[:, 0:NE], lhsT=(maskA[:, :] if tb2 == tb else ones_bf[:, :]), rhs=m01[:, tb2, :], start=(tb2 == 0), stop=(tb2 == tb)), sig=(tb2 == tb))
            DVE.wait(e)
            a = DVE.op(lambda h: h.tensor_tensor(out=slotv[:, :], in0=psS1[:, 0:NE], in1=ebase[:, :], op=ALU.add))
            sfree[1] = a
            DVE.wait(a)
            for k_ in range(4):
                DVE.op(lambda h, k_=k_: h.scalar_tensor_tensor(out=rtm[:, :], in0=lg[:, :], scalar=mx8[:, k_:k_ + 1], in1=slotv[:, :], op0=ALU.is_equal, op1=ALU.mult))
                DVE.wait((DVE.sem, DVE.n, DVE.name))
                DVE.op(lambda h, k_=k_: h.reduce_sum(out=destf[:, k_:k_ + 1], in_=rtm[:, :], axis=AX.X))
                DVE.wait((DVE.sem, DVE.n, DVE.name))
            a = DVE.op(lambda h: h.tensor_copy(out=desti[:, tb, :], in_=destf[:, :]))
            DVE.op(lambda h: h.tensor_scalar(out=gw4[:, tb, :], in0=sm[:, 4:8], scalar1=rsm[:, tb:tb + 1], scalar2=None, op0=ALU.mult))
            DVE.wait(st.get("h1b_free"))
            a = DVE.op(lambda h: h.tensor_copy(out=h1b[:, :], in_=xf[:, :]))
            POOL.wait(a, ZFILL)
            for k_ in range(4):
                sc = POOL.dma(lambda h, k_=k_: h.indirect_dma_start(out=Xg[:, :], out_offset=bass.IndirectOffsetOnAxis(ap=desti[:, tb, k_:k_ + 1], axis=0), in_=h1b[:, :], in_offset=None), d_sc)
            st["h1b_free"] = sc
            xf_fr[s_] = [stv_ev[tb], e_tr, a]
            g_last["misc"] = a
            st["route_done"] = a

        PE.wait(PW, YA_DONE, YB_DONE)
        DVE.wait(PW)
        for w in range(4):
            tok = slice(w * 512, (w + 1) * 512)
            SP.wait(ybw_free)
            ybw_ev = SP.dma(lambda h, tok=tok: h.dma_start(out=ybw[:, :, :], in_=ybd.rearrange("p (h t) -> p h t", h=8)[:, :, tok]), d_ybw)
            for cp in range(4):
                sA, evA = load_w(6144 + cp * 256)
                sB, evB = load_w(7168 + cp * 256)
                for fcl in range(2):
                    fc = 2 * cp + fcl
                    PE.wait(evA, sfree[0])
                    for kc in range(8):
                        e = PE.op(lambda h, kc=kc, sA=sA, fcl=fcl, tok=tok: h.matmul(psS0[:, :], lhsT=Wr[sA][:, kc, fcl * 128:(fcl + 1) * 128], rhs=xTo[:, kc, tok], start=(kc == 0), stop=(kc == 7)), sig=(kc == 7))
                    if fcl == 1:
                        wfree[sA] = e
                    ACT.wait(e, g_last["ga"], CONST)
                    ga = ACT.op(lambda h, fc=fc: h.activation(out=ft[0][:, :], in_=psS0[:, :], func=AF.Sigmoid, bias=bgt[:, fc:fc + 1], scale=1.0))
                    sfree[0] = ga
                    PE.wait(evB, sfree[1])
                    for kc in range(8):
                        e = PE.op(lambda h, kc=kc, sB=sB, fcl=fcl, tok=tok: h.matmul(psS1[:, :], lhsT=Wr[sB][:, kc, fcl * 128:(fcl + 1) * 128], rhs=xTo[:, kc, tok], start=(kc == 0), stop=(kc == 7)), sig=(kc == 7))
                    if fcl == 1:
                        wfree[sB] = e
                    ACT.wait(e, g_last["gb"])
                    gb = ACT.op(lambda h, fc=fc: h.activation(out=ft[1][:, :], in_=psS1[:, :], func=AF.Sigmoid, bias=bgt[:, 8 + fc:9 + fc], scale=1.0))
                    sfree[1] = gb
                    PE.wait(ofree[0])
                    for kc in range(4):
                        e = PE.op(lambda h, kc=kc, fc=fc, tok=tok: h.matmul(psO0[:, :], lhsT=woa[:, kc, fc * 128:(fc + 1) * 128], rhs=yaT[:, kc, tok], start=(kc == 0), stop=(kc == 3)), sig=(kc == 3))
                    oa = e
                    PE.wait(ofree[1], ybw_ev)
                    for hh_ in range(8):
                        e = PE.op(lambda h, hh_=hh_, fc=fc: h.matmul(psO1[:, :], lhsT=wob[0:64, hh_, fc * 128:(fc + 1) * 128], rhs=ybw[0:64, hh_, :], start=(hh_ == 0), stop=(hh_ == 7)), sig=(hh_ == 7))
                    obv = e
                    if fc == 7:
                        ybw_free = e
                    DVE.wait(ga, oa, g_last["misc"])
                    a = DVE.op(lambda h: h.tensor_tensor(out=ft[2][:, :], in0=psO0[:, :], in1=ft[0][:, :], op=ALU.mult))
                    ofree[0] = a
                    g_last["ga"] = a
                    DVE.wait(gb, obv)
                    b = DVE.op(lambda h: h.tensor_tensor(out=ft[3][:, :], in0=psO1[:, :], in1=ft[1][:, :], op=ALU.mult))
                    ofree[1] = b
                    g_last["gb"] = b
                    DVE.wait(a, b, g_last["mT"] if fc == 0 else None)
                    c_ = DVE.op(lambda h, fc=fc: h.tensor_tensor(out=mT[:, fc, :], in0=ft[2][:, :], in1=ft[3][:, :], op=ALU.add))
                    g_last["misc"] = c_
            MT_READY = c_
            for tbl in range(4):
                tb = w * 4 + tbl
                stageA(tb, tbl, MT_READY)
                if pendB[0] is not None:
                    stageB(pendB[0])
                pendB[0] = tb
        stageB(pendB[0])
        ROUTE_DONE = st["route_done"]
        xf_free = [xf_fr[0], xf_fr[1]]
        P3_DONE = [ROUTE_DONE, st["psP_free"], xf_free]
        H1D_DONE = [d_h1r[0].ev(), d_h1r[1].ev()]

        Wd = [av(i * 16 * K, [128, 8, D], BF16) for i in range(2)]
        Xe = [av(32 * K + i * 6 * K, [128, 3, D], BF16) for i in range(2)]
        XT = [av(44 * K + i * 6 * K, [128, 8, CAP], BF16) for i in range(2)]
        actT = [av(56 * K + i * 6 * K, [128, 8, CAP], BF16) for i in range(2)]
        Yt = [av(68 * K + i * 4 * K, [128, D], F32) for i in range(2)]
        Wgu = [av(112 * K + i * 32 * K, [128, 8, 2048], BF16) for i in range(2)]
        tg = [av(100 * K + i * 6 * K, [128, CAP], F32) for i in range(2)]
        tsg = [av(102 * K + i * 6 * K, [128, CAP], F32) for i in range(2)]
        tu = [av(104 * K + i * 6 * K, [128, CAP], F32) for i in range(2)]
        d_gu = [dsem(f"d_gu{i}") for i in range(2)]
        d_dn = [dsem(f"d_dn{i}") for i in range(2)]
        d_xe = [dsem("d_xe0"), dsem("d_xe1")]
        d_yg = [dsem("d_yg0"), dsem("d_yg1")]
        gu_free = [None] * 2
        dn_free = [None] * 2
        xe_free = [None, None]
        yt_free = [None, None]
        items = [(e, q) for e in range(NE) for q in range(4)]
        gu_ev = {}
        dn_ev = {}
        xe_ev = {}
        xt_ready = {}

        def issue_gu(e):
            s = e % 2
            POOL.wait(gu_free[s])
            gu_ev[e] = POOL.dma(lambda h, s=s, e=e: h.dma_start(out=Wgu[s][:, :, :], in_=w_gu[e].rearrange("(k p) c -> p k c", p=128)), d_gu[s])

        def issue_dn(e):
            s = e % 2
            POOL.wait(dn_free[s])
            dn_ev[e] = POOL.dma(lambda h, s=s, e=e: h.dma_start(out=Wd[s][:, :, :], in_=w_down[e].rearrange("(k p) c -> p k c", p=128)), d_dn[s])

        def issue_xe(e):
            s = e % 2
            SP.wait(xe_free[s])
            xe_ev[e] = SP.dma(lambda h, s=s, e=e: h.dma_start(out=Xe[s][:, :, :], in_=Xg[e * CAP:(e + 1) * CAP, :].rearrange("(n p) d -> p n d", p=128)), d_xe[s])

        def do_transposes(e):
            s = e % 2
            for n in range(3):
                PE.wait(xe_ev[e], st["psT_free"])
                for kc in range(8):
                    ev_ = PE.op(lambda h, kc=kc, n=n, s=s: h.transpose(out=psTb[:, kc * 128:(kc + 1) * 128], in_=Xe[s][:, n, kc * 128:(kc + 1) * 128], identity=identb[:, :]), sig=(kc == 7))
                ACT.wait(ev_)
                st["psT_free"] = ACT.op(lambda h, n=n, s=s: h.activation(out=XT[s][:, :, n * 128:(n + 1) * 128], in_=psTb[:, :].rearrange("p (k t) -> p k t", k=8), func=AF.Copy))
            xe_free[s] = ev_
            xt_ready[e] = st["psT_free"]

        SCAT_DONE = d_sc.ev()
        POOL.wait(P3_DONE, H1D_DONE)
        PE.wait(P3_DONE)
        DVE.wait(P3_DONE, H1D_DONE)
        ACT.wait(P3_DONE, H1D_DONE)
        SP.wait(P3_DONE, SCAT_DONE)
        issue_gu(0)
        issue_dn(0)
        issue_xe(0)
        issue_xe(1)
        do_transposes(0)
        banks = [(psS0, psS1), (psD0, psD1)]
        bfree = [[sfree[0], sfree[1]], [st.get("dfree2"), st.get("dfree2")]]
        obk = [psO0, psO1, psP]
        obkfree = [ofree[0], ofree[1], st["psP_free"]]
        tfree = [None, None]
        stepc = 0
        ocnt = 0
        act_last = None
        for e in range(NE):
            xs = e % 2
            if e + 1 < NE:
                issue_gu(e + 1)
                issue_dn(e + 1)
            s = e % 2
            for q in range(4):
                i = e
                for jj in range(2):
                    j = 2 * q + jj
                    b = stepc % 2
                    stepc += 1
                    Pg, Pu = banks[b]
                    PE.wait(gu_ev[i], xt_ready[e], bfree[b][0], bfree[b][1])
                    for kc in range(8):
                        eg = PE.op(lambda h, kc=kc, Pg=Pg, s=s, j=j, xs=xs: h.matmul(Pg[:, 0:CAP], lhsT=Wgu[s][:, kc, j * 128:(j + 1) * 128], rhs=XT[xs][:, kc, :], start=(kc == 0), stop=(kc == 7)), sig=(kc == 7))
                    for kc in range(8):
                        eu = PE.op(lambda h, kc=kc, Pu=Pu, s=s, j=j, xs=xs: h.matmul(Pu[:, 0:CAP], lhsT=Wgu[s][:, kc, 1024 + j * 128:1024 + (j + 1) * 128], rhs=XT[xs][:, kc, :], start=(kc == 0), stop=(kc == 7)), sig=(kc == 7))
                    if j == 7:
                        gu_free[s] = eu
                    DVE.wait(eg, tfree[b])
                    a1 = DVE.op(lambda h, Pg=Pg, e=e, j=j, b=b: h.tensor_scalar(out=tg[b][:, :], in0=Pg[:, 0:CAP], scalar1=bgu[:, e * 16 + j:e * 16 + j + 1], scalar2=7.0, op0=ALU.add, op1=ALU.min))
                    bfree[b][0] = a1
                    ACT.wait(a1, eu)
                    ACT.op(lambda h, b=b: h.activation(out=tsg[b][:, :], in_=tg[b][:, :], func=AF.Sigmoid, scale=1.702), sig=False)
                    a2 = ACT.op(lambda h, Pu=Pu, e=e, j=j, b=b: h.activation(out=tu[b][:, :], in_=Pu[:, 0:CAP], func=AF.Identity, bias=bgu[:, e * 16 + 8 + j:e * 16 + 9 + j], scale=1.0))
                    bfree[b][1] = a2
                    DVE.wait(a2)
                    a3 = DVE.op(lambda h, b=b: h.tensor_scalar(out=tu[b][:, :], in0=tu[b][:, :], scalar1=7.0, scalar2=-7.0, op0=ALU.min, op1=ALU.max))
                    DVE.wait(a3)
                    a4 = DVE.op(lambda h, b=b: h.scalar_tensor_tensor(out=tu[b][:, :], in0=tu[b][:, :], scalar=1.0, in1=tg[b][:, :], op0=ALU.add, op1=ALU.mult))
                    DVE.wait(a4)
                    a5 = DVE.op(lambda h, j=j, b=b, xs=xs: h.tensor_tensor(out=actT[xs][:, j, :], in0=tu[b][:, :], in1=tsg[b][:, :], op=ALU.mult))
                    tfree[b] = a5
                    act_last = a5
            if e + 1 < NE:
                do_transposes(e + 1)
            if e + 2 < NE:
                issue_xe(e + 2)
            ds_ = e % 2
            PE.wait(act_last, dn_ev[e])
            for nb_ in range(3):
                ys = (e * 3 + nb_) % 2
                for dh in range(2):
                    oi = ocnt % 3
                    ocnt += 1
                    O = obk[oi]
                    PE.wait(obkfree[oi])
                    for fc in range(8):
                        em = PE.op(lambda h, O=O, fc=fc, nb_=nb_, dh=dh, ds_=ds_, xs=xs: h.matmul(O[:, :], lhsT=actT[xs][:, fc, nb_ * 128:(nb_ + 1) * 128], rhs=Wd[ds_][:, fc, dh * 512:(dh + 1) * 512], start=(fc == 0), stop=(fc == 7)), sig=(fc == 7))
                    ACT.wait(em, yt_free[ys] if dh == 0 else None)
                    ac = ACT.op(lambda h, O=O, ys=ys, dh=dh: h.activation(out=Yt[ys][:, dh * 512:(dh + 1) * 512], in_=O[:, :], func=AF.Copy))
                    obkfree[oi] = ac
                SP.wait(ac)
                yt_free[ys] = SP.dma(lambda h, ys=ys, e=e, nb_=nb_: h.dma_start(out=Yg[e * CAP + nb_ * 128:e * CAP + (nb_ + 1) * 128, :], in_=Yt[ys][:, :]), d_yg[ys])
            dn_free[ds_] = em
        MOE_DONE = [ac, em, d_yg[0].ev(), d_yg[1].ev()]

        G = [[av((s_ * 4 + k_) * 4 * K, [128, D], F32) for k_ in range(4)] for s_ in range(2)]
        d_hl = [dsem("d_hl0"), dsem("d_hl1")]
        d_out = [dsem("d_out0"), dsem("d_out1")]
        d_g = [dsem("d_g0"), dsem("d_g1")]
        d_l2 = dsem("d_l2")
        buf_free = [None, None]
        g_free = [None, None]
        gT_free = None
        SP.wait(H1D_DONE, MOE_DONE)
        POOL.wait(MOE_DONE)
        PE.wait(MOE_DONE)
        DVE.wait(MOE_DONE)
        l2ev = SP.dma(lambda h: h.dma_start(out=lnp2[:, :, :], in_=lnp_d[:, 2 * D:4 * D].rearrange("p (a c) -> p a c", a=2)), d_l2)
        o5free = [obkfree[0], obkfree[1]]

        def issue_gather(tb):
            s = tb % 2
            POOL.wait(g_free[s])
            for k_ in range(4):
                gev_ = POOL.dma(lambda h, s=s, tb=tb, k_=k_: h.indirect_dma_start(out=G[s][k_][:, :], out_offset=None, in_=Yg[:, :], in_offset=bass.IndirectOffsetOnAxis(ap=desti[:, tb, k_:k_ + 1], axis=0)), d_g[s])
            return gev_

        gq = {0: issue_gather(0)}
        for tb in range(16):
            s = tb % 2
            if tb + 1 < 16:
                gq[tb + 1] = issue_gather(tb + 1)
            SP.wait(buf_free[s])
            hev = SP.dma(lambda h, s=s, tb=tb: h.dma_start(out=xf5[s][:, :], in_=h1d[tb * 128:(tb + 1) * 128, :]), d_hl[s])
            PE.wait(bfree[0][0], bfree[0][1], gT_free)
            e = PE.op(lambda h, tb=tb: h.transpose(out=psS0[0:NE, 0:128], in_=gwd[:, tb, :], identity=identf[:, :]))
            ACT.wait(e, gT_free)
            a = ACT.op(lambda h: h.activation(out=gT[:, :], in_=psS0[0:NE, 0:128], func=AF.Copy))
            bfree[0][0] = a
            PE.wait(a, o5free[0], o5free[1])
            for dh in range(2):
                e = PE.op(lambda h, dh=dh: h.matmul([psO0, psO1][dh][:, :], lhsT=gT[:, :], rhs=bdn[:, dh * 512:(dh + 1) * 512], start=True, stop=True))
            gT_free = e
            G0 = G[s][0]
            DVE.wait(gq[tb])
            a = DVE.op(lambda h, G0=G0, tb=tb: h.tensor_scalar(out=G0[:, :], in0=G0[:, :], scalar1=gw4[:, tb, 0:1], scalar2=None, op0=ALU.mult))
            for k_ in range(1, 4):
                DVE.wait(a)
                a = DVE.op(lambda h, G0=G0, s=s, k_=k_, tb=tb: h.scalar_tensor_tensor(out=G0[:, :], in0=G[s][k_][:, :], scalar=gw4[:, tb, k_:k_ + 1], in1=G0[:, :], op0=ALU.mult, op1=ALU.add))
            DVE.wait(a, e)
            DVE.op(lambda h, G0=G0: h.tensor_tensor(out=G0[:, 0:512], in0=G0[:, 0:512], in1=psO0[:, :], op=ALU.add), sig=False)
            a = DVE.op(lambda h, G0=G0: h.tensor_tensor(out=G0[:, 512:1024], in0=G0[:, 512:1024], in1=psO1[:, :], op=ALU.add))
            o5free = [a, a]
            DVE.wait(a, hev)
            a = DVE.op(lambda h, s=s, G0=G0: h.scalar_tensor_tensor(out=xf5[s][:, :], in0=xf5[s][:, :], scalar=ALPHA, in1=G0[:, :], op0=ALU.mult, op1=ALU.add))
            g_free[s] = a
            DVE.wait(l2ev)
            oev = layer_norm(xf5[s], lnp2, a)
            SP.wait(oev)
            buf_free[s] = SP.dma(lambda h, s=s, tb=tb: h.dma_start(out=out_d[tb * 128:(tb + 1) * 128, :], in_=xf5[s][:, :]), d_out[s])
        SP.wait(d_out[0].ev(), d_out[1].ev())
        if debug:
            SP.wait(d_dbg.ev())

        with nc.Block() as block:
            @block.tensor
            def _(h):
                for f in PE.ops:
                    f(h)

            @block.scalar
            def _(h):
                for f in ACT.ops:
                    f(h)

            @block.vector
            def _(h):
                for f in DVE.ops:
                    f(h)

            @block.gpsimd
            def _(h):
                for f in POOL.ops:
                    f(h)

            @block.sync
            def _(h):
                for f in SP.ops:
                    f(h)
    return nc


def rope_tables_np(positions):
    inv = 500000.0 ** (-np.arange(0, 16, 2, dtype=np.float32) / 16.0)
    ang = positions.astype(np.float32)[:, None] * inv[None, :]
    return np.cos(ang).astype(np.float32), np.sin(ang).astype(np.float32)


def make_consts(hf):
    TIDX, NTAB = tab_index()
    tab = np.zeros((128, NTAB, 16), np.float32)
    p = np.arange(128)
    pos0 = hf * 2048

    def put(slot, positions):
        c, s = rope_tables_np(positions)
        tab[:, slot, 0:8] = c
        tab[:, slot, 8:16] = s

    for b in range(32):
        if b < 16:
            put(TIDX[("n", 0, b)], b * 128 + p)
        else:
            put(TIDX[("n", 0, b)], pos0 + (b - 16) * 128 + p)
    for g, d in enumerate(DILS):
        nb = 16 // d
        for ob in range(16):
            r, n = ob // nb, ob % nb
            put(TIDX[("o", g, ob)], pos0 + (n * 128 + p) * d + r)
        for pb in range(d):
            put(TIDX[("p", g, pb)], 2048 - 128 * d + p * d + pb)
    k = np.arange(128)[:, None]
    q = np.arange(512)[None, :]
    cm = np.stack([(128 * v + k > q) for v in range(4)], axis=1).astype(np.float32)
    mA = (np.arange(128)[:, None] < np.arange(128)[None, :]).astype(np.float32)
    bf = ml_dtypes.bfloat16
    return {
        "rope": tab.reshape(128, NTAB * 16),
        "prevbias": np.full((128, 1), 0.0 if hf == 1 else NEG, np.float32),
        "cmask": cm.reshape(128, 4 * 512).astype(bf),
        "maskA": mA.astype(bf),
        "negI": (NEG * np.eye(128, dtype=np.float32)).astype(bf),
        "identb": np.eye(128, dtype=np.float32).astype(bf),
        "identf": np.eye(128, dtype=np.float32),
    }


_NC_CACHE = {}


def kernel(x, w_in, b_gate, lam_q1, lam_k1, lam_q2, lam_k2, subln_g, w_oa, w_ob, w_o,
           ln1_g, ln1_b, w_router, b_router, w_gu, b_gu, w_down, b_down, ln2_g, ln2_b, _debug=False):
    f32 = np.float32
    x = np.asarray(x, f32)
    key = bool(_debug)
    if key not in _NC_CACHE:
        _NC_CACHE[key] = build_nc(debug=_debug)
    nc = _NC_CACHE[key]
    c = lambda a: np.ascontiguousarray(np.asarray(a, f32))
    shared = {
        "w_in": c(w_in[0]), "w_oa": c(w_oa[0]), "w_ob": c(w_ob[0]), "w_o": c(w_o[0]),
        "w_router": c(w_router[0]), "w_gu": c(w_gu[0]), "w_down": c(w_down[0]),
        "lamv": c(np.broadcast_to(np.stack([lam_q1[0], lam_k1[0], lam_q2[0], lam_k2[0]])[None], (128, 4, 64)).reshape(128, 256)),
        "subg": c(np.asarray(subln_g[0]).reshape(128, 1)),
        "bgt": c(np.asarray(b_gate[0]).reshape(16, 128).T),
        "bgu_t": c(np.asarray(b_gu[0]).reshape(NE, 16, 128).transpose(2, 0, 1).reshape(128, NE * 16)),
        "brt": c(np.broadcast_to(np.asarray(b_router[0])[None], (128, NE))),
        "bdn": c(b_down[0]),
        "ebase": c(np.broadcast_to((np.arange(NE, dtype=np.float32) * CAP)[None], (128, NE))),
        "lnp": c(np.broadcast_to(np.stack([ln1_g[0], ln1_b[0], ln2_g[0], ln2_b[0]])[None], (128, 4, D)).reshape(128, 4 * D)),
    }
    consts = [make_consts(0), make_consts(1)]
    in_maps = []
    for core in range(8):
        b, hf = core // 2, core % 2
        m = dict(shared)
        m.update(consts[hf])
        m["x_own"] = c(x[b, hf * 2048:(hf + 1) * 2048])
        m["x_prev"] = c(x[b, 0:2048])
        in_maps.append(m)
    res = run_bass_kernel_spmd(nc, in_maps, core_ids=list(range(8)))
    out = np.empty((4, 4096, D), f32)
    for core in range(8):
        b, hf = core // 2, core % 2
        out[b, hf * 2048:(hf + 1) * 2048] = res.results[core]["out"]
    if _debug:
        return out, res.results
    return out
```

```python
import contextlib
import numpy as np
import ml_dtypes
import concourse.bass as bass
import concourse.mybir as mybir
from concourse.bass_utils import run_bass_kernel_spmd

F32 = mybir.dt.float32
BF16 = mybir.dt.bfloat16
ALU = mybir.AluOpType
AF = mybir.ActivationFunctionType
AX = mybir.AxisListType

S_OWN = 2048
D = 1024
NE = 32
ALPHA = 2.0 ** 0.25
EPS = 1e-5
LAMBDA_INIT = 0.2
NEG = -30000.0
DILS = (1, 4, 16)
SAME_ENGINE_WAITS = True
CAP = 384
I32 = mybir.dt.int32


def tab_index():
    idx = {}
    n = 0
    for b in range(32):
        idx[("n", 0, b)] = n; n += 1
    for g, d in enumerate(DILS):
        for ob in range(16):
            idx[("o", g, ob)] = n; n += 1
        for pb in range(d):
            idx[("p", g, pb)] = n; n += 1
    return idx, n


class Eng:
    def __init__(self, name, sem):
        self.name, self.sem, self.n, self.ops, self.seen = name, sem, 0, [], {}

    def wait(self, *evs):
        for ev in evs:
            if ev is None:
                continue
            if isinstance(ev, list):
                self.wait(*ev)
                continue
            sem, val, key = ev
            if key == self.name and not SAME_ENGINE_WAITS:
                continue
            if self.seen.get(key, 0) >= val:
                continue
            self.seen[key] = val
            self.ops.append(lambda h, sem=sem, val=val: h.wait_ge(sem, val))

    def op(self, fn, sig=True):
        if sig:
            self.n += 1
            self.ops.append(lambda h, fn=fn, sem=self.sem: fn(h).then_inc(sem, 1))
            return (self.sem, self.n, self.name)
        self.ops.append(lambda h, fn=fn: fn(h))
        return None

    def dma(self, fn, ds):
        ds.n += 16
        self.ops.append(lambda h, fn=fn, sem=ds.sem: fn(h).then_inc(sem, 16))
        return (ds.sem, ds.n, ds.name)


class DSem:
    def __init__(self, name, sem):
        self.name, self.sem, self.n = name, sem, 0

    def ev(self):
        return (self.sem, self.n, self.name)


def build_nc(debug=False):
    nc = bass.Bass("TRN2", target_bir_lowering=False)
    TIDX, NTAB = tab_index()

    def din(name, shape, dt=F32):
        return nc.dram_tensor(name, list(shape), dt, kind="ExternalInput").ap()

    x_own = din("x_own", [S_OWN, D])
    x_prev = din("x_prev", [S_OWN, D])
    w_in = din("w_in", [D, 8192])
    w_oa = din("w_oa", [512, D])
    w_ob = din("w_ob", [512, D])
    w_o = din("w_o", [D, D])
    w_router = din("w_router", [D, NE])
    w_gu = din("w_gu", [NE, D, 2048])
    w_down = din("w_down", [NE, D, D])
    rope = din("rope", [128, NTAB * 16])
    prevbias_d = din("prevbias", [128, 1])
    cmask_d = din("cmask", [128, 4 * 512], BF16)
    maskA_d = din("maskA", [128, 128], BF16)
    negI_d = din("negI", [128, 128], BF16)
    identb_d = din("identb", [128, 128], BF16)
    identf_d = din("identf", [128, 128])
    lamv_d = din("lamv", [128, 4 * 64])
    subg_d = din("subg", [128, 1])
    bgt_d = din("bgt", [128, 16])
    bgu_d = din("bgu_t", [128, NE * 16])
    brt_d = din("brt", [128, NE])
    bdn_d = din("bdn", [NE, D])
    lnp_d = din("lnp", [128, 4 * D])
    ebase_d = din("ebase", [128, NE])
    Xg = nc.dram_tensor("Xg", [NE * CAP, D], BF16, kind="Internal").ap()
    Yg = nc.dram_tensor("Yg", [NE * CAP, D], F32, kind="Internal").ap()
    out_d = nc.dram_tensor("out", [S_OWN, D], F32, kind="ExternalOutput").ap()
    h1d = nc.dram_tensor("h1d", [S_OWN, D], F32, kind="ExternalOutput" if debug else "Internal").ap()
    if debug:
        dbg_ya = nc.dram_tensor("dbg_ya", [128, 4 * 2048], BF16, kind="ExternalOutput").ap()
    ybd = nc.dram_tensor("ybd", [64, 8 * 2048], BF16, kind="ExternalOutput" if debug else "Internal").ap()

    es = contextlib.ExitStack()
    with es:
        def sb(name, shape, dt):
            return es.enter_context(nc.sbuf_tensor("sb_" + name, list(shape), dt))

        def pst(name):
            return es.enter_context(nc.psum_tensor(name, [128, 512], F32))

        _semc = [0]

        def newsem(name):
            _semc[0] += 1
            return es.enter_context(nc.semaphore(name))

        PE = Eng("pe", newsem("s_pe"))
        ACT = Eng("act", newsem("s_act"))
        DVE = Eng("dve", newsem("s_dve"))
        POOL = Eng("pool", newsem("s_pool"))
        SP = Eng("sp", newsem("s_sp"))

        def dsem(name):
            return DSem(name, newsem(name))

        K = 1024
        ARENA = sb("arena", [128, 92 * K], BF16)

        def av(off, shape, dt, parts=128):
            esz = 4 if dt == F32 else 2
            n = 1
            for d_ in shape[1:]:
                n *= d_
            a = ARENA[0:parts, off // 2: off // 2 + n * esz // 2]
            if dt == F32:
                a = a.bitcast(F32)
            if len(shape) == 3:
                a = a.rearrange("p (a b) -> p a b", a=shape[1])
            elif len(shape) == 4:
                a = a.rearrange("p (a b c) -> p a b c", a=shape[1], b=shape[2])
            return a

        xTp = av(0, [128, 8, 2048], BF16)
        xTo = av(32 * K, [128, 8, 2048], BF16)
        ybH = av(64 * K, [64, 4, 2048], BF16, parts=64)
        acc = av(80 * K, [128, 4, 2048], F32)
        yaT = av(80 * K, [128, 4, 2048], BF16)
        ft = [av(96 * K + i * 2 * K, [128, 512], F32) for i in range(5)]
        xld = [av(100 * K + i * 2 * K, [128, 1024], BF16) for i in range(2)]
        QT = av(112 * K, [128, 2, 2048], BF16)
        KT = av(120 * K, [128, 2, 4096], BF16)
        VVf = av(136 * K, [128, 32 * 260], BF16)
        Wr = [av(153 * K + i * 4 * K, [128, 8, 256], BF16) for i in range(3)]
        Tt = [av(165 * K + i * 512, [128, 256], BF16) for i in range(2)]
        rtmp = [av(166 * K + i * 512, [128, 4, 32], F32) for i in range(2)]
        ropet = av(167 * K, [128, NTAB, 16], F32)
        cmask = av(167 * K + 6656, [128, 4, 512], BF16)
        PT = [av(167 * K + 6656 + 4 * K + i * K, [128, 512], BF16) for i in range(4)]
        PT2 = [av(167 * K + 6656 + 4 * K + i * 2 * K, [128, 1024], BF16) for i in range(2)]
        lamv = av(167 * K + 6656 + 8 * K, [128, 4, 64], F32)
        maskA = av(167 * K + 6656 + 9 * K, [128, 128], BF16)
        negI = av(167 * K + 6656 + 9 * K + 256, [128, 128], BF16)
        ybw = av(64 * K, [64, 8, 512], BF16, parts=64)
        mT = av(72 * K, [128, 8, 512], BF16)
        h1Tf = av(106 * K, [128, 8, 128], F32)
        wrt = av(110 * K, [128, 8, NE], F32)
        woa = av(112 * K, [128, 4, D], BF16)
        wob = av(120 * K, [64, 8, D], BF16, parts=64)
        wo = av(136 * K, [128, 8, D], BF16)
        lnp1 = av(165 * K, [128, 2, D], F32)
        xf3 = av(173 * K, [128, D], F32)
        xf5 = [av(128 * K + i * 4 * K, [128, D], F32) for i in range(2)]
        lnp2 = av(136 * K, [128, 2, D], F32)

        identb = sb("identb", [128, 128], BF16)
        identf = sb("identf", [128, 128], F32)
        ones_bf = sb("ones_bf", [128, 128], BF16)
        onesF = sb("onesF", [128, 128], F32)
        prevbias = sb("prevbias", [128, 1], F32)
        lamt = sb("lamt", [128, 2, 64], F32)
        lams = sb("lams", [128, 4], F32)
        subg = sb("subg", [128, 1], F32)
        epsc = sb("epsc", [128, 1], F32)
        bgt = sb("bgt", [128, 16], F32)
        bgu = sb("bgu", [128, NE * 16], F32)
        brt = sb("brt", [128, NE], F32)
        bdn = sb("bdn", [NE, D], F32)
        gwd = sb("gwd", [128, 16, NE], F32)
        rsm = sb("rsm", [128, 16], F32)
        ftA = sb("ftA", [128, 512], F32)
        stt = sb("stt", [128, 2, 6], F32)
        mv = sb("mv", [128, 2], F32)
        sm = sb("sm", [128, 8], F32)
        lg = sb("lg", [128, NE], F32)
        mx8 = sb("mx8", [128, 8], F32)
        el = sb("el", [128, NE], F32)
        gT = sb("gT", [NE, 128], F32)
        ebase = sb("ebase", [128, NE], F32)
        m01 = sb("m01", [128, 16, NE], BF16)
        slotv = sb("slotv", [128, NE], F32)
        rtm = sb("rtm", [128, NE], F32)
        destf = sb("destf", [128, 4], F32)
        desti = sb("desti", [128, 16, 4], I32)
        gw4 = sb("gw4", [128, 16, 4], F32)

        pairA = es.enter_context(nc.psum_tensor("psA", [128, 1024], F32))
        pairB = es.enter_context(nc.psum_tensor("psB", [128, 1024], F32))
        psS0, psS1 = pairA[:, 0:512], pairA[:, 512:1024]
        psP, psT = pairB[:, 0:512], pairB[:, 512:1024]
        psO0, psO1, psD0, psD1 = [pst(f"ps{i}") for i in range(4)]
        psTb = psT[:, :].bitcast(BF16)

        d_const = dsem("d_const")
        for (dst, src) in [
            (ropet[:, :, :], rope.rearrange("p (n c) -> p n c", c=16)),
            (prevbias[:, :], prevbias_d), (cmask[:, :, :], cmask_d.rearrange("p (v q) -> p v q", v=4)),
            (maskA[:, :], maskA_d), (negI[:, :], negI_d), (identb[:, :], identb_d), (identf[:, :], identf_d),
            (lamv[:, :, :], lamv_d.rearrange("p (a b) -> p a b", a=4)), (subg[:, :], subg_d),
            (bgt[:, :], bgt_d), (bgu[:, :], bgu_d), (brt[:, :], brt_d), (bdn[:, :], bdn_d), (ebase[:, :], ebase_d),
        ]:
            SP.dma(lambda h, dst=dst, src=src: h.dma_start(out=dst, in_=src), d_const)
        CONST = d_const.ev()
        e1 = POOL.op(lambda h: h.memset(ones_bf[:, :], 1.0))
        e2 = POOL.op(lambda h: h.memset(onesF[:, :], 1.0))
        e3 = POOL.op(lambda h: h.memset(epsc[:, :], EPS))
        e4 = POOL.op(lambda h: h.memset(VVf[:, :], 1.0))
        MEMS = [e1, e2, e3, e4]
        zt = av(112 * K, [128, 8192], BF16)
        ez = POOL.op(lambda h: h.memset(zt[:, :], 0.0))
        d_z = dsem("d_z")
        SP.wait(ez)
        xg_flat = Xg.rearrange("(p n) d -> p (n d)", p=128)
        for i_ in range(NE * CAP * D // 128 // 8192):
            SP.dma(lambda h, i_=i_: h.dma_start(out=xg_flat[:, i_ * 8192:(i_ + 1) * 8192], in_=zt[:, :]), d_z)
        ZFILL = d_z.ev()
        DVE.wait(CONST)
        ev = DVE.op(lambda h: h.tensor_tensor(out=lamt[:, :, :], in0=lamv[:, 0:4:2, :], in1=lamv[:, 1:4:2, :], op=ALU.mult))
        DVE.wait(ev)
        ev = DVE.op(lambda h: h.reduce_sum(out=lams[:, 0:2], in_=lamt[:, :, :], axis=AX.X))
        ACT.wait(ev)
        ev = ACT.op(lambda h: h.activation(out=lams[:, 2:4], in_=lams[:, 0:2], func=AF.Exp))
        DVE.wait(ev)
        ev = DVE.op(lambda h: h.tensor_tensor(out=lams[:, 0:1], in0=lams[:, 3:4], in1=lams[:, 2:3], op=ALU.subtract))
        DVE.wait(ev)
        ev = DVE.op(lambda h: h.tensor_scalar(out=lams[:, 0:1], in0=lams[:, 0:1], scalar1=-LAMBDA_INIT, scalar2=None, op0=ALU.add))
        DVE.wait(ev)
        ev = DVE.op(lambda h: h.tensor_scalar(out=lams[:, 1:2], in0=subg[:, :], scalar1=1.0 - LAMBDA_INIT, scalar2=None, op0=ALU.mult))
        LAMEV = ev
        neglam = lams[:, 0:1]
        gsc = lams[:, 1:2]

        d_x = [dsem("d_x0"), dsem("d_x1")]
        xfree = [None, None]
        psT_free = None
        PE.wait(CONST)
        for blk in range(32):
            s = blk % 2
            src = (x_prev if blk < 16 else x_own)[(blk % 16) * 128:(blk % 16 + 1) * 128, :]
            POOL.wait(xfree[s])
            ld = POOL.dma(lambda h, s=s, src=src: h.dma_start(out=xld[s][:, :], in_=src), d_x[s])
            PE.wait(ld, psT_free)
            for kc in range(8):
                ev = PE.op(lambda h, s=s, kc=kc: h.transpose(out=psTb[:, kc * 128:(kc + 1) * 128], in_=xld[s][:, kc * 128:(kc + 1) * 128], identity=identb[:, :]), sig=(kc == 7))
            xfree[s] = ev
            ACT.wait(ev)
            dst = (xTp if blk < 16 else xTo)[:, :, (blk % 16) * 128:(blk % 16 + 1) * 128]
            psT_free = ACT.op(lambda h, dst=dst: h.activation(out=dst, in_=psTb[:, :].rearrange("p (k t) -> p k t", k=8), func=AF.Copy))
        XT_DONE = psT_free

        d_w = [dsem(f"d_w{i}") for i in range(3)]
        wfree = [None, None, None]
        wcnt = [0]

        def load_w(col0):
            s = wcnt[0] % 3
            wcnt[0] += 1
            POOL.wait(wfree[s])
            ev = POOL.dma(lambda h, s=s, col0=col0: h.dma_start(out=Wr[s][:, :, :], in_=w_in[:, col0:col0 + 256].rearrange("(k p) c -> p k c", p=128)), d_w[s])
            return s, ev

        st = {"psP_free": None, "psT_free": XT_DONE, "tcnt": 0, "Tfree": [None, None], "rfree": [None, None], "pend": None,
              "pf": [None, None], "pcnt": 0}
        pbanks = [psP, psD1]

        def proj_mm(xap_fn, ws, wev):
            pb = st["pcnt"] % 2
            st["pcnt"] += 1
            Pb = pbanks[pb]
            PE.wait(wev, st["pf"][pb], st["psP_free"] if pb == 0 else None, XT_DONE)
            for kc in range(8):
                ev = PE.op(lambda h, kc=kc, Pb=Pb: h.matmul(Pb[:, 0:256], lhsT=xap_fn(kc), rhs=Wr[ws][:, kc, :], start=(kc == 0), stop=(kc == 7)), sig=(kc == 7))
            wfree[ws] = ev
            return ev, pb

        def flush_pend():
            if st["pend"] is None:
                return
            ti, tev, dst = st["pend"]
            st["pend"] = None
            PE.wait(tev, st["psT_free"])
            for hh_ in range(2):
                ev = PE.op(lambda h, hh_=hh_, ti=ti: h.transpose(out=psTb[:, hh_ * 128:(hh_ + 1) * 128], in_=Tt[ti][:, hh_ * 128:(hh_ + 1) * 128], identity=identb[:, :]), sig=(hh_ == 1))
            st["Tfree"][ti] = ev
            ACT.wait(ev)
            st["psT_free"] = ACT.op(lambda h, dst=dst: h.activation(out=dst, in_=psTb[:, 0:256].rearrange("p (a t) -> p a t", a=2), func=AF.Copy))
            st["last_qk"] = st["psT_free"]

        def qk_tile(xap_fn, ws, wev, tab, dst):
            mmev, pb = proj_mm(xap_fn, ws, wev)
            flush_pend()
            ti = st["tcnt"] % 2
            st["tcnt"] += 1
            p3 = pbanks[pb][:, 0:256].rearrange("p (a c) -> p a c", a=4)
            t3 = Tt[ti][:, :].rearrange("p (a c) -> p a c", a=4)
            cos = ropet[:, tab, 0:8].unsqueeze(1).broadcast_to([128, 4, 8])
            sin = ropet[:, tab, 8:16].unsqueeze(1).broadcast_to([128, 4, 8])
            rt = rtmp[ti]
            DVE.wait(mmev, st["Tfree"][ti], CONST)
            a = DVE.op(lambda h: h.tensor_tensor(out=rt[:, :, 0:8], in0=p3[:, :, 0:8], in1=cos, op=ALU.mult), sig=False)
            a = DVE.op(lambda h: h.tensor_tensor(out=rt[:, :, 8:16], in0=p3[:, :, 8:16], in1=cos, op=ALU.mult), sig=False)
            a = DVE.op(lambda h: h.tensor_tensor(out=rt[:, :, 16:24], in0=p3[:, :, 8:16], in1=sin, op=ALU.mult), sig=False)
            a = DVE.op(lambda h: h.tensor_tensor(out=rt[:, :, 24:32], in0=p3[:, :, 0:8], in1=sin, op=ALU.mult), sig=False)
            a = DVE.op(lambda h: h.tensor_copy(out=t3[:, :, 16:64], in_=p3[:, :, 16:64]))
            st["pf"][pb] = a
            if pb == 0:
                st["psP_free"] = a
            DVE.wait(a)
            a = DVE.op(lambda h: h.tensor_tensor(out=t3[:, :, 0:8], in0=rt[:, :, 0:8], in1=rt[:, :, 16:24], op=ALU.subtract), sig=False)
            a = DVE.op(lambda h: h.tensor_tensor(out=t3[:, :, 8:16], in0=rt[:, :, 8:16], in1=rt[:, :, 24:32], op=ALU.add))
            st["pend"] = (ti, a, dst)

        def v_tile(xap_fn, ws, wev, dst, srcf):
            mmev, pb = proj_mm(xap_fn, ws, wev)
            ACT.wait(mmev, MEMS)
            a = ACT.op(lambda h: h.activation(out=dst, in_=srcf(pbanks[pb]), func=AF.Copy))
            st["pf"][pb] = a
            if pb == 0:
                st["psP_free"] = a
            st["last_v"] = a

        def xnat(blk):
            t = xTp if blk < 16 else xTo
            b = blk % 16
            return lambda kc: t[:, kc, b * 128:(b + 1) * 128]

        Vaug = VVf[:, :].rearrange("p (b h c) -> p b h c", b=32, h=4)
        bufc = [0]
        d_yb = dsem("d_yb")
        sfree = [None, None]
        ptfree = [None] * 4
        ofree = [None, None]
        ptc = [0]
        psSb = [psS0, psS1]
        psOb = [psO0, psO1]
        acc_last = [None]

        for hh in range(2):
            for g, d in enumerate(DILS):
                nb = 16 // d
                base = 1536 + g * 1536 + hh * 256
                stage_guard = acc_last[0]
                ACT.wait(stage_guard, ZFILL)
                sq, evq = load_w(base)
                sk, evk = load_w(base + 512)
                sv, evv = load_w(base + 1024)

                def xown(ob, d=d, nb=nb):
                    r, n = ob // nb, ob % nb
                    return lambda kc: xTo[:, kc, :].rearrange("p (l d) -> p d l", d=d)[:, r, n * 128:(n + 1) * 128]

                def xprev(pb, d=d):
                    return lambda kc: xTp[:, kc, 2048 - 128 * d:2048].rearrange("p (l d) -> p d l", d=d)[:, pb, :]

                for ob in range(16):
                    qk_tile(xown(ob), sq, evq, TIDX[("o", g, ob)], QT[:, :, ob * 128:(ob + 1) * 128])
                for ob in range(16):
                    qk_tile(xown(ob), sk, evk, TIDX[("o", g, ob)], KT[:, :, 2048 + ob * 128:2048 + (ob + 1) * 128])
                for pb in range(d):
                    qk_tile(xprev(pb), sk, evk, TIDX[("p", g, pb)], KT[:, :, pb * 128:(pb + 1) * 128])
                flush_pend()
                for ob in range(16):
                    v_tile(xown(ob), sv, evv, Vaug[:, 16 + ob, :, 0:64], lambda P_: P_[:, 0:256].rearrange("p (a c) -> p a c", a=4))
                for pb in range(d):
                    v_tile(xprev(pb), sv, evv, Vaug[:, pb, :, 0:64], lambda P_: P_[:, 0:256].rearrange("p (a c) -> p a c", a=4))
                PROJ_DONE = [st["last_qk"], st["last_v"]]
                PE.wait(PROJ_DONE)
                work = [(r, hl, n) for r in range(d) for hl in range(4) for n in range(nb)]

                def dil_S(item, d=d, nb=nb):
                    r, hl, n = item
                    p, s_ = hl // 2, hl % 2
                    lo, hi = s_ * 64, s_ * 64 + 64
                    ob = r * nb + n
                    qpos = ob * 128
                    if n == 0:
                        kposA, blkA = r * 128, r
                    else:
                        kposA, blkA = 2048 + (ob - 1) * 128, 16 + ob - 1
                    kposB, blkB = 2048 + ob * 128, 16 + ob
                    bi = bufc[0] % 2
                    bufc[0] += 1
                    S = psSb[bi]
                    PE.wait(sfree[bi])
                    qap = QT[lo:hi, p, qpos:qpos + 128]
                    PE.op(lambda h: h.matmul(S[:, 0:128], lhsT=KT[lo:hi, p, kposA:kposA + 128], rhs=qap, start=True, stop=False), sig=False)
                    PE.op(lambda h: h.matmul(S[:, 0:128], lhsT=negI[:, :], rhs=maskA[:, :], start=False, stop=True), sig=False)
                    PE.op(lambda h: h.matmul(S[:, 128:256], lhsT=KT[lo:hi, p, kposB:kposB + 128], rhs=qap, start=True, stop=False), sig=False)
                    sev = PE.op(lambda h: h.matmul(S[:, 128:256], lhsT=negI[:, :], rhs=cmask[:, 0, 0:128], start=False, stop=True))
                    pi = ptc[0] % 4
                    ptc[0] += 1
                    ACT.wait(sev, ptfree[pi])
                    if n == 0:
                        ACT.op(lambda h: h.activation(out=PT[pi][:, 0:128], in_=S[:, 0:128], func=AF.Exp, bias=prevbias[:, 0:1], scale=0.125), sig=False)
                        aev = ACT.op(lambda h: h.activation(out=PT[pi][:, 128:256], in_=S[:, 128:256], func=AF.Exp, scale=0.125))
                    else:
                        aev = ACT.op(lambda h: h.activation(out=PT[pi][:, 0:256], in_=S[:, 0:256], func=AF.Exp, scale=0.125))
                    sfree[bi] = aev
                    return (aev, pi, bi, blkA, blkB)

                def dil_AV(item, pend_, g=g, d=d):
                    r, hl, n = item
                    aev, pi, bi, blkA, blkB = pend_
                    O = psOb[bi]
                    PE.wait(aev, ofree[bi])
                    PE.op(lambda h: h.matmul(O[0:65, 0:128], lhsT=Vaug[:, blkA, hl, :], rhs=PT[pi][:, 0:128], start=True, stop=False), sig=False)
                    oev = PE.op(lambda h: h.matmul(O[0:65, 0:128], lhsT=Vaug[:, blkB, hl, :], rhs=PT[pi][:, 128:256], start=False, stop=True))
                    ptfree[pi] = oev
                    dst = acc[0:65, hl, :].rearrange("p (l d) -> p d l", d=d)[:, r, n * 128:(n + 1) * 128]
                    DVE.wait(oev, acc_last[0] if g > 0 else None)
                    if g == 0:
                        dev = DVE.op(lambda h: h.tensor_copy(out=dst, in_=O[0:65, 0:128]))
                    else:
                        dev = DVE.op(lambda h: h.tensor_tensor(out=dst, in0=dst, in1=O[0:65, 0:128], op=ALU.add))
                    ofree[bi] = dev
                    acc_last[0] = dev

                pend_ = dil_S(work[0])
                for wi, item in enumerate(work):
                    nxt = dil_S(work[wi + 1]) if wi + 1 < len(work) else None
                    dil_AV(item, pend_)
                    pend_ = nxt
            DVE.wait(st.get("ybstore"))
            for hl in range(4):
                for w in range(4):
                    PE.wait(acc_last[0], MEMS, st.get("dfree"))
                    bev = PE.op(lambda h, hl=hl, w=w: h.matmul(psD0[0:64, :], lhsT=onesF[64:65, 0:64], rhs=acc[64:65, hl, w * 512:(w + 1) * 512], start=True, stop=True))
                    ACT.wait(bev, acc_last[0])
                    rev = ACT.op(lambda h: h.activation(out=ftA[0:64, :], in_=psD0[0:64, :], func=AF.Ln))
                    st["dfree"] = rev
                    ACT.wait(rev)
                    rev = ACT.op(lambda h: h.activation(out=ftA[0:64, :], in_=ftA[0:64, :], func=AF.Exp, scale=-1.0))
                    DVE.wait(rev)
                    fev = DVE.op(lambda h, hl=hl, w=w: h.tensor_tensor(out=ybH[0:64, hl, w * 512:(w + 1) * 512], in0=acc[0:64, hl, w * 512:(w + 1) * 512], in1=ftA[0:64, :], op=ALU.mult))
                    acc_last[0] = fev
            SP.wait(acc_last[0])
            st["ybstore"] = SP.dma(lambda h, hh=hh: h.dma_start(out=ybd[:, hh * 8192:(hh + 1) * 8192], in_=ybH[:, :, :].rearrange("p h t -> p (h t)")), d_yb)
        YB_DONE = [acc_last[0], st["ybstore"]]

        Vd = VVf[:, 0:32 * 256].rearrange("p (b c) -> p b c", b=32)
        psOd = [psO0, psO1]
        psDd = [psD0, psD1]
        fin_free = YB_DONE
        st["sbf"] = [[sfree[0], sfree[1]], None]
        st["acc_prev"] = [None, None]
        att_last = YB_DONE
        ya_last = None
        for pp in range(2):
            ACT.wait(att_last)
            DVE.wait(att_last)
            sq, evq = load_w(pp * 256)
            sk, evk = load_w(512 + pp * 256)
            sv, evv = load_w(1024 + pp * 256)
            for ob in range(16):
                qk_tile(xnat(16 + ob), sq, evq, TIDX[("n", 0, 16 + ob)], QT[:, :, ob * 128:(ob + 1) * 128])
            for blk in range(32):
                qk_tile(xnat(blk), sk, evk, TIDX[("n", 0, blk)], KT[:, :, blk * 128:(blk + 1) * 128])
            flush_pend()
            for blk in range(32):
                v_tile(xnat(blk), sv, evv, Vd[:, blk, :], lambda P_: P_[:, 0:256])
            PE.wait(st["last_qk"], st["last_v"])
            for hl in range(2):
                for j in range(4):
                    nkb = 16 + 4 * (j + 1)
                    PE.wait(fin_free, st["pf"][1], st["pf"][0], st["psP_free"], st["psT_free"])
                    SBK = [(psS0, psS1), (psP, psT)]
                    accD = [av(106 * K, [128, 512], F32), av(108 * K, [128, 512], F32)]
                    accE = [DVE, DVE]

                    def dif_S(kb, hl=hl, j=j):
                        bp = bufc[0] % 2
                        bufc[0] += 1
                        diag = kb >= 16 + 4 * j
                        PE.wait(st["sbf"][bp])
                        for c in range(2):
                            lo, hi = c * 64, c * 64 + 64
                            S = SBK[bp][c]
                            sev = PE.op(lambda h, S=S, lo=lo, hi=hi: h.matmul(S[:, :], lhsT=KT[lo:hi, hl, kb * 128:(kb + 1) * 128], rhs=QT[lo:hi, hl, j * 512:(j + 1) * 512], start=True, stop=not diag), sig=(c == 1 and not diag))
                            if diag:
                                v = kb - 16 - 4 * j
                                sev = PE.op(lambda h, S=S, v=v: h.matmul(S[:, :], lhsT=negI[:, :], rhs=cmask[:, v, :], start=False, stop=True), sig=(c == 1))
                        pis = [(2 * bp) % 4, (2 * bp + 1) % 4]
                        ACT.wait(sev, ptfree[pis[0]], ptfree[pis[1]])
                        SS = [pairA, pairB][bp]
                        if kb < 16:
                            aev = ACT.op(lambda h: h.activation(out=PT2[bp][:, :], in_=SS[:, :], func=AF.Exp, bias=prevbias[:, 0:1], scale=0.125))
                        else:
                            aev = ACT.op(lambda h: h.activation(out=PT2[bp][:, :], in_=SS[:, :], func=AF.Exp, scale=0.125))
                        st["sbf"][bp] = aev
                        if bp == 1:
                            st["psP_free"] = aev
                            st["psT_free"] = aev
                            st["pf"][0] = aev
                        else:
                            sfree[0] = aev
                            sfree[1] = aev
                        return (aev, pis)

                    def dif_AV(kb, pend_, hl=hl, nkb=nkb):
                        aev, pis = pend_
                        even = False
                        PE.wait(aev)
                        PE.op(lambda h: h.matmul(psOd[0][:, :], lhsT=Vd[:, kb, hl * 128:(hl + 1) * 128], rhs=PT[pis[0]][:, :], start=(kb == 0), stop=(kb == nkb - 1)), sig=False)
                        oev_ = PE.op(lambda h: h.matmul(psOd[1][:, :], lhsT=Vd[:, kb, hl * 128:(hl + 1) * 128], rhs=PT[pis[1]][:, :], start=(kb == 0), stop=(kb == nkb - 1)), sig=not even)
                        if even:
                            PE.op(lambda h: h.matmul(psDd[0][:, :], lhsT=ones_bf[:, :], rhs=PT[pis[0]][:, :], start=(kb == 0), stop=False), sig=False)
                            oev_ = PE.op(lambda h: h.matmul(psDd[1][:, :], lhsT=ones_bf[:, :], rhs=PT[pis[1]][:, :], start=(kb == 0), stop=False))
                            ptfree[pis[0]] = oev_
                            ptfree[pis[1]] = oev_
                            return oev_
                        devs = []
                        for c in range(2):
                            E_ = accE[c]
                            E_.wait(aev, st["acc_prev"][c])
                            if kb == 0:
                                dv = E_.op(lambda h, c=c: h.tensor_copy(out=accD[c][:, :], in_=PT[pis[c]][:, :]))
                            else:
                                dv = E_.op(lambda h, c=c: h.tensor_tensor(out=accD[c][:, :], in0=accD[c][:, :], in1=PT[pis[c]][:, :], op=ALU.add))
                            st["acc_prev"][c] = dv
                            devs.append(dv)
                        ptfree[pis[0]] = [oev_, devs[0]]
                        ptfree[pis[1]] = [oev_, devs[1]]
                        return oev_

                    pend_ = dif_S(0)
                    for kb in range(nkb):
                        nxt = dif_S(kb + 1) if kb + 1 < nkb else None
                        oev = dif_AV(kb, pend_)
                        pend_ = nxt
                    PE.wait(st["acc_prev"][0], st["acc_prev"][1], MEMS)
                    PE.op(lambda h: h.matmul(psD0[:, :], lhsT=onesF[:, :], rhs=accD[0][:, :], start=True, stop=True), sig=False)
                    oev = PE.op(lambda h: h.matmul(psD1[:, :], lhsT=onesF[:, :], rhs=accD[1][:, :], start=True, stop=True))
                    st["acc_prev"] = [oev, oev]
                    DVE.wait(oev, LAMEV, ya_last)
                    ACT.wait(oev, ya_last)
                    ACT.op(lambda h: h.activation(out=ft[0][:, :], in_=psD0[:, :], func=AF.Ln), sig=False)
                    a = ACT.op(lambda h: h.activation(out=ft[1][:, :], in_=psD1[:, :], func=AF.Ln))
                    ACT.wait(a)
                    ACT.op(lambda h: h.activation(out=ft[0][:, :], in_=ft[0][:, :], func=AF.Exp, scale=-1.0), sig=False)
                    a = ACT.op(lambda h: h.activation(out=ft[1][:, :], in_=ft[1][:, :], func=AF.Exp, scale=-1.0))
                    DVE.wait(a)
                    a = DVE.op(lambda h: h.tensor_tensor(out=ft[0][:, :], in0=psO0[:, :], in1=ft[0][:, :], op=ALU.mult), sig=False)
                    a = DVE.op(lambda h: h.tensor_tensor(out=ft[1][:, :], in0=psO1[:, :], in1=ft[1][:, :], op=ALU.mult))
                    fin_free = a
                    st["pf"][1] = a
                    DVE.wait(a)
                    a = DVE.op(lambda h: h.scalar_tensor_tensor(out=ft[2][:, :], in0=ft[1][:, :], scalar=neglam, in1=ft[0][:, :], op0=ALU.mult, op1=ALU.add))
                    DVE.wait(a)
                    a = DVE.op(lambda h: h.tensor_tensor(out=ft[3][:, :], in0=ft[2][:, :], in1=ft[2][:, :], op=ALU.mult))
                    PE.wait(a, st["sbf"][0])
                    m = PE.op(lambda h: h.matmul(psS0[:, :], lhsT=onesF[:, :], rhs=ft[3][:, :], start=True, stop=True))
                    ACT.wait(m)
                    a = ACT.op(lambda h: h.activation(out=ft[4][:, :], in_=psS0[:, :], func=AF.Ln, bias=epsc[:, 0:1], scale=1.0 / 128.0))
                    st["sbf"][0] = a
                    sfree[0] = a
                    ACT.wait(a)
                    a = ACT.op(lambda h: h.activation(out=ft[4][:, :], in_=ft[4][:, :], func=AF.Exp, scale=-0.5))
                    DVE.wait(a)
                    a = DVE.op(lambda h, pp=pp, hl=hl, j=j: h.scalar_tensor_tensor(out=yaT[:, 2 * pp + hl, j * 512:(j + 1) * 512], in0=ft[2][:, :], scalar=gsc, in1=ft[4][:, :], op0=ALU.mult, op1=ALU.mult))
                    ya_last = a
                    att_last = a
        YA_DONE = ya_last

        d_dbg = dsem("d_dbg")
        if debug:
            SP.wait(YA_DONE, YB_DONE)
            SP.dma(lambda h: h.dma_start(out=dbg_ya, in_=yaT[:, :, :].rearrange("p h t -> p (h t)")), d_dbg)

        d_pw = dsem("d_pw")
        POOL.wait(YA_DONE, YB_DONE)
        SP.wait(YA_DONE, YB_DONE)
        if debug:
            POOL.wait(d_dbg.ev())
            SP.wait(d_dbg.ev())
        POOL.dma(lambda h: h.dma_start(out=woa[:, :, :], in_=w_oa.rearrange("(k p) c -> p k c", p=128)), d_pw)
        POOL.dma(lambda h: h.dma_start(out=wob[:, :, :], in_=w_ob.rearrange("(k p) c -> p k c", p=64)), d_pw)
        POOL.dma(lambda h: h.dma_start(out=wo[:, :, :], in_=w_o.rearrange("(k p) c -> p k c", p=128)), d_pw)
        SP.dma(lambda h: h.dma_start(out=lnp1[:, :, :], in_=lnp_d[:, 0:2 * D].rearrange("p (a c) -> p a c", a=2)), d_pw)
        SP.dma(lambda h: h.dma_start(out=wrt[:, :, :], in_=w_router.rearrange("(k p) c -> p k c", p=128)), d_pw)
        PW = d_pw.ev()

        h1T = xTp
        d_xf = dsem("d_xf")
        d_h1 = dsem("d_h1")
        d_ybw = dsem("d_ybw")
        d_sc = dsem("d_sc")
        h1b = av(177 * K, [128, D], BF16)
        xf_free = None
        ybw_free = None
        g_last = {"ga": None, "gb": None, "mT": None, "h1Tf": None, "misc": None}

        def layer_norm(buf, lnv, pre_wait):
            DVE.wait(pre_wait)
            DVE.op(lambda h: h.bn_stats(out=stt[:, 0, :], in_=buf[:, 0:512]), sig=False)
            a = DVE.op(lambda h: h.bn_stats(out=stt[:, 1, :], in_=buf[:, 512:1024]))
            DVE.wait(a)
            a = DVE.op(lambda h: h.bn_aggr(out=mv[:, :], in_=stt[:, :, :].rearrange("p a b -> p (a b)")))
            ACT.wait(a)
            a = ACT.op(lambda h: h.activation(out=sm[:, 0:1], in_=mv[:, 1:2], func=AF.Ln, bias=epsc[:, 0:1], scale=1.0))
            ACT.wait(a)
            a = ACT.op(lambda h: h.activation(out=sm[:, 1:2], in_=sm[:, 0:1], func=AF.Exp, scale=-0.5))
            DVE.wait(a)
            a = DVE.op(lambda h: h.tensor_scalar(out=buf[:, :], in0=buf[:, :], scalar1=mv[:, 0:1], scalar2=sm[:, 1:2], op0=ALU.subtract, op1=ALU.mult))
            DVE.wait(a)
            a = DVE.op(lambda h: h.tensor_tensor(out=buf[:, :], in0=buf[:, :], in1=lnv[:, 0, :], op=ALU.mult))
            DVE.wait(a)
            a = DVE.op(lambda h: h.tensor_tensor(out=buf[:, :], in0=buf[:, :], in1=lnv[:, 1, :], op=ALU.add))
            return a

        xf3r = [xf3, av(0, [128, D], F32)]
        h1Tfr = [h1Tf, av(4 * K, [128, 8, 128], F32)]
        d_xfr = [dsem("d_xfr0"), dsem("d_xfr1")]
        d_h1r = [dsem("d_h1r0"), dsem("d_h1r1")]
        xf_fr = [None, None]
        h1Tf_fr = [None, None]
        tr_ev = {}
        stv_ev = {}
        pendB = [None]

        def stageA(tb, tbl, mt_ready):
            s_ = tb % 2
            xf = xf3r[s_]
            hT = h1Tfr[s_]
            SP.wait(xf_fr[s_])
            xev = SP.dma(lambda h: h.dma_start(out=xf[:, :], in_=x_own[tb * 128:(tb + 1) * 128, :]), d_xfr[s_])
            PE.wait(mt_ready, st.get("dfree2"))
            for dh in range(2):
                for fc in range(8):
                    e = PE.op(lambda h, dh=dh, fc=fc: h.matmul(psDd[dh][:, :], lhsT=mT[:, fc, tbl * 128:(tbl + 1) * 128], rhs=wo[:, fc, dh * 512:(dh + 1) * 512], start=(fc == 0), stop=(fc == 7)), sig=(fc == 7 and dh == 1))
            mm_o = e
            if tbl == 3:
                g_last["mT"] = mm_o
            DVE.wait(mm_o, xev)
            DVE.op(lambda h: h.scalar_tensor_tensor(out=xf[:, 0:512], in0=xf[:, 0:512], scalar=ALPHA, in1=psD0[:, :], op0=ALU.mult, op1=ALU.add), sig=False)
            a = DVE.op(lambda h: h.scalar_tensor_tensor(out=xf[:, 512:1024], in0=xf[:, 512:1024], scalar=ALPHA, in1=psD1[:, :], op0=ALU.mult, op1=ALU.add))
            st["dfree2"] = a
            h1ev = layer_norm(xf, lnp1, a)
            SP.wait(h1ev)
            stv_ev[tb] = SP.dma(lambda h: h.dma_start(out=h1d[tb * 128:(tb + 1) * 128, :], in_=xf[:, :]), d_h1r[s_])
            for half in range(2):
                PE.wait(h1ev, st["psP_free"])
                for q_ in range(4):
                    kc = half * 4 + q_
                    e = PE.op(lambda h, kc=kc, q_=q_: h.transpose(out=psP[:, q_ * 128:(q_ + 1) * 128], in_=xf[:, kc * 128:(kc + 1) * 128], identity=identf[:, :]), sig=(q_ == 3))
                ACT.wait(e, h1Tf_fr[s_] if half == 0 else None)
                a = ACT.op(lambda h, half=half: h.activation(out=hT[:, half * 4:(half + 1) * 4, :], in_=psP[:, :].rearrange("p (k t) -> p k t", k=4), func=AF.Copy))
                st["psP_free"] = a
            tr_ev[tb] = (a, e)

        def stageB(tb):
            s_ = tb % 2
            xf = xf3r[s_]
            hT = h1Tfr[s_]
            a, e_tr = tr_ev[tb]
            PE.wait(a, sfree[0])
            for kc in range(8):
                e = PE.op(lambda h, kc=kc: h.matmul(psS0[:, 0:NE], lhsT=hT[:, kc, :], rhs=wrt[:, kc, :], start=(kc == 0), stop=(kc == 7)), sig=(kc == 7))
            h1Tf_fr[s_] = e
            DVE.wait(e)
            a = DVE.op(lambda h: h.tensor_tensor(out=lg[:, :], in0=psS0[:, 0:NE], in1=brt[:, :], op=ALU.add))
            sfree[0] = a
            DVE.wait(a)
            a = DVE.op(lambda h: h.max(out=mx8[:, :], in_=lg[:, :]))
            DVE.wait(a)
            a = DVE.op(lambda h: h.tensor_scalar(out=sm[:, 2:3], in0=mx8[:, 0:1], scalar1=-1.0, scalar2=None, op0=ALU.mult))
            ACT.wait(a)
            ACT.op(lambda h: h.activation(out=sm[:, 4:8], in_=mx8[:, 0:4], func=AF.Exp, bias=sm[:, 2:3], scale=1.0), sig=False)
            a = ACT.op(lambda h: h.activation(out=el[:, :], in_=lg[:, :], func=AF.Exp, bias=sm[:, 2:3], scale=1.0))
            DVE.wait(a)
            a = DVE.op(lambda h: h.reduce_sum(out=sm[:, 3:4], in_=sm[:, 4:8], axis=AX.X))
            DVE.wait(a)
            a = DVE.op(lambda h: h.reciprocal(out=rsm[:, tb:tb + 1], in_=sm[:, 3:4]))
            DVE.wait(a)
            a = DVE.op(lambda h: h.tensor_scalar(out=el[:, :], in0=el[:, :], scalar1=rsm[:, tb:tb + 1], scalar2=None, op0=ALU.mult))
            DVE.wait(a)
            a = DVE.op(lambda h: h.scalar_tensor_tensor(out=gwd[:, tb, :], in0=lg[:, :], scalar=mx8[:, 3:4], in1=el[:, :], op0=ALU.is_ge, op1=ALU.mult))
            a = DVE.op(lambda h: h.tensor_scalar(out=m01[:, tb, :], in0=lg[:, :], scalar1=mx8[:, 3:4], scalar2=None, op0=ALU.is_ge))
            PE.wait(a, sfree[1])
            for tb2 in range(tb + 1):
                e = PE.op(lambda h, tb2=tb2: h.matmul(psS1[:, 0:NE], lhsT=(maskA[:, :] if tb2 == tb else ones_bf[:, :]), rhs=m01[:, tb2, :], start=(tb2 == 0), stop=(tb2 == tb)), sig=(tb2 == tb))
            DVE.wait(e)
            a = DVE.op(lambda h: h.tensor_tensor(out=slotv[:, :], in0=psS1[:, 0:NE], in1=ebase[:, :], op=ALU.add))
            sfree[1] = a
            DVE.wait(a)
            for k_ in range(4):
                DVE.op(lambda h, k_=k_: h.scalar_tensor_tensor(out=rtm[:, :], in0=lg[:, :], scalar=mx8[:, k_:k_ + 1], in1=slotv[:, :], op0=ALU.is_equal, op1=ALU.mult))
                DVE.wait((DVE.sem, DVE.n, DVE.name))
                DVE.op(lambda h, k_=k_: h.reduce_sum(out=destf[:, k_:k_ + 1], in_=rtm[:, :], axis=AX.X))
                DVE.wait((DVE.sem, DVE.n, DVE.name))
            a = DVE.op(lambda h: h.tensor_copy(out=desti[:, tb, :], in_=destf[:, :]))
            DVE.op(lambda h: h.tensor_scalar(out=gw4[:, tb, :], in0=sm[:, 4:8], scalar1=rsm[:, tb:tb + 1], scalar2=None, op0=ALU.mult))
            DVE.wait(st.get("h1b_free"))
            a = DVE.op(lambda h: h.tensor_copy(out=h1b[:, :], in_=xf[:, :]))
            POOL.wait(a, ZFILL)
            for k_ in range(4):
                sc = POOL.dma(lambda h, k_=k_: h.indirect_dma_start(out=Xg[:, :], out_offset=bass.IndirectOffsetOnAxis(ap=desti[:, tb, k_:k_ + 1], axis=0), in_=h1b[:, :], in_offset=None), d_sc)
            st["h1b_free"] = sc
            xf_fr[s_] = [stv_ev[tb], e_tr, a]
            g_last["misc"] = a
            st["route_done"] = a

        PE.wait(PW, YA_DONE, YB_DONE)
        DVE.wait(PW)
        for w in range(4):
            tok = slice(w * 512, (w + 1) * 512)
            SP.wait(ybw_free)
            ybw_ev = SP.dma(lambda h, tok=tok: h.dma_start(out=ybw[:, :, :], in_=ybd.rearrange("p (h t) -> p h t", h=8)[:, :, tok]), d_ybw)
            for cp in range(4):
                sA, evA = load_w(6144 + cp * 256)
                sB, evB = load_w(7168 + cp * 256)
                for fcl in range(2):
                    fc = 2 * cp + fcl
                    PE.wait(evA, sfree[0])
                    for kc in range(8):
                        e = PE.op(lambda h, kc=kc, sA=sA, fcl=fcl, tok=tok: h.matmul(psS0[:, :], lhsT=Wr[sA][:, kc, fcl * 128:(fcl + 1) * 128], rhs=xTo[:, kc, tok], start=(kc == 0), stop=(kc == 7)), sig=(kc == 7))
                    if fcl == 1:
                        wfree[sA] = e
                    ACT.wait(e, g_last["ga"], CONST)
                    ga = ACT.op(lambda h, fc=fc: h.activation(out=ft[0][:, :], in_=psS0[:, :], func=AF.Sigmoid, bias=bgt[:, fc:fc + 1], scale=1.0))
                    sfree[0] = ga
                    PE.wait(evB, sfree[1])
                    for kc in range(8):
                        e = PE.op(lambda h, kc=kc, sB=sB, fcl=fcl, tok=tok: h.matmul(psS1[:, :], lhsT=Wr[sB][:, kc, fcl * 128:(fcl + 1) * 128], rhs=xTo[:, kc, tok], start=(kc == 0), stop=(kc == 7)), sig=(kc == 7))
                    if fcl == 1:
                        wfree[sB] = e
                    ACT.wait(e, g_last["gb"])
                    gb = ACT.op(lambda h, fc=fc: h.activation(out=ft[1][:, :], in_=psS1[:, :], func=AF.Sigmoid, bias=bgt[:, 8 + fc:9 + fc], scale=1.0))
                    sfree[1] = gb
                    PE.wait(ofree[0])
                    for kc in range(4):
                        e = PE.op(lambda h, kc=kc, fc=fc, tok=tok: h.matmul(psO0[:, :], lhsT=woa[:, kc, fc * 128:(fc + 1) * 128], rhs=yaT[:, kc, tok], start=(kc == 0), stop=(kc == 3)), sig=(kc == 3))
                    oa = e
                    PE.wait(ofree[1], ybw_ev)
                    for hh_ in range(8):
                        e = PE.op(lambda h, hh_=hh_, fc=fc: h.matmul(psO1[:, :], lhsT=wob[0:64, hh_, fc * 128:(fc + 1) * 128], rhs=ybw[0:64, hh_, :], start=(hh_ == 0), stop=(hh_ == 7)), sig=(hh_ == 7))
                    obv = e
                    if fc == 7:
                        ybw_free = e
                    DVE.wait(ga, oa, g_last["misc"])
                    a = DVE.op(lambda h: h.tensor_tensor(out=ft[2][:, :], in0=psO0[:, :], in1=ft[0][:, :], op=ALU.mult))
                    ofree[0] = a
                    g_last["ga"] = a
                    DVE.wait(gb, obv)
                    b = DVE.op(lambda h: h.tensor_tensor(out=ft[3][:, :], in0=psO1[:, :], in1=ft[1][:, :], op=ALU.mult))
                    ofree[1] = b
                    g_last["gb"] = b
                    DVE.wait(a, b, g_last["mT"] if fc == 0 else None)
                    c_ = DVE.op(lambda h, fc=fc: h.tensor_tensor(out=mT[:, fc, :], in0=ft[2][:, :], in1=ft[3][:, :], op=ALU.add))
                    g_last["misc"] = c_
            MT_READY = c_
            for tbl in range(4):
                tb = w * 4 + tbl
                stageA(tb, tbl, MT_READY)
                if pendB[0] is not None:
                    stageB(pendB[0])
                pendB[0] = tb
        stageB(pendB[0])
        ROUTE_DONE = st["route_done"]
        xf_free = [xf_fr[0], xf_fr[1]]
        P3_DONE = [ROUTE_DONE, st["psP_free"], xf_free]
        H1D_DONE = [d_h1r[0].ev(), d_h1r[1].ev()]

        Wd = [av(i * 16 * K, [128, 8, D], BF16) for i in range(2)]
        Xe = [av(32 * K + i * 6 * K, [128, 3, D], BF16) for i in range(2)]
        XT = [av(44 * K + i * 6 * K, [128, 8, CAP], BF16) for i in range(2)]
        actT = [av(56 * K + i * 6 * K, [128, 8, CAP], BF16) for i in range(2)]
        Yt = [av(68 * K + i * 4 * K, [128, D], F32) for i in range(2)]
        Wgu = [av(112 * K + i * 32 * K, [128, 8, 2048], BF16) for i in range(2)]
        tg = [av(100 * K + i * 6 * K, [128, CAP], F32) for i in range(2)]
        tsg = [av(102 * K + i * 6 * K, [128, CAP], F32) for i in range(2)]
        tu = [av(104 * K + i * 6 * K, [128, CAP], F32) for i in range(2)]
        d_gu = [dsem(f"d_gu{i}") for i in range(2)]
        d_dn = [dsem(f"d_dn{i}") for i in range(2)]
        d_xe = [dsem("d_xe0"), dsem("d_xe1")]
        d_yg = [dsem("d_yg0"), dsem("d_yg1")]
        gu_free = [None] * 2
        dn_free = [None] * 2
        xe_free = [None, None]
        yt_free = [None, None]
        items = [(e, q) for e in range(NE) for q in range(4)]
        gu_ev = {}
        dn_ev = {}
        xe_ev = {}
        xt_ready = {}

        def issue_gu(e):
            s = e % 2
            POOL.wait(gu_free[s])
            gu_ev[e] = POOL.dma(lambda h, s=s, e=e: h.dma_start(out=Wgu[s][:, :, :], in_=w_gu[e].rearrange("(k p) c -> p k c", p=128)), d_gu[s])

        def issue_dn(e):
            s = e % 2
            POOL.wait(dn_free[s])
            dn_ev[e] = POOL.dma(lambda h, s=s, e=e: h.dma_start(out=Wd[s][:, :, :], in_=w_down[e].rearrange("(k p) c -> p k c", p=128)), d_dn[s])

        def issue_xe(e):
            s = e % 2
            SP.wait(xe_free[s])
            xe_ev[e] = SP.dma(lambda h, s=s, e=e: h.dma_start(out=Xe[s][:, :, :], in_=Xg[e * CAP:(e + 1) * CAP, :].rearrange("(n p) d -> p n d", p=128)), d_xe[s])

        def do_transposes(e):
            s = e % 2
            for n in range(3):
                PE.wait(xe_ev[e], st["psT_free"])
                for kc in range(8):
                    ev_ = PE.op(lambda h, kc=kc, n=n, s=s: h.transpose(out=psTb[:, kc * 128:(kc + 1) * 128], in_=Xe[s][:, n, kc * 128:(kc + 1) * 128], identity=identb[:, :]), sig=(kc == 7))
                ACT.wait(ev_)
                st["psT_free"] = ACT.op(lambda h, n=n, s=s: h.activation(out=XT[s][:, :, n * 128:(n + 1) * 128], in_=psTb[:, :].rearrange("p (k t) -> p k t", k=8), func=AF.Copy))
            xe_free[s] = ev_
            xt_ready[e] = st["psT_free"]

        SCAT_DONE = d_sc.ev()
        POOL.wait(P3_DONE, H1D_DONE)
        PE.wait(P3_DONE)
        DVE.wait(P3_DONE, H1D_DONE)
        ACT.wait(P3_DONE, H1D_DONE)
        SP.wait(P3_DONE, SCAT_DONE)
        issue_gu(0)
        issue_dn(0)
        issue_xe(0)
        issue_xe(1)
        do_transposes(0)
        banks = [(psS0, psS1), (psD0, psD1)]
        bfree = [[sfree[0], sfree[1]], [st.get("dfree2"), st.get("dfree2")]]
        obk = [psO0, psO1, psP]
        obkfree = [ofree[0], ofree[1], st["psP_free"]]
        tfree = [None, None]
        stepc = 0
        ocnt = 0
        act_last = None
        for e in range(NE):
            xs = e % 2
            if e + 1 < NE:
                issue_gu(e + 1)
                issue_dn(e + 1)
            s = e % 2
            for q in range(4):
                i = e
                for jj in range(2):
                    j = 2 * q + jj
                    b = stepc % 2
                    stepc += 1
                    Pg, Pu = banks[b]
                    PE.wait(gu_ev[i], xt_ready[e], bfree[b][0], bfree[b][1])
                    for kc in range(8):
                        eg = PE.op(lambda h, kc=kc, Pg=Pg, s=s, j=j, xs=xs: h.matmul(Pg[:, 0:CAP], lhsT=Wgu[s][:, kc, j * 128:(j + 1) * 128], rhs=XT[xs][:, kc, :], start=(kc == 0), stop=(kc == 7)), sig=(kc == 7))
                    for kc in range(8):
                        eu = PE.op(lambda h, kc=kc, Pu=Pu, s=s, j=j, xs=xs: h.matmul(Pu[:, 0:CAP], lhsT=Wgu[s][:, kc, 1024 + j * 128:1024 + (j + 1) * 128], rhs=XT[xs][:, kc, :], start=(kc == 0), stop=(kc == 7)), sig=(kc == 7))
                    if j == 7:
                        gu_free[s] = eu
                    DVE.wait(eg, tfree[b])
                    a1 = DVE.op(lambda h, Pg=Pg, e=e, j=j, b=b: h.tensor_scalar(out=tg[b][:, :], in0=Pg[:, 0:CAP], scalar1=bgu[:, e * 16 + j:e * 16 + j + 1], scalar2=7.0, op0=ALU.add, op1=ALU.min))
                    bfree[b][0] = a1
                    ACT.wait(a1, eu)
                    ACT.op(lambda h, b=b: h.activation(out=tsg[b][:, :], in_=tg[b][:, :], func=AF.Sigmoid, scale=1.702), sig=False)
                    a2 = ACT.op(lambda h, Pu=Pu, e=e, j=j, b=b: h.activation(out=tu[b][:, :], in_=Pu[:, 0:CAP], func=AF.Identity, bias=bgu[:, e * 16 + 8 + j:e * 16 + 9 + j], scale=1.0))
                    bfree[b][1] = a2
                    DVE.wait(a2)
                    a3 = DVE.op(lambda h, b=b: h.tensor_scalar(out=tu[b][:, :], in0=tu[b][:, :], scalar1=7.0, scalar2=-7.0, op0=ALU.min, op1=ALU.max))
                    DVE.wait(a3)
                    a4 = DVE.op(lambda h, b=b: h.scalar_tensor_tensor(out=tu[b][:, :], in0=tu[b][:, :], scalar=1.0, in1=tg[b][:, :], op0=ALU.add, op1=ALU.mult))
                    DVE.wait(a4)
                    a5 = DVE.op(lambda h, j=j, b=b, xs=xs: h.tensor_tensor(out=actT[xs][:, j, :], in0=tu[b][:, :], in1=tsg[b][:, :], op=ALU.mult))
                    tfree[b] = a5
                    act_last = a5
            if e + 1 < NE:
                do_transposes(e + 1)
            if e + 2 < NE:
                issue_xe(e + 2)
            ds_ = e % 2
            PE.wait(act_last, dn_ev[e])
            for nb_ in range(3):
                ys = (e * 3 + nb_) % 2
                for dh in range(2):
                    oi = ocnt % 3
                    ocnt += 1
                    O = obk[oi]
                    PE.wait(obkfree[oi])
                    for fc in range(8):
                        em = PE.op(lambda h, O=O, fc=fc, nb_=nb_, dh=dh, ds_=ds_, xs=xs: h.matmul(O[:, :], lhsT=actT[xs][:, fc, nb_ * 128:(nb_ + 1) * 128], rhs=Wd[ds_][:, fc, dh * 512:(dh + 1) * 512], start=(fc == 0), stop=(fc == 7)), sig=(fc == 7))
                    ACT.wait(em, yt_free[ys] if dh == 0 else None)
                    ac = ACT.op(lambda h, O=O, ys=ys, dh=dh: h.activation(out=Yt[ys][:, dh * 512:(dh + 1) * 512], in_=O[:, :], func=AF.Copy))
                    obkfree[oi] = ac
                SP.wait(ac)
                yt_free[ys] = SP.dma(lambda h, ys=ys, e=e, nb_=nb_: h.dma_start(out=Yg[e * CAP + nb_ * 128:e * CAP + (nb_ + 1) * 128, :], in_=Yt[ys][:, :]), d_yg[ys])
            dn_free[ds_] = em
        MOE_DONE = [ac, em, d_yg[0].ev(), d_yg[1].ev()]

        G = [[av((s_ * 4 + k_) * 4 * K, [128, D], F32) for k_ in range(4)] for s_ in range(2)]
        d_hl = [dsem("d_hl0"), dsem("d_hl1")]
        d_out = [dsem("d_out0"), dsem("d_out1")]
        d_g = [dsem("d_g0"), dsem("d_g1")]
        d_l2 = dsem("d_l2")
        buf_free = [None, None]
        g_free = [None, None]
        gT_free = None
        SP.wait(H1D_DONE, MOE_DONE)
        POOL.wait(MOE_DONE)
        PE.wait(MOE_DONE)
        DVE.wait(MOE_DONE)
        l2ev = SP.dma(lambda h: h.dma_start(out=lnp2[:, :, :], in_=lnp_d[:, 2 * D:4 * D].rearrange("p (a c) -> p a c", a=2)), d_l2)
        o5free = [obkfree[0], obkfree[1]]

        def issue_gather(tb):
            s = tb % 2
            POOL.wait(g_free[s])
            for k_ in range(4):
                gev_ = POOL.dma(lambda h, s=s, tb=tb, k_=k_: h.indirect_dma_start(out=G[s][k_][:, :], out_offset=None, in_=Yg[:, :], in_offset=bass.IndirectOffsetOnAxis(ap=desti[:, tb, k_:k_ + 1], axis=0)), d_g[s])
            return gev_

        gq = {0: issue_gather(0)}
        for tb in range(16):
            s = tb % 2
            if tb + 1 < 16:
                gq[tb + 1] = issue_gather(tb + 1)
            SP.wait(buf_free[s])
            hev = SP.dma(lambda h, s=s, tb=tb: h.dma_start(out=xf5[s][:, :], in_=h1d[tb * 128:(tb + 1) * 128, :]), d_hl[s])
            PE.wait(bfree[0][0], bfree[0][1], gT_free)
            e = PE.op(lambda h, tb=tb: h.transpose(out=psS0[0:NE, 0:128], in_=gwd[:, tb, :], identity=identf[:, :]))
            ACT.wait(e, gT_free)
            a = ACT.op(lambda h: h.activation(out=gT[:, :], in_=psS0[0:NE, 0:128], func=AF.Copy))
            bfree[0][0] = a
            PE.wait(a, o5free[0], o5free[1])
            for dh in range(2):
                e = PE.op(lambda h, dh=dh: h.matmul([psO0, psO1][dh][:, :], lhsT=gT[:, :], rhs=bdn[:, dh * 512:(dh + 1) * 512], start=True, stop=True))
            gT_free = e
            G0 = G[s][0]
            DVE.wait(gq[tb])
            a = DVE.op(lambda h, G0=G0, tb=tb: h.tensor_scalar(out=G0[:, :], in0=G0[:, :], scalar1=gw4[:, tb, 0:1], scalar2=None, op0=ALU.mult))
            for k_ in range(1, 4):
                DVE.wait(a)
                a = DVE.op(lambda h, G0=G0, s=s, k_=k_, tb=tb: h.scalar_tensor_tensor(out=G0[:, :], in0=G[s][k_][:, :], scalar=gw4[:, tb, k_:k_ + 1], in1=G0[:, :], op0=ALU.mult, op1=ALU.add))
            DVE.wait(a, e)
            DVE.op(lambda h, G0=G0: h.tensor_tensor(out=G0[:, 0:512], in0=G0[:, 0:512], in1=psO0[:, :], op=ALU.add), sig=False)
            a = DVE.op(lambda h, G0=G0: h.tensor_tensor(out=G0[:, 512:1024], in0=G0[:, 512:1024], in1=psO1[:, :], op=ALU.add))
            o5free = [a, a]
            DVE.wait(a, hev)
            a = DVE.op(lambda h, s=s, G0=G0: h.scalar_tensor_tensor(out=xf5[s][:, :], in0=xf5[s][:, :], scalar=ALPHA, in1=G0[:, :], op0=ALU.mult, op1=ALU.add))
            g_free[s] = a
            DVE.wait(l2ev)
            oev = layer_norm(xf5[s], lnp2, a)
            SP.wait(oev)
            buf_free[s] = SP.dma(lambda h, s=s, tb=tb: h.dma_start(out=out_d[tb * 128:(tb + 1) * 128, :], in_=xf5[s][:, :]), d_out[s])
        SP.wait(d_out[0].ev(), d_out[1].ev())
        if debug:
            SP.wait(d_dbg.ev())

        with nc.Block() as block:
            @block.tensor
            def _(h):
                for f in PE.ops:
                    f(h)

            @block.scalar
            def _(h):
                for f in ACT.ops:
                    f(h)

            @block.vector
            def _(h):
                for f in DVE.ops:
                    f(h)

            @block.gpsimd
            def _(h):
                for f in POOL.ops:
                    f(h)

            @block.sync
            def _(h):
                for f in SP.ops:
                    f(h)
    return nc


def rope_tables_np(positions):
    inv = 500000.0 ** (-np.arange(0, 16, 2, dtype=np.float32) / 16.0)
    ang = positions.astype(np.float32)[:, None] * inv[None, :]
    return np.cos(ang).astype(np.float32), np.sin(ang).astype(np.float32)


def make_consts(hf):
    TIDX, NTAB = tab_index()
    tab = np.zeros((128, NTAB, 16), np.float32)
    p = np.arange(128)
    pos0 = hf * 2048

    def put(slot, positions):
        c, s = rope_tables_np(positions)
        tab[:, slot, 0:8] = c
        tab[:, slot, 8:16] = s

    for b in range(32):
        if b < 16:
            put(TIDX[("n", 0, b)], b * 128 + p)
        else:
            put(TIDX[("n", 0, b)], pos0 + (b - 16) * 128 + p)
    for g, d in enumerate(DILS):
        nb = 16 // d
        for ob in range(16):
            r, n = ob // nb, ob % nb
            put(TIDX[("o", g, ob)], pos0 + (n * 128 + p) * d + r)
        for pb in range(d):
            put(TIDX[("p", g, pb)], 2048 - 128 * d + p * d + pb)
    k = np.arange(128)[:, None]
    q = np.arange(512)[None, :]
    cm = np.stack([(128 * v + k > q) for v in range(4)], axis=1).astype(np.float32)
    mA = (np.arange(128)[:, None] < np.arange(128)[None, :]).astype(np.float32)
    bf = ml_dtypes.bfloat16
    return {
        "rope": tab.reshape(128, NTAB * 16),
        "prevbias": np.full((128, 1), 0.0 if hf == 1 else NEG, np.float32),
        "cmask": cm.reshape(128, 4 * 512).astype(bf),
        "maskA": mA.astype(bf),
        "negI": (NEG * np.eye(128, dtype=np.float32)).astype(bf),
        "identb": np.eye(128, dtype=np.float32).astype(bf),
        "identf": np.eye(128, dtype=np.float32),
    }


_NC_CACHE = {}


def kernel(x, w_in, b_gate, lam_q1, lam_k1, lam_q2, lam_k2, subln_g, w_oa, w_ob, w_o,
           ln1_g, ln1_b, w_router, b_router, w_gu, b_gu, w_down, b_down, ln2_g, ln2_b, _debug=False):
    f32 = np.float32
    x = np.asarray(x, f32)
    key = bool(_debug)
    if key not in _NC_CACHE:
        _NC_CACHE[key] = build_nc(debug=_debug)
    nc = _NC_CACHE[key]
    c = lambda a: np.ascontiguousarray(np.asarray(a, f32))
    shared = {
        "w_in": c(w_in[0]), "w_oa": c(w_oa[0]), "w_ob": c(w_ob[0]), "w_o": c(w_o[0]),
        "w_router": c(w_router[0]), "w_gu": c(w_gu[0]), "w_down": c(w_down[0]),
        "lamv": c(np.broadcast_to(np.stack([lam_q1[0], lam_k1[0], lam_q2[0], lam_k2[0]])[None], (128, 4, 64)).reshape(128, 256)),
        "subg": c(np.asarray(subln_g[0]).reshape(128, 1)),
        "bgt": c(np.asarray(b_gate[0]).reshape(16, 128).T),
        "bgu_t": c(np.asarray(b_gu[0]).reshape(NE, 16, 128).transpose(2, 0, 1).reshape(128, NE * 16)),
        "brt": c(np.broadcast_to(np.asarray(b_router[0])[None], (128, NE))),
        "bdn": c(b_down[0]),
        "ebase": c(np.broadcast_to((np.arange(NE, dtype=np.float32) * CAP)[None], (128, NE))),
        "lnp": c(np.broadcast_to(np.stack([ln1_g[0], ln1_b[0], ln2_g[0], ln2_b[0]])[None], (128, 4, D)).reshape(128, 4 * D)),
    }
    consts = [make_consts(0), make_consts(1)]
    in_maps = []
    for core in range(8):
        b, hf = core // 2, core % 2
        m = dict(shared)
        m.update(consts[hf])
        m["x_own"] = c(x[b, hf * 2048:(hf + 1) * 2048])
        m["x_prev"] = c(x[b, 0:2048])
        in_maps.append(m)
    res = run_bass_kernel_spmd(nc, in_maps, core_ids=list(range(8)))
    out = np.empty((4, 4096, D), f32)
    for core in range(8):
        b, hf = core // 2, core % 2
        out[b, hf * 2048:(hf + 1) * 2048] = res.results[core]["out"]
    if _debug:
        return out, res.results
    return out
```

```python
import contextlib
import numpy as np
import ml_dtypes
import concourse.bass as bass
import concourse.mybir as mybir
from concourse.bass_utils import run_bass_kernel_spmd

F32 = mybir.dt.float32
BF16 = mybir.dt.bfloat16
ALU = mybir.AluOpType
AF = mybir.ActivationFunctionType
AX = mybir.AxisListType

S_OWN = 2048
D = 1024
NE = 32
ALPHA = 2.0 ** 0.25
EPS = 1e-5
LAMBDA_INIT = 0.2
NEG = -30000.0
DILS = (1, 4, 16)
SAME_ENGINE_WAITS = True
CAP = 384
I32 = mybir.dt.int32


def tab_index():
    idx = {}
    n = 0
    for b in range(32):
        idx[("n", 0, b)] = n; n += 1
    for g, d in enumerate(DILS):
        for ob in range(16):
            idx[("o", g, ob)] = n; n += 1
        for pb in range(d):
            idx[("p", g, pb)] = n; n += 1
    return idx, n


class Eng:
    def __init__(self, name, sem):
        self.name, self.sem, self.n, self.ops, self.seen = name, sem, 0, [], {}

    def wait(self, *evs):
        for ev in evs:
            if ev is None:
                continue
            if isinstance(ev, list):
                self.wait(*ev)
                continue
            sem, val, key = ev
            if key == self.name and not SAME_ENGINE_WAITS:
                continue
            if self.seen.get(key, 0) >= val:
                continue
            self.seen[key] = val
            self.ops.append(lambda h, sem=sem, val=val: h.wait_ge(sem, val))

    def op(self, fn, sig=True):
        if sig:
            self.n += 1
            self.ops.append(lambda h, fn=fn, sem=self.sem: fn(h).then_inc(sem, 1))
            return (self.sem, self.n, self.name)
        self.ops.append(lambda h, fn=fn: fn(h))
        return None

    def dma(self, fn, ds):
        ds.n += 16
        self.ops.append(lambda h, fn=fn, sem=ds.sem: fn(h).then_inc(sem, 16))
        return (ds.sem, ds.n, ds.name)


class DSem:
    def __init__(self, name, sem):
        self.name, self.sem, self.n = name, sem, 0

    def ev(self):
        return (self.sem, self.n, self.name)


def build_nc(debug=False):
    nc = bass.Bass("TRN2", target_bir_lowering=False)
    TIDX, NTAB = tab_index()

    def din(name, shape, dt=F32):
        return nc.dram_tensor(name, list(shape), dt, kind="ExternalInput").ap()

    x_own = din("x_own", [S_OWN, D])
    x_prev = din("x_prev", [S_OWN, D])
    w_in = din("w_in", [D, 8192])
    w_oa = din("w_oa", [512, D])
    w_ob = din("w_ob", [512, D])
    w_o = din("w_o", [D, D])
    w_router = din("w_router", [D, NE])
    w_gu = din("w_gu", [NE, D, 2048])
    w_down = din("w_down", [NE, D, D])
    rope = din("rope", [128, NTAB * 16])
    prevbias_d = din("prevbias", [128, 1])
    cmask_d = din("cmask", [128, 4 * 512], BF16)
    maskA_d = din("maskA", [128, 128], BF16)
    negI_d = din("negI", [128, 128], BF16)
    identb_d = din("identb", [128, 128], BF16)
    identf_d = din("identf", [128, 128])
    lamv_d = din("lamv", [128, 4 * 64])
    subg_d = din("subg", [128, 1])
    bgt_d = din("bgt", [128, 16])
    bgu_d = din("bgu_t", [128, NE * 16])
    brt_d = din("brt", [128, NE])
    bdn_d = din("bdn", [NE, D])
    lnp_d = din("lnp", [128, 4 * D])
    ebase_d = din("ebase", [128, NE])
    Xg = nc.dram_tensor("Xg", [NE * CAP, D], BF16, kind="Internal").ap()
    Yg = nc.dram_tensor("Yg", [NE * CAP, D], F32, kind="Internal").ap()
    out_d = nc.dram_tensor("out", [S_OWN, D], F32, kind="ExternalOutput").ap()
    h1d = nc.dram_tensor("h1d", [S_OWN, D], F32, kind="ExternalOutput" if debug else "Internal").ap()
    if debug:
        dbg_ya = nc.dram_tensor("dbg_ya", [128, 4 * 2048], BF16, kind="ExternalOutput").ap()
    ybd = nc.dram_tensor("ybd", [64, 8 * 2048], BF16, kind="ExternalOutput" if debug else "Internal").ap()

    es = contextlib.ExitStack()
    with es:
        def sb(name, shape, dt):
            return es.enter_context(nc.sbuf_tensor("sb_" + name, list(shape), dt))

        def pst(name):
            return es.enter_context(nc.psum_tensor(name, [128, 512], F32))

        _semc = [0]

        def newsem(name):
            _semc[0] += 1
            return es.enter_context(nc.semaphore(name))

        PE = Eng("pe", newsem("s_pe"))
        ACT = Eng("act", newsem("s_act"))
        DVE = Eng("dve", newsem("s_dve"))
        POOL = Eng("pool", newsem("s_pool"))
        SP = Eng("sp", newsem("s_sp"))

        def dsem(name):
            return DSem(name, newsem(name))

        K = 1024
        ARENA = sb("arena", [128, 92 * K], BF16)

        def av(off, shape, dt, parts=128):
            esz = 4 if dt == F32 else 2
            n = 1
            for d_ in shape[1:]:
                n *= d_
            a = ARENA[0:parts, off // 2: off // 2 + n * esz // 2]
            if dt == F32:
                a = a.bitcast(F32)
            if len(shape) == 3:
                a = a.rearrange("p (a b) -> p a b", a=shape[1])
            elif len(shape) == 4:
                a = a.rearrange("p (a b c) -> p a b c", a=shape[1], b=shape[2])
            return a

        xTp = av(0, [128, 8, 2048], BF16)
        xTo = av(32 * K, [128, 8, 2048], BF16)
        ybH = av(64 * K, [64, 4, 2048], BF16, parts=64)
        acc = av(80 * K, [128, 4, 2048], F32)
        yaT = av(80 * K, [128, 4, 2048], BF16)
        ft = [av(96 * K + i * 2 * K, [128, 512], F32) for i in range(5)]
        xld = [av(100 * K + i * 2 * K, [128, 1024], BF16) for i in range(2)]
        QT = av(112 * K, [128, 2, 2048], BF16)
        KT = av(120 * K, [128, 2, 4096], BF16)
        VVf = av(136 * K, [128, 32 * 260], BF16)
        Wr = [av(153 * K + i * 4 * K, [128, 8, 256], BF16) for i in range(3)]
        Tt = [av(165 * K + i * 512, [128, 256], BF16) for i in range(2)]
        rtmp = [av(166 * K + i * 512, [128, 4, 32], F32) for i in range(2)]
        ropet = av(167 * K, [128, NTAB, 16], F32)
        cmask = av(167 * K + 6656, [128, 4, 512], BF16)
        PT = [av(167 * K + 6656 + 4 * K + i * K, [128, 512], BF16) for i in range(4)]
        PT2 = [av(167 * K + 6656 + 4 * K + i * 2 * K, [128, 1024], BF16) for i in range(2)]
        lamv = av(167 * K + 6656 + 8 * K, [128, 4, 64], F32)
        maskA = av(167 * K + 6656 + 9 * K, [128, 128], BF16)
        negI = av(167 * K + 6656 + 9 * K + 256, [128, 128], BF16)
        ybw = av(64 * K, [64, 8, 512], BF16, parts=64)
        mT = av(72 * K, [128, 8, 512], BF16)
        h1Tf = av(106 * K, [128, 8, 128], F32)
        wrt = av(110 * K, [128, 8, NE], F32)
        woa = av(112 * K, [128, 4, D], BF16)
        wob = av(120 * K, [64, 8, D], BF16, parts=64)
        wo = av(136 * K, [128, 8, D], BF16)
        lnp1 = av(165 * K, [128, 2, D], F32)
        xf3 = av(173 * K, [128, D], F32)
        xf5 = [av(128 * K + i * 4 * K, [128, D], F32) for i in range(2)]
        lnp2 = av(136 * K, [128, 2, D], F32)

        identb = sb("identb", [128, 128], BF16)
        identf = sb("identf", [128, 128], F32)
        ones_bf = sb("ones_bf", [128, 128], BF16)
        onesF = sb("onesF", [128, 128], F32)
        prevbias = sb("prevbias", [128, 1], F32)
        lamt = sb("lamt", [128, 2, 64], F32)
        lams = sb("lams", [128, 4], F32)
        subg = sb("subg", [128, 1], F32)
        epsc = sb("epsc", [128, 1], F32)
        bgt = sb("bgt", [128, 16], F32)
        bgu = sb("bgu", [128, NE * 16], F32)
        brt = sb("brt", [128, NE], F32)
        bdn = sb("bdn", [NE, D], F32)
        gwd = sb("gwd", [128, 16, NE], F32)
        rsm = sb("rsm", [128, 16], F32)
        ftA = sb("ftA", [128, 512], F32)
        stt = sb("stt", [128, 2, 6], F32)
        mv = sb("mv", [128, 2], F32)
        sm = sb("sm", [128, 8], F32)
        lg = sb("lg", [128, NE], F32)
        mx8 = sb("mx8", [128, 8], F32)
        el = sb("el", [128, NE], F32)
        gT = sb("gT", [NE, 128], F32)
        ebase = sb("ebase", [128, NE], F32)
        m01 = sb("m01", [128, 16, NE], BF16)
        slotv = sb("slotv", [128, NE], F32)
        rtm = sb("rtm", [128, NE], F32)
        destf = sb("destf", [128, 4], F32)
        desti = sb("desti", [128, 16, 4], I32)
        gw4 = sb("gw4", [128, 16, 4], F32)

        pairA = es.enter_context(nc.psum_tensor("psA", [128, 1024], F32))
        pairB = es.enter_context(nc.psum_tensor("psB", [128, 1024], F32))
        psS0, psS1 = pairA[:, 0:512], pairA[:, 512:1024]
        psP, psT = pairB[:, 0:512], pairB[:, 512:1024]
        psO0, psO1, psD0, psD1 = [pst(f"ps{i}") for i in range(4)]
        psTb = psT[:, :].bitcast(BF16)

        d_const = dsem("d_const")
        for (dst, src) in [
            (ropet[:, :, :], rope.rearrange("p (n c) -> p n c", c=16)),
            (prevbias[:, :], prevbias_d), (cmask[:, :, :], cmask_d.rearrange("p (v q) -> p v q", v=4)),
            (maskA[:, :], maskA_d), (negI[:, :], negI_d), (identb[:, :], identb_d), (identf[:, :], identf_d),
            (lamv[:, :, :], lamv_d.rearrange("p (a b) -> p a b", a=4)), (subg[:, :], subg_d),
            (bgt[:, :], bgt_d), (bgu[:, :], bgu_d), (brt[:, :], brt_d), (bdn[:, :], bdn_d), (ebase[:, :], ebase_d),
        ]:
            SP.dma(lambda h, dst=dst, src=src: h.dma_start(out=dst, in_=src), d_const)
        CONST = d_const.ev()
        e1 = POOL.op(lambda h: h.memset(ones_bf[:, :], 1.0))
        e2 = POOL.op(lambda h: h.memset(onesF[:, :], 1.0))
        e3 = POOL.op(lambda h: h.memset(epsc[:, :], EPS))
        e4 = POOL.op(lambda h: h.memset(VVf[:, :], 1.0))
        MEMS = [e1, e2, e3, e4]
        zt = av(112 * K, [128, 8192], BF16)
        ez = POOL.op(lambda h: h.memset(zt[:, :], 0.0))
        d_z = dsem("d_z")
        SP.wait(ez)
        xg_flat = Xg.rearrange("(p n) d -> p (n d)", p=128)
        for i_ in range(NE * CAP * D // 128 // 8192):
            SP.dma(lambda h, i_=i_: h.dma_start(out=xg_flat[:, i_ * 8192:(i_ + 1) * 8192], in_=zt[:, :]), d_z)
        ZFILL = d_z.ev()
        DVE.wait(CONST)
        ev = DVE.op(lambda h: h.tensor_tensor(out=lamt[:, :, :], in0=lamv[:, 0:4:2, :], in1=lamv[:, 1:4:2, :], op=ALU.mult))
        DVE.wait(ev)
        ev = DVE.op(lambda h: h.reduce_sum(out=lams[:, 0:2], in_=lamt[:, :, :], axis=AX.X))
        ACT.wait(ev)
        ev = ACT.op(lambda h: h.activation(out=lams[:, 2:4], in_=lams[:, 0:2], func=AF.Exp))
        DVE.wait(ev)
        ev = DVE.op(lambda h: h.tensor_tensor(out=lams[:, 0:1], in0=lams[:, 3:4], in1=lams[:, 2:3], op=ALU.subtract))
        DVE.wait(ev)
        ev = DVE.op(lambda h: h.tensor_scalar(out=lams[:, 0:1], in0=lams[:, 0:1], scalar1=-LAMBDA_INIT, scalar2=None, op0=ALU.add))
        DVE.wait(ev)
        ev = DVE.op(lambda h: h.tensor_scalar(out=lams[:, 1:2], in0=subg[:, :], scalar1=1.0 - LAMBDA_INIT, scalar2=None, op0=ALU.mult))
        LAMEV = ev
        neglam = lams[:, 0:1]
        gsc = lams[:, 1:2]

        d_x = [dsem("d_x0"), dsem("d_x1")]
        xfree = [None, None]
        psT_free = None
        PE.wait(CONST)
        for blk in range(32):
            s = blk % 2
            src = (x_prev if blk < 16 else x_own)[(blk % 16) * 128:(blk % 16 + 1) * 128, :]
            POOL.wait(xfree[s])
            ld = POOL.dma(lambda h, s=s, src=src: h.dma_start(out=xld[s][:, :], in_=src), d_x[s])
            PE.wait(ld, psT_free)
            for kc in range(8):
                ev = PE.op(lambda h, s=s, kc=kc: h.transpose(out=psTb[:, kc * 128:(kc + 1) * 128], in_=xld[s][:, kc * 128:(kc + 1) * 128], identity=identb[:, :]), sig=(kc == 7))
            xfree[s] = ev
            ACT.wait(ev)
            dst = (xTp if blk < 16 else xTo)[:, :, (blk % 16) * 128:(blk % 16 + 1) * 128]
            psT_free = ACT.op(lambda h, dst=dst: h.activation(out=dst, in_=psTb[:, :].rearrange("p (k t) -> p k t", k=8), func=AF.Copy))
        XT_DONE = psT_free

        d_w = [dsem(f"d_w{i}") for i in range(3)]
        wfree = [None, None, None]
        wcnt = [0]

        def load_w(col0):
            s = wcnt[0] % 3
            wcnt[0] += 1
            POOL.wait(wfree[s])
            ev = POOL.dma(lambda h, s=s, col0=col0: h.dma_start(out=Wr[s][:, :, :], in_=w_in[:, col0:col0 + 256].rearrange("(k p) c -> p k c", p=128)), d_w[s])
            return s, ev

        st = {"psP_free": None, "psT_free": XT_DONE, "tcnt": 0, "Tfree": [None, None], "rfree": [None, None], "pend": None,
              "pf": [None, None], "pcnt": 0}
        pbanks = [psP, psD1]

        def proj_mm(xap_fn, ws, wev):
            pb = st["pcnt"] % 2
            st["pcnt"] += 1
            Pb = pbanks[pb]
            PE.wait(wev, st["pf"][pb], st["psP_free"] if pb == 0 else None, XT_DONE)
            for kc in range(8):
                ev = PE.op(lambda h, kc=kc, Pb=Pb: h.matmul(Pb[:, 0:256], lhsT=xap_fn(kc), rhs=Wr[ws][:, kc, :], start=(kc == 0), stop=(kc == 7)), sig=(kc == 7))
            wfree[ws] = ev
            return ev, pb

        def flush_pend():
            if st["pend"] is None:
                return
            ti, tev, dst = st["pend"]
            st["pend"] = None
            PE.wait(tev, st["psT_free"])
            for hh_ in range(2):
                ev = PE.op(lambda h, hh_=hh_, ti=ti: h.transpose(out=psTb[:, hh_ * 128:(hh_ + 1) * 128], in_=Tt[ti][:, hh_ * 128:(hh_ + 1) * 128], identity=identb[:, :]), sig=(hh_ == 1))
            st["Tfree"][ti] = ev
            ACT.wait(ev)
            st["psT_free"] = ACT.op(lambda h, dst=dst: h.activation(out=dst, in_=psTb[:, 0:256].rearrange("p (a t) -> p a t", a=2), func=AF.Copy))
            st["last_qk"] = st["psT_free"]

        def qk_tile(xap_fn, ws, wev, tab, dst):
            mmev, pb = proj_mm(xap_fn, ws, wev)
            flush_pend()
            ti = st["tcnt"] % 2
            st["tcnt"] += 1
            p3 = pbanks[pb][:, 0:256].rearrange("p (a c) -> p a c", a=4)
            t3 = Tt[ti][:, :].rearrange("p (a c) -> p a c", a=4)
            cos = ropet[:, tab, 0:8].unsqueeze(1).broadcast_to([128, 4, 8])
            sin = ropet[:, tab, 8:16].unsqueeze(1).broadcast_to([128, 4, 8])
            rt = rtmp[ti]
            DVE.wait(mmev, st["Tfree"][ti], CONST)
            a = DVE.op(lambda h: h.tensor_tensor(out=rt[:, :, 0:8], in0=p3[:, :, 0:8], in1=cos, op=ALU.mult), sig=False)
            a = DVE.op(lambda h: h.tensor_tensor(out=rt[:, :, 8:16], in0=p3[:, :, 8:16], in1=cos, op=ALU.mult), sig=False)
            a = DVE.op(lambda h: h.tensor_tensor(out=rt[:, :, 16:24], in0=p3[:, :, 8:16], in1=sin, op=ALU.mult), sig=False)
            a = DVE.op(lambda h: h.tensor_tensor(out=rt[:, :, 24:32], in0=p3[:, :, 0:8], in1=sin, op=ALU.mult), sig=False)
            a = DVE.op(lambda h: h.tensor_copy(out=t3[:, :, 16:64], in_=p3[:, :, 16:64]))
            st["pf"][pb] = a
            if pb == 0:
                st["psP_free"] = a
            DVE.wait(a)
            a = DVE.op(lambda h: h.tensor_tensor(out=t3[:, :, 0:8], in0=rt[:, :, 0:8], in1=rt[:, :, 16:24], op=ALU.subtract), sig=False)
            a = DVE.op(lambda h: h.tensor_tensor(out=t3[:, :, 8:16], in0=rt[:, :, 8:16], in1=rt[:, :, 24:32], op=ALU.add))
            st["pend"] = (ti, a, dst)

        def v_tile(xap_fn, ws, wev, dst, srcf):
            mmev, pb = proj_mm(xap_fn, ws, wev)
            ACT.wait(mmev, MEMS)
            a = ACT.op(lambda h: h.activation(out=dst, in_=srcf(pbanks[pb]), func=AF.Copy))
            st["pf"][pb] = a
            if pb == 0:
                st["psP_free"] = a
            st["last_v"] = a

        def xnat(blk):
            t = xTp if blk < 16 else xTo
            b = blk % 16
            return lambda kc: t[:, kc, b * 128:(b + 1) * 128]

        Vaug = VVf[:, :].rearrange("p (b h c) -> p b h c", b=32, h=4)
        bufc = [0]
        d_yb = dsem("d_yb")
        sfree = [None, None]
        ptfree = [None] * 4
        ofree = [None, None]
        ptc = [0]
        psSb = [psS0, psS1]
        psOb = [psO0, psO1]
        acc_last = [None]

        for hh in range(2):
            for g, d in enumerate(DILS):
                nb = 16 // d
                base = 1536 + g * 1536 + hh * 256
                stage_guard = acc_last[0]
                ACT.wait(stage_guard, ZFILL)
                sq, evq = load_w(base)
                sk, evk = load_w(base + 512)
                sv, evv = load_w(base + 1024)

                def xown(ob, d=d, nb=nb):
                    r, n = ob // nb, ob % nb
                    return lambda kc: xTo[:, kc, :].rearrange("p (l d) -> p d l", d=d)[:, r, n * 128:(n + 1) * 128]

                def xprev(pb, d=d):
                    return lambda kc: xTp[:, kc, 2048 - 128 * d:2048].rearrange("p (l d) -> p d l", d=d)[:, pb, :]

                for ob in range(16):
                    qk_tile(xown(ob), sq, evq, TIDX[("o", g, ob)], QT[:, :, ob * 128:(ob + 1) * 128])
                for ob in range(16):
                    qk_tile(xown(ob), sk, evk, TIDX[("o", g, ob)], KT[:, :, 2048 + ob * 128:2048 + (ob + 1) * 128])
                for pb in range(d):
                    qk_tile(xprev(pb), sk, evk, TIDX[("p", g, pb)], KT[:, :, pb * 128:(pb + 1) * 128])
                flush_pend()
                for ob in range(16):
                    v_tile(xown(ob), sv, evv, Vaug[:, 16 + ob, :, 0:64], lambda P_: P_[:, 0:256].rearrange("p (a c) -> p a c", a=4))
                for pb in range(d):
                    v_tile(xprev(pb), sv, evv, Vaug[:, pb, :, 0:64], lambda P_: P_[:, 0:256].rearrange("p (a c) -> p a c", a=4))
                PROJ_DONE = [st["last_qk"], st["last_v"]]
                PE.wait(PROJ_DONE)
                work = [(r, hl, n) for r in range(d) for hl in range(4) for n in range(nb)]

                def dil_S(item, d=d, nb=nb):
                    r, hl, n = item
                    p, s_ = hl // 2, hl % 2
                    lo, hi = s_ * 64, s_ * 64 + 64
                    ob = r * nb + n
                    qpos = ob * 128
                    if n == 0:
                        kposA, blkA = r * 128, r
                    else:
                        kposA, blkA = 2048 + (ob - 1) * 128, 16 + ob - 1
                    kposB, blkB = 2048 + ob * 128, 16 + ob
                    bi = bufc[0] % 2
                    bufc[0] += 1
                    S = psSb[bi]
                    PE.wait(sfree[bi])
                    qap = QT[lo:hi, p, qpos:qpos + 128]
                    PE.op(lambda h: h.matmul(S[:, 0:128], lhsT=KT[lo:hi, p, kposA:kposA + 128], rhs=qap, start=True, stop=False), sig=False)
                    PE.op(lambda h: h.matmul(S[:, 0:128], lhsT=negI[:, :], rhs=maskA[:, :], start=False, stop=True), sig=False)
                    PE.op(lambda h: h.matmul(S[:, 128:256], lhsT=KT[lo:hi, p, kposB:kposB + 128], rhs=qap, start=True, stop=False), sig=False)
                    sev = PE.op(lambda h: h.matmul(S[:, 128:256], lhsT=negI[:, :], rhs=cmask[:, 0, 0:128], start=False, stop=True))
                    pi = ptc[0] % 4
                    ptc[0] += 1
                    ACT.wait(sev, ptfree[pi])
                    if n == 0:
                        ACT.op(lambda h: h.activation(out=PT[pi][:, 0:128], in_=S[:, 0:128], func=AF.Exp, bias=prevbias[:, 0:1], scale=0.125), sig=False)
                        aev = ACT.op(lambda h: h.activation(out=PT[pi][:, 128:256], in_=S[:, 128:256], func=AF.Exp, scale=0.125))
                    else:
                        aev = ACT.op(lambda h: h.activation(out=PT[pi][:, 0:256], in_=S[:, 0:256], func=AF.Exp, scale=0.125))
                    sfree[bi] = aev
                    return (aev, pi, bi, blkA, blkB)

                def dil_AV(item, pend_, g=g, d=d):
                    r, hl, n = item
                    aev, pi, bi, blkA, blkB = pend_
                    O = psOb[bi]
                    PE.wait(aev, ofree[bi])
                    PE.op(lambda h: h.matmul(O[0:65, 0:128], lhsT=Vaug[:, blkA, hl, :], rhs=PT[pi][:, 0:128], start=True, stop=False), sig=False)
                    oev = PE.op(lambda h: h.matmul(O[0:65, 0:128], lhsT=Vaug[:, blkB, hl, :], rhs=PT[pi][:, 128:256], start=False, stop=True))
                    ptfree[pi] = oev
                    dst = acc[0:65, hl, :].rearrange("p (l d) -> p d l", d=d)[:, r, n * 128:(n + 1) * 128]
                    DVE.wait(oev, acc_last[0] if g > 0 else None)
                    if g == 0:
                        dev = DVE.op(lambda h: h.tensor_copy(out=dst, in_=O[0:65, 0:128]))
                    else:
                        dev = DVE.op(lambda h: h.tensor_tensor(out=dst, in0=dst, in1=O[0:65, 0:128], op=ALU.add))
                    ofree[bi] = dev
                    acc_last[0] = dev

                pend_ = dil_S(work[0])
                for wi, item in enumerate(work):
                    nxt = dil_S(work[wi + 1]) if wi + 1 < len(work) else None
                    dil_AV(item, pend_)
                    pend_ = nxt
            DVE.wait(st.get("ybstore"))
            for hl in range(4):
                for w in range(4):
                    PE.wait(acc_last[0], MEMS, st.get("dfree"))
                    bev = PE.op(lambda h, hl=hl, w=w: h.matmul(psD0[0:64, :], lhsT=onesF[64:65, 0:64], rhs=acc[64:65, hl, w * 512:(w + 1) * 512], start=True, stop=True))
                    ACT.wait(bev, acc_last[0])
                    rev = ACT.op(lambda h: h.activation(out=ftA[0:64, :], in_=psD0[0:64, :], func=AF.Ln))
                    st["dfree"] = rev
                    ACT.wait(rev)
                    rev = ACT.op(lambda h: h.activation(out=ftA[0:64, :], in_=ftA[0:64, :], func=AF.Exp, scale=-1.0))
                    DVE.wait(rev)
                    fev = DVE.op(lambda h, hl=hl, w=w: h.tensor_tensor(out=ybH[0:64, hl, w * 512:(w + 1) * 512], in0=acc[0:64, hl, w * 512:(w + 1) * 512], in1=ftA[0:64, :], op=ALU.mult))
                    acc_last[0] = fev
            SP.wait(acc_last[0])
            st["ybstore"] = SP.dma(lambda h, hh=hh: h.dma_start(out=ybd[:, hh * 8192:(hh + 1) * 8192], in_=ybH[:, :, :].rearrange("p h t -> p (h t)")), d_yb)
        YB_DONE = [acc_last[0], st["ybstore"]]

        Vd = VVf[:, 0:32 * 256].rearrange("p (b c) -> p b c", b=32)
        psOd = [psO0, psO1]
        psDd = [psD0, psD1]
        fin_free = YB_DONE
        st["sbf"] = [[sfree[0], sfree[1]], None]
        st["acc_prev"] = [None, None]
        att_last = YB_DONE
        ya_last = None
        for pp in range(2):
            ACT.wait(att_last)
            DVE.wait(att_last)
            sq, evq = load_w(pp * 256)
            sk, evk = load_w(512 + pp * 256)
            sv, evv = load_w(1024 + pp * 256)
            for ob in range(16):
                qk_tile(xnat(16 + ob), sq, evq, TIDX[("n", 0, 16 + ob)], QT[:, :, ob * 128:(ob + 1) * 128])
            for blk in range(32):
                qk_tile(xnat(blk), sk, evk, TIDX[("n", 0, blk)], KT[:, :, blk * 128:(blk + 1) * 128])
            flush_pend()
            for blk in range(32):
                v_tile(xnat(blk), sv, evv, Vd[:, blk, :], lambda P_: P_[:, 0:256])
            PE.wait(st["last_qk"], st["last_v"])
            for hl in range(2):
                for j in range(4):
                    nkb = 16 + 4 * (j + 1)
                    PE.wait(fin_free, st["pf"][1], st["pf"][0], st["psP_free"], st["psT_free"])
                    SBK = [(psS0, psS1), (psP, psT)]
                    accD = [av(106 * K, [128, 512], F32), av(108 * K, [128, 512], F32)]
                    accE = [DVE, DVE]

                    def dif_S(kb, hl=hl, j=j):
                        bp = bufc[0] % 2
                        bufc[0] += 1
                        diag = kb >= 16 + 4 * j
                        PE.wait(st["sbf"][bp])
                        for c in range(2):
                            lo, hi = c * 64, c * 64 + 64
                            S = SBK[bp][c]
                            sev = PE.op(lambda h, S=S, lo=lo, hi=hi: h.matmul(S[:, :], lhsT=KT[lo:hi, hl, kb * 128:(kb + 1) * 128], rhs=QT[lo:hi, hl, j * 512:(j + 1) * 512], start=True, stop=not diag), sig=(c == 1 and not diag))
                            if diag:
                                v = kb - 16 - 4 * j
                                sev = PE.op(lambda h, S=S, v=v: h.matmul(S[:, :], lhsT=negI[:, :], rhs=cmask[:, v, :], start=False, stop=True), sig=(c == 1))
                        pis = [(2 * bp) % 4, (2 * bp + 1) % 4]
                        ACT.wait(sev, ptfree[pis[0]], ptfree[pis[1]])
                        SS = [pairA, pairB][bp]
                        if kb < 16:
                            aev = ACT.op(lambda h: h.activation(out=PT2[bp][:, :], in_=SS[:, :], func=AF.Exp, bias=prevbias[:, 0:1], scale=0.125))
                        else:
                            aev = ACT.op(lambda h: h.activation(out=PT2[bp][:, :], in_=SS[:, :], func=AF.Exp, scale=0.125))
                        st["sbf"][bp] = aev
                        if bp == 1:
                            st["psP_free"] = aev
                            st["psT_free"] = aev
                            st["pf"][0] = aev
                        else:
                            sfree[0] = aev
                            sfree[1] = aev
                        return (aev, pis)

                    def dif_AV(kb, pend_, hl=hl, nkb=nkb):
                        aev, pis = pend_
                        even = False
                        PE.wait(aev)
                        PE.op(lambda h: h.matmul(psOd[0][:, :], lhsT=Vd[:, kb, hl * 128:(hl + 1) * 128], rhs=PT[pis[0]][:, :], start=(kb == 0), stop=(kb == nkb - 1)), sig=False)
                        oev_ = PE.op(lambda h: h.matmul(psOd[1][:, :], lhsT=Vd[:, kb, hl * 128:(hl + 1) * 128], rhs=PT[pis[1]][:, :], start=(kb == 0), stop=(kb == nkb - 1)), sig=not even)
                        if even:
                            PE.op(lambda h: h.matmul(psDd[0][:, :], lhsT=ones_bf[:, :], rhs=PT[pis[0]][:, :], start=(kb == 0), stop=False), sig=False)
                            oev_ = PE.op(lambda h: h.matmul(psDd[1][:, :], lhsT=ones_bf[:, :], rhs=PT[pis[1]][:, :], start=(kb == 0), stop=False))
                            ptfree[pis[0]] = oev_
                            ptfree[pis[1]] = oev_
                            return oev_
                        devs = []
                        for c in range(2):
                            E_ = accE[c]
                            E_.wait(aev, st["acc_prev"][c])
                            if kb == 0:
                                dv = E_.op(lambda h, c=c: h.tensor_copy(out=accD[c][:, :], in_=PT[pis[c]][:, :]))
                            else:
                                dv = E_.op(lambda h, c=c: h.tensor_tensor(out=accD[c][:, :], in0=accD[c][:, :], in1=PT[pis[c]][:, :], op=ALU.add))
                            st["acc_prev"][c] = dv
                            devs.append(dv)
                        ptfree[pis[0]] = [oev_, devs[0]]
                        ptfree[pis[1]] = [oev_, devs[1]]
                        return oev_

                    pend_ = dif_S(0)
                    for kb in range(nkb):
                        nxt = dif_S(kb + 1) if kb + 1 < nkb else None
                        oev = dif_AV(kb, pend_)
                        pend_ = nxt
                    PE.wait(st["acc_prev"][0], st["acc_prev"][1], MEMS)
                    PE.op(lambda h: h.matmul(psD0[:, :], lhsT=onesF[:, :], rhs=accD[0][:, :], start=True, stop=True), sig=False)
                    oev = PE.op(lambda h: h.matmul(psD1[:, :], lhsT=onesF[:, :], rhs=accD[1][:, :], start=True, stop=True))
                    st["acc_prev"] = [oev, oev]
                    DVE.wait(oev, LAMEV, ya_last)
                    ACT.wait(oev, ya_last)
                    ACT.op(lambda h: h.activation(out=ft[0][:, :], in_=psD0[:, :], func=AF.Ln), sig=False)
                    a = ACT.op(lambda h: h.activation(out=ft[1][:, :], in_=psD1[:, :], func=AF.Ln))
                    ACT.wait(a)
                    ACT.op(lambda h: h.activation(out=ft[0][:, :], in_=ft[0][:, :], func=AF.Exp, scale=-1.0), sig=False)
                    a = ACT.op(lambda h: h.activation(out=ft[1][:, :], in_=ft[1][:, :], func=AF.Exp, scale=-1.0))
                    DVE.wait(a)
                    a = DVE.op(lambda h: h.tensor_tensor(out=ft[0][:, :], in0=psO0[:, :], in1=ft[0][:, :], op=ALU.mult), sig=False)
                    a = DVE.op(lambda h: h.tensor_tensor(out=ft[1][:, :], in0=psO1[:, :], in1=ft[1][:, :], op=ALU.mult))
                    fin_free = a
                    st["pf"][1] = a
                    DVE.wait(a)
                    a = DVE.op(lambda h: h.scalar_tensor_tensor(out=ft[2][:, :], in0=ft[1][:, :], scalar=neglam, in1=ft[0][:, :], op0=ALU.mult, op1=ALU.add))
                    DVE.wait(a)
                    a = DVE.op(lambda h: h.tensor_tensor(out=ft[3][:, :], in0=ft[2][:, :], in1=ft[2][:, :], op=ALU.mult))
                    PE.wait(a, st["sbf"][0])
                    m = PE.op(lambda h: h.matmul(psS0[:, :], lhsT=onesF[:, :], rhs=ft[3][:, :], start=True, stop=True))
                    ACT.wait(m)
                    a = ACT.op(lambda h: h.activation(out=ft[4][:, :], in_=psS0[:, :], func=AF.Ln, bias=epsc[:, 0:1], scale=1.0 / 128.0))
                    st["sbf"][0] = a
                    sfree[0] = a
                    ACT.wait(a)
                    a = ACT.op(lambda h: h.activation(out=ft[4][:, :], in_=ft[4][:, :], func=AF.Exp, scale=-0.5))
                    DVE.wait(a)
                    a = DVE.op(lambda h, pp=pp, hl=hl, j=j: h.scalar_tensor_tensor(out=yaT[:, 2 * pp + hl, j * 512:(j + 1) * 512], in0=ft[2][:, :], scalar=gsc, in1=ft[4][:, :], op0=ALU.mult, op1=ALU.mult))
                    ya_last = a
                    att_last = a
        YA_DONE = ya_last

        d_dbg = dsem("d_dbg")
        if debug:
            SP.wait(YA_DONE, YB_DONE)
            SP.dma(lambda h: h.dma_start(out=dbg_ya, in_=yaT[:, :, :].rearrange("p h t -> p (h t)")), d_dbg)

        d_pw = dsem("d_pw")
        POOL.wait(YA_DONE, YB_DONE)
        SP.wait(YA_DONE, YB_DONE)
        if debug:
            POOL.wait(d_dbg.ev())
            SP.wait(d_dbg.ev())
        POOL.dma(lambda h: h.dma_start(out=woa[:, :, :], in_=w_oa.rearrange("(k p) c -> p k c", p=128)), d_pw)
        POOL.dma(lambda h: h.dma_start(out=wob[:, :, :], in_=w_ob.rearrange("(k p) c -> p k c", p=64)), d_pw)
        POOL.dma(lambda h: h.dma_start(out=wo[:, :, :], in_=w_o.rearrange("(k p) c -> p k c", p=128)), d_pw)
        d_pw2 = dsem("d_pw2")
        SP.dma(lambda h: h.dma_start(out=lnp1[:, :, :], in_=lnp_d[:, 0:2 * D].rearrange("p (a c) -> p a c", a=2)), d_pw2)
        SP.dma(lambda h: h.dma_start(out=wrt[:, :, :], in_=w_router.rearrange("(k p) c -> p k c", p=128)), d_pw2)
        PW = [d_pw.ev(), d_pw2.ev()]

        h1T = xTp
        d_xf = dsem("d_xf")
        d_h1 = dsem("d_h1")
        d_ybw = dsem("d_ybw")
        d_sc = dsem("d_sc")
        h1b = av(177 * K, [128, D], BF16)
        xf_free = None
        ybw_free = None
        g_last = {"ga": None, "gb": None, "mT": None, "h1Tf": None, "misc": None}

        def layer_norm(buf, lnv, pre_wait):
            DVE.wait(pre_wait)
            DVE.op(lambda h: h.bn_stats(out=stt[:, 0, :], in_=buf[:, 0:512]), sig=False)
            a = DVE.op(lambda h: h.bn_stats(out=stt[:, 1, :], in_=buf[:, 512:1024]))
            DVE.wait(a)
            a = DVE.op(lambda h: h.bn_aggr(out=mv[:, :], in_=stt[:, :, :].rearrange("p a b -> p (a b)")))
            ACT.wait(a)
            a = ACT.op(lambda h: h.activation(out=sm[:, 0:1], in_=mv[:, 1:2], func=AF.Ln, bias=epsc[:, 0:1], scale=1.0))
            ACT.wait(a)
            a = ACT.op(lambda h: h.activation(out=sm[:, 1:2], in_=sm[:, 0:1], func=AF.Exp, scale=-0.5))
            DVE.wait(a)
            a = DVE.op(lambda h: h.tensor_scalar(out=buf[:, :], in0=buf[:, :], scalar1=mv[:, 0:1], scalar2=sm[:, 1:2], op0=ALU.subtract, op1=ALU.mult))
            DVE.wait(a)
            a = DVE.op(lambda h: h.tensor_tensor(out=buf[:, :], in0=buf[:, :], in1=lnv[:, 0, :], op=ALU.mult))
            DVE.wait(a)
            a = DVE.op(lambda h: h.tensor_tensor(out=buf[:, :], in0=buf[:, :], in1=lnv[:, 1, :], op=ALU.add))
            return a

        xf3r = [xf3, av(0, [128, D], F32)]
        h1Tfr = [h1Tf, av(4 * K, [128, 8, 128], F32)]
        d_xfr = [dsem("d_xfr0"), dsem("d_xfr1")]
        d_h1r = [dsem("d_h1r0"), dsem("d_h1r1")]
        xf_fr = [None, None]
        h1Tf_fr = [None, None]
        tr_ev = {}
        stv_ev = {}
        pendB = [None]

        def stageA(tb, tbl, mt_ready):
            s_ = tb % 2
            xf = xf3r[s_]
            hT = h1Tfr[s_]
            SP.wait(xf_fr[s_])
            xev = SP.dma(lambda h: h.dma_start(out=xf[:, :], in_=x_own[tb * 128:(tb + 1) * 128, :]), d_xfr[s_])
            PE.wait(mt_ready, st.get("dfree2"))
            for dh in range(2):
                for fc in range(8):
                    e = PE.op(lambda h, dh=dh, fc=fc: h.matmul(psDd[dh][:, :], lhsT=mT[:, fc, tbl * 128:(tbl + 1) * 128], rhs=wo[:, fc, dh * 512:(dh + 1) * 512], start=(fc == 0), stop=(fc == 7)), sig=(fc == 7 and dh == 1))
            mm_o = e
            if tbl == 3:
                g_last["mT"] = mm_o
            DVE.wait(mm_o, xev)
            DVE.op(lambda h: h.scalar_tensor_tensor(out=xf[:, 0:512], in0=xf[:, 0:512], scalar=ALPHA, in1=psD0[:, :], op0=ALU.mult, op1=ALU.add), sig=False)
            a = DVE.op(lambda h: h.scalar_tensor_tensor(out=xf[:, 512:1024], in0=xf[:, 512:1024], scalar=ALPHA, in1=psD1[:, :], op0=ALU.mult, op1=ALU.add))
            st["dfree2"] = a
            h1ev = layer_norm(xf, lnp1, a)
            SP.wait(h1ev)
            stv_ev[tb] = SP.dma(lambda h: h.dma_start(out=h1d[tb * 128:(tb + 1) * 128, :], in_=xf[:, :]), d_h1r[s_])
            for half in range(2):
                PE.wait(h1ev, st["psP_free"])
                for q_ in range(4):
                    kc = half * 4 + q_
                    e = PE.op(lambda h, kc=kc, q_=q_: h.transpose(out=psP[:, q_ * 128:(q_ + 1) * 128], in_=xf[:, kc * 128:(kc + 1) * 128], identity=identf[:, :]), sig=(q_ == 3))
                ACT.wait(e, h1Tf_fr[s_] if half == 0 else None)
                a = ACT.op(lambda h, half=half: h.activation(out=hT[:, half * 4:(half + 1) * 4, :], in_=psP[:, :].rearrange("p (k t) -> p k t", k=4), func=AF.Copy))
                st["psP_free"] = a
            tr_ev[tb] = (a, e)

        def stageB(tb):
            s_ = tb % 2
            xf = xf3r[s_]
            hT = h1Tfr[s_]
            a, e_tr = tr_ev[tb]
            PE.wait(a, sfree[0])
            for kc in range(8):
                e = PE.op(lambda h, kc=kc: h.matmul(psS0[:, 0:NE], lhsT=hT[:, kc, :], rhs=wrt[:, kc, :], start=(kc == 0), stop=(kc == 7)), sig=(kc == 7))
            h1Tf_fr[s_] = e
            DVE.wait(e)
            a = DVE.op(lambda h: h.tensor_tensor(out=lg[:, :], in0=psS0[:, 0:NE], in1=brt[:, :], op=ALU.add))
            sfree[0] = a
            DVE.wait(a)
            a = DVE.op(lambda h: h.max(out=mx8[:, :], in_=lg[:, :]))
            DVE.wait(a)
            a = DVE.op(lambda h: h.tensor_scalar(out=sm[:, 2:3], in0=mx8[:, 0:1], scalar1=-1.0, scalar2=None, op0=ALU.mult))
            ACT.wait(a)
            ACT.op(lambda h: h.activation(out=sm[:, 4:8], in_=mx8[:, 0:4], func=AF.Exp, bias=sm[:, 2:3], scale=1.0), sig=False)
            a = ACT.op(lambda h: h.activation(out=el[:, :], in_=lg[:, :], func=AF.Exp, bias=sm[:, 2:3], scale=1.0))
            DVE.wait(a)
            a = DVE.op(lambda h: h.reduce_sum(out=sm[:, 3:4], in_=sm[:, 4:8], axis=AX.X))
            DVE.wait(a)
            a = DVE.op(lambda h: h.reciprocal(out=rsm[:, tb:tb + 1], in_=sm[:, 3:4]))
            DVE.wait(a)
            a = DVE.op(lambda h: h.tensor_scalar(out=el[:, :], in0=el[:, :], scalar1=rsm[:, tb:tb + 1], scalar2=None, op0=ALU.mult))
            DVE.wait(a)
            a = DVE.op(lambda h: h.scalar_tensor_tensor(out=gwd[:, tb, :], in0=lg[:, :], scalar=mx8[:, 3:4], in1=el[:, :], op0=ALU.is_ge, op1=ALU.mult))
            a = DVE.op(lambda h: h.tensor_scalar(out=m01[:, tb, :], in0=lg[:, :], scalar1=mx8[:, 3:4], scalar2=None, op0=ALU.is_ge))
            PE.wait(a, sfree[1])
            for tb2 in range(tb + 1):
                e = PE.op(lambda h, tb2=tb2: h.matmul(psS1[:, 0:NE], lhsT=(maskA[:, :] if tb2 == tb else ones_bf[:, :]), rhs=m01[:, tb2, :], start=(tb2 == 0), stop=(tb2 == tb)), sig=(tb2 == tb))
            DVE.wait(e)
            a = DVE.op(lambda h: h.tensor_tensor(out=slotv[:, :], in0=psS1[:, 0:NE], in1=ebase[:, :], op=ALU.add))
            sfree[1] = a
            DVE.wait(a)
            for k_ in range(4):
                DVE.op(lambda h, k_=k_: h.scalar_tensor_tensor(out=rtm[:, :], in0=lg[:, :], scalar=mx8[:, k_:k_ + 1], in1=slotv[:, :], op0=ALU.is_equal, op1=ALU.mult))
                DVE.wait((DVE.sem, DVE.n, DVE.name))
                DVE.op(lambda h, k_=k_: h.reduce_sum(out=destf[:, k_:k_ + 1], in_=rtm[:, :], axis=AX.X))
                DVE.wait((DVE.sem, DVE.n, DVE.name))
            a = DVE.op(lambda h: h.tensor_copy(out=desti[:, tb, :], in_=destf[:, :]))
            DVE.op(lambda h: h.tensor_scalar(out=gw4[:, tb, :], in0=sm[:, 4:8], scalar1=rsm[:, tb:tb + 1], scalar2=None, op0=ALU.mult))
            DVE.wait(st.get("h1b_free"))
            a = DVE.op(lambda h: h.tensor_copy(out=h1b[:, :], in_=xf[:, :]))
            POOL.wait(a, ZFILL)
            for k_ in range(4):
                sc = POOL.dma(lambda h, k_=k_: h.indirect_dma_start(out=Xg[:, :], out_offset=bass.IndirectOffsetOnAxis(ap=desti[:, tb, k_:k_ + 1], axis=0), in_=h1b[:, :], in_offset=None), d_sc)
            st["h1b_free"] = sc
            xf_fr[s_] = [stv_ev[tb], e_tr, a]
            g_last["misc"] = a
            st["route_done"] = a

        PE.wait(PW, YA_DONE, YB_DONE)
        DVE.wait(PW)
        for w in range(4):
            tok = slice(w * 512, (w + 1) * 512)
            SP.wait(ybw_free)
            ybw_ev = SP.dma(lambda h, tok=tok: h.dma_start(out=ybw[:, :, :], in_=ybd.rearrange("p (h t) -> p h t", h=8)[:, :, tok]), d_ybw)
            for cp in range(4):
                sA, evA = load_w(6144 + cp * 256)
                sB, evB = load_w(7168 + cp * 256)
                for fcl in range(2):
                    fc = 2 * cp + fcl
                    PE.wait(evA, sfree[0])
                    for kc in range(8):
                        e = PE.op(lambda h, kc=kc, sA=sA, fcl=fcl, tok=tok: h.matmul(psS0[:, :], lhsT=Wr[sA][:, kc, fcl * 128:(fcl + 1) * 128], rhs=xTo[:, kc, tok], start=(kc == 0), stop=(kc == 7)), sig=(kc == 7))
                    if fcl == 1:
                        wfree[sA] = e
                    ACT.wait(e, g_last["ga"], CONST)
                    ga = ACT.op(lambda h, fc=fc: h.activation(out=ft[0][:, :], in_=psS0[:, :], func=AF.Sigmoid, bias=bgt[:, fc:fc + 1], scale=1.0))
                    sfree[0] = ga
                    PE.wait(evB, sfree[1])
                    for kc in range(8):
                        e = PE.op(lambda h, kc=kc, sB=sB, fcl=fcl, tok=tok: h.matmul(psS1[:, :], lhsT=Wr[sB][:, kc, fcl * 128:(fcl + 1) * 128], rhs=xTo[:, kc, tok], start=(kc == 0), stop=(kc == 7)), sig=(kc == 7))
                    if fcl == 1:
                        wfree[sB] = e
                    ACT.wait(e, g_last["gb"])
                    gb = ACT.op(lambda h, fc=fc: h.activation(out=ft[1][:, :], in_=psS1[:, :], func=AF.Sigmoid, bias=bgt[:, 8 + fc:9 + fc], scale=1.0))
                    sfree[1] = gb
                    PE.wait(ofree[0])
                    for kc in range(4):
                        e = PE.op(lambda h, kc=kc, fc=fc, tok=tok: h.matmul(psO0[:, :], lhsT=woa[:, kc, fc * 128:(fc + 1) * 128], rhs=yaT[:, kc, tok], start=(kc == 0), stop=(kc == 3)), sig=(kc == 3))
                    oa = e
                    PE.wait(ofree[1], ybw_ev)
                    for hh_ in range(8):
                        e = PE.op(lambda h, hh_=hh_, fc=fc: h.matmul(psO1[:, :], lhsT=wob[0:64, hh_, fc * 128:(fc + 1) * 128], rhs=ybw[0:64, hh_, :], start=(hh_ == 0), stop=(hh_ == 7)), sig=(hh_ == 7))
                    obv = e
                    if fc == 7:
                        ybw_free = e
                    DVE.wait(ga, oa, g_last["misc"])
                    a = DVE.op(lambda h: h.tensor_tensor(out=ft[2][:, :], in0=psO0[:, :], in1=ft[0][:, :], op=ALU.mult))
                    ofree[0] = a
                    g_last["ga"] = a
                    DVE.wait(gb, obv)
                    b = DVE.op(lambda h: h.tensor_tensor(out=ft[3][:, :], in0=psO1[:, :], in1=ft[1][:, :], op=ALU.mult))
                    ofree[1] = b
                    g_last["gb"] = b
                    DVE.wait(a, b, g_last["mT"] if fc == 0 else None)
                    c_ = DVE.op(lambda h, fc=fc: h.tensor_tensor(out=mT[:, fc, :], in0=ft[2][:, :], in1=ft[3][:, :], op=ALU.add))
                    g_last["misc"] = c_
            MT_READY = c_
            for tbl in range(4):
                tb = w * 4 + tbl
                stageA(tb, tbl, MT_READY)
                if pendB[0] is not None:
                    stageB(pendB[0])
                pendB[0] = tb
        stageB(pendB[0])
        ROUTE_DONE = st["route_done"]
        xf_free = [xf_fr[0], xf_fr[1]]
        P3_DONE = [ROUTE_DONE, st["psP_free"], xf_free]
        H1D_DONE = [d_h1r[0].ev(), d_h1r[1].ev()]

        Wd = [av(i * 16 * K, [128, 8, D], BF16) for i in range(2)]
        Xe = [av(32 * K + i * 6 * K, [128, 3, D], BF16) for i in range(2)]
        XT = [av(44 * K + i * 6 * K, [128, 8, CAP], BF16) for i in range(2)]
        actT = [av(56 * K + i * 6 * K, [128, 8, CAP], BF16) for i in range(2)]
        Yt = [av(68 * K + i * 4 * K, [128, D], F32) for i in range(2)]
        Wgu = [av(112 * K + i * 32 * K, [128, 8, 2048], BF16) for i in range(2)]
        tg = [av(100 * K + i * 6 * K, [128, CAP], F32) for i in range(2)]
        tsg = [av(102 * K + i * 6 * K, [128, CAP], F32) for i in range(2)]
        tu = [av(104 * K + i * 6 * K, [128, CAP], F32) for i in range(2)]
        d_gu = [dsem(f"d_gu{i}") for i in range(2)]
        d_dn = [dsem(f"d_dn{i}") for i in range(2)]
        d_xe = [dsem("d_xe0"), dsem("d_xe1")]
        d_yg = [dsem("d_yg0"), dsem("d_yg1")]
        gu_free = [None] * 2
        dn_free = [None] * 2
        xe_free = [None, None]
        yt_free = [None, None]
        items = [(e, q) for e in range(NE) for q in range(4)]
        gu_ev = {}
        dn_ev = {}
        xe_ev = {}
        xt_ready = {}

        def issue_gu(e):
            s = e % 2
            POOL.wait(gu_free[s])
            gu_ev[e] = POOL.dma(lambda h, s=s, e=e: h.dma_start(out=Wgu[s][:, :, :], in_=w_gu[e].rearrange("(k p) c -> p k c", p=128)), d_gu[s])

        def issue_dn(e):
            s = e % 2
            POOL.wait(dn_free[s])
            dn_ev[e] = POOL.dma(lambda h, s=s, e=e: h.dma_start(out=Wd[s][:, :, :], in_=w_down[e].rearrange("(k p) c -> p k c", p=128)), d_dn[s])

        def issue_xe(e):
            s = e % 2
            SP.wait(xe_free[s])
            xe_ev[e] = SP.dma(lambda h, s=s, e=e: h.dma_start(out=Xe[s][:, :, :], in_=Xg[e * CAP:(e + 1) * CAP, :].rearrange("(n p) d -> p n d", p=128)), d_xe[s])

        def do_transposes(e):
            s = e % 2
            for n in range(3):
                PE.wait(xe_ev[e], st["psT_free"])
                for kc in range(8):
                    ev_ = PE.op(lambda h, kc=kc, n=n, s=s: h.transpose(out=psTb[:, kc * 128:(kc + 1) * 128], in_=Xe[s][:, n, kc * 128:(kc + 1) * 128], identity=identb[:, :]), sig=(kc == 7))
                ACT.wait(ev_)
                st["psT_free"] = ACT.op(lambda h, n=n, s=s: h.activation(out=XT[s][:, :, n * 128:(n + 1) * 128], in_=psTb[:, :].rearrange("p (k t) -> p k t", k=8), func=AF.Copy))
            xe_free[s] = ev_
            xt_ready[e] = st["psT_free"]

        SCAT_DONE = d_sc.ev()
        POOL.wait(P3_DONE, H1D_DONE)
        PE.wait(P3_DONE)
        DVE.wait(P3_DONE, H1D_DONE)
        ACT.wait(P3_DONE, H1D_DONE)
        SP.wait(P3_DONE, SCAT_DONE)
        issue_gu(0)
        issue_dn(0)
        issue_xe(0)
        issue_xe(1)
        do_transposes(0)
        banks = [(psS0, psS1), (psD0, psD1)]
        bfree = [[sfree[0], sfree[1]], [st.get("dfree2"), st.get("dfree2")]]
        obk = [psO0, psO1, psP]
        obkfree = [ofree[0], ofree[1], st["psP_free"]]
        tfree = [None, None]
        stepc = 0
        ocnt = 0
        act_last = None
        for e in range(NE):
            xs = e % 2
            if e + 1 < NE:
                issue_gu(e + 1)
                issue_dn(e + 1)
            s = e % 2
            for q in range(4):
                i = e
                for jj in range(2):
                    j = 2 * q + jj
                    b = stepc % 2
                    stepc += 1
                    Pg, Pu = banks[b]
                    PE.wait(gu_ev[i], xt_ready[e], bfree[b][0], bfree[b][1])
                    for kc in range(8):
                        eg = PE.op(lambda h, kc=kc, Pg=Pg, s=s, j=j, xs=xs: h.matmul(Pg[:, 0:CAP], lhsT=Wgu[s][:, kc, j * 128:(j + 1) * 128], rhs=XT[xs][:, kc, :], start=(kc == 0), stop=(kc == 7)), sig=(kc == 7))
                    for kc in range(8):
                        eu = PE.op(lambda h, kc=kc, Pu=Pu, s=s, j=j, xs=xs: h.matmul(Pu[:, 0:CAP], lhsT=Wgu[s][:, kc, 1024 + j * 128:1024 + (j + 1) * 128], rhs=XT[xs][:, kc, :], start=(kc == 0), stop=(kc == 7)), sig=(kc == 7))
                    if j == 7:
                        gu_free[s] = eu
                    DVE.wait(eg, tfree[b])
                    a1 = DVE.op(lambda h, Pg=Pg, e=e, j=j, b=b: h.tensor_scalar(out=tg[b][:, :], in0=Pg[:, 0:CAP], scalar1=bgu[:, e * 16 + j:e * 16 + j + 1], scalar2=7.0, op0=ALU.add, op1=ALU.min))
                    bfree[b][0] = a1
                    ACT.wait(a1, eu)
                    ACT.op(lambda h, b=b: h.activation(out=tsg[b][:, :], in_=tg[b][:, :], func=AF.Sigmoid, scale=1.702), sig=False)
                    a2 = ACT.op(lambda h, Pu=Pu, e=e, j=j, b=b: h.activation(out=tu[b][:, :], in_=Pu[:, 0:CAP], func=AF.Identity, bias=bgu[:, e * 16 + 8 + j:e * 16 + 9 + j], scale=1.0))
                    bfree[b][1] = a2
                    DVE.wait(a2)
                    a3 = DVE.op(lambda h, b=b: h.tensor_scalar(out=tu[b][:, :], in0=tu[b][:, :], scalar1=7.0, scalar2=-7.0, op0=ALU.min, op1=ALU.max))
                    DVE.wait(a3)
                    a4 = DVE.op(lambda h, b=b: h.scalar_tensor_tensor(out=tu[b][:, :], in0=tu[b][:, :], scalar=1.0, in1=tg[b][:, :], op0=ALU.add, op1=ALU.mult))
                    DVE.wait(a4)
                    a5 = DVE.op(lambda h, j=j, b=b, xs=xs: h.tensor_tensor(out=actT[xs][:, j, :], in0=tu[b][:, :], in1=tsg[b][:, :], op=ALU.mult))
                    tfree[b] = a5
                    act_last = a5
            if e + 1 < NE:
                do_transposes(e + 1)
            if e + 2 < NE:
                issue_xe(e + 2)
            ds_ = e % 2
            PE.wait(act_last, dn_ev[e])
            for nb_ in range(3):
                ys = (e * 3 + nb_) % 2
                for dh in range(2):
                    oi = ocnt % 3
                    ocnt += 1
                    O = obk[oi]
                    PE.wait(obkfree[oi])
                    for fc in range(8):
                        em = PE.op(lambda h, O=O, fc=fc, nb_=nb_, dh=dh, ds_=ds_, xs=xs: h.matmul(O[:, :], lhsT=actT[xs][:, fc, nb_ * 128:(nb_ + 1) * 128], rhs=Wd[ds_][:, fc, dh * 512:(dh + 1) * 512], start=(fc == 0), stop=(fc == 7)), sig=(fc == 7))
                    ACT.wait(em, yt_free[ys] if dh == 0 else None)
                    ac = ACT.op(lambda h, O=O, ys=ys, dh=dh: h.activation(out=Yt[ys][:, dh * 512:(dh + 1) * 512], in_=O[:, :], func=AF.Copy))
                    obkfree[oi] = ac
                SP.wait(ac)
                yt_free[ys] = SP.dma(lambda h, ys=ys, e=e, nb_=nb_: h.dma_start(out=Yg[e * CAP + nb_ * 128:e * CAP + (nb_ + 1) * 128, :], in_=Yt[ys][:, :]), d_yg[ys])
            dn_free[ds_] = em
        MOE_DONE = [ac, em, d_yg[0].ev(), d_yg[1].ev()]

        G = [[av((s_ * 4 + k_) * 4 * K, [128, D], F32) for k_ in range(4)] for s_ in range(2)]
        d_hl = [dsem("d_hl0"), dsem("d_hl1")]
        d_out = [dsem("d_out0"), dsem("d_out1")]
        d_g = [dsem("d_g0"), dsem("d_g1")]
        d_l2 = dsem("d_l2")
        buf_free = [None, None]
        g_free = [None, None]
        gT_free = None
        SP.wait(H1D_DONE, MOE_DONE)
        POOL.wait(MOE_DONE)
        PE.wait(MOE_DONE)
        DVE.wait(MOE_DONE)
        l2ev = SP.dma(lambda h: h.dma_start(out=lnp2[:, :, :], in_=lnp_d[:, 2 * D:4 * D].rearrange("p (a c) -> p a c", a=2)), d_l2)
        o5free = [obkfree[0], obkfree[1]]

        def issue_gather(tb):
            s = tb % 2
            POOL.wait(g_free[s])
            for k_ in range(4):
                gev_ = POOL.dma(lambda h, s=s, tb=tb, k_=k_: h.indirect_dma_start(out=G[s][k_][:, :], out_offset=None, in_=Yg[:, :], in_offset=bass.IndirectOffsetOnAxis(ap=desti[:, tb, k_:k_ + 1], axis=0)), d_g[s])
            return gev_

        gq = {0: issue_gather(0)}
        for tb in range(16):
            s = tb % 2
            if tb + 1 < 16:
                gq[tb + 1] = issue_gather(tb + 1)
            SP.wait(buf_free[s])
            hev = SP.dma(lambda h, s=s, tb=tb: h.dma_start(out=xf5[s][:, :], in_=h1d[tb * 128:(tb + 1) * 128, :]), d_hl[s])
            PE.wait(bfree[0][0], bfree[0][1], gT_free)
            e = PE.op(lambda h, tb=tb: h.transpose(out=psS0[0:NE, 0:128], in_=gwd[:, tb, :], identity=identf[:, :]))
            ACT.wait(e, gT_free)
            a = ACT.op(lambda h: h.activation(out=gT[:, :], in_=psS0[0:NE, 0:128], func=AF.Copy))
            bfree[0][0] = a
            PE.wait(a, o5free[0], o5free[1])
            for dh in range(2):
                e = PE.op(lambda h, dh=dh: h.matmul([psO0, psO1][dh][:, :], lhsT=gT[:, :], rhs=bdn[:, dh * 512:(dh + 1) * 512], start=True, stop=True))
            gT_free = e
            G0 = G[s][0]
            DVE.wait(gq[tb])
            a = DVE.op(lambda h, G0=G0, tb=tb: h.tensor_scalar(out=G0[:, :], in0=G0[:, :], scalar1=gw4[:, tb, 0:1], scalar2=None, op0=ALU.mult))
            for k_ in range(1, 4):
                DVE.wait(a)
                a = DVE.op(lambda h, G0=G0, s=s, k_=k_, tb=tb: h.scalar_tensor_tensor(out=G0[:, :], in0=G[s][k_][:, :], scalar=gw4[:, tb, k_:k_ + 1], in1=G0[:, :], op0=ALU.mult, op1=ALU.add))
            DVE.wait(a, e)
            DVE.op(lambda h, G0=G0: h.tensor_tensor(out=G0[:, 0:512], in0=G0[:, 0:512], in1=psO0[:, :], op=ALU.add), sig=False)
            a = DVE.op(lambda h, G0=G0: h.tensor_tensor(out=G0[:, 512:1024], in0=G0[:, 512:1024], in1=psO1[:, :], op=ALU.add))
            o5free = [a, a]
            DVE.wait(a, hev)
            a = DVE.op(lambda h, s=s, G0=G0: h.scalar_tensor_tensor(out=xf5[s][:, :], in0=xf5[s][:, :], scalar=ALPHA, in1=G0[:, :], op0=ALU.mult, op1=ALU.add))
            g_free[s] = a
            DVE.wait(l2ev)
            oev = layer_norm(xf5[s], lnp2, a)
            SP.wait(oev)
            buf_free[s] = SP.dma(lambda h, s=s, tb=tb: h.dma_start(out=out_d[tb * 128:(tb + 1) * 128, :], in_=xf5[s][:, :]), d_out[s])
        SP.wait(d_out[0].ev(), d_out[1].ev())
        if debug:
            SP.wait(d_dbg.ev())

        with nc.Block() as block:
            @block.tensor
            def _(h):
                for f in PE.ops:
                    f(h)

            @block.scalar
            def _(h):
                for f in ACT.ops:
                    f(h)

            @block.vector
            def _(h):
                for f in DVE.ops:
                    f(h)

            @block.gpsimd
            def _(h):
                for f in POOL.ops:
                    f(h)

            @block.sync
            def _(h):
                for f in SP.ops:
                    f(h)
    return nc


def rope_tables_np(positions):
    inv = 500000.0 ** (-np.arange(0, 16, 2, dtype=np.float32) / 16.0)
    ang = positions.astype(np.float32)[:, None] * inv[None, :]
    return np.cos(ang).astype(np.float32), np.sin(ang).astype(np.float32)


def make_consts(hf):
    TIDX, NTAB = tab_index()
    tab = np.zeros((128, NTAB, 16), np.float32)
    p = np.arange(128)
    pos0 = hf * 2048

    def put(slot, positions):
        c, s = rope_tables_np(positions)
        tab[:, slot, 0:8] = c
        tab[:, slot, 8:16] = s

    for b in range(32):
        if b < 16:
            put(TIDX[("n", 0, b)], b * 128 + p)
        else:
            put(TIDX[("n", 0, b)], pos0 + (b - 16) * 128 + p)
    for g, d in enumerate(DILS):
        nb = 16 // d
        for ob in range(16):
            r, n = ob // nb, ob % nb
            put(TIDX[("o", g, ob)], pos0 + (n * 128 + p) * d + r)
        for pb in range(d):
            put(TIDX[("p", g, pb)], 2048 - 128 * d + p * d + pb)
    k = np.arange(128)[:, None]
    q = np.arange(512)[None, :]
    cm = np.stack([(128 * v + k > q) for v in range(4)], axis=1).astype(np.float32)
    mA = (np.arange(128)[:, None] < np.arange(128)[None, :]).astype(np.float32)
    bf = ml_dtypes.bfloat16
    return {
        "rope": tab.reshape(128, NTAB * 16),
        "prevbias": np.full((128, 1), 0.0 if hf == 1 else NEG, np.float32),
        "cmask": cm.reshape(128, 4 * 512).astype(bf),
        "maskA": mA.astype(bf),
        "negI": (NEG * np.eye(128, dtype=np.float32)).astype(bf),
        "identb": np.eye(128, dtype=np.float32).astype(bf),
        "identf": np.eye(128, dtype=np.float32),
    }


_NC_CACHE = {}


def kernel(x, w_in, b_gate, lam_q1, lam_k1, lam_q2, lam_k2, subln_g, w_oa, w_ob, w_o,
           ln1_g, ln1_b, w_router, b_router, w_gu, b_gu, w_down, b_down, ln2_g, ln2_b, _debug=False):
    f32 = np.float32
    x = np.asarray(x, f32)
    key = bool(_debug)
    if key not in _NC_CACHE:
        _NC_CACHE[key] = build_nc(debug=_debug)
    nc = _NC_CACHE[key]
    c = lambda a: np.ascontiguousarray(np.asarray(a, f32))
    shared = {
        "w_in": c(w_in[0]), "w_oa": c(w_oa[0]), "w_ob": c(w_ob[0]), "w_o": c(w_o[0]),
        "w_router": c(w_router[0]), "w_gu": c(w_gu[0]), "w_down": c(w_down[0]),
        "lamv": c(np.broadcast_to(np.stack([lam_q1[0], lam_k1[0], lam_q2[0], lam_k2[0]])[None], (128, 4, 64)).reshape(128, 256)),
        "subg": c(np.asarray(subln_g[0]).reshape(128, 1)),
        "bgt": c(np.asarray(b_gate[0]).reshape(16, 128).T),
        "bgu_t": c(np.asarray(b_gu[0]).reshape(NE, 16, 128).transpose(2, 0, 1).reshape(128, NE * 16)),
        "brt": c(np.broadcast_to(np.asarray(b_router[0])[None], (128, NE))),
        "bdn": c(b_down[0]),
        "ebase": c(np.broadcast_to((np.arange(NE, dtype=np.float32) * CAP)[None], (128, NE))),
        "lnp": c(np.broadcast_to(np.stack([ln1_g[0], ln1_b[0], ln2_g[0], ln2_b[0]])[None], (128, 4, D)).reshape(128, 4 * D)),
    }
    consts = [make_consts(0), make_consts(1)]
    in_maps = []
    for core in range(8):
        b, hf = core // 2, core % 2
        m = dict(shared)
        m.update(consts[hf])
        m["x_own"] = c(x[b, hf * 2048:(hf + 1) * 2048])
        m["x_prev"] = c(x[b, 0:2048])
        in_maps.append(m)
    res = run_bass_kernel_spmd(nc, in_maps, core_ids=list(range(8)))
    out = np.empty((4, 4096, D), f32)
    for core in range(8):
        b, hf = core // 2, core % 2
        out[b, hf * 2048:(hf + 1) * 2048] = res.results[core]["out"]
    if _debug:
        return out, res.results
    return out
```
